# Optimizing a Trainium2 kernel written in Bass

```python
import math
import jax
import jax.numpy as jnp
from jax import lax
import numpy as np

D_MODEL = 1024
BATCH = 2
SEQ = 8192
DEPTH = 4

GRID_W = 64
CTX_LEN = 256
N_MIXERS = 4
NORM_EPS = 1e-6

MB_D_INNER = 2 * D_MODEL
MB_HEAD_DIM = 64
MB_HEADS = MB_D_INNER // MB_HEAD_DIM
MB_GROUPS = 4
MB_HPG = MB_HEADS // MB_GROUPS
MB_STATE = 128
MB_CONV = 5
MB_CHUNK = 128
MB_XBC = MB_D_INNER + 2 * MB_GROUPS * MB_STATE
MB_IN = MB_D_INNER + MB_XBC + 2 * MB_HEADS

RW_HEAD = 64
RW_HEADS = D_MODEL // RW_HEAD
RW_DECAY_LORA = 64
RW_ICLR_LORA = 64
RW_GATE_LORA = 128
RW_DECAY_SCALE = 0.606531
RW_LN_EPS = 64e-5

POOL_WINDOWS = (2, 4, 8, 16)
POOL_GROUP = D_MODEL // len(POOL_WINDOWS)

AT_HEAD = 64
AT_HEADS = D_MODEL // AT_HEAD
AT_KV_HEADS = 4
AT_GROUP = AT_HEADS // AT_KV_HEADS
AT_BLOCK = 128
ROPE_THETA = 10000.0

FF_DENSE = 2816
N_EXPERTS = 8
TOP_K = 2
FF_EXPERT = 3584

kernel_name = 'hybrid_ssd_rwkv7_pool_gqa_moe_dit'


def _rmsnorm(x, g, eps=NORM_EPS):
    xf = x.astype(jnp.float32)
    y = xf * lax.rsqrt(jnp.mean(xf * xf, axis=-1, keepdims=True) + eps)
    return (y * g.astype(jnp.float32)).astype(x.dtype)


def _flip(t):
    return jnp.flip(t, axis=1)


def _dwconv_centred(u, w, b):
    ch = u.shape[-1]
    pad = (w.shape[0] - 1) // 2
    y = lax.conv_general_dilated(u, w[:, None, :].astype(u.dtype), window_strides=(1,),
                                 padding=[(pad, pad)], dimension_numbers=('NWC', 'WIO', 'NWC'),
                                 feature_group_count=ch)
    return y + b


def _ssd_scan(x, dt, a_neg, bm, cm, s0, need_y):
    f32 = jnp.float32
    b, L, G, E, P = x.shape
    N = bm.shape[-1]
    nc = L // MB_CHUNK
    x = x.astype(f32).reshape(b, nc, MB_CHUNK, G, E, P)
    dt = dt.astype(f32).reshape(b, nc, MB_CHUNK, G, E)
    bm = bm.astype(f32).reshape(b, nc, MB_CHUNK, G, N)
    cm = cm.astype(f32).reshape(b, nc, MB_CHUNK, G, N)
    cum = jnp.cumsum(dt * a_neg, axis=2)
    last = cum[:, :, -1]
    xdt = x * (jnp.exp(last[:, :, None] - cum) * dt)[..., None]
    chunk_states = jnp.einsum('bcqgn,bcqgep->bcgepn', bm, xdt)

    def step(s, inp):
        st, dec = inp
        return s * dec[..., None, None] + st, s

    s_fin, s_prev = lax.scan(step, s0.astype(f32),
                             (jnp.moveaxis(chunk_states, 1, 0), jnp.moveaxis(jnp.exp(last), 1, 0)))
    if not need_y:
        return None, s_fin
    s_prev = jnp.moveaxis(s_prev, 0, 1)
    pos = jnp.arange(MB_CHUNK)
    lower = (pos[:, None] >= pos[None, :])[:, :, None, None]
    seg = cum[:, :, :, None] - cum[:, :, None, :]
    decay = jnp.exp(jnp.where(lower, seg, -jnp.inf))
    cb = jnp.einsum('bcign,bcjgn->bcijg', cm, bm)
    w = cb[..., None] * decay * dt[:, :, None]
    y = jnp.einsum('bcijge,bcjgep->bcigep', w, x)
    y = y + jnp.einsum('bcign,bcgepn->bcigep', cm, s_prev) * jnp.exp(cum)[..., None]
    return y.reshape(b, L, G, E, P), s_fin


def _mamba_project(u, in_w, conv_w, conv_b, dt_bias):
    b, L, _ = u.shape
    zxd = u @ in_w
    z = zxd[..., :MB_D_INNER]
    xbc = jax.nn.silu(_dwconv_centred(zxd[..., MB_D_INNER:MB_D_INNER + MB_XBC], conv_w, conv_b))
    dt_raw = zxd[..., MB_D_INNER + MB_XBC:].reshape(b, L, 2, MB_HEADS)
    gn = MB_GROUPS * MB_STATE
    xs = xbc[..., :MB_D_INNER].reshape(b, L, MB_GROUPS, MB_HPG, MB_HEAD_DIM)
    bm = xbc[..., MB_D_INNER:MB_D_INNER + gn].reshape(b, L, MB_GROUPS, MB_STATE)
    cm = xbc[..., MB_D_INNER + gn:].reshape(b, L, MB_GROUPS, MB_STATE)
    dt = jax.nn.softplus(dt_raw.astype(jnp.float32) + dt_bias.astype(jnp.float32))
    return z, xs, bm, cm, dt.reshape(b, L, 2, MB_GROUPS, MB_HPG)


def _mamba_mixer(h, hc, want_ctx, in_w, conv_w, conv_b, dt_bias, a_log, d_skip, norm_g, out_w):
    f32 = jnp.float32
    a_neg = -jnp.exp(a_log.astype(f32)).reshape(2, MB_GROUPS, MB_HPG)
    s0 = jnp.zeros((h.shape[0], MB_GROUPS, MB_HPG, MB_HEAD_DIM, MB_STATE), f32)

    def bidir(u, init_f, init_b, need_y):
        z, xs, bm, cm, dt = _mamba_project(u, in_w, conv_w, conv_b, dt_bias)
        y_f, s_f = _ssd_scan(xs, dt[:, :, 0], a_neg[0], bm, cm, init_f, need_y)
        y_b, s_b = _ssd_scan(_flip(xs), _flip(dt[:, :, 1]), a_neg[1], _flip(bm), _flip(cm), init_b, need_y)
        if not need_y:
            return None, s_f, s_b
        y = y_f + _flip(y_b) + xs.astype(f32) * d_skip.astype(f32).reshape(MB_GROUPS, MB_HPG, 1)
        y = y.reshape(u.shape[0], u.shape[1], MB_D_INNER).astype(u.dtype)
        return _rmsnorm(y * jax.nn.silu(z), norm_g) @ out_w, s_f, s_b

    yc, sc_f, sc_b = bidir(hc, s0, s0, want_ctx)
    y, _, _ = bidir(h, sc_f, sc_b, True)
    return y, yc


def _centred_shift(u):
    prev = jnp.pad(u, ((0, 0), (1, 0), (0, 0)))[:, :-1]
    nxt = jnp.pad(u, ((0, 0), (0, 1), (0, 0)))[:, 1:]
    return 0.5 * (prev + nxt) - u


def _heads(t):
    return t.reshape(t.shape[0], t.shape[1], RW_HEADS, RW_HEAD)


def _rwkv_features(u, mix, rkv_w, w0, w1, w2, a0, a1, a2, g1, g2, k_k, k_a):
    f32 = jnp.float32
    xx = _centred_shift(u)
    xr, xw, xk, xv, xa, xg = [u + xx * mix[j] for j in range(6)]
    r = xr @ rkv_w[0]
    k = xk @ rkv_w[1]
    v = xv @ rkv_w[2]
    g = jax.nn.sigmoid(xg @ g1) @ g2
    kk = _heads((k * k_k).astype(f32))
    kk = kk / jnp.maximum(jnp.sqrt(jnp.sum(kk * kk, axis=-1, keepdims=True)), 1e-12)
    logw, iclr, kd = [], [], []
    for d in range(2):
        logw.append(_heads(-RW_DECAY_SCALE * jax.nn.sigmoid((w0[d] + jnp.tanh(xw @ w1[d]) @ w2[d]).astype(f32))))
        a = jax.nn.sigmoid((a0[d] + (xa @ a1[d]) @ a2[d]).astype(f32))
        iclr.append(_heads(a))
        kd.append(_heads(k.astype(f32) * (1.0 + (a - 1.0) * k_a.astype(f32))))
    return _heads(r.astype(f32)), _heads(v.astype(f32)), g, kk, logw, iclr, kd


def _rwkv_scan(r, logw, k, v, kk, a, s0, need_y):
    f32 = jnp.float32
    tm = lambda t: jnp.moveaxis(t.astype(f32), 1, 0)
    seq = (tm(r), tm(jnp.exp(logw)), tm(k), tm(v), tm(kk), tm(kk * a))

    def step(S, inp):
        r_t, w_t, k_t, v_t, kk_t, b_t = inp
        sa = jnp.einsum('bhvk,bhk->bhv', S, kk_t)
        S = S * w_t[:, :, None, :] - sa[..., None] * b_t[:, :, None, :] + v_t[..., None] * k_t[:, :, None, :]
        return S, (jnp.einsum('bhvk,bhk->bhv', S, r_t) if need_y else None)

    s_fin, ys = lax.scan(step, s0, seq)
    return (jnp.moveaxis(ys, 0, 1) if need_y else None), s_fin


def _rwkv_mixer(h, hc, want_ctx, mix, rkv_w, w0, w1, w2, a0, a1, a2, g1, g2, k_k, k_a,
                r_k, ln_g, ln_b, out_w):
    f32 = jnp.float32
    s0 = jnp.zeros((h.shape[0], RW_HEADS, RW_HEAD, RW_HEAD), f32)

    def bidir(u, inits, need_y):
        r, v, g, kk, logw, iclr, kd = _rwkv_features(u, mix, rkv_w, w0, w1, w2, a0, a1, a2, g1, g2, k_k, k_a)
        ys, states = [], []
        for d in range(2):
            f = (lambda t: t) if d == 0 else _flip
            y_d, s_d = _rwkv_scan(f(r), f(logw[d]), f(kd[d]), f(v), f(kk), f(iclr[d]), inits[d], need_y)
            ys.append(f(y_d) if need_y else None)
            states.append(s_d)
        if not need_y:
            return None, states
        y = ys[0] + ys[1]
        mu = jnp.mean(y, axis=-1, keepdims=True)
        var = jnp.mean(jnp.square(y - mu), axis=-1, keepdims=True)
        yn = (y - mu) * lax.rsqrt(var + RW_LN_EPS) * ln_g.astype(f32).reshape(RW_HEADS, RW_HEAD) \
            + ln_b.astype(f32).reshape(RW_HEADS, RW_HEAD)
        bonus = jnp.sum(r * r_k.astype(f32) * (0.5 * (kd[0] + kd[1])), axis=-1, keepdims=True) * v
        o = (yn + bonus).reshape(u.shape).astype(u.dtype) * g
        return o @ out_w, states

    yc, sc = bidir(hc, [s0, s0], want_ctx)
    y, _ = bidir(h, sc, True)
    return y, yc


def _pool_mixer(u, w, scale):
    b, L, _ = u.shape
    cs = jnp.cumsum(u.astype(jnp.float32), axis=1)
    cs = jnp.concatenate([jnp.zeros_like(cs[:, :1]), cs], axis=1)
    t = jnp.arange(L)
    means = []
    for gi, win in enumerate(POOL_WINDOWS):
        lo = jnp.clip(t - win // 2, 0, L)
        hi = jnp.clip(t + win // 2, 0, L)
        csg = cs[:, :, gi * POOL_GROUP:(gi + 1) * POOL_GROUP]
        cnt = (hi - lo).astype(jnp.float32)[None, :, None]
        means.append((csg[:, hi] - csg[:, lo]) / cnt)
    pooled = jnp.concatenate(means, axis=-1).astype(u.dtype) - u
    y = jnp.einsum('blgc,gcd->blgd', pooled.reshape(b, L, len(POOL_WINDOWS), POOL_GROUP), w)
    return y.reshape(b, L, -1) * scale


def _axial_rope(x, rows, cols):
    f32 = jnp.float32
    half = x.shape[-1] // 2
    quarter = half // 2
    inv = ROPE_THETA ** (-jnp.arange(quarter, dtype=f32) / quarter)

    def rot(xp, pos):
        ang = pos.astype(f32)[:, None] * inv
        cos = jnp.cos(ang)[None, :, None, :].astype(x.dtype)
        sin = jnp.sin(ang)[None, :, None, :].astype(x.dtype)
        x1, x2 = xp[..., :quarter], xp[..., quarter:]
        return jnp.concatenate([x1 * cos - x2 * sin, x2 * cos + x1 * sin], axis=-1)

    return jnp.concatenate([rot(x[..., :half], rows), rot(x[..., half:], cols)], axis=-1)


def _gqa_attend(q, k, v):
    s = jnp.einsum('bqkgd,bskd->bkgqs', q, k).astype(jnp.float32)
    p = jax.nn.softmax(s, axis=-1).astype(v.dtype)
    return jnp.einsum('bkgqs,bskd->bqkgd', p, v)


def _attn_mixer(h, hc, want_ctx, rows, cols, qkv_w, q_g, k_g, out_w):
    nq = AT_HEADS * AT_HEAD
    nk = AT_KV_HEADS * AT_HEAD
    scale = AT_HEAD ** -0.5

    def proj(u):
        b, L, _ = u.shape
        qkv = u @ qkv_w
        q = _rmsnorm(qkv[..., :nq].reshape(b, L, AT_HEADS, AT_HEAD), q_g)
        k = _rmsnorm(qkv[..., nq:nq + nk].reshape(b, L, AT_KV_HEADS, AT_HEAD), k_g)
        v = qkv[..., nq + nk:].reshape(b, L, AT_KV_HEADS, AT_HEAD)
        return q, k, v

    qc, kc, vc = proj(hc)
    q, k, v = proj(h)
    q = _axial_rope(q, rows, cols) * scale
    k = _axial_rope(k, rows, cols)
    k_all = jnp.concatenate([kc, k], axis=1)
    v_all = jnp.concatenate([vc, v], axis=1)
    b, L = h.shape[:2]
    nblk = L // AT_BLOCK
    qb = jnp.moveaxis(q.reshape(b, nblk, AT_BLOCK, AT_KV_HEADS, AT_GROUP, AT_HEAD), 1, 0)
    o = lax.map(lambda blk: _gqa_attend(blk, k_all, v_all), qb)
    y = jnp.moveaxis(o, 0, 1).reshape(b, L, nq) @ out_w
    yc = None
    if want_ctx:
        bc, lc = hc.shape[:2]
        oc = _gqa_attend(qc.reshape(bc, lc, AT_KV_HEADS, AT_GROUP, AT_HEAD) * scale, kc, vc)
        yc = oc.reshape(bc, lc, nq) @ out_w
    return y, yc


def _swiglu(u, w_in, w_out):
    gu = u @ w_in
    f = w_out.shape[0]
    return (jax.nn.silu(gu[..., :f]) * gu[..., f:]) @ w_out


def _moe(u, router_w, w_in, w_out):
    logits = (u @ router_w).astype(jnp.float32)
    top_v, top_i = lax.top_k(logits, TOP_K)
    wts = jax.nn.softmax(top_v, axis=-1)
    gates = jnp.einsum('btk,btke->bte', wts, jax.nn.one_hot(top_i, N_EXPERTS, dtype=jnp.float32)).astype(u.dtype)
    out = jnp.zeros_like(u)
    for e in range(N_EXPERTS):
        out = out + gates[..., e:e + 1] * _swiglu(u, w_in[e], w_out[e])
    return out


def setup_inputs(seed: int = 0) -> dict:
    key = jax.random.key(seed)
    keys = iter(jax.random.split(key, 64))
    f32 = jnp.float32
    D = D_MODEL

    def nrm(shape, s):
        return jax.random.normal(next(keys), shape, f32) * s

    def gain(shape):
        return 1.0 + nrm(shape, 0.05)

    na, nb, npool, nattn = [len(range(k, DEPTH, N_MIXERS)) for k in range(N_MIXERS)]
    n_dense = len(range(0, DEPTH, 2))
    n_moe = len(range(1, DEPTH, 2))
    dt0 = jnp.exp(jax.random.uniform(next(keys), (na, 2, MB_HEADS), f32, math.log(1e-3), math.log(1e-1)))
    return {
        'x': nrm((BATCH, SEQ, D), 1.0),
        'c': nrm((BATCH, D), 1.0),
        'ctx': nrm((BATCH, CTX_LEN, D), 1.0),
        'c_ctx': nrm((D,), 1.0),
        'mod_w': nrm((DEPTH, D, 6 * D), 0.5 * D ** -0.5),
        'mod_b': nrm((DEPTH, 6 * D), 0.02),
        'norm1_g': gain((DEPTH, D)),
        'norm2_g': gain((DEPTH, D)),
        'final_g': gain((D,)),
        'mb_in_w': nrm((na, D, MB_IN), D ** -0.5),
        'mb_conv_w': nrm((na, MB_CONV, MB_XBC), MB_CONV ** -0.5),
        'mb_conv_b': nrm((na, MB_XBC), 0.02),
        'mb_dt_bias': dt0 + jnp.log(-jnp.expm1(-dt0)),
        'mb_a_log': jnp.log(jax.random.uniform(next(keys), (na, 2, MB_HEADS), f32, 1.0, 16.0)),
        'mb_d': gain((na, MB_HEADS)),
        'mb_norm_g': gain((na, MB_D_INNER)),
        'mb_out_w': nrm((na, MB_D_INNER, D), MB_D_INNER ** -0.5),
        'rw_mix': jax.random.uniform(next(keys), (nb, 6, D), f32),
        'rw_rkv_w': nrm((nb, 3, D, D), D ** -0.5),
        'rw_w0': nrm((nb, 2, D), 1.0),
        'rw_w1': nrm((nb, 2, D, RW_DECAY_LORA), D ** -0.5),
        'rw_w2': nrm((nb, 2, RW_DECAY_LORA, D), 0.5 * RW_DECAY_LORA ** -0.5),
        'rw_a0': nrm((nb, 2, D), 0.5),
        'rw_a1': nrm((nb, 2, D, RW_ICLR_LORA), D ** -0.5),
        'rw_a2': nrm((nb, 2, RW_ICLR_LORA, D), 0.5 * RW_ICLR_LORA ** -0.5),
        'rw_g1': nrm((nb, D, RW_GATE_LORA), D ** -0.5),
        'rw_g2': nrm((nb, RW_GATE_LORA, D), RW_GATE_LORA ** -0.5),
        'rw_k_k': 0.85 + nrm((nb, D), 0.05),
        'rw_k_a': gain((nb, D)),
        'rw_r_k': nrm((nb, RW_HEADS, RW_HEAD), 0.1),
        'rw_ln_g': gain((nb, D)),
        'rw_ln_b': nrm((nb, D), 0.02),
        'rw_out_w': nrm((nb, D, D), D ** -0.5),
        'pl_w': nrm((npool, len(POOL_WINDOWS), POOL_GROUP, POOL_GROUP), POOL_GROUP ** -0.5),
        'pl_scale': gain((npool, D)),
        'at_qkv_w': nrm((nattn, D, (AT_HEADS + 2 * AT_KV_HEADS) * AT_HEAD), D ** -0.5),
        'at_q_g': gain((nattn, AT_HEAD)),
        'at_k_g': gain((nattn, AT_HEAD)),
        'at_out_w': nrm((nattn, AT_HEADS * AT_HEAD, D), (AT_HEADS * AT_HEAD) ** -0.5),
        'ff_in_w': nrm((n_dense, D, 2 * FF_DENSE), D ** -0.5),
        'ff_out_w': nrm((n_dense, FF_DENSE, D), FF_DENSE ** -0.5),
        'moe_router_w': nrm((n_moe, D, N_EXPERTS), D ** -0.5),
        'moe_in_w': nrm((n_moe, N_EXPERTS, D, 2 * FF_EXPERT), D ** -0.5),
        'moe_out_w': nrm((n_moe, N_EXPERTS, FF_EXPERT, D), FF_EXPERT ** -0.5),
    }


def reference(x, c, ctx, c_ctx, mod_w, mod_b, norm1_g, norm2_g, final_g,
              mb_in_w, mb_conv_w, mb_conv_b, mb_dt_bias, mb_a_log, mb_d, mb_norm_g, mb_out_w,
              rw_mix, rw_rkv_w, rw_w0, rw_w1, rw_w2, rw_a0, rw_a1, rw_a2, rw_g1, rw_g2,
              rw_k_k, rw_k_a, rw_r_k, rw_ln_g, rw_ln_b, rw_out_w,
              pl_w, pl_scale, at_qkv_w, at_q_g, at_k_g, at_out_w,
              ff_in_w, ff_out_w, moe_router_w, moe_in_w, moe_out_w):
    b, L, _ = x.shape
    n_rows = L // GRID_W
    rows = jnp.repeat(jnp.arange(n_rows, dtype=jnp.int32), GRID_W)
    cols = jnp.arange(L, dtype=jnp.int32) % GRID_W
    n_ctx = ctx.shape[1]
    s_lat = jax.nn.silu(c)[:, None, :]
    s_ctx = jax.nn.silu(c_ctx)
    xc = ctx
    for i in range(DEPTH):
        last = i == DEPTH - 1
        kind = i % N_MIXERS
        j = i // N_MIXERS
        m = jnp.split(s_lat @ mod_w[i] + mod_b[i], 6, axis=-1)
        h = _rmsnorm(x, norm1_g[i]) * (1 + m[1]) + m[0]
        need_hc = (not last) or kind != 2
        mc, hc = None, None
        if need_hc:
            mc = jnp.split(s_ctx @ mod_w[i] + mod_b[i], 6, axis=-1)
            hc = _rmsnorm(xc, norm1_g[i]) * (1 + mc[1]) + mc[0]
        if kind == 0:
            y, yc = _mamba_mixer(h, hc, not last, mb_in_w[j], mb_conv_w[j], mb_conv_b[j], mb_dt_bias[j],
                                 mb_a_log[j], mb_d[j], mb_norm_g[j], mb_out_w[j])
        elif kind == 1:
            y, yc = _rwkv_mixer(h, hc, not last, rw_mix[j], rw_rkv_w[j], rw_w0[j], rw_w1[j], rw_w2[j],
                                rw_a0[j], rw_a1[j], rw_a2[j], rw_g1[j], rw_g2[j], rw_k_k[j], rw_k_a[j],
                                rw_r_k[j], rw_ln_g[j], rw_ln_b[j], rw_out_w[j])
        elif kind == 2:
            y = _pool_mixer(h, pl_w[j], pl_scale[j])
            yc = None if last else _pool_mixer(hc, pl_w[j], pl_scale[j])
        else:
            y, yc = _attn_mixer(h, hc, not last, rows, cols, at_qkv_w[j], at_q_g[j], at_k_g[j], at_out_w[j])
        x = x + m[2] * y
        h2 = _rmsnorm(x, norm2_g[i]) * (1 + m[4]) + m[3]
        if not last:
            xc = xc + mc[2] * yc
            h2c = _rmsnorm(xc, norm2_g[i]) * (1 + mc[4]) + mc[3]
            hcat = jnp.concatenate([h2c, h2], axis=1)
        else:
            hcat = h2
        if i % 2 == 0:
            f = _swiglu(hcat, ff_in_w[i // 2], ff_out_w[i // 2])
        else:
            f = _moe(hcat, moe_router_w[i // 2], moe_in_w[i // 2], moe_out_w[i // 2])
        if last:
            x = x + m[5] * f
        else:
            xc = xc + mc[5] * f[:, :n_ctx]
            x = x + m[5] * f[:, n_ctx:]
    return _rmsnorm(x, final_g)
```

```python
import numpy as np
from contextlib import ExitStack
import concourse.bass as bass
import concourse.mybir as mybir
from concourse.bass_utils import run_bass_kernel_spmd

F32 = mybir.dt.float32
F32R = mybir.dt.float32r
I32 = mybir.dt.int32
AF = mybir.ActivationFunctionType
ALU = mybir.AluOpType
AX = mybir.AxisListType


def R(ap):
    return ap.bitcast(F32R)

D = 1024
KC = 8
NCORES = 8
EPS = 1e-6


class Buf:
    __slots__ = ("lw", "rd", "name", "parent")

    def __init__(self, name="", parent=None):
        self.lw = None
        self.rd = {}
        self.name = name
        self.parent = parent


class TT:
    def __init__(self, t, name):
        self.t = t
        self.name = name
        self.b = Buf(name)
        self.subs = {}

    def sub(self, key):
        if key not in self.subs:
            self.subs[key] = Buf(f"{self.name}/{key}", self.b)
        return self.subs[key]

    def __getitem__(self, idx):
        return self.t[idx]


class _SubView:
    def __init__(self, tt, subs):
        self.tt = tt
        self.subs = subs

    def __getitem__(self, idx):
        return self.tt.t[idx]


def _bufs(lst):
    out = []
    for x in lst:
        if x is None:
            continue
        if isinstance(x, TT):
            out.append(x.b)
            out.extend(x.subs.values())
        elif isinstance(x, _SubView):
            out.extend(x.subs)
        else:
            out.append(x)
    return out


class Prog:
    NDS = 24

    def __init__(self, nc, stack):
        self.nc = nc
        self.stack = stack
        self.eng = {"pe": nc.tensor, "dve": nc.vector, "act": nc.scalar, "pool": nc.gpsimd, "sp": nc.sync}
        self.sem = {k: stack.enter_context(nc.semaphore("s_" + k)) for k in self.eng}
        self.cnt = {k: 0 for k in self.eng}
        self.waited = {k: {} for k in self.eng}
        self.ops = {k: [] for k in self.eng}
        self.dsem = [stack.enter_context(nc.semaphore(f"d{i}")) for i in range(self.NDS)]
        self.dval = [0] * self.NDS
        self.dnext = 0
        self.nalloc = 0
        self.psum_banks = None
        self.psum_next = 0

    def sb(self, name, shape, dtype=F32):
        self.nalloc += 1
        t = self.stack.enter_context(self.nc.sbuf_tensor(f"{name}_{self.nalloc}", list(shape), dtype))
        return TT(t, name)

    def ps(self, name, shape, dtype=F32):
        self.nalloc += 1
        t = self.stack.enter_context(self.nc.psum_tensor(f"{name}_{self.nalloc}", list(shape), dtype))
        return TT(t, name)

    def init_psum(self, n=8):
        self.psum_banks = [self.ps(f"bank{i}", [128, 512]) for i in range(n)]

    def bank(self):
        b = self.psum_banks[self.psum_next % len(self.psum_banks)]
        self.psum_next += 1
        return b

    def dram(self, name, shape, dtype=F32, kind="Internal"):
        t = self.nc.dram_tensor(name, list(shape), dtype, kind=kind)
        return TT(t.ap(), name)

    def _collect(self, engine, reads, writes, extra=None):
        need = {}

        def add(ev):
            if ev is None:
                return
            k, v = ev
            if need.get(k, 0) < v:
                need[k] = v

        for b in reads:
            add(b.lw)
            if b.parent is not None:
                add(b.parent.lw)
        for b in writes:
            add(b.lw)
            for k, v in b.rd.items():
                add((k, v))
            if b.parent is not None:
                add(b.parent.lw)
                for k, v in b.parent.rd.items():
                    add((k, v))
        if extra:
            for ev in extra:
                add(ev)
        if engine == "pe":
            need.pop(("e", "pe"), None)
        w = self.waited[engine]
        waits = []
        for k, v in need.items():
            if w.get(k, 0) < v:
                waits.append((k, v))
                w[k] = v
        return waits

    def _mark(self, ev, reads, writes):
        k, v = ev
        for b in reads:
            if b.rd.get(k, 0) < v:
                b.rd[k] = v
        for b in writes:
            b.lw = ev
            b.rd = {}

    def op(self, engine, fn, reads=(), writes=()):
        reads = _bufs(reads)
        writes = _bufs(writes)
        waits = self._collect(engine, reads, writes)
        self.cnt[engine] += 1
        ev = (("e", engine), self.cnt[engine])
        self._mark(ev, reads, writes)
        self.ops[engine].append((waits, fn, None, self.cnt[engine]))

    def dma(self, out, in_, reads=(), writes=(), queue="sp", r=False, **kw):
        if r:
            out = out.bitcast(F32R)
            in_ = in_.bitcast(F32R)
        reads = _bufs(reads)
        writes = _bufs(writes)
        i = self.dnext
        self.dnext = (i + 1) % self.NDS
        extra = [(("d", i), self.dval[i])] if self.dval[i] else None
        waits = self._collect(queue, reads, writes, extra)
        self.dval[i] += 16
        ev = (("d", i), self.dval[i])
        self._mark(ev, reads, writes)
        self.ops[queue].append((waits, lambda e: e.dma_start(out=out, in_=in_, **kw), i, None))

    def dbg(self, name, ap, shape, reads):
        if not getattr(self, "debug", False):
            return
        if name in getattr(self, "_dbg_done", set()):
            return
        self.__dict__.setdefault("_dbg_done", set()).add(name)
        t = self.nc.dram_tensor(name, list(shape), F32, kind="ExternalOutput").ap()
        self.dma(t, ap, reads, [])

    def _semof(self, k):
        return self.sem[k[1]] if k[0] == "e" else self.dsem[k[1]]

    def emit(self):
        fin = []
        for i in range(self.NDS):
            if self.dval[i] and self.waited["sp"].get(("d", i), 0) < self.dval[i]:
                fin.append((("d", i), self.dval[i]))
        for k in self.eng:
            if k != "sp" and self.cnt[k]:
                fin.append((("e", k), self.cnt[k]))
        needed = {k: set() for k in self.eng}
        for engine in self.eng:
            for waits, fn, di, idx in self.ops[engine]:
                for k, v in waits:
                    if k[0] == "e":
                        needed[k[1]].add(v)
        for k, v in fin:
            if k[0] == "e":
                needed[k[1]].add(v)
        rank = {}
        for k in self.eng:
            srt = sorted(needed[k])
            rank[k] = {v: i + 1 for i, v in enumerate(srt)}
            assert len(srt) < 30000, (k, len(srt))
        self.sem_counts = {k: len(rank[k]) for k in self.eng}

        def semval(k, v):
            return rank[k[1]][v] if k[0] == "e" else v

        with self.nc.Block() as block:
            def mk(engine):
                def body(e):
                    for waits, fn, di, idx in self.ops[engine]:
                        for k, v in waits:
                            e.wait_ge(self._semof(k), semval(k, v))
                        inst = fn(e)
                        if di is None:
                            if idx in rank[engine]:
                                inst.then_inc(self.sem[engine], 1)
                        else:
                            inst.then_inc(self.dsem[di], 16)
                    if engine == "sp":
                        for k, v in fin:
                            e.wait_ge(self._semof(k), semval(k, v))
                return body
            block.sync(mk("sp"))
            block.tensor(mk("pe"))
            block.vector(mk("dve"))
            block.scalar(mk("act"))
            block.gpsimd(mk("pool"))

    def mm(self, out, lhsT, rhs, start, stop, reads, writes, r=True):
        if r:
            lhsT = lhsT.bitcast(F32R)
            rhs = rhs.bitcast(F32R)
        self.op("pe", lambda e: e.matmul(out, lhsT, rhs, start=start, stop=stop), reads, writes)

    def transpose(self, out, in_, ident, reads, writes):
        self.op("pe", lambda e: e.transpose(out, in_, ident), reads, writes)

    def act(self, out, in_, func, reads, writes, bias=None, scale=None):
        kw = {}
        if bias is not None:
            kw["bias"] = bias
        if scale is not None:
            kw["scale"] = scale
        self.op("act", lambda e: e.activation(out=out, in_=in_, func=func, **kw), reads, writes)

    def tt(self, out, in0, in1, op, reads, writes, eng="dve"):
        self.op(eng, lambda e: e.tensor_tensor(out=out, in0=in0, in1=in1, op=op), reads, writes)

    def ts(self, out, in0, s1, op0, reads, writes, s2=None, op1=None, eng="dve"):
        if op1 is None:
            self.op(eng, lambda e: e.tensor_scalar(out=out, in0=in0, scalar1=s1, scalar2=None, op0=op0), reads, writes)
        else:
            self.op(eng, lambda e: e.tensor_scalar(out=out, in0=in0, scalar1=s1, scalar2=s2, op0=op0, op1=op1), reads, writes)

    def stt(self, out, in0, scalar, in1, op0, op1, reads, writes):
        self.op("dve", lambda e: e.scalar_tensor_tensor(out=out, in0=in0, scalar=scalar, in1=in1, op0=op0, op1=op1), reads, writes)

    def copy(self, out, in_, reads, writes, eng="dve"):
        if eng == "act":
            self.op("act", lambda e: e.copy(out=out, in_=in_), reads, writes)
        else:
            self.op(eng, lambda e: e.tensor_copy(out=out, in_=in_), reads, writes)

    def memset(self, ap, val, writes, eng="dve"):
        self.op(eng, lambda e: e.memset(ap, val), (), writes)


def make_consts(P):
    ident = P.sb("ident", [128, 128])
    ones = P.sb("ones", [128, 128])
    tmp = P.sb("iota_tmp", [128, 128])
    P.op("pool", lambda e: e.iota(tmp[:], pattern=[[1, 128]], base=0, channel_multiplier=-1,
                                  allow_small_or_imprecise_dtypes=True), (), [tmp])
    P.ts(ident[:], tmp[:], 0.0, ALU.is_equal, [tmp], [ident])
    P.ts(R(ones[:]), tmp[:], 0.0, ALU.mult, [tmp], [ones], s2=1.0, op1=ALU.add)
    return ident, ones


def load_cols(P, ident, dst, dst_ap, src_rows_ap, n):
    rows = P.sb("lc_rows", [n, 128])
    P.dma(rows[:], src_rows_ap, [], [rows])
    pb = P.bank()
    P.transpose(pb[:, 0:n], rows[:], ident[0:n, 0:n], [rows, ident], [pb])
    P.copy(dst_ap, pb[:, 0:n], [pb], [dst])


NT = 2112
NH = 1056
NTILE = 352
HALVES = [(0, [(0, 64, 1), (64, 1056, 0)]), (1056, [(0, 1056, 0)])]


def segs(half_idx, a, b):
    out = []
    for (c0, c1, w) in HALVES[half_idx][1]:
        lo, hi = max(a, c0), min(b, c1)
        if lo < hi:
            out.append((lo, hi, w))
    return out


class Ctx:
    pass


def emit_mod(P, C, mod_w_ap, mod_b_rows_ap, name):
    modT = P.sb(name, [128, 48, 2])
    bcols = P.sb(name + "_b", [128, 48])
    load_cols(P, C.ident, bcols, bcols[:], mod_b_rows_ap, 48)
    for blk in range(12):
        wb = C.wbuf()
        wv = wb.t[:, 0:4096].rearrange("p (a b) -> p a b", b=512)
        P.dma(wv, mod_w_ap[:, blk * 512:(blk + 1) * 512].rearrange("(kc p) n -> p kc n", p=128), [], [wb], r=True)
        pb = P.bank()
        for j in range(4):
            for kc in range(8):
                P.mm(pb[:, 2 * j:2 * j + 2], wv[:, kc, j * 128:(j + 1) * 128], C.sT[:, kc, :], kc == 0, kc == 7,
                     [wb, C.sT], [pb], r=False)
        P.tt(modT[:, blk * 4:(blk + 1) * 4, :], pb[:, 0:8].rearrange("p (a b) -> p a b", b=2),
             bcols[:, blk * 4:(blk + 1) * 4].unsqueeze(2).to_broadcast([128, 4, 2]), ALU.add, [pb, bcols], [modT])
    return modT


def emit_scale_vec(P, C, modT, g_rows_ap, sc_chunk0, name):
    g = P.sb(name + "_g", [128, 8])
    load_cols(P, C.ident, g, g[:], g_rows_ap, 8)
    gs = P.sb(name, [128, 8, 2])
    P.ts(gs[:], modT[:, sc_chunk0:sc_chunk0 + 8, :], 1.0, ALU.add, [modT], [gs])
    P.tt(gs[:], gs[:], g[:].unsqueeze(2).to_broadcast([128, 8, 2]), ALU.mult, [gs, g], [gs])
    return gs


def emit_rstd(P, C, src, nch, n0, n1, dst, inv_d, eps):
    pb = P.bank()
    w = n1 - n0
    for c in range(nch):
        sq = C.sqbuf()
        P.act(R(sq[:, 0:w]), src[:, c, n0:n1], AF.Square, [src], [sq])
        P.mm(pb[:, 0:w], C.ones[:, :], sq[:, 0:w], c == 0, c == nch - 1, [C.ones, sq], [pb])
    P.act(dst[:, n0:n1], pb[:, 0:w], AF.Ln, [pb, C.epsb], [dst], bias=C.epsb[:, 0:1] if eps == EPS else C.epsb[:, 1:2], scale=inv_d)
    P.act(dst[:, n0:n1], dst[:, n0:n1], AF.Exp, [dst], [dst], scale=-0.5)


def emit_norm_mod(P, C, hi, x, dst, gs, modT, shift_chunk0):
    for ti in range(3):
        n0, n1 = ti * NTILE, (ti + 1) * NTILE
        emit_rstd(P, C, x, 8, n0, n1, C.rstd, 1.0 / D, EPS)
        for c in range(8):
            for (a, b, w) in segs(hi, n0, n1):
                if modT is not None:
                    P.stt(R(dst[:, c, a:b]), x[:, c, a:b], gs[:, c, w:w + 1], C.rstd[:, a:b], ALU.mult, ALU.mult,
                          [x, gs, C.rstd], [dst])
                    P.act(R(dst[:, c, a:b]), dst[:, c, a:b], AF.Identity, [dst, modT], [dst],
                          bias=modT[:, shift_chunk0 + c, w:w + 1])
                else:
                    P.stt(R(dst[:, c, a:b]), x[:, c, a:b], gs[:, c:c + 1], C.rstd[:, a:b], ALU.mult, ALU.mult,
                          [x, gs, C.rstd], [dst])


def emit_linear_add(P, C, hi, src, kchunks, w_ap, x, gate):
    for dc in range(8):
        wb = C.wbuf()
        wv = wb.t[:, 0:kchunks * 128].rearrange("p (a b) -> p a b", b=128)
        P.dma(wv, w_ap[:, dc * 128:(dc + 1) * 128].rearrange("(kc p) n -> p kc n", p=128), [], [wb], r=True)
        for ti in range(3):
            n0, n1 = ti * NTILE, (ti + 1) * NTILE
            pb = P.bank()
            for kc in range(kchunks):
                P.mm(pb[:, 0:NTILE], wv[:, kc, :], src[:, kc, n0:n1], kc == 0, kc == kchunks - 1, [wb, src], [pb])
            for (a, b, w) in segs(hi, n0, n1):
                P.stt(x[:, dc, a:b], pb[:, a - n0:b - n0], gate[:, dc, w:w + 1], x[:, dc, a:b], ALU.mult, ALU.add,
                      [pb, gate, x], [x])


def emit_ffn(P, C, hi, h2, x, gate2, w_in_ap, w_out_ap, F, gbc=None):
    FC = F // 128
    G = FC // 2
    act = C.big
    for gi in range(2):
        f0 = gi * G
        fl = 0
        while fl < G:
            nb = min(4, G - fl)
            wg = C.wbuf()
            wu = C.wbuf()
            wgv = wg.t[:, 0:8 * nb * 128].rearrange("p (a b) -> p a b", b=nb * 128)
            wuv = wu.t[:, 0:8 * nb * 128].rearrange("p (a b) -> p a b", b=nb * 128)
            c0 = (f0 + fl) * 128
            P.dma(wgv, w_in_ap[:, c0:c0 + nb * 128].rearrange("(kc p) n -> p kc n", p=128), [], [wg], r=True)
            P.dma(wuv, w_in_ap[:, F + c0:F + c0 + nb * 128].rearrange("(kc p) n -> p kc n", p=128), [], [wu], r=True)
            for j in range(nb):
                for ti in range(3):
                    n0, n1 = ti * NTILE, (ti + 1) * NTILE
                    pg = P.bank()
                    pu = P.bank()
                    for kc in range(8):
                        P.mm(pg[:, 0:NTILE], wgv[:, kc, j * 128:(j + 1) * 128], h2[:, kc, n0:n1], kc == 0, kc == 7, [wg, h2], [pg])
                    for kc in range(8):
                        P.mm(pu[:, 0:NTILE], wuv[:, kc, j * 128:(j + 1) * 128], h2[:, kc, n0:n1], kc == 0, kc == 7, [wu, h2], [pu])
                    sg = C.sgbuf()
                    P.act(sg[:, 0:NTILE], pg[:, 0:NTILE], AF.Silu, [pg], [sg])
                    P.tt(R(act[:, fl + j, n0:n1]), sg[:, 0:NTILE], pu[:, 0:NTILE], ALU.mult, [sg, pu], [act.sub(fl + j)])
            fl += nb
        for dc in range(8):
            wb = C.wbuf()
            wv = wb.t[:, 0:G * 128].rearrange("p (a b) -> p a b", b=128)
            P.dma(wv, w_out_ap[f0 * 128:(f0 + G) * 128, dc * 128:(dc + 1) * 128].rearrange("(kc p) n -> p kc n", p=128), [], [wb], r=True)
            for ti in range(3):
                n0, n1 = ti * NTILE, (ti + 1) * NTILE
                pb = P.bank()
                for k in range(G):
                    P.mm(pb[:, 0:NTILE], wv[:, k, :], act[:, k, n0:n1], k == 0, k == G - 1, [wb, act.sub(k)], [pb])
                for (a, b, w) in segs(hi, n0, n1):
                    if gbc is None:
                        P.stt(x[:, dc, a:b], pb[:, a - n0:b - n0], gate2[:, dc, w:w + 1], x[:, dc, a:b], ALU.mult, ALU.add,
                              [pb, gate2, x], [x])
                    else:
                        tmp = C.sgbuf()
                        P.tt(tmp[:, 0:b - a], pb[:, a - n0:b - n0], gbc[:, a:b], ALU.mult, [pb, gbc], [tmp])
                        P.stt(x[:, dc, a:b], tmp[:, 0:b - a], gate2[:, dc, w:w + 1], x[:, dc, a:b], ALU.mult, ALU.add,
                              [tmp, gate2, x], [x])


def emit_pool(P, C, hi, hp_ctx, hp_lat, inv_ap):
    h0 = HALVES[hi][0]
    inv = C.hb
    for g in range(4):
        P.dma(inv[:, g, :], inv_ap[g:g + 1, h0:h0 + NH].to_broadcast([128, NH]), [], [inv], r=True)
    for c in range(8):
        g = c // 2
        w = 2 << g
        for (a, b, isctx) in HALVES[hi][1]:
            n = b - a
            hh = C.poolbuf[0]
            if isctx:
                src = hp_ctx[c * 128:(c + 1) * 128, 0:n + 16]
            else:
                l0 = h0 + a - 64
                src = hp_lat[c * 128:(c + 1) * 128, l0:l0 + n + 16]
            P.dma(hh[:, 0:n + 16], src, [], [hh])
            cur = hh
            sh = 1
            k = 0
            while sh < w:
                nxt = C.poolbuf[1 + (k % 2)]
                lo = 2 * sh - 1
                P.tt(nxt[:, lo:n + 16], cur[:, lo:n + 16], cur[:, lo - sh:n + 16 - sh], ALU.add, [cur], [nxt])
                cur = nxt
                sh *= 2
                k += 1
            off = 8 + w // 2 - 1
            P.tt(R(C.big[:, c, a:b]), cur[:, off:off + n], inv[:, g, a:b], ALU.mult, [cur, inv], [C.big.sub(c)])
            P.tt(R(C.big[:, c, a:b]), C.big[:, c, a:b], hh[:, 8:8 + n], ALU.subtract, [C.big.sub(c), hh], [C.big.sub(c)])


def emit_moe_gates(P, C, hi, h2, router_ap):
    rw = P.sb("router", [128, 8, 8])
    P.dma(rw[:], router_ap.rearrange("(kc p) e -> p kc e", p=128), [], [rw])
    t0 = 0
    while t0 < NH:
        m = min(128, NH - t0)
        pb = P.bank()
        for kc in range(8):
            P.mm(pb[0:m, 0:8], h2[:, kc, t0:t0 + m], rw[:, kc, :], kc == 0, kc == 7, [h2, rw], [pb], r=False)
        lg = P.sb("lg", [128, 8])
        P.copy(lg[0:m, :], pb[0:m, 0:8], [pb], [lg])
        mx = P.sb("mx", [128, 8])
        P.op("dve", lambda e, mx=mx, lg=lg, m=m: e.max(out=mx[0:m, :], in_=lg[0:m, :]), [lg], [mx])
        dd = P.sb("dd", [128, 4])
        P.tt(dd[0:m, 0:1], mx[0:m, 1:2], mx[0:m, 0:1], ALU.subtract, [mx], [dd])
        P.act(dd[0:m, 1:2], dd[0:m, 0:1], AF.Exp, [dd], [dd])
        P.ts(dd[0:m, 1:2], dd[0:m, 1:2], 1.0, ALU.add, [dd], [dd])
        P.op("dve", lambda e, dd=dd, m=m: e.reciprocal(out=dd[0:m, 2:3], in_=dd[0:m, 1:2]), [dd], [dd])
        P.ts(dd[0:m, 3:4], dd[0:m, 2:3], -1.0, ALU.mult, [dd], [dd], s2=1.0, op1=ALU.add)
        g1 = P.sb("g1", [128, 8])
        g2 = P.sb("g2", [128, 8])
        P.ts(g1[0:m, :], lg[0:m, :], mx[0:m, 0:1], ALU.is_equal, [lg, mx, dd], [g1], s2=dd[0:m, 2:3], op1=ALU.mult)
        P.ts(g2[0:m, :], lg[0:m, :], mx[0:m, 1:2], ALU.is_equal, [lg, mx, dd], [g2], s2=dd[0:m, 3:4], op1=ALU.mult)
        P.tt(g1[0:m, :], g1[0:m, :], g2[0:m, :], ALU.add, [g1, g2], [g1])
        pt = P.bank()
        P.transpose(pt[0:8, 0:m], g1[0:m, :], C.ident[0:m, 0:m], [g1, C.ident], [pt])
        P.copy(C.gatesT[:, t0:t0 + m], pt[0:8, 0:m], [pt], [C.gatesT])
        t0 += m


def emit_gbc(P, C, e_idx):
    for ti in range(3):
        n0, n1 = ti * NTILE, (ti + 1) * NTILE
        pb = P.bank()
        P.mm(pb[:, 0:NTILE], C.sel[:, e_idx, :], C.gatesT[:, n0:n1], True, True, [C.sel, C.gatesT], [pb], r=False)
        P.copy(C.gbc[:, n0:n1], pb[:, 0:NTILE], [pb], [C.gbc], eng="act")


def build_ts(layer, post, ffn, nxt):
    nc = bass.Bass("TRN2", target_bir_lowering=False)
    with ExitStack() as stack:
        nc.dge_precook = False
        P = Prog(nc, stack)
        P.init_psum(8)
        C = Ctx()

        def din(name, shape):
            return nc.dram_tensor(name, list(shape), F32, kind="ExternalInput").ap()

        def dout(name, shape):
            return nc.dram_tensor(name, list(shape), F32, kind="ExternalOutput").ap()

        xT = din("xT", [D, NT])
        cvec = din("cvec", [16, 128])
        if post is not None or ffn is not None:
            mod_w = din("mod_w", [D, 6 * D])
            mod_b = din("mod_b", [48, 128])
            n2g = din("n2g", [8, 128])
        if post == "mamba":
            mT = din("mT", [2048, NT])
            mb_ng = din("mb_ng", [16, 128])
            w_post = din("w_post", [2048, D])
        elif post == "lin":
            mT = din("mT", [D, NT])
            w_post = din("w_post", [D, D])
        elif post == "pool":
            hp_ctx = din("hp_ctx", [D, 80])
            hp_lat = din("hp_lat", [D, 2048 + 16])
            inv_cnt = din("inv_cnt", [4, NT])
            pl_w = din("pl_w", [4, 256, 256])
            pl_scale = din("pl_scale", [8, 128])
        if ffn == "dense":
            F = 2816
            w_in = din("w_in", [D, 2 * F])
            w_out = din("w_out", [F, D])
        elif ffn == "moe":
            F = 3584
            router = din("router", [D, 8])
            w_in = din("w_in", [8, D, 2 * F])
            w_out = din("w_out", [8, F, D])
        if nxt == "norm":
            mod_w_n = din("mod_w_n", [D, 6 * D])
            mod_b_n = din("mod_b_n", [48, 128])
            n1g_n = din("n1g_n", [8, 128])
            xT_out = dout("xT_out", [D, NT])
            hT_out = dout("hT_out", [D, NT])
        else:
            fin_g = din("fin_g", [8, 128])
            out_T = dout("out_T", [D, NT])

        C.ident, C.ones = make_consts(P)
        wbufs = [P.sb(f"wb{i}", [128, 4096]) for i in range(3)]
        wi = [0]

        def wbuf():
            b = wbufs[wi[0] % 3]
            wi[0] += 1
            return b
        C.wbuf = wbuf
        sqs = [P.sb(f"sq{i}", [128, NTILE]) for i in range(3)]
        si = [0]

        def sqbuf():
            b = sqs[si[0] % 3]
            si[0] += 1
            return b
        C.sqbuf = sqbuf
        sgs = [P.sb(f"sg{i}", [128, NTILE]) for i in range(3)]
        gi_ = [0]

        def sgbuf():
            b = sgs[gi_[0] % 3]
            gi_[0] += 1
            return b
        C.sgbuf = sgbuf
        C.rstd = P.sb("rstd", [128, NH])
        C.epsb = P.sb("epsb", [128, 2])
        P.memset(C.epsb[:, 0:1], EPS, [C.epsb])
        P.memset(C.epsb[:, 1:2], EPS, [C.epsb])
        craw = P.sb("craw", [128, 16])
        load_cols(P, C.ident, craw, craw[:], cvec, 16)
        C.sT = P.sb("sT", [128, 8, 2])
        P.act(C.sT[:, :, 0], craw[:, 0:8], AF.Silu, [craw], [C.sT])
        P.act(C.sT[:, :, 1], craw[:, 8:16], AF.Silu, [craw], [C.sT])

        x = P.sb("x", [128, 8, NH])
        C.hb = P.sb("hb", [128, 8, NH])
        need_big = post is not None or ffn is not None
        if need_big:
            nbig = 16 if post == "mamba" else (14 if ffn == "moe" else 11)
            C.big = P.sb("big", [128, nbig, NH])
        if post == "pool":
            C.poolbuf = [P.sb(f"pb{i}", [128, NH + 16]) for i in range(3)]
        if ffn == "moe":
            C.gatesT = P.sb("gatesT", [8, NH])
            C.gbc = P.sb("gbc", [128, NH])
            C.sel = P.sb("sel", [8, 8, 128])
            P.memset(C.sel[:], 0.0, [C.sel])
            for e_ in range(8):
                P.ts(C.sel[:, e_, :], C.ones[0:8, :], C.ident[0:8, e_:e_ + 1], ALU.mult, [C.ones, C.ident, C.sel], [C.sel])

        if post is not None or ffn is not None:
            modT = emit_mod(P, C, mod_w, mod_b, "modT")
            gs2 = emit_scale_vec(P, C, modT, n2g, 32, "gs2")
            gate1 = P.sb("gate1", [128, 8, 2])
            P.copy(gate1[:], modT[:, 16:24, :], [modT], [gate1])
            gate2 = P.sb("gate2", [128, 8, 2])
            P.copy(gate2[:], modT[:, 40:48, :], [modT], [gate2])
            if post == "pool":
                psc = P.sb("psc", [128, 8])
                load_cols(P, C.ident, psc, psc[:], pl_scale, 8)
                P.tt(gate1[:], gate1[:], psc[:].unsqueeze(2).to_broadcast([128, 8, 2]), ALU.mult, [gate1, psc], [gate1])
            if post == "mamba":
                mng = P.sb("mng", [128, 16])
                load_cols(P, C.ident, mng, mng[:], mb_ng, 16)
        if nxt == "norm":
            modN = emit_mod(P, C, mod_w_n, mod_b_n, "modN")
            gs1n = emit_scale_vec(P, C, modN, n1g_n, 8, "gs1n")
        else:
            fg = P.sb("fg", [128, 8])
            load_cols(P, C.ident, fg, fg[:], fin_g, 8)

        for hi in range(2):
            h0 = HALVES[hi][0]
            for c in range(8):
                P.dma(x[:, c, :], xT[c * 128:(c + 1) * 128, h0:h0 + NH], [], [x])
            if post == "mamba":
                big = C.big
                for c in range(16):
                    P.dma(big[:, c, :], mT[c * 128:(c + 1) * 128, h0:h0 + NH], [], [big.sub(c)], r=True)
                allb = [big.sub(c) for c in range(16)]
                for ti in range(3):
                    n0, n1 = ti * NTILE, (ti + 1) * NTILE
                    pb = P.bank()
                    for c in range(16):
                        sq = C.sqbuf()
                        P.act(R(sq[:, 0:NTILE]), big[:, c, n0:n1], AF.Square, [big.sub(c)], [sq])
                        P.mm(pb[:, 0:NTILE], C.ones[:, :], sq[:, 0:NTILE], c == 0, c == 15, [C.ones, sq], [pb])
                    P.act(C.rstd[:, n0:n1], pb[:, 0:NTILE], AF.Ln, [pb, C.epsb], [C.rstd], bias=C.epsb[:, 0:1], scale=1.0 / 2048)
                    P.act(C.rstd[:, n0:n1], C.rstd[:, n0:n1], AF.Exp, [C.rstd], [C.rstd], scale=-0.5)
                    for c in range(16):
                        P.stt(R(big[:, c, n0:n1]), big[:, c, n0:n1], mng[:, c:c + 1], C.rstd[:, n0:n1], ALU.mult, ALU.mult,
                              [big.sub(c), mng, C.rstd], [big.sub(c)])
                emit_linear_add(P, C, hi, _SubView(big, allb), 16, w_post, x, gate1)
            elif post == "lin":
                big = C.big
                for c in range(8):
                    P.dma(big[:, c, :], mT[c * 128:(c + 1) * 128, h0:h0 + NH], [], [big.sub(c)], r=True)
                emit_linear_add(P, C, hi, _SubView(big, [big.sub(c) for c in range(8)]), 8, w_post, x, gate1)
            elif post == "pool":
                emit_pool(P, C, hi, hp_ctx, hp_lat, inv_cnt)
                big = C.big
                for dc in range(8):
                    g, j = dc // 2, dc % 2
                    wb = C.wbuf()
                    wv = wb.t[:, 0:256].rearrange("p (a b) -> p a b", b=128)
                    P.dma(wv, pl_w[g, :, j * 128:(j + 1) * 128].rearrange("(kc p) n -> p kc n", p=128), [], [wb], r=True)
                    for ti in range(3):
                        n0, n1 = ti * NTILE, (ti + 1) * NTILE
                        pb = P.bank()
                        for kc in range(2):
                            P.mm(pb[:, 0:NTILE], wv[:, kc, :], big[:, 2 * g + kc, n0:n1], kc == 0, kc == 1,
                                 [wb, big.sub(2 * g + kc)], [pb])
                        for (a, b, w) in segs(hi, n0, n1):
                            P.stt(x[:, dc, a:b], pb[:, a - n0:b - n0], gate1[:, dc, w:w + 1], x[:, dc, a:b], ALU.mult, ALU.add,
                                  [pb, gate1, x], [x])
            if ffn is not None:
                h2 = C.hb
                emit_norm_mod(P, C, hi, x, h2, gs2, modT, 24)
                if ffn == "dense":
                    emit_ffn(P, C, hi, h2, x, gate2, w_in, w_out, F)
                else:
                    emit_moe_gates(P, C, hi, h2, router)
                    for e_ in range(8):
                        emit_gbc(P, C, e_)
                        emit_ffn(P, C, hi, h2, x, gate2, w_in[e_], w_out[e_], F, gbc=C.gbc)
            if nxt == "norm":
                for c in range(8):
                    P.dma(xT_out[c * 128:(c + 1) * 128, h0:h0 + NH], x[:, c, :], [x], [])
                emit_norm_mod(P, C, hi, x, C.hb, gs1n, modN, 0)
                for c in range(8):
                    P.dma(hT_out[c * 128:(c + 1) * 128, h0:h0 + NH], C.hb[:, c, :], [C.hb], [])
            else:
                emit_norm_mod(P, C, hi, x, C.hb, fg, None, 0)
                for c in range(8):
                    P.dma(out_T[c * 128:(c + 1) * 128, h0:h0 + NH], C.hb[:, c, :], [C.hb], [])
        P.emit()
    return nc


def rows128(v):
    return np.ascontiguousarray(np.asarray(v, np.float32).reshape(-1, 128))


def to_cores_T(lat, ctx):
    outs = []
    for core in range(NCORES):
        b, q = core // 4, core % 4
        a = np.concatenate([ctx[b, q * 64:(q + 1) * 64], lat[b, q * 2048:(q + 1) * 2048]], axis=0)
        outs.append(np.ascontiguousarray(a.T))
    return outs


def from_cores_T(arrs):
    Cc = arrs[0].shape[0]
    lat = np.empty((2, 8192, Cc), np.float32)
    ctx = np.empty((2, 256, Cc), np.float32)
    for core in range(NCORES):
        b, q = core // 4, core % 4
        a = arrs[core].T
        ctx[b, q * 64:(q + 1) * 64] = a[0:64]
        lat[b, q * 2048:(q + 1) * 2048] = a[64:]
    return lat, ctx


def cvec_for(c, c_ctx, core):
    b = core // 4
    return np.ascontiguousarray(np.concatenate([np.asarray(c[b], np.float32).reshape(8, 128),
                                                np.asarray(c_ctx, np.float32).reshape(8, 128)], axis=0))


_NC_CACHE = {}


def get_ts(layer, post, ffn, nxt):
    key = ("ts", post, ffn, nxt)
    if key not in _NC_CACHE:
        _NC_CACHE[key] = build_ts(layer, post, ffn, nxt)
    return _NC_CACHE[key]


def run(nc, in_maps):
    res = run_bass_kernel_spmd(nc, in_maps, core_ids=list(range(NCORES)))
    return res.results


def pool_inputs(h_lat, h_ctx):
    outs = []
    for core in range(NCORES):
        b, q = core // 4, core % 4
        lat = np.zeros((2048 + 16, D), np.float32)
        lo, hi = q * 2048 - 8, (q + 1) * 2048 + 8
        s0, s1 = max(lo, 0), min(hi, 8192)
        lat[s0 - lo:s1 - lo] = h_lat[b, s0:s1]
        cx = np.zeros((64 + 16, D), np.float32)
        lo, hi = q * 64 - 8, (q + 1) * 64 + 8
        s0, s1 = max(lo, 0), min(hi, 256)
        cx[s0 - lo:s1 - lo] = h_ctx[b, s0:s1]
        inv = np.empty((4, NT), np.float32)
        for g, w in enumerate((2, 4, 8, 16)):
            t = np.arange(q * 64, (q + 1) * 64)
            inv[g, 0:64] = 1.0 / (np.minimum(t + w // 2, 256) - np.maximum(t - w // 2, 0))
            t = np.arange(q * 2048, (q + 1) * 2048)
            inv[g, 64:] = 1.0 / (np.minimum(t + w // 2, 8192) - np.maximum(t - w // 2, 0))
        outs.append({"hp_ctx": np.ascontiguousarray(cx.T), "hp_lat": np.ascontiguousarray(lat.T), "inv_cnt": inv})
    return outs


NKEY = 8448
NQ = 8192


def build_attn():
    nc = bass.Bass("TRN2", target_bir_lowering=False)
    with ExitStack() as stack:
        nc.dge_precook = False
        P = Prog(nc, stack)
        P.init_psum(6)
        oacc = P.ps("oacc", [128, 512])
        bcp = P.ps("bcp", [128, 512])

        def din(name, shape):
            return nc.dram_tensor(name, list(shape), F32, kind="ExternalInput").ap()

        hT = din("hT", [D, NKEY])
        wq = din("wq", [D, 256])
        wk = din("wk", [D, 64])
        wv = din("wv", [D, 64])
        qg = din("qg", [64, 1])
        kg = din("kg", [64, 1])
        cosT = din("cosT", [64, NKEY])
        sinS = din("sinS", [64, NKEY])
        prot = din("prot", [64, 64])
        oT = nc.dram_tensor("oT", [256, NQ], F32, kind="ExternalOutput").ap()

        ident, ones = make_consts(P)
        wq_s = P.sb("wq_s", [128, 8, 256])
        wk_s = P.sb("wk_s", [128, 8, 64])
        wv_s = P.sb("wv_s", [128, 8, 64])
        P.dma(wq_s[:], wq.rearrange("(kc p) n -> p kc n", p=128), [], [wq_s], r=True)
        P.dma(wk_s[:], wk.rearrange("(kc p) n -> p kc n", p=128), [], [wk_s], r=True)
        P.dma(wv_s[:], wv.rearrange("(kc p) n -> p kc n", p=128), [], [wv_s], r=True)
        qg_s = P.sb("qg_s", [64, 1])
        kg_s = P.sb("kg_s", [64, 1])
        P.dma(qg_s[:], qg, [], [qg_s])
        P.dma(kg_s[:], kg, [], [kg_s])
        prot_s = P.sb("prot_s", [64, 64])
        P.dma(prot_s[:], prot, [], [prot_s], r=True)
        epsb = P.sb("epsb", [128, 1])
        P.memset(epsb[:], EPS, [epsb])
        KT = P.sb("KT", [64, NKEY])
        Vx = P.sb("Vx", [128, 66, 65])
        P.ts(R(Vx[:, :, 64:65]), Vx[:, :, 64:65], 0.0, ALU.mult, [], [Vx], s2=1.0, op1=ALU.add)
        hts = [P.sb(f"ht{i}", [128, 8, 512]) for i in range(2)]
        cst = [P.sb(f"cs{i}", [64, 512]) for i in range(2)]
        snt = [P.sb(f"sn{i}", [64, 512]) for i in range(2)]
        QTs = [P.sb(f"QT{i}", [64, 4, 512]) for i in range(2)]
        pts = [P.sb(f"pt{i}", [128, 512]) for i in range(3)]
        sqb = [P.sb(f"sqb{i}", [64, 512]) for i in range(2)]
        rsb = [P.sb(f"rsb{i}", [64, 512]) for i in range(2)]
        qnb = [P.sb(f"qnb{i}", [64, 512]) for i in range(2)]
        t1b = [P.sb(f"t1b{i}", [64, 512]) for i in range(2)]
        t2b = [P.sb(f"t2b{i}", [64, 512]) for i in range(2)]
        obuf = [P.sb(f"ob{i}", [64, 4, 512]) for i in range(2)]
        lrow = P.sb("lrow", [128, 512])
        bcs = P.sb("bcs", [64, 512])
        cnt = [0]

        def normrope(src_ps, w, g_s, cs, sn, dst_ap, dst_tt):
            i = cnt[0] % 2
            cnt[0] += 1
            sq, rs, qn, t1, t2 = sqb[i], rsb[i], qnb[i], t1b[i], t2b[i]
            P.act(R(sq[:, 0:w]), src_ps, AF.Square, [src_tt[0]], [sq])
            pb = P.bank()
            P.mm(pb[0:64, 0:w], ones[0:64, 0:64], sq[:, 0:w], True, True, [ones, sq], [pb])
            P.act(rs[:, 0:w], pb[0:64, 0:w], AF.Ln, [pb, epsb], [rs], bias=epsb[0:64, 0:1], scale=1.0 / 64)
            P.act(rs[:, 0:w], rs[:, 0:w], AF.Exp, [rs], [rs], scale=-0.5)
            P.stt(R(qn[:, 0:w]), src_ps, g_s[:, 0:1], rs[:, 0:w], ALU.mult, ALU.mult, [src_tt[0], g_s, rs], [qn])
            pr = P.bank()
            P.mm(pr[0:64, 0:w], prot_s[:, :], qn[:, 0:w], True, True, [prot_s, qn], [pr])
            P.tt(t1[:, 0:w], qn[:, 0:w], cs, ALU.mult, [qn, cs_tt[0]], [t1])
            P.tt(t2[:, 0:w], pr[0:64, 0:w], sn, ALU.mult, [pr, sn_tt[0]], [t2])
            P.tt(R(dst_ap), t1[:, 0:w], t2[:, 0:w], ALU.add, [t1, t2], [dst_tt])

        src_tt = [None]
        cs_tt = [None]
        sn_tt = [None]

        ntile_k = [(i * 512, 512) for i in range(16)] + [(8192, 256)]
        for ti, (c0, w) in enumerate(ntile_k):
            ht = hts[ti % 2]
            cs = cst[ti % 2]
            sn = snt[ti % 2]
            for kc in range(8):
                P.dma(ht[:, kc, 0:w], hT[kc * 128:(kc + 1) * 128, c0:c0 + w], [], [ht], r=True)
            P.dma(cs[:, 0:w], cosT[:, c0:c0 + w], [], [cs])
            P.dma(sn[:, 0:w], sinS[:, c0:c0 + w], [], [sn])
            pk = P.bank()
            for kc in range(8):
                P.mm(pk[0:64, 0:w], wk_s[:, kc, :], ht[:, kc, 0:w], kc == 0, kc == 7, [wk_s, ht], [pk])
            src_tt[0], cs_tt[0], sn_tt[0] = pk, cs, sn
            normrope(pk[0:64, 0:w], w, kg_s, cs[:, 0:w], sn[:, 0:w], KT[:, c0:c0 + w], KT.sub(ti))
            for j in range(w // 128):
                ch = c0 // 128 + j
                pv = P.bank()
                for kc in range(8):
                    P.mm(pv[:, 0:64], ht[:, kc, j * 128:(j + 1) * 128], wv_s[:, kc, :], kc == 0, kc == 7, [ht, wv_s], [pv])
                P.copy(R(Vx[:, ch, 0:64]), pv[:, 0:64], [pv], [Vx.sub(ch)], eng="act")
        KTall = [KT.sub(ti) for ti in range(len(ntile_k))]

        for ti in range(16):
            c0 = 256 + ti * 512
            ht = hts[ti % 2]
            cs = cst[ti % 2]
            sn = snt[ti % 2]
            QT = QTs[ti % 2]
            ob = obuf[ti % 2]
            for kc in range(8):
                P.dma(ht[:, kc, :], hT[kc * 128:(kc + 1) * 128, c0:c0 + 512], [], [ht], r=True)
            P.dma(cs[:, :], cosT[:, c0:c0 + 512], [], [cs])
            P.dma(sn[:, :], sinS[:, c0:c0 + 512], [], [sn])
            for hq in range(4):
                pq = P.bank()
                for kc in range(8):
                    P.mm(pq[0:64, :], wq_s[:, kc, hq * 64:(hq + 1) * 64], ht[:, kc, :], kc == 0, kc == 7, [wq_s, ht], [pq])
                src_tt[0], cs_tt[0], sn_tt[0] = pq, cs, sn
                normrope(pq[0:64, :], 512, qg_s, cs[:, :], sn[:, :], QT[:, hq, :], QT)
            for qb in range(4):
                rhs_q = QT[:, :, qb * 128:(qb + 1) * 128]
                for ch in range(66):
                    pst = P.bank()
                    P.mm(pst[:, :].rearrange("p (h q) -> p h q", h=4), KT[:, ch * 128:(ch + 1) * 128], rhs_q, True, True,
                         [KT.sub(ch // 4), QT], [pst])
                    pt = pts[ch % 3]
                    P.act(R(pt[:, :]), pst[:, :], AF.Exp, [pst], [pt], scale=0.125)
                    P.mm(oacc[0:65, :], Vx[:, ch, :], pt[:, :], ch == 0, ch == 65, [Vx.sub(ch), pt], [oacc])
                P.copy(lrow[64:65, :], oacc[64:65, :], [oacc], [lrow], eng="act")
                P.op("dve", lambda e: e.reciprocal(out=lrow[64:65, :], in_=lrow[64:65, :]), [lrow], [lrow])
                P.mm(bcp[0:64, :], ones[64:65, 0:64], lrow[64:65, :], True, True, [ones, lrow], [bcp], r=False)
                P.copy(bcs[:, :], bcp[0:64, :], [bcp], [bcs], eng="act")
                P.tt(ob[:, :, qb * 128:(qb + 1) * 128], oacc[0:64, :].rearrange("p (h q) -> p h q", h=4),
                     bcs[:, :].rearrange("p (h q) -> p h q", h=4), ALU.mult, [oacc, bcs], [ob])
            q0 = ti * 512
            for hq in range(4):
                P.dma(oT[hq * 64:(hq + 1) * 64, q0:q0 + 512], ob[:, hq, :], [ob], [])
        P.emit()
    return nc


def rope_tables():
    quarter = 16
    inv = (10000.0 ** (-np.arange(quarter, dtype=np.float32) / quarter)).astype(np.float32)
    t = np.arange(8192)
    rows = (t // 64).astype(np.float32)
    cols = (t % 64).astype(np.float32)
    cosT = np.ones((64, NKEY), np.float32)
    sinS = np.zeros((64, NKEY), np.float32)
    prot = np.zeros((64, 64), np.float32)
    for i in range(64):
        half, within = i // 32, i % 32
        j, first = within % 16, within < 16
        pos = rows if half == 0 else cols
        ang = (pos * inv[j]).astype(np.float32)
        cosT[i, 256:] = np.cos(ang)
        sinS[i, 256:] = -np.sin(ang) if first else np.sin(ang)
        prot[i + 16 if first else i - 16, i] = 1.0
    return cosT, sinS, prot


def build_mamba(debug=False):
    nc = bass.Bass("TRN2", target_bir_lowering=False)
    with ExitStack() as stack:
        nc.dge_precook = False
        P = Prog(nc, stack)
        P.debug = debug
        P.init_psum(6)
        pybanks = [P.ps("pyb0", [128, 512]), P.ps("pyb1", [128, 512])]
        pyi = [0]

        def din(name, shape):
            return nc.dram_tensor(name, list(shape), F32, kind="ExternalInput").ap()

        hT = din("hT", [D, NKEY])
        wz = din("wz", [D, 512])
        wxbc = din("wxbc", [D, 768])
        wdt = din("wdt", [D, 16])
        cw = din("cw", [128, 30])
        cb = din("cb", [128, 6])
        dtb = din("dtb", [1, 16])
        alog = din("alog", [1, 16])
        dsk = din("dsk", [128, 4])
        uT = nc.dram_tensor("uT", [512, NKEY], F32, kind="ExternalOutput").ap()
        ybs = nc.dram_tensor("ybs", [512, NKEY], F32, kind="Internal").ap()
        ybs_t = TT(ybs, "ybs")

        ident, ones = make_consts(P)
        val = P.sb("val", [128, 128])
        P.op("pool", lambda e: e.iota(val[:], pattern=[[1, 128]], base=0, channel_multiplier=-1,
                                      allow_small_or_imprecise_dtypes=True), (), [val])
        Uf = P.sb("Uf", [128, 128]); Tf = P.sb("Tf", [128, 128]); Ub = P.sb("Ub", [128, 128]); Tb = P.sb("Tb", [128, 128])
        P.ts(Uf[:], val[:], 0.0, ALU.is_lt, [val], [Uf])
        P.ts(Tf[:], val[:], 0.0, ALU.is_ge, [val], [Tf])
        P.ts(Ub[:], val[:], 0.0, ALU.is_gt, [val], [Ub])
        P.ts(Tb[:], val[:], 0.0, ALU.is_le, [val], [Tb])
        UU = [Uf, Ub]
        TTm = [Tf, Tb]

        wz_s = P.sb("wz_s", [128, 8, 512])
        wx_s = P.sb("wx_s", [128, 8, 768])
        wd_s = P.sb("wd_s", [128, 8, 16])
        P.dma(wz_s[:], wz.rearrange("(kc p) n -> p kc n", p=128), [], [wz_s], r=True)
        P.dma(wx_s[:], wxbc.rearrange("(kc p) n -> p kc n", p=128), [], [wx_s], r=True)
        P.dma(wd_s[:], wdt.rearrange("(kc p) n -> p kc n", p=128), [], [wd_s], r=True)
        cw_s = P.sb("cw_s", [128, 30]); cb_s = P.sb("cb_s", [128, 6]); dsk_s = P.sb("dsk_s", [128, 4])
        P.dma(cw_s[:], cw, [], [cw_s]); P.dma(cb_s[:], cb, [], [cb_s]); P.dma(dsk_s[:], dsk, [], [dsk_s])
        dtb_s = P.sb("dtb_s", [128, 16]); aneg = P.sb("aneg", [128, 16])
        P.dma(dtb_s[:], dtb.to_broadcast([128, 16]), [], [dtb_s])
        P.dma(aneg[:], alog.to_broadcast([128, 16]), [], [aneg])
        P.act(aneg[:], aneg[:], AF.Exp, [aneg], [aneg])
        P.ts(aneg[:], aneg[:], -1.0, ALU.mult, [aneg], [aneg])
        oneb = P.sb("oneb", [128, 1])
        P.memset(oneb[:], 1.0, [oneb])

        W = 256
        ht = [P.sb(f"ht{i}", [128, 8, W + 4]) for i in range(2)]
        raw = P.sb("raw", [128, 6, W + 4])
        acc = [P.sb(f"acc{i}", [128, W]) for i in range(2)]
        xTc = P.sb("xTc", [128, 4, W])
        BT = P.sb("BT", [128, W]); CT = P.sb("CT", [128, W])
        x_tok = P.sb("x_tok", [128, 2, 512]); B_tok = P.sb("B_tok", [128, 2, 128]); dt_tok = P.sb("dt_tok", [128, 2, 16])
        zs = P.sb("zs", [128, 4, W])
        ST = [P.sb(f"ST{d}", [128, 512]) for d in range(2)]
        for d_ in range(2):
            P.memset(ST[d_][:], 0.0, [ST[d_]])
        sm = [P.sb(f"sm{i}", [128, 16]) for i in range(8)]
        GM = P.sb("GM", [128, 128])
        lD = [P.sb(f"lD{i}", [128, 128]) for i in range(2)]
        Lx = [P.sb(f"Lx{i}", [128, 128]) for i in range(2)]
        WT = [P.sb(f"WT{i}", [128, 128]) for i in range(2)]
        A1 = [P.sb(f"A1{i}", [128, 128]) for i in range(2)]
        Ec = [P.sb(f"Ec{i}", [128, 128]) for i in range(2)]
        Cd = [P.sb(f"Cd{i}", [128, 128]) for i in range(2)]
        xdt = P.sb("xdt", [128, 512])
        ybuf = P.sb("ybuf", [128, 4, 128])
        ubuf = [P.sb(f"ubuf{i}", [128, 4, 128]) for i in range(2)]

        def stage_a(ti, c0, q0, q1, fwd):
            h = ht[ti % 2]
            lo, hi = max(c0 - 2, q0), min(c0 + W + 2, q1)
            off = lo - (c0 - 2)
            n = hi - lo
            for kc in range(8):
                P.dma(h[:, kc, off:off + n], hT[kc * 128:(kc + 1) * 128, lo:hi], [], [h], r=True)
            if off > 0:
                P.ts(R(h[:, :, 0:off]), h[:, :, 0:off], 0.0, ALU.mult, [], [h])
            for cc in range(6):
                pb = P.bank()
                for kc in range(8):
                    P.mm(pb[:, 0:n], wx_s[:, kc, cc * 128:(cc + 1) * 128], h[:, kc, off:off + n], kc == 0, kc == 7, [wx_s, h], [pb])
                if off > 0:
                    P.memset(raw[:, cc, 0:off], 0.0, [raw.sub(cc)])
                if off + n < W + 4:
                    P.memset(raw[:, cc, off + n:W + 4], 0.0, [raw.sub(cc)])
                P.copy(raw[:, cc, off:off + n], pb[:, 0:n], [pb], [raw.sub(cc)], eng="act")
                a_ = acc[cc % 2]
                P.ts(a_[:, :], raw[:, cc, 0:W], cw_s[:, cc * 5:cc * 5 + 1], ALU.mult, [raw.sub(cc), cw_s], [a_])
                for k in range(1, 5):
                    P.stt(a_[:, :], raw[:, cc, k:k + W], cw_s[:, cc * 5 + k:cc * 5 + k + 1], a_[:, :], ALU.mult, ALU.add,
                          [raw.sub(cc), cw_s, a_], [a_])
                if cc < 4:
                    P.act(xTc[:, cc, :], a_[:, :], AF.Silu, [a_, cb_s], [xTc.sub(cc)], bias=cb_s[:, cc:cc + 1])
                elif cc == 4:
                    P.act(BT[:, :], a_[:, :], AF.Silu, [a_, cb_s], [BT], bias=cb_s[:, cc:cc + 1])
                else:
                    P.act(CT[:, :], a_[:, :], AF.Silu, [a_, cb_s], [CT], bias=cb_s[:, cc:cc + 1])
            for j in range(2):
                pt = P.bank()
                for cc in range(4):
                    P.transpose(pt[:, cc * 128:(cc + 1) * 128], xTc[:, cc, j * 128:(j + 1) * 128], ident[:, :], [xTc.sub(cc), ident], [pt])
                P.copy(x_tok[:, j, :], pt[:, :], [pt], [x_tok.sub(j)], eng="act")
                pb = P.bank()
                P.transpose(pb[:, 0:128], BT[:, j * 128:(j + 1) * 128], ident[:, :], [BT, ident], [pb])
                P.copy(R(B_tok[:, j, :]), pb[:, 0:128], [pb], [B_tok.sub(j)])
                pd = P.bank()
                for kc in range(8):
                    P.mm(pd[:, 0:16], h[:, kc, 2 + j * 128:2 + (j + 1) * 128], wd_s[:, kc, :], kc == 0, kc == 7, [h, wd_s], [pd], r=False)
                xx, ax, ee, rr = sm[0], sm[1], sm[2], sm[3]
                P.tt(xx[:, :], pd[:, 0:16], dtb_s[:, :], ALU.add, [pd, dtb_s], [xx])
                P.stt(ax[:, :], xx[:, :], -1.0, xx[:, :], ALU.mult, ALU.max, [xx], [ax])
                P.act(ee[:, :], ax[:, :], AF.Exp, [ax], [ee], scale=-1.0)
                P.act(ee[:, :], ee[:, :], AF.Ln, [ee, oneb], [ee], bias=oneb[:, 0:1])
                P.ts(rr[:, :], xx[:, :], 0.0, ALU.max, [xx], [rr])
                P.tt(dt_tok[:, j, :], rr[:, :], ee[:, :], ALU.add, [rr, ee], [dt_tok.sub(j)])
            if fwd:
                P.dbg("d_xTc", xTc[:, :, :], [128, 4, W], [xTc])
                P.dbg("d_BT", BT[:, :], [128, W], [BT])
                P.dbg("d_CT", CT[:, :], [128, W], [CT])
                P.dbg("d_xtok", x_tok[:, :, :], [128, 2, 512], [x_tok])
                P.dbg("d_Btok", B_tok[:, :, :], [128, 2, 128], [B_tok])
                P.dbg("d_dt", dt_tok[:, :, :], [128, 2, 16], [dt_tok])
                P.dbg("d_raw", raw[:, :, :], [128, 6, W + 4], [raw])
            if fwd:
                for cc in range(4):
                    pz = P.bank()
                    for kc in range(8):
                        P.mm(pz[:, 0:W], wz_s[:, kc, cc * 128:(cc + 1) * 128], h[:, kc, 2:2 + W], kc == 0, kc == 7, [wz_s, h], [pz])
                    P.act(zs[:, cc, :], pz[:, 0:W], AF.Silu, [pz], [zs.sub(cc)])

        def chunk_step(d, j, col0, ui):
            S = ST[d]
            dts = dt_tok[:, j, d * 8:(d + 1) * 8]
            a_s, cum_s, w_s, et_s = sm[4], sm[5], sm[6], sm[7]
            P.tt(a_s[:, 0:8], dts, aneg[:, d * 8:(d + 1) * 8], ALU.mult, [dt_tok.sub(j), aneg], [a_s])
            pc = P.bank()
            P.mm(pc[:, 0:8], TTm[d][:, :], a_s[:, 0:8], True, True, [TTm[d], a_s], [pc], r=False)
            P.mm(pc[:, 8:16], ones[:, :], a_s[:, 0:8], True, True, [ones, a_s], [pc], r=False)
            P.copy(cum_s[:, 0:16], pc[:, 0:16], [pc], [cum_s])
            P.tt(w_s[:, 0:8], cum_s[:, 8:16], cum_s[:, 0:8], ALU.subtract, [cum_s], [w_s])
            P.act(w_s[:, 0:8], w_s[:, 0:8], AF.Exp, [w_s], [w_s])
            P.tt(w_s[:, 0:8], w_s[:, 0:8], dts, ALU.mult, [w_s, dt_tok.sub(j)], [w_s])
            P.act(et_s[:, 0:8], cum_s[:, 8:16], AF.Exp, [cum_s], [et_s])
            pg = P.bank()
            P.mm(pg[:, 0:128], BT[:, j * 128:(j + 1) * 128], CT[:, j * 128:(j + 1) * 128], True, True, [BT, CT], [pg], r=False)
            P.tt(GM[:, :], pg[:, 0:128], TTm[d][:, :], ALU.mult, [pg, TTm[d]], [GM])
            py = pybanks[pyi[0] % 2]
            pyi[0] += 1
            for e_ in range(8):
                i2 = e_ % 2
                P.ts(lD[i2][:, :], UU[d][:, :], a_s[:, e_:e_ + 1], ALU.mult, [UU[d], a_s], [lD[i2]])
                pD = P.bank()
                P.mm(pD[:, 0:128], lD[i2][:, :], TTm[d][:, :], True, True, [lD[i2], TTm[d]], [pD], r=False)
                P.act(Lx[i2][:, :], pD[:, 0:128], AF.Exp, [pD], [Lx[i2]])
                P.stt(WT[i2][:, :], Lx[i2][:, :], dts[:, e_:e_ + 1], GM[:, :], ALU.mult, ALU.mult, [Lx[i2], dt_tok.sub(j), GM], [WT[i2]])
                P.ts(A1[i2][:, :], ones[:, :], a_s[:, e_:e_ + 1], ALU.mult, [ones, a_s], [A1[i2]])
                pE = P.bank()
                P.mm(pE[:, 0:128], A1[i2][:, :], TTm[d][:, :], True, True, [A1[i2], TTm[d]], [pE], r=False)
                P.act(Ec[i2][:, :], pE[:, 0:128], AF.Exp, [pE], [Ec[i2]])
                P.tt(Cd[i2][:, :], CT[:, j * 128:(j + 1) * 128], Ec[i2][:, :], ALU.mult, [CT, Ec[i2]], [Cd[i2]])
                p0 = (e_ % 2) * 64
                cc = e_ // 2
                P.mm(py[p0:p0 + 64, cc * 128:(cc + 1) * 128], x_tok[:, j, e_ * 64:(e_ + 1) * 64], WT[i2][:, :], True, False,
                     [x_tok.sub(j), WT[i2]], [py], r=False)
                P.mm(py[p0:p0 + 64, cc * 128:(cc + 1) * 128], S[:, e_ * 64:(e_ + 1) * 64], Cd[i2][:, :], False, True,
                     [S, Cd[i2]], [py], r=False)
            P.tt(R(xdt[:, :].rearrange("p (e q) -> p e q", e=8)), x_tok[:, j, :].rearrange("p (e q) -> p e q", e=8),
                 w_s[:, 0:8].unsqueeze(2).to_broadcast([128, 8, 64]), ALU.mult, [x_tok.sub(j), w_s], [xdt])
            pu = P.bank()
            P.mm(pu[:, :], B_tok[:, j, :], xdt[:, :], True, True, [B_tok.sub(j), xdt], [pu])
            P.tt(S[:, :].rearrange("p (e q) -> p e q", e=8), S[:, :].rearrange("p (e q) -> p e q", e=8),
                 et_s[:, 0:8].unsqueeze(2).to_broadcast([128, 8, 64]), ALU.mult, [S, et_s], [S])
            P.tt(S[:, :], S[:, :], pu[:, :], ALU.add, [S, pu], [S])
            if d == 0:
                P.dbg("d_GM", GM[:, :], [128, 128], [GM])
                P.dbg("d_WT", WT[1][:, :], [128, 128], [WT[1]])
                P.dbg("d_Lx", Lx[1][:, :], [128, 128], [Lx[1]])
                P.dbg("d_Cd", Cd[1][:, :], [128, 128], [Cd[1]])
                P.dbg("d_cum", cum_s[:, :], [128, 16], [cum_s])
                P.dbg("d_w", w_s[:, :], [128, 16], [w_s])
                P.dbg("d_S", S[:, :], [128, 512], [S])
            pyv = py[:, :].rearrange("p (c t) -> p c t", c=4)
            if d == 1:
                P.copy(ybuf[:, :, :], pyv, [py], [ybuf])
                P.dma(ybs[:, col0:col0 + 128].rearrange("(c p) t -> p c t", p=128), ybuf[:, :, :], [ybuf], [ybs_t])
            else:
                ub = ubuf[ui % 2]
                P.dma(ybuf[:, :, :], ybs[:, col0:col0 + 128].rearrange("(c p) t -> p c t", p=128), [ybs_t], [ybuf])
                P.tt(ybuf[:, :, :], ybuf[:, :, :], pyv, ALU.add, [ybuf, py], [ybuf])
                for cc in range(4):
                    P.stt(ub[:, cc, :], xTc[:, cc, j * 128:(j + 1) * 128], dsk_s[:, cc:cc + 1], ybuf[:, cc, :], ALU.mult, ALU.add,
                          [xTc.sub(cc), dsk_s, ybuf], [ub])
                P.dbg("d_ysum", ybuf[:, :, :], [128, 4, 128], [ybuf])
                P.dbg("d_zs", zs[:, :, :], [128, 4, W], [zs])
                P.tt(ub[:, :, :], ub[:, :, :], zs[:, :, j * 128:(j + 1) * 128], ALU.mult, [ub, zs], [ub])
                P.dma(uT[:, col0:col0 + 128].rearrange("(c p) t -> p c t", p=128), ub[:, :, :], [ub], [])

        tiles = [(0, 0, 256)] + [(256 + 256 * t, 256, NKEY) for t in range(32)]
        order_b = [tiles[0]] + tiles[:0:-1]
        ti = 0
        for (c0, q0, q1) in order_b:
            stage_a(ti, c0, q0, q1, False)
            for j in (1, 0):
                chunk_step(1, j, c0 + j * 128, 0)
            ti += 1
        ui = 0
        for (c0, q0, q1) in tiles:
            stage_a(ti, c0, q0, q1, True)
            for j in (0, 1):
                chunk_step(0, j, c0 + j * 128, ui)
                ui += 1
            ti += 1
        P.emit()
    return nc


def mamba_maps(inp, h_lat, h_ctx):
    W = np.asarray(inp["mb_in_w"][0], np.float32)
    cwf = np.asarray(inp["mb_conv_w"][0], np.float32)
    cbf = np.asarray(inp["mb_conv_b"][0], np.float32)
    maps = []
    for core in range(NCORES):
        b, g = core // 4, core % 4
        hT = np.ascontiguousarray(np.concatenate([h_ctx[b], h_lat[b]], axis=0).T)
        xcols = np.arange(g * 512, (g + 1) * 512)
        bcols = 2048 + np.arange(g * 128, (g + 1) * 128)
        ccols = 2048 + 512 + np.arange(g * 128, (g + 1) * 128)
        ch = np.concatenate([xcols, bcols, ccols])
        wxbc = np.ascontiguousarray(W[:, 2048 + ch])
        wz = np.ascontiguousarray(W[:, g * 512:(g + 1) * 512])
        dcols = np.concatenate([5120 + g * 8 + np.arange(8), 5120 + 32 + g * 8 + np.arange(8)])
        wdt = np.ascontiguousarray(W[:, dcols])
        cw = np.ascontiguousarray(cwf[:, ch].reshape(5, 6, 128).transpose(2, 1, 0).reshape(128, 30))
        cb = np.ascontiguousarray(cbf[ch].reshape(6, 128).T)
        dtb = np.ascontiguousarray(np.asarray(inp["mb_dt_bias"][0], np.float32)[:, g * 8:(g + 1) * 8].reshape(1, 16))
        alog = np.ascontiguousarray(np.asarray(inp["mb_a_log"][0], np.float32)[:, g * 8:(g + 1) * 8].reshape(1, 16))
        dsk = np.ascontiguousarray(np.repeat(np.asarray(inp["mb_d"][0], np.float32)[g * 8:(g + 1) * 8], 64).reshape(4, 128).T)
        maps.append({"hT": hT, "wz": wz, "wxbc": wxbc, "wdt": wdt, "cw": cw, "cb": cb, "dtb": dtb, "alog": alog, "dsk": dsk})
    return maps


def mamba_gather(res):
    u_lat = np.empty((2, 8192, 2048), np.float32)
    u_ctx = np.empty((2, 256, 2048), np.float32)
    for core in range(NCORES):
        b, g = core // 4, core % 4
        u = res[core]["uT"].T
        u_ctx[b, :, g * 512:(g + 1) * 512] = u[0:256]
        u_lat[b, :, g * 512:(g + 1) * 512] = u[256:]
    return u_lat, u_ctx


RW_SCALE = 0.606531
RW_EPS = 64e-5


def build_rwkv():
    nc = bass.Bass("TRN2", target_bir_lowering=False)
    with ExitStack() as stack:
        nc.dge_precook = False
        P = Prog(nc, stack)
        P.init_psum(8)

        def din(name, shape):
            return nc.dram_tensor(name, list(shape), F32, kind="ExternalInput").ap()

        hT = din("hT", [D, NKEY])
        mixr = din("mixr", [48, 128])
        wr = din("wr", [D, 256]); wk = din("wk", [D, 256]); wv = din("wv", [D, 256])
        w1 = din("w1", [2, D, 64]); w2 = din("w2", [2, 64, 256]); w0 = din("w0", [128, 4])
        a1 = din("a1", [2, D, 64]); a2 = din("a2", [2, 64, 256]); a0 = din("a0", [128, 4])
        g1 = din("g1", [D, 128]); g2 = din("g2", [128, 256])
        kkv = din("kkv", [128, 2]); kav = din("kav", [128, 2]); rkv = din("rkv", [128, 2])
        lng = din("lng", [2, 128, 64]); lnb = din("lnb", [2, 128, 64])
        oo = nc.dram_tensor("oo", [NKEY, 256], F32, kind="ExternalOutput").ap()
        ybs = nc.dram_tensor("ybs", [NKEY, 256], F32, kind="Internal").ap()
        ybs_t = TT(ybs, "ybs")

        ident, ones = make_consts(P)
        val = P.sb("val", [128, 64])
        for hs in range(2):
            P.op("pool", lambda e, hs=hs: e.iota(val[hs * 64:(hs + 1) * 64, :], pattern=[[1, 64]], base=0, channel_multiplier=-1,
                                                allow_small_or_imprecise_dtypes=True), (), [val])
        mgt = P.sb("mgt", [128, 64]); mge = P.sb("mge", [128, 64]); mlt = P.sb("mlt", [128, 64]); mle = P.sb("mle", [128, 64])
        identp = P.sb("identp", [128, 64])
        P.ts(mgt[:], val[:], 0.0, ALU.is_gt, [val], [mgt]); P.ts(mge[:], val[:], 0.0, ALU.is_ge, [val], [mge])
        P.ts(mlt[:], val[:], 0.0, ALU.is_lt, [val], [mlt]); P.ts(mle[:], val[:], 0.0, ALU.is_le, [val], [mle])
        P.ts(identp[:], val[:], 0.0, ALU.is_equal, [val], [identp])
        maskT = [P.sb("maskTf", [128, 128]), P.sb("maskTb", [128, 128])]
        P.copy(maskT[0][:, 0:64], mgt[:], [mgt], [maskT[0]]); P.copy(maskT[0][:, 64:128], mge[:], [mge], [maskT[0]])
        P.copy(maskT[1][:, 0:64], mlt[:], [mlt], [maskT[1]]); P.copy(maskT[1][:, 64:128], mle[:], [mle], [maskT[1]])
        maskA = [mlt, mgt]
        blk = P.sb("blk", [128, 128])
        P.memset(blk[:], 0.0, [blk])
        P.memset(blk[0:64, 0:64], 1.0, [blk]); P.memset(blk[64:128, 64:128], 1.0, [blk])
        half = P.sb("half", [128, 2])
        P.memset(half[:], 0.5, [half])
        tiny = P.sb("tiny", [128, 1])
        P.memset(tiny[:], RW_EPS, [tiny])

        def wload(name, ap, shape, rr=True):
            t = P.sb(name, shape)
            P.dma(t[:], ap, [], [t], r=rr)
            return t
        wr_s = wload("wr_s", wr.rearrange("(kc p) n -> p kc n", p=128), [128, 8, 256])
        wk_s = wload("wk_s", wk.rearrange("(kc p) n -> p kc n", p=128), [128, 8, 256])
        wv_s = wload("wv_s", wv.rearrange("(kc p) n -> p kc n", p=128), [128, 8, 256])
        g1_s = wload("g1_s", g1.rearrange("(kc p) n -> p kc n", p=128), [128, 8, 128])
        g2_s = wload("g2_s", g2, [128, 256])
        w1_s = [wload(f"w1_{d}", w1[d].rearrange("(kc p) n -> p kc n", p=128), [128, 8, 64]) for d in range(2)]
        a1_s = [wload(f"a1_{d}", a1[d].rearrange("(kc p) n -> p kc n", p=128), [128, 8, 64]) for d in range(2)]
        w2_s = [wload(f"w2_{d}", w2[d], [64, 256]) for d in range(2)]
        a2_s = [wload(f"a2_{d}", a2[d], [64, 256]) for d in range(2)]
        w0_s = wload("w0_s", w0, [128, 4], False); a0_s = wload("a0_s", a0, [128, 4], False)
        kk_s = wload("kk_s", kkv, [128, 2], False); ka_s = wload("ka_s", kav, [128, 2], False); rk_s = wload("rk_s", rkv, [128, 2], False)
        omka = P.sb("omka", [128, 2])
        P.ts(omka[:], ka_s[:], -1.0, ALU.mult, [ka_s], [omka], s2=1.0, op1=ALU.add)
        lng_s = [wload(f"lng{i}", lng[i], [128, 64], False) for i in range(2)]
        lnb_s = [wload(f"lnb{i}", lnb[i], [128, 64], False) for i in range(2)]
        mixc = P.sb("mixc", [128, 48])
        load_cols(P, ident, mixc, mixc[:], mixr, 48)

        W = 256
        NCH = 4
        ht = [P.sb(f"ht{i}", [128, 8, W + 2]) for i in range(2)]
        xx = P.sb("xx", [128, 8, W])
        xm = [P.sb(f"xm{i}", [128, 8, W]) for i in range(2)]
        rT = P.sb("rT", [128, 2, W]); kT = P.sb("kT", [128, 2, W]); kkT = P.sb("kkT", [128, 2, W])
        lwT = P.sb("lwT", [128, 2, W]); clT = P.sb("clT", [128, 2, W]); cleT = P.sb("cleT", [128, 2, W])
        aT = [P.sb(f"aT{d}", [128, 2, W]) for d in range(2)]
        kdT = [P.sb(f"kdT{d}", [128, 2, W]) for d in range(2)]
        bT = P.sb("bT", [128, 2, W])
        e1 = P.sb("e1", [128, 2, W]); e2 = P.sb("e2", [128, 2, W]); e3 = P.sb("e3", [128, 2, W])
        lam = P.sb("lam", [128, 2, NCH])
        KR = P.sb("KR", [128, 2, NCH, 128]); BK = P.sb("BK", [128, 2, NCH, 128])
        tw = P.sb("tw", [64, W]); t1 = P.sb("t1", [128, W])
        Vt = P.sb("Vt", [128, NCH, 2, 64]); Gt = P.sb("Gt", [128, NCH, 2, 64])
        prodT = P.sb("prodT", [128, 2, W]); bsc = P.sb("bsc", [128, NCH, 2, 2])
        sq = P.sb("sqk", [128, W]); rn = P.sb("rnk", [128, W])
        onesW = P.sb("onesW", [128, W])
        P.memset(onesW[:], 1.0, [onesW])
        ST = [[P.sb(f"ST{d}{hp}", [128, 64]) for hp in range(2)] for d in range(2)]
        for d_ in range(2):
            for hp in range(2):
                P.memset(ST[d_][hp][:], 0.0, [ST[d_][hp]])

        class Slot:
            pass
        slots = []
        for si in range(4):
            s_ = Slot()
            for nm, shp in (("AWu", [128, 128]), ("BWv", [128, 128]), ("PPa", [128, 128]), ("PPb", [128, 128]), ("X", [128, 64]),
                            ("RH", [128, 128]), ("UK", [128, 128]), ("BKt", [128, 128]), ("MpT", [128, 64]), ("Nc", [128, 64]),
                            ("R2T", [128, 64]), ("Y0", [128, 64])):
                setattr(s_, nm, P.sb(f"{nm}{si}", shp))
            slots.append(s_)
        ysb = [P.sb(f"ysb{i}", [128, 64]) for i in range(4)]
        ybb = [P.sb(f"ybb{i}", [128, 64]) for i in range(4)]
        stt6 = [P.sb(f"st6{i}", [128, 6]) for i in range(2)]
        mv = [P.sb(f"mv{i}", [128, 4]) for i in range(2)]

        def stage_a(ti, c0, q0, q1, dirs, fwd):
            h = ht[ti % 2]
            lo, hi = max(c0 - 1, q0), min(c0 + W + 1, q1)
            off = lo - (c0 - 1)
            n = hi - lo
            if off > 0:
                P.memset(h[:, :, 0:off], 0.0, [h])
            if off + n < W + 2:
                P.memset(h[:, :, off + n:W + 2], 0.0, [h])
            for kc in range(8):
                P.dma(h[:, kc, off:off + n], hT[kc * 128:(kc + 1) * 128, lo:hi], [], [h])
            P.tt(xx[:, :, :], h[:, :, 0:W], h[:, :, 2:W + 2], ALU.add, [h], [xx])
            P.stt(xx[:, :, :], xx[:, :, :], 0.5, h[:, :, 1:W + 1], ALU.mult, ALU.subtract, [xx, h], [xx])
            mi = [0]

            def mixed(j):
                m_ = xm[mi[0] % 2]
                mi[0] += 1
                for kc in range(8):
                    P.stt(R(m_[:, kc, :]), xx[:, kc, :], mixc[:, j * 8 + kc:j * 8 + kc + 1], h[:, kc, 1:W + 1], ALU.mult, ALU.add,
                          [xx, mixc, h], [m_])
                return m_

            def proj2(m_, w_s, dst):
                for oc in range(2):
                    pb = P.bank()
                    for kc in range(8):
                        P.mm(pb[:, 0:W], w_s[:, kc, oc * 128:(oc + 1) * 128], m_[:, kc, :], kc == 0, kc == 7, [w_s, m_], [pb])
                    P.copy(dst[:, oc, :], pb[:, 0:W], [pb], [dst], eng="act")

            def lora(m_, l1_s, l2_s, bias_s, d, dst, mid_func):
                pb = P.bank()
                for kc in range(8):
                    P.mm(pb[0:64, 0:W], l1_s[:, kc, :], m_[:, kc, :], kc == 0, kc == 7, [l1_s, m_], [pb])
                P.act(R(tw[:, :]), pb[0:64, 0:W], mid_func, [pb], [tw])
                for oc in range(2):
                    p2 = P.bank()
                    P.mm(p2[:, 0:W], l2_s[:, oc * 128:(oc + 1) * 128], tw[:, :], True, True, [l2_s, tw], [p2])
                    P.act(dst[:, oc, :], p2[:, 0:W], AF.Sigmoid, [p2, bias_s], [dst], bias=bias_s[:, d * 2 + oc:d * 2 + oc + 1])

            m_ = mixed(0)
            proj2(m_, wr_s, rT)
            m_ = mixed(1)
            dd = dirs[0] if len(dirs) == 1 else 0
            lora(m_, w1_s[dd], w2_s[dd], w0_s, dd, lwT, AF.Tanh)
            P.ts(lwT[:, :, :], lwT[:, :, :], -RW_SCALE, ALU.mult, [lwT], [lwT])
            m_ = mixed(2)
            proj2(m_, wk_s, kT)
            m_ = mixed(3)
            for j4 in range(NCH):
                pv = P.bank()
                for hq in range(4):
                    p0 = (hq % 2) * 64
                    hp = hq // 2
                    for kc in range(8):
                        P.mm(pv[p0:p0 + 64, hp * 64:(hp + 1) * 64], m_[:, kc, j4 * 64:(j4 + 1) * 64], wv_s[:, kc, hq * 64:(hq + 1) * 64],
                             kc == 0, kc == 7, [m_, wv_s], [pv], r=False)
                P.copy(Vt[:, j4, :, :], pv[:, 0:128].rearrange("p (a b) -> p a b", a=2), [pv], [Vt.sub(j4)], eng="act")
            m_ = mixed(4)
            adirs = [0, 1] if fwd else dirs
            for d in adirs:
                lora(m_, a1_s[d], a2_s[d], a0_s, d, aT[d], AF.Copy)
            if fwd:
                m_ = mixed(5)
                pb = P.bank()
                for kc in range(8):
                    P.mm(pb[:, 0:W], g1_s[:, kc, :], m_[:, kc, :], kc == 0, kc == 7, [g1_s, m_], [pb])
                P.act(t1[:, :], pb[:, 0:W], AF.Sigmoid, [pb], [t1])
                for j4 in range(NCH):
                    pg = P.bank()
                    for hq in range(4):
                        p0 = (hq % 2) * 64
                        hp = hq // 2
                        P.mm(pg[p0:p0 + 64, hp * 64:(hp + 1) * 64], t1[:, j4 * 64:(j4 + 1) * 64], g2_s[:, hq * 64:(hq + 1) * 64],
                             True, True, [t1, g2_s], [pg], r=False)
                    P.copy(Gt[:, j4, :, :], pg[:, 0:128].rearrange("p (a b) -> p a b", a=2), [pg], [Gt.sub(j4)], eng="act")
            for oc in range(2):
                P.ts(kkT[:, oc, :], kT[:, oc, :], kk_s[:, oc:oc + 1], ALU.mult, [kT, kk_s], [kkT])
                P.act(sq[:, :], kkT[:, oc, :], AF.Square, [kkT], [sq])
                pb = P.bank()
                P.mm(pb[:, 0:W], blk[:, :], sq[:, :], True, True, [blk, sq], [pb], r=False)
                P.ts(rn[:, :], pb[:, 0:W], 1e-24, ALU.max, [pb], [rn])
                P.act(rn[:, :], rn[:, :], AF.Ln, [rn], [rn])
                P.act(rn[:, :], rn[:, :], AF.Exp, [rn], [rn], scale=-0.5)
                P.tt(kkT[:, oc, :], kkT[:, oc, :], rn[:, :], ALU.mult, [kkT, rn], [kkT])
                for d in adirs:
                    P.ts(kdT[d][:, oc, :], aT[d][:, oc, :], ka_s[:, oc:oc + 1], ALU.mult, [aT[d], ka_s, omka], [kdT[d]],
                         s2=omka[:, oc:oc + 1], op1=ALU.add)
                    P.tt(kdT[d][:, oc, :], kdT[d][:, oc, :], kT[:, oc, :], ALU.mult, [kdT[d], kT], [kdT[d]])
            if fwd:
                P.tt(prodT[:, :, :], kdT[0][:, :, :], kdT[1][:, :, :], ALU.add, [kdT[0], kdT[1]], [prodT])
                for oc in range(2):
                    P.stt(prodT[:, oc, :], rT[:, oc, :], rk_s[:, oc:oc + 1], prodT[:, oc, :], ALU.mult, ALU.mult, [rT, rk_s, prodT], [prodT])
                for j4 in range(NCH):
                    pb = P.bank()
                    for hq in range(4):
                        p0 = (hq % 2) * 64
                        hp = hq // 2
                        P.mm(pb[p0:p0 + 64, hp * 2:hp * 2 + 2], prodT[p0:p0 + 64, hp, j4 * 64:(j4 + 1) * 64], half[p0:p0 + 64, 0:2],
                             True, True, [prodT, half], [pb], r=False)
                    P.copy(bsc[:, j4, :, :], pb[:, 0:4].rearrange("p (a b) -> p a b", a=2), [pb], [bsc.sub(j4)])
            d = dirs[0] if len(dirs) == 1 else 0
            P.tt(bT[:, :, :], kkT[:, :, :], aT[d][:, :, :], ALU.mult, [kkT, aT[d]], [bT])
            for oc in range(2):
                P.op("dve", lambda e, oc=oc: e.tensor_tensor_scan(out=clT[:, oc, :], data0=onesW[:, :], data1=lwT[:, oc, :], initial=0.0,
                                                                   op0=ALU.mult, op1=ALU.add), [onesW, lwT], [clT])
                for j4 in range(NCH - 1, 0, -1):
                    P.ts(clT[:, oc, j4 * 64:(j4 + 1) * 64], clT[:, oc, j4 * 64:(j4 + 1) * 64], clT[:, oc, j4 * 64 - 1:j4 * 64], ALU.subtract,
                         [clT], [clT])
                if d == 0:
                    P.tt(cleT[:, oc, :], clT[:, oc, :], lwT[:, oc, :], ALU.subtract, [clT, lwT], [cleT])
                else:
                    for j4 in range(NCH):
                        P.ts(cleT[:, oc, j4 * 64:(j4 + 1) * 64], clT[:, oc, j4 * 64:(j4 + 1) * 64], -1.0, ALU.mult, [clT], [cleT],
                             s2=clT[:, oc, j4 * 64 + 63:j4 * 64 + 64], op1=ALU.add)
                    P.tt(clT[:, oc, :], cleT[:, oc, :], lwT[:, oc, :], ALU.add, [cleT, lwT, clT], [clT])
            P.act(e1[:, :, :], cleT[:, :, :], AF.Exp, [cleT], [e1])
            P.act(e2[:, :, :], clT[:, :, :], AF.Exp, [clT], [e2], scale=-1.0)
            P.act(e3[:, :, :], clT[:, :, :], AF.Exp, [clT], [e3])
            for oc in range(2):
                e3v = e3[:, oc, :].rearrange("p (c t) -> p c t", c=NCH)
                P.copy(lam[:, oc, :], e3v[:, :, 63] if d == 0 else e3v[:, :, 0], [e3], [lam])
                kkv_ = kkT[:, oc, :].rearrange("p (c t) -> p c t", c=NCH)
                P.tt(KR[:, oc, :, 0:64], kkv_, e1[:, oc, :].rearrange("p (c t) -> p c t", c=NCH), ALU.mult, [kkT, e1], [KR])
                P.tt(KR[:, oc, :, 64:128], rT[:, oc, :].rearrange("p (c t) -> p c t", c=NCH), e3v, ALU.mult, [rT, e3], [KR])
                e2v = e2[:, oc, :].rearrange("p (c t) -> p c t", c=NCH)
                P.tt(BK[:, oc, :, 0:64], bT[:, oc, :].rearrange("p (c t) -> p c t", c=NCH), e2v, ALU.mult, [bT, e2], [BK])
                P.tt(BK[:, oc, :, 64:128], kdT[d][:, oc, :].rearrange("p (c t) -> p c t", c=NCH), e2v, ALU.mult, [kdT[d], e2], [BK])

        def pre(sl, hp, d, j4):
            kr = KR[:, hp, j4, :]
            bk = BK[:, hp, j4, :]
            V = Vt[:, j4, hp, :]
            hs2 = [(0, 64), (64, 128)]
            pA = P.bank()
            for (a, b) in hs2:
                P.mm(pA[a:b, 0:128], bk[a:b, 0:64], kr[a:b, :], True, True, [BK, KR], [pA], r=False)
                P.mm(pA[a:b, 128:256], bk[a:b, 64:128], kr[a:b, :], True, True, [BK, KR], [pA], r=False)
                P.mm(pA[a:b, 256:320], kr[a:b, 0:64], bk[a:b, 0:64], True, True, [BK, KR], [pA], r=False)
            P.tt(sl.AWu[:, :], pA[:, 0:128], maskT[d][:, :], ALU.mult, [pA, maskT[d]], [sl.AWu])
            P.tt(sl.BWv[:, :], pA[:, 128:256], maskT[d][:, :], ALU.mult, [pA, maskT[d]], [sl.BWv])
            P.stt(sl.PPa[:, 0:64], pA[:, 256:320], -1.0, maskA[d][:, :], ALU.mult, ALU.mult, [pA, maskA[d]], [sl.PPa])
            P.ts(sl.PPa[:, 64:128], sl.AWu[:, 0:64], -1.0, ALU.mult, [sl.AWu], [sl.PPa])
            P.tt(sl.X[:, :], identp[:, :], sl.PPa[:, 64:128], ALU.add, [identp, sl.PPa], [sl.X])
            cur, nxt = sl.PPa, sl.PPb
            for m in range(1, 6):
                pL = P.bank()
                for (a, b) in hs2:
                    P.mm(pL[a:b, 0:64], cur[a:b, 64:128], cur[a:b, 0:64], True, True, [cur], [pL], r=False)
                    if m < 5:
                        P.mm(pL[a:b, 64:128], cur[a:b, 0:64], cur[a:b, 64:128], True, True, [cur], [pL], r=False)
                if m < 5:
                    P.copy(nxt[:, :], pL[:, 0:128], [pL], [nxt], eng="act")
                else:
                    P.copy(nxt[:, 0:64], pL[:, 0:64], [pL], [nxt], eng="act")
                pX = P.bank()
                for (a, b) in hs2:
                    P.mm(pX[a:b, 0:64], nxt[a:b, 0:64], sl.X[a:b, :], True, True, [nxt, sl.X], [pX], r=False)
                P.tt(sl.X[:, :], sl.X[:, :], pX[:, 0:64], ALU.add, [sl.X, pX], [sl.X])
                cur, nxt = nxt, cur
            pR = P.bank()
            for (a, b) in hs2:
                P.mm(pR[a:b, 0:64], sl.BWv[a:b, 0:64], V[a:b, :], True, True, [sl.BWv, Vt.sub(j4)], [pR], r=False)
                P.mm(pR[a:b, 64:128], kr[a:b, 0:64], ident[a:b, a:b], True, True, [KR, ident], [pR], r=False)
            P.copy(sl.RH[:, :], pR[:, 0:128], [pR], [sl.RH], eng="act")
            pXR = P.bank()
            for (a, b) in hs2:
                P.mm(pXR[a:b, 0:128], sl.X[a:b, :], sl.RH[a:b, :], True, True, [sl.X, sl.RH], [pXR], r=False)
            P.ts(sl.UK[:, 0:64], pXR[:, 0:64], -1.0, ALU.mult, [pXR], [sl.UK])
            P.copy(sl.UK[:, 64:128], pXR[:, 64:128], [pXR], [sl.UK], eng="act")
            pT = P.bank()
            for (a, b) in hs2:
                P.mm(pT[a:b, 0:64], bk[a:b, 0:64], ident[a:b, a:b], True, True, [BK, ident], [pT], r=False)
                P.mm(pT[a:b, 64:128], bk[a:b, 64:128], ident[a:b, a:b], True, True, [BK, ident], [pT], r=False)
            P.copy(sl.BKt[:, :], pT[:, 0:128], [pT], [sl.BKt], eng="act")
            pM = P.bank()
            for (a, b) in hs2:
                P.mm(pM[a:b, 0:64], sl.UK[a:b, 64:128], sl.BKt[a:b, 0:64], True, True, [sl.UK, sl.BKt], [pM], r=False)
                P.mm(pM[a:b, 64:128], sl.BKt[a:b, 0:64], sl.UK[a:b, 0:64], True, False, [sl.UK, sl.BKt], [pM], r=False)
                P.mm(pM[a:b, 64:128], sl.BKt[a:b, 64:128], V[a:b, :], False, True, [sl.BKt, Vt.sub(j4)], [pM], r=False)
                P.mm(pM[a:b, 128:192], sl.UK[a:b, 64:128], sl.AWu[a:b, 64:128], True, True, [sl.UK, sl.AWu], [pM], r=False)
            P.tt(sl.MpT[:, :], identp[:, :], pM[:, 0:64], ALU.subtract, [identp, pM], [sl.MpT])
            P.ts(sl.Nc[:, :], pM[:, 64:128], lam[:, hp, j4:j4 + 1], ALU.mult, [pM, lam], [sl.Nc])
            P.tt(sl.R2T[:, :], kr[:, 64:128], pM[:, 128:192], ALU.subtract, [KR, pM], [sl.R2T])
            pY = P.bank()
            for (a, b) in hs2:
                P.mm(pY[a:b, 0:64], sl.AWu[a:b, 64:128], sl.UK[a:b, 0:64], True, False, [sl.AWu, sl.UK], [pY], r=False)
                P.mm(pY[a:b, 0:64], sl.BWv[a:b, 64:128], V[a:b, :], False, True, [sl.BWv, Vt.sub(j4)], [pY], r=False)
            P.copy(sl.Y0[:, :], pY[:, 0:64], [pY], [sl.Y0], eng="act")

        def seq(sl, hp, d, j4, col0, k):
            S = ST[d][hp]
            hs2 = [(0, 64), (64, 128)]
            pYs = P.bank()
            for (a, b) in hs2:
                P.mm(pYs[a:b, 0:64], sl.R2T[a:b, :], S[a:b, :], True, True, [sl.R2T, S], [pYs], r=False)
            ys = ysb[k % 4]
            P.tt(ys[:, :], sl.Y0[:, :], pYs[:, 0:64], ALU.add, [sl.Y0, pYs], [ys])
            pS = P.bank()
            for (a, b) in hs2:
                P.mm(pS[a:b, 0:64], sl.MpT[a:b, :], S[a:b, :], True, True, [sl.MpT, S], [pS], r=False)
            P.stt(S[:, :], pS[:, 0:64], lam[:, hp, j4:j4 + 1], sl.Nc[:, :], ALU.mult, ALU.add, [pS, lam, sl.Nc], [S])
            if d == 1:
                for hs, (a, b) in enumerate(hs2):
                    hq = hp * 2 + hs
                    P.dma(ybs[col0:col0 + 64, hq * 64:(hq + 1) * 64], ys[a:b, :], [ys], [ybs_t])
            else:
                yb = ybb[k % 4]
                for hs, (a, b) in enumerate(hs2):
                    hq = hp * 2 + hs
                    P.dma(yb[a:b, :], ybs[col0:col0 + 64, hq * 64:(hq + 1) * 64], [ybs_t], [yb])
                P.tt(ys[:, :], ys[:, :], yb[:, :], ALU.add, [ys, yb], [ys])
                s6 = stt6[k % 2]
                m2 = mv[k % 2]
                P.op("dve", lambda e, s6=s6, ys=ys: e.bn_stats(out=s6[:, :], in_=ys[:, :]), [ys], [s6])
                P.op("dve", lambda e, s6=s6, m2=m2: e.bn_aggr(out=m2[:, 0:2], in_=s6[:, :]), [s6], [m2])
                P.act(m2[:, 2:3], m2[:, 1:2], AF.Sqrt, [m2, tiny], [m2], bias=tiny[:, 0:1])
                P.op("dve", lambda e, m2=m2: e.reciprocal(out=m2[:, 3:4], in_=m2[:, 2:3]), [m2], [m2])
                P.ts(ys[:, :], ys[:, :], m2[:, 0:1], ALU.subtract, [ys, m2], [ys], s2=m2[:, 3:4], op1=ALU.mult)
                P.tt(ys[:, :], ys[:, :], lng_s[hp][:, :], ALU.mult, [ys, lng_s[hp]], [ys])
                P.tt(ys[:, :], ys[:, :], lnb_s[hp][:, :], ALU.add, [ys, lnb_s[hp]], [ys])
                P.stt(yb[:, :], Vt[:, j4, hp, :], bsc[:, j4, hp, 0:1], ys[:, :], ALU.mult, ALU.add, [Vt.sub(j4), bsc.sub(j4), ys], [yb])
                P.tt(yb[:, :], yb[:, :], Gt[:, j4, hp, :], ALU.mult, [yb, Gt.sub(j4)], [yb])
                for hs, (a, b) in enumerate(hs2):
                    hq = hp * 2 + hs
                    P.dma(oo[col0:col0 + 64, hq * 64:(hq + 1) * 64], yb[a:b, :], [yb], [])

        tiles = [(0, 0, 256)] + [(256 + 256 * t, 256, NKEY) for t in range(32)]
        order_b = [tiles[0]] + tiles[:0:-1]
        ti = 0
        k = 0
        for d, order, chunks in ((1, order_b, (3, 2, 1, 0)), (0, tiles, (0, 1, 2, 3))):
            for (c0, q0, q1) in order:
                stage_a(ti, c0, q0, q1, [d], d == 0)
                ti += 1
                units = [(j4, hp) for j4 in chunks for hp in range(2)]
                pre(slots[k % 4], units[0][1], d, units[0][0])
                for ui, (j4, hp) in enumerate(units):
                    if ui + 1 < len(units):
                        pre(slots[(k + 1) % 4], units[ui + 1][1], d, units[ui + 1][0])
                    seq(slots[k % 4], hp, d, j4, c0 + j4 * 64, k)
                    k += 1
        P.emit()
    return nc


def rwkv_maps(inp, h_lat, h_ctx):
    f = lambda k: np.asarray(inp[k][0], np.float32)
    rkv, w0, w1, w2 = f("rw_rkv_w"), f("rw_w0"), f("rw_w1"), f("rw_w2")
    a0, a1, a2, g1, g2 = f("rw_a0"), f("rw_a1"), f("rw_a2"), f("rw_g1"), f("rw_g2")
    k_k, k_a, r_k, ln_g, ln_b = f("rw_k_k"), f("rw_k_a"), f("rw_r_k").reshape(-1), f("rw_ln_g"), f("rw_ln_b")
    mixr = rows128(f("rw_mix"))
    maps = []
    for core in range(NCORES):
        b, hg = core // 4, core % 4
        sl = slice(hg * 256, (hg + 1) * 256)
        hT = np.ascontiguousarray(np.concatenate([h_ctx[b], h_lat[b]], axis=0).T)

        def pc(v):
            return np.ascontiguousarray(v.reshape(2, 128).T)

        def pc2(v2):
            return np.ascontiguousarray(v2.reshape(2, 2, 128).transpose(2, 0, 1).reshape(128, 4))

        def pairrows(v):
            hh = v.reshape(2, 2, 1, 64)
            return np.ascontiguousarray(np.broadcast_to(hh, (2, 2, 64, 64)).reshape(2, 128, 64))
        maps.append({
            "hT": hT, "mixr": mixr,
            "wr": np.ascontiguousarray(rkv[0][:, sl]), "wk": np.ascontiguousarray(rkv[1][:, sl]), "wv": np.ascontiguousarray(rkv[2][:, sl]),
            "w1": w1, "w2": np.ascontiguousarray(w2[:, :, sl]), "w0": pc2(w0[:, sl]),
            "a1": a1, "a2": np.ascontiguousarray(a2[:, :, sl]), "a0": pc2(a0[:, sl]),
            "g1": g1, "g2": np.ascontiguousarray(g2[:, sl]),
            "kkv": pc(k_k[sl]), "kav": pc(k_a[sl]), "rkv": pc(r_k[sl]),
            "lng": pairrows(ln_g[sl]), "lnb": pairrows(ln_b[sl]),
        })
    return maps


def rwkv_gather(res):
    o_lat = np.empty((2, 8192, 1024), np.float32)
    o_ctx = np.empty((2, 256, 1024), np.float32)
    for core in range(NCORES):
        b, hg = core // 4, core % 4
        o = res[core]["oo"]
        o_ctx[b, :, hg * 256:(hg + 1) * 256] = o[0:256]
        o_lat[b, :, hg * 256:(hg + 1) * 256] = o[256:]
    return o_lat, o_ctx


def _get(key, builder):
    if key not in _NC_CACHE:
        _NC_CACHE[key] = builder()
    return _NC_CACHE[key]


def attn_maps(inp, h_lat, h_ctx):
    cosT, sinS, prot = rope_tables()
    Wq = np.asarray(inp["at_qkv_w"][0], np.float32)
    qg = np.asarray(inp["at_q_g"][0], np.float32).reshape(64, 1)
    kg = np.asarray(inp["at_k_g"][0], np.float32).reshape(64, 1)
    maps = []
    for core in range(NCORES):
        b, kv = core // 4, core % 4
        hT = np.ascontiguousarray(np.concatenate([h_ctx[b], h_lat[b]], axis=0).T)
        maps.append({"hT": hT, "wq": np.ascontiguousarray(Wq[:, kv * 256:(kv + 1) * 256]),
                     "wk": np.ascontiguousarray(Wq[:, 1024 + kv * 64:1024 + (kv + 1) * 64]),
                     "wv": np.ascontiguousarray(Wq[:, 1280 + kv * 64:1280 + (kv + 1) * 64]),
                     "qg": qg, "kg": kg, "cosT": cosT, "sinS": sinS, "prot": prot})
    return maps


def attn_gather(res):
    o = np.zeros((2, 8192, 1024), np.float32)
    for core in range(NCORES):
        b, kv = core // 4, core % 4
        o[b, :, kv * 256:(kv + 1) * 256] = res[core]["oT"].T
    return o


def kernel(**inp):
    f32 = lambda a: np.asarray(a, np.float32)
    x, c, ctx, c_ctx = f32(inp["x"]), f32(inp["c"]), f32(inp["ctx"]), f32(inp["c_ctx"])
    mod_w, mod_b = f32(inp["mod_w"]), f32(inp["mod_b"])
    n1g, n2g = f32(inp["norm1_g"]), f32(inp["norm2_g"])
    cv = [cvec_for(c, c_ctx, core) for core in range(NCORES)]

    def nxt_params(i):
        return {"mod_w_n": mod_w[i], "mod_b_n": rows128(mod_b[i]), "n1g_n": rows128(n1g[i])}

    def cur_params(i):
        return {"mod_w": mod_w[i], "mod_b": rows128(mod_b[i]), "n2g": rows128(n2g[i])}

    xs = to_cores_T(x, ctx)
    res = run(_get(("ts", None, None, "norm"), lambda: build_ts(0, None, None, "norm")),
              [dict(xT=xs[k], cvec=cv[k], **nxt_params(0)) for k in range(NCORES)])
    h_lat, h_ctx = from_cores_T([r["hT_out"] for r in res])
    mres = run(_get("mamba", build_mamba), mamba_maps(inp, h_lat, h_ctx))
    u_lat, u_ctx = mamba_gather(mres)
    ms = to_cores_T(u_lat, u_ctx)
    res = run(_get(("ts", "mamba", "dense", "norm"), lambda: build_ts(0, "mamba", "dense", "norm")),
              [dict(xT=xs[k], mT=ms[k], cvec=cv[k], mb_ng=rows128(f32(inp["mb_norm_g"])[0]), w_post=f32(inp["mb_out_w"])[0],
                    w_in=f32(inp["ff_in_w"])[0], w_out=f32(inp["ff_out_w"])[0], **cur_params(0), **nxt_params(1)) for k in range(NCORES)])
    xs = [r["xT_out"] for r in res]
    h_lat, h_ctx = from_cores_T([r["hT_out"] for r in res])
    rres = run(_get("rwkv", build_rwkv), rwkv_maps(inp, h_lat, h_ctx))
    o_lat, o_ctx = rwkv_gather(rres)
    ms = to_cores_T(o_lat, o_ctx)
    ts_moe_n = _get(("ts", "lin", "moe", "norm"), lambda: build_ts(1, "lin", "moe", "norm"))
    res = run(ts_moe_n,
              [dict(xT=xs[k], mT=ms[k], cvec=cv[k], w_post=f32(inp["rw_out_w"])[0], router=f32(inp["moe_router_w"])[0],
                    w_in=f32(inp["moe_in_w"])[0], w_out=f32(inp["moe_out_w"])[0], **cur_params(1), **nxt_params(2)) for k in range(NCORES)])
    xs = [r["xT_out"] for r in res]
    h_lat, h_ctx = from_cores_T([r["hT_out"] for r in res])
    pin = pool_inputs(h_lat, h_ctx)
    res = run(_get(("ts", "pool", "dense", "norm"), lambda: build_ts(2, "pool", "dense", "norm")),
              [dict(xT=xs[k], cvec=cv[k], pl_w=f32(inp["pl_w"])[0], pl_scale=rows128(f32(inp["pl_scale"])[0]),
                    w_in=f32(inp["ff_in_w"])[1], w_out=f32(inp["ff_out_w"])[1], **pin[k], **cur_params(2), **nxt_params(3))
               for k in range(NCORES)])
    xs = [r["xT_out"] for r in res]
    h_lat, h_ctx = from_cores_T([r["hT_out"] for r in res])
    ares = run(_get("attn", build_attn), attn_maps(inp, h_lat, h_ctx))
    o_lat = attn_gather(ares)
    ms = to_cores_T(o_lat, np.zeros((2, 256, 1024), np.float32))
    res = run(_get(("ts", "lin", "moe", "final"), lambda: build_ts(3, "lin", "moe", "final")),
              [dict(xT=xs[k], mT=ms[k], cvec=cv[k], w_post=f32(inp["at_out_w"])[0], router=f32(inp["moe_router_w"])[1],
                    w_in=f32(inp["moe_in_w"])[1], w_out=f32(inp["moe_out_w"])[1], fin_g=rows128(f32(inp["final_g"])), **cur_params(3))
               for k in range(NCORES)])
    out_lat, _ = from_cores_T([r["out_T"] for r in res])
    return out_lat
```

```python
import numpy as np
from contextlib import ExitStack
import concourse.bass as bass
import concourse.mybir as mybir
from concourse.bass_utils import run_bass_kernel_spmd

F32 = mybir.dt.float32
F32R = mybir.dt.float32r
I32 = mybir.dt.int32
AF = mybir.ActivationFunctionType
ALU = mybir.AluOpType
AX = mybir.AxisListType


def R(ap):
    return ap.bitcast(F32R)

D = 1024
KC = 8
NCORES = 8
EPS = 1e-6


class Buf:
    __slots__ = ("lw", "rd", "name", "parent")

    def __init__(self, name="", parent=None):
        self.lw = None
        self.rd = {}
        self.name = name
        self.parent = parent


class TT:
    def __init__(self, t, name):
        self.t = t
        self.name = name
        self.b = Buf(name)
        self.subs = {}

    def sub(self, key):
        if key not in self.subs:
            self.subs[key] = Buf(f"{self.name}/{key}", self.b)
        return self.subs[key]

    def __getitem__(self, idx):
        return self.t[idx]


class _SubView:
    def __init__(self, tt, subs):
        self.tt = tt
        self.subs = subs

    def __getitem__(self, idx):
        return self.tt.t[idx]


def _bufs(lst):
    out = []
    for x in lst:
        if x is None:
            continue
        if isinstance(x, TT):
            out.append(x.b)
            out.extend(x.subs.values())
        elif isinstance(x, _SubView):
            out.extend(x.subs)
        else:
            out.append(x)
    return out


class Prog:
    NDS = 24

    def __init__(self, nc, stack):
        self.nc = nc
        self.stack = stack
        self.eng = {"pe": nc.tensor, "dve": nc.vector, "act": nc.scalar, "pool": nc.gpsimd, "sp": nc.sync}
        self.sem = {k: stack.enter_context(nc.semaphore("s_" + k)) for k in self.eng}
        self.cnt = {k: 0 for k in self.eng}
        self.waited = {k: {} for k in self.eng}
        self.ops = {k: [] for k in self.eng}
        self.dsem = [stack.enter_context(nc.semaphore(f"d{i}")) for i in range(self.NDS)]
        self.dval = [0] * self.NDS
        self.dnext = 0
        self.nalloc = 0
        self.psum_banks = None
        self.psum_next = 0

    def sb(self, name, shape, dtype=F32):
        self.nalloc += 1
        t = self.stack.enter_context(self.nc.sbuf_tensor(f"{name}_{self.nalloc}", list(shape), dtype))
        return TT(t, name)

    def ps(self, name, shape, dtype=F32):
        self.nalloc += 1
        t = self.stack.enter_context(self.nc.psum_tensor(f"{name}_{self.nalloc}", list(shape), dtype))
        return TT(t, name)

    def init_psum(self, n=8):
        self.psum_banks = [self.ps(f"bank{i}", [128, 512]) for i in range(n)]

    def bank(self):
        b = self.psum_banks[self.psum_next % len(self.psum_banks)]
        self.psum_next += 1
        return b

    def dram(self, name, shape, dtype=F32, kind="Internal"):
        t = self.nc.dram_tensor(name, list(shape), dtype, kind=kind)
        return TT(t.ap(), name)

    def _collect(self, engine, reads, writes, extra=None):
        need = {}

        def add(ev):
            if ev is None:
                return
            k, v = ev
            if need.get(k, 0) < v:
                need[k] = v

        for b in reads:
            add(b.lw)
            if b.parent is not None:
                add(b.parent.lw)
        for b in writes:
            add(b.lw)
            for k, v in b.rd.items():
                add((k, v))
            if b.parent is not None:
                add(b.parent.lw)
                for k, v in b.parent.rd.items():
                    add((k, v))
        if extra:
            for ev in extra:
                add(ev)
        if engine == "pe":
            need.pop(("e", "pe"), None)
        w = self.waited[engine]
        waits = []
        for k, v in need.items():
            if w.get(k, 0) < v:
                waits.append((k, v))
                w[k] = v
        return waits

    def _mark(self, ev, reads, writes):
        k, v = ev
        for b in reads:
            if b.rd.get(k, 0) < v:
                b.rd[k] = v
        for b in writes:
            b.lw = ev
            b.rd = {}

    def op(self, engine, fn, reads=(), writes=()):
        reads = _bufs(reads)
        writes = _bufs(writes)
        waits = self._collect(engine, reads, writes)
        self.cnt[engine] += 1
        ev = (("e", engine), self.cnt[engine])
        self._mark(ev, reads, writes)
        self.ops[engine].append((waits, fn, None, self.cnt[engine]))

    def dma(self, out, in_, reads=(), writes=(), queue="sp", r=False, **kw):
        if r:
            out = out.bitcast(F32R)
            in_ = in_.bitcast(F32R)
        reads = _bufs(reads)
        writes = _bufs(writes)
        i = self.dnext
        self.dnext = (i + 1) % self.NDS
        extra = [(("d", i), self.dval[i])] if self.dval[i] else None
        waits = self._collect(queue, reads, writes, extra)
        self.dval[i] += 16
        ev = (("d", i), self.dval[i])
        self._mark(ev, reads, writes)
        self.ops[queue].append((waits, lambda e: e.dma_start(out=out, in_=in_, **kw), i, None))

    def dbg(self, name, ap, shape, reads):
        if not getattr(self, "debug", False):
            return
        if name in getattr(self, "_dbg_done", set()):
            return
        self.__dict__.setdefault("_dbg_done", set()).add(name)
        t = self.nc.dram_tensor(name, list(shape), F32, kind="ExternalOutput").ap()
        self.dma(t, ap, reads, [])

    def _semof(self, k):
        return self.sem[k[1]] if k[0] == "e" else self.dsem[k[1]]

    def emit(self):
        fin = []
        for i in range(self.NDS):
            if self.dval[i] and self.waited["sp"].get(("d", i), 0) < self.dval[i]:
                fin.append((("d", i), self.dval[i]))
        for k in self.eng:
            if k != "sp" and self.cnt[k]:
                fin.append((("e", k), self.cnt[k]))
        needed = {k: set() for k in self.eng}
        for engine in self.eng:
            for waits, fn, di, idx in self.ops[engine]:
                for k, v in waits:
                    if k[0] == "e":
                        needed[k[1]].add(v)
        for k, v in fin:
            if k[0] == "e":
                needed[k[1]].add(v)
        rank = {}
        for k in self.eng:
            srt = sorted(needed[k])
            rank[k] = {v: i + 1 for i, v in enumerate(srt)}
            assert len(srt) < 30000, (k, len(srt))
        self.sem_counts = {k: len(rank[k]) for k in self.eng}

        def semval(k, v):
            return rank[k[1]][v] if k[0] == "e" else v

        with self.nc.Block() as block:
            def mk(engine):
                def body(e):
                    for waits, fn, di, idx in self.ops[engine]:
                        for k, v in waits:
                            e.wait_ge(self._semof(k), semval(k, v))
                        inst = fn(e)
                        if di is None:
                            if idx in rank[engine]:
                                inst.then_inc(self.sem[engine], 1)
                        else:
                            inst.then_inc(self.dsem[di], 16)
                    if engine == "sp":
                        for k, v in fin:
                            e.wait_ge(self._semof(k), semval(k, v))
                return body
            block.sync(mk("sp"))
            block.tensor(mk("pe"))
            block.vector(mk("dve"))
            block.scalar(mk("act"))
            block.gpsimd(mk("pool"))

    def mm(self, out, lhsT, rhs, start, stop, reads, writes, r=True):
        if r:
            lhsT = lhsT.bitcast(F32R)
            rhs = rhs.bitcast(F32R)
        self.op("pe", lambda e: e.matmul(out, lhsT, rhs, start=start, stop=stop), reads, writes)

    def transpose(self, out, in_, ident, reads, writes):
        self.op("pe", lambda e: e.transpose(out, in_, ident), reads, writes)

    def act(self, out, in_, func, reads, writes, bias=None, scale=None):
        kw = {}
        if bias is not None:
            kw["bias"] = bias
        if scale is not None:
            kw["scale"] = scale
        self.op("act", lambda e: e.activation(out=out, in_=in_, func=func, **kw), reads, writes)

    def tt(self, out, in0, in1, op, reads, writes, eng="dve"):
        self.op(eng, lambda e: e.tensor_tensor(out=out, in0=in0, in1=in1, op=op), reads, writes)

    def ts(self, out, in0, s1, op0, reads, writes, s2=None, op1=None, eng="dve"):
        if op1 is None:
            self.op(eng, lambda e: e.tensor_scalar(out=out, in0=in0, scalar1=s1, scalar2=None, op0=op0), reads, writes)
        else:
            self.op(eng, lambda e: e.tensor_scalar(out=out, in0=in0, scalar1=s1, scalar2=s2, op0=op0, op1=op1), reads, writes)

    def stt(self, out, in0, scalar, in1, op0, op1, reads, writes):
        self.op("dve", lambda e: e.scalar_tensor_tensor(out=out, in0=in0, scalar=scalar, in1=in1, op0=op0, op1=op1), reads, writes)

    def copy(self, out, in_, reads, writes, eng="dve"):
        if eng == "act":
            self.op("act", lambda e: e.copy(out=out, in_=in_), reads, writes)
        else:
            self.op(eng, lambda e: e.tensor_copy(out=out, in_=in_), reads, writes)

    def memset(self, ap, val, writes, eng="dve"):
        self.op(eng, lambda e: e.memset(ap, val), (), writes)


def make_consts(P):
    ident = P.sb("ident", [128, 128])
    ones = P.sb("ones", [128, 128])
    tmp = P.sb("iota_tmp", [128, 128])
    P.op("pool", lambda e: e.iota(tmp[:], pattern=[[1, 128]], base=0, channel_multiplier=-1,
                                  allow_small_or_imprecise_dtypes=True), (), [tmp])
    P.ts(ident[:], tmp[:], 0.0, ALU.is_equal, [tmp], [ident])
    P.ts(R(ones[:]), tmp[:], 0.0, ALU.mult, [tmp], [ones], s2=1.0, op1=ALU.add)
    return ident, ones


def load_cols(P, ident, dst, dst_ap, src_rows_ap, n):
    rows = P.sb("lc_rows", [n, 128])
    P.dma(rows[:], src_rows_ap, [], [rows])
    pb = P.bank()
    P.transpose(pb[:, 0:n], rows[:], ident[0:n, 0:n], [rows, ident], [pb])
    P.copy(dst_ap, pb[:, 0:n], [pb], [dst])


NT = 2112
NH = 1056
NTILE = 352
HALVES = [(0, [(0, 64, 1), (64, 1056, 0)]), (1056, [(0, 1056, 0)])]


def segs(half_idx, a, b):
    out = []
    for (c0, c1, w) in HALVES[half_idx][1]:
        lo, hi = max(a, c0), min(b, c1)
        if lo < hi:
            out.append((lo, hi, w))
    return out


class Ctx:
    pass


def emit_mod(P, C, mod_w_ap, mod_b_rows_ap, name):
    modT = P.sb(name, [128, 48, 2])
    bcols = P.sb(name + "_b", [128, 48])
    load_cols(P, C.ident, bcols, bcols[:], mod_b_rows_ap, 48)
    for blk in range(12):
        wb = C.wbuf()
        wv = wb.t[:, 0:4096].rearrange("p (a b) -> p a b", b=512)
        P.dma(wv, mod_w_ap[:, blk * 512:(blk + 1) * 512].rearrange("(kc p) n -> p kc n", p=128), [], [wb], r=True)
        pb = P.bank()
        for j in range(4):
            for kc in range(8):
                P.mm(pb[:, 2 * j:2 * j + 2], wv[:, kc, j * 128:(j + 1) * 128], C.sT[:, kc, :], kc == 0, kc == 7,
                     [wb, C.sT], [pb], r=False)
        P.tt(modT[:, blk * 4:(blk + 1) * 4, :], pb[:, 0:8].rearrange("p (a b) -> p a b", b=2),
             bcols[:, blk * 4:(blk + 1) * 4].unsqueeze(2).to_broadcast([128, 4, 2]), ALU.add, [pb, bcols], [modT])
    return modT


def emit_scale_vec(P, C, modT, g_rows_ap, sc_chunk0, name):
    g = P.sb(name + "_g", [128, 8])
    load_cols(P, C.ident, g, g[:], g_rows_ap, 8)
    gs = P.sb(name, [128, 8, 2])
    P.ts(gs[:], modT[:, sc_chunk0:sc_chunk0 + 8, :], 1.0, ALU.add, [modT], [gs])
    P.tt(gs[:], gs[:], g[:].unsqueeze(2).to_broadcast([128, 8, 2]), ALU.mult, [gs, g], [gs])
    return gs


def emit_rstd(P, C, src, nch, n0, n1, dst, inv_d, eps):
    pb = P.bank()
    w = n1 - n0
    for c in range(nch):
        sq = C.sqbuf()
        P.act(R(sq[:, 0:w]), src[:, c, n0:n1], AF.Square, [src], [sq])
        P.mm(pb[:, 0:w], C.ones[:, :], sq[:, 0:w], c == 0, c == nch - 1, [C.ones, sq], [pb])
    P.act(dst[:, n0:n1], pb[:, 0:w], AF.Ln, [pb, C.epsb], [dst], bias=C.epsb[:, 0:1] if eps == EPS else C.epsb[:, 1:2], scale=inv_d)
    P.act(dst[:, n0:n1], dst[:, n0:n1], AF.Exp, [dst], [dst], scale=-0.5)


def emit_norm_mod(P, C, hi, x, dst, gs, modT, shift_chunk0):
    for ti in range(3):
        n0, n1 = ti * NTILE, (ti + 1) * NTILE
        emit_rstd(P, C, x, 8, n0, n1, C.rstd, 1.0 / D, EPS)
        for c in range(8):
            for (a, b, w) in segs(hi, n0, n1):
                if modT is not None:
                    P.stt(R(dst[:, c, a:b]), x[:, c, a:b], gs[:, c, w:w + 1], C.rstd[:, a:b], ALU.mult, ALU.mult,
                          [x, gs, C.rstd], [dst])
                    P.act(R(dst[:, c, a:b]), dst[:, c, a:b], AF.Identity, [dst, modT], [dst],
                          bias=modT[:, shift_chunk0 + c, w:w + 1])
                else:
                    P.stt(R(dst[:, c, a:b]), x[:, c, a:b], gs[:, c:c + 1], C.rstd[:, a:b], ALU.mult, ALU.mult,
                          [x, gs, C.rstd], [dst])


def emit_linear_add(P, C, hi, src, kchunks, w_ap, x, gate):
    for dc in range(8):
        wb = C.wbuf()
        wv = wb.t[:, 0:kchunks * 128].rearrange("p (a b) -> p a b", b=128)
        P.dma(wv, w_ap[:, dc * 128:(dc + 1) * 128].rearrange("(kc p) n -> p kc n", p=128), [], [wb], r=True)
        for ti in range(3):
            n0, n1 = ti * NTILE, (ti + 1) * NTILE
            pb = P.bank()
            for kc in range(kchunks):
                P.mm(pb[:, 0:NTILE], wv[:, kc, :], src[:, kc, n0:n1], kc == 0, kc == kchunks - 1, [wb, src], [pb])
            for (a, b, w) in segs(hi, n0, n1):
                P.stt(x[:, dc, a:b], pb[:, a - n0:b - n0], gate[:, dc, w:w + 1], x[:, dc, a:b], ALU.mult, ALU.add,
                      [pb, gate, x], [x])


def emit_ffn(P, C, hi, h2, x, gate2, w_in_ap, w_out_ap, F, gbc=None):
    FC = F // 128
    G = FC // 2
    act = C.big
    for gi in range(2):
        f0 = gi * G
        fl = 0
        while fl < G:
            nb = min(4, G - fl)
            wg = C.wbuf()
            wu = C.wbuf()
            wgv = wg.t[:, 0:8 * nb * 128].rearrange("p (a b) -> p a b", b=nb * 128)
            wuv = wu.t[:, 0:8 * nb * 128].rearrange("p (a b) -> p a b", b=nb * 128)
            c0 = (f0 + fl) * 128
            P.dma(wgv, w_in_ap[:, c0:c0 + nb * 128].rearrange("(kc p) n -> p kc n", p=128), [], [wg], r=True)
            P.dma(wuv, w_in_ap[:, F + c0:F + c0 + nb * 128].rearrange("(kc p) n -> p kc n", p=128), [], [wu], r=True)
            for j in range(nb):
                for ti in range(3):
                    n0, n1 = ti * NTILE, (ti + 1) * NTILE
                    pg = P.bank()
                    pu = P.bank()
                    for kc in range(8):
                        P.mm(pg[:, 0:NTILE], wgv[:, kc, j * 128:(j + 1) * 128], h2[:, kc, n0:n1], kc == 0, kc == 7, [wg, h2], [pg])
                    for kc in range(8):
                        P.mm(pu[:, 0:NTILE], wuv[:, kc, j * 128:(j + 1) * 128], h2[:, kc, n0:n1], kc == 0, kc == 7, [wu, h2], [pu])
                    sg = C.sgbuf()
                    P.act(sg[:, 0:NTILE], pg[:, 0:NTILE], AF.Silu, [pg], [sg])
                    P.tt(R(act[:, fl + j, n0:n1]), sg[:, 0:NTILE], pu[:, 0:NTILE], ALU.mult, [sg, pu], [act.sub(fl + j)])
            fl += nb
        for dc in range(8):
            wb = C.wbuf()
            wv = wb.t[:, 0:G * 128].rearrange("p (a b) -> p a b", b=128)
            P.dma(wv, w_out_ap[f0 * 128:(f0 + G) * 128, dc * 128:(dc + 1) * 128].rearrange("(kc p) n -> p kc n", p=128), [], [wb], r=True)
            for ti in range(3):
                n0, n1 = ti * NTILE, (ti + 1) * NTILE
                pb = P.bank()
                for k in range(G):
                    P.mm(pb[:, 0:NTILE], wv[:, k, :], act[:, k, n0:n1], k == 0, k == G - 1, [wb, act.sub(k)], [pb])
                for (a, b, w) in segs(hi, n0, n1):
                    if gbc is None:
                        P.stt(x[:, dc, a:b], pb[:, a - n0:b - n0], gate2[:, dc, w:w + 1], x[:, dc, a:b], ALU.mult, ALU.add,
                              [pb, gate2, x], [x])
                    else:
                        tmp = C.sgbuf()
                        P.tt(tmp[:, 0:b - a], pb[:, a - n0:b - n0], gbc[:, a:b], ALU.mult, [pb, gbc], [tmp])
                        P.stt(x[:, dc, a:b], tmp[:, 0:b - a], gate2[:, dc, w:w + 1], x[:, dc, a:b], ALU.mult, ALU.add,
                              [tmp, gate2, x], [x])


def emit_pool(P, C, hi, hp_ctx, hp_lat, inv_ap):
    h0 = HALVES[hi][0]
    inv = C.hb
    for g in range(4):
        P.dma(inv[:, g, :], inv_ap[g:g + 1, h0:h0 + NH].to_broadcast([128, NH]), [], [inv], r=True)
    for c in range(8):
        g = c // 2
        w = 2 << g
        for (a, b, isctx) in HALVES[hi][1]:
            n = b - a
            hh = C.poolbuf[0]
            if isctx:
                src = hp_ctx[c * 128:(c + 1) * 128, 0:n + 16]
            else:
                l0 = h0 + a - 64
                src = hp_lat[c * 128:(c + 1) * 128, l0:l0 + n + 16]
            P.dma(hh[:, 0:n + 16], src, [], [hh])
            cur = hh
            sh = 1
            k = 0
            while sh < w:
                nxt = C.poolbuf[1 + (k % 2)]
                lo = 2 * sh - 1
                P.tt(nxt[:, lo:n + 16], cur[:, lo:n + 16], cur[:, lo - sh:n + 16 - sh], ALU.add, [cur], [nxt])
                cur = nxt
                sh *= 2
                k += 1
            off = 8 + w // 2 - 1
            P.tt(R(C.big[:, c, a:b]), cur[:, off:off + n], inv[:, g, a:b], ALU.mult, [cur, inv], [C.big.sub(c)])
            P.tt(R(C.big[:, c, a:b]), C.big[:, c, a:b], hh[:, 8:8 + n], ALU.subtract, [C.big.sub(c), hh], [C.big.sub(c)])


def emit_moe_gates(P, C, hi, h2, router_ap):
    rw = P.sb("router", [128, 8, 8])
    P.dma(rw[:], router_ap.rearrange("(kc p) e -> p kc e", p=128), [], [rw])
    t0 = 0
    while t0 < NH:
        m = min(128, NH - t0)
        pb = P.bank()
        for kc in range(8):
            P.mm(pb[0:m, 0:8], h2[:, kc, t0:t0 + m], rw[:, kc, :], kc == 0, kc == 7, [h2, rw], [pb], r=False)
        lg = P.sb("lg", [128, 8])
        P.copy(lg[0:m, :], pb[0:m, 0:8], [pb], [lg])
        mx = P.sb("mx", [128, 8])
        P.op("dve", lambda e, mx=mx, lg=lg, m=m: e.max(out=mx[0:m, :], in_=lg[0:m, :]), [lg], [mx])
        dd = P.sb("dd", [128, 4])
        P.tt(dd[0:m, 0:1], mx[0:m, 1:2], mx[0:m, 0:1], ALU.subtract, [mx], [dd])
        P.act(dd[0:m, 1:2], dd[0:m, 0:1], AF.Exp, [dd], [dd])
        P.ts(dd[0:m, 1:2], dd[0:m, 1:2], 1.0, ALU.add, [dd], [dd])
        P.op("dve", lambda e, dd=dd, m=m: e.reciprocal(out=dd[0:m, 2:3], in_=dd[0:m, 1:2]), [dd], [dd])
        P.ts(dd[0:m, 3:4], dd[0:m, 2:3], -1.0, ALU.mult, [dd], [dd], s2=1.0, op1=ALU.add)
        g1 = P.sb("g1", [128, 8])
        g2 = P.sb("g2", [128, 8])
        P.ts(g1[0:m, :], lg[0:m, :], mx[0:m, 0:1], ALU.is_equal, [lg, mx, dd], [g1], s2=dd[0:m, 2:3], op1=ALU.mult)
        P.ts(g2[0:m, :], lg[0:m, :], mx[0:m, 1:2], ALU.is_equal, [lg, mx, dd], [g2], s2=dd[0:m, 3:4], op1=ALU.mult)
        P.tt(g1[0:m, :], g1[0:m, :], g2[0:m, :], ALU.add, [g1, g2], [g1])
        pt = P.bank()
        P.transpose(pt[0:8, 0:m], g1[0:m, :], C.ident[0:m, 0:m], [g1, C.ident], [pt])
        P.copy(C.gatesT[:, t0:t0 + m], pt[0:8, 0:m], [pt], [C.gatesT])
        t0 += m


def emit_gbc(P, C, e_idx):
    for ti in range(3):
        n0, n1 = ti * NTILE, (ti + 1) * NTILE
        pb = P.bank()
        P.mm(pb[:, 0:NTILE], C.sel[:, e_idx, :], C.gatesT[:, n0:n1], True, True, [C.sel, C.gatesT], [pb], r=False)
        P.copy(C.gbc[:, n0:n1], pb[:, 0:NTILE], [pb], [C.gbc], eng="act")


def build_ts(layer, post, ffn, nxt):
    nc = bass.Bass("TRN2", target_bir_lowering=False)
    with ExitStack() as stack:
        nc.dge_precook = False
        P = Prog(nc, stack)
        P.init_psum(8)
        C = Ctx()

        def din(name, shape):
            return nc.dram_tensor(name, list(shape), F32, kind="ExternalInput").ap()

        def dout(name, shape):
            return nc.dram_tensor(name, list(shape), F32, kind="ExternalOutput").ap()

        xT = din("xT", [D, NT])
        cvec = din("cvec", [16, 128])
        if post is not None or ffn is not None:
            mod_w = din("mod_w", [D, 6 * D])
            mod_b = din("mod_b", [48, 128])
            n2g = din("n2g", [8, 128])
        if post == "mamba":
            mT = din("mT", [2048, NT])
            mb_ng = din("mb_ng", [16, 128])
            w_post = din("w_post", [2048, D])
        elif post == "lin":
            mT = din("mT", [D, NT])
            w_post = din("w_post", [D, D])
        elif post == "pool":
            hp_ctx = din("hp_ctx", [D, 80])
            hp_lat = din("hp_lat", [D, 2048 + 16])
            inv_cnt = din("inv_cnt", [4, NT])
            pl_w = din("pl_w", [4, 256, 256])
            pl_scale = din("pl_scale", [8, 128])
        if ffn == "dense":
            F = 2816
            w_in = din("w_in", [D, 2 * F])
            w_out = din("w_out", [F, D])
        elif ffn == "moe":
            F = 3584
            router = din("router", [D, 8])
            w_in = din("w_in", [8, D, 2 * F])
            w_out = din("w_out", [8, F, D])
        if nxt == "norm":
            mod_w_n = din("mod_w_n", [D, 6 * D])
            mod_b_n = din("mod_b_n", [48, 128])
            n1g_n = din("n1g_n", [8, 128])
            xT_out = dout("xT_out", [D, NT])
            hT_out = dout("hT_out", [D, NT])
        else:
            fin_g = din("fin_g", [8, 128])
            out_T = dout("out_T", [D, NT])

        C.ident, C.ones = make_consts(P)
        wbufs = [P.sb(f"wb{i}", [128, 4096]) for i in range(3)]
        wi = [0]

        def wbuf():
            b = wbufs[wi[0] % 3]
            wi[0] += 1
            return b
        C.wbuf = wbuf
        sqs = [P.sb(f"sq{i}", [128, NTILE]) for i in range(3)]
        si = [0]

        def sqbuf():
            b = sqs[si[0] % 3]
            si[0] += 1
            return b
        C.sqbuf = sqbuf
        sgs = [P.sb(f"sg{i}", [128, NTILE]) for i in range(3)]
        gi_ = [0]

        def sgbuf():
            b = sgs[gi_[0] % 3]
            gi_[0] += 1
            return b
        C.sgbuf = sgbuf
        C.rstd = P.sb("rstd", [128, NH])
        C.epsb = P.sb("epsb", [128, 2])
        P.memset(C.epsb[:, 0:1], EPS, [C.epsb])
        P.memset(C.epsb[:, 1:2], EPS, [C.epsb])
        craw = P.sb("craw", [128, 16])
        load_cols(P, C.ident, craw, craw[:], cvec, 16)
        C.sT = P.sb("sT", [128, 8, 2])
        P.act(C.sT[:, :, 0], craw[:, 0:8], AF.Silu, [craw], [C.sT])
        P.act(C.sT[:, :, 1], craw[:, 8:16], AF.Silu, [craw], [C.sT])

        x = P.sb("x", [128, 8, NH])
        C.hb = P.sb("hb", [128, 8, NH])
        need_big = post is not None or ffn is not None
        if need_big:
            nbig = 16 if post == "mamba" else (14 if ffn == "moe" else 11)
            C.big = P.sb("big", [128, nbig, NH])
        if post == "pool":
            C.poolbuf = [P.sb(f"pb{i}", [128, NH + 16]) for i in range(3)]
        if ffn == "moe":
            C.gatesT = P.sb("gatesT", [8, NH])
            C.gbc = P.sb("gbc", [128, NH])
            C.sel = P.sb("sel", [8, 8, 128])
            P.memset(C.sel[:], 0.0, [C.sel])
            for e_ in range(8):
                P.ts(C.sel[:, e_, :], C.ones[0:8, :], C.ident[0:8, e_:e_ + 1], ALU.mult, [C.ones, C.ident, C.sel], [C.sel])

        if post is not None or ffn is not None:
            modT = emit_mod(P, C, mod_w, mod_b, "modT")
            gs2 = emit_scale_vec(P, C, modT, n2g, 32, "gs2")
            gate1 = P.sb("gate1", [128, 8, 2])
            P.copy(gate1[:], modT[:, 16:24, :], [modT], [gate1])
            gate2 = P.sb("gate2", [128, 8, 2])
            P.copy(gate2[:], modT[:, 40:48, :], [modT], [gate2])
            if post == "pool":
                psc = P.sb("psc", [128, 8])
                load_cols(P, C.ident, psc, psc[:], pl_scale, 8)
                P.tt(gate1[:], gate1[:], psc[:].unsqueeze(2).to_broadcast([128, 8, 2]), ALU.mult, [gate1, psc], [gate1])
            if post == "mamba":
                mng = P.sb("mng", [128, 16])
                load_cols(P, C.ident, mng, mng[:], mb_ng, 16)
        if nxt == "norm":
            modN = emit_mod(P, C, mod_w_n, mod_b_n, "modN")
            gs1n = emit_scale_vec(P, C, modN, n1g_n, 8, "gs1n")
        else:
            fg = P.sb("fg", [128, 8])
            load_cols(P, C.ident, fg, fg[:], fin_g, 8)

        for hi in range(2):
            h0 = HALVES[hi][0]
            for c in range(8):
                P.dma(x[:, c, :], xT[c * 128:(c + 1) * 128, h0:h0 + NH], [], [x])
            if post == "mamba":
                big = C.big
                for c in range(16):
                    P.dma(big[:, c, :], mT[c * 128:(c + 1) * 128, h0:h0 + NH], [], [big.sub(c)], r=True)
                allb = [big.sub(c) for c in range(16)]
                for ti in range(3):
                    n0, n1 = ti * NTILE, (ti + 1) * NTILE
                    pb = P.bank()
                    for c in range(16):
                        sq = C.sqbuf()
                        P.act(R(sq[:, 0:NTILE]), big[:, c, n0:n1], AF.Square, [big.sub(c)], [sq])
                        P.mm(pb[:, 0:NTILE], C.ones[:, :], sq[:, 0:NTILE], c == 0, c == 15, [C.ones, sq], [pb])
                    P.act(C.rstd[:, n0:n1], pb[:, 0:NTILE], AF.Ln, [pb, C.epsb], [C.rstd], bias=C.epsb[:, 0:1], scale=1.0 / 2048)
                    P.act(C.rstd[:, n0:n1], C.rstd[:, n0:n1], AF.Exp, [C.rstd], [C.rstd], scale=-0.5)
                    for c in range(16):
                        P.stt(R(big[:, c, n0:n1]), big[:, c, n0:n1], mng[:, c:c + 1], C.rstd[:, n0:n1], ALU.mult, ALU.mult,
                              [big.sub(c), mng, C.rstd], [big.sub(c)])
                emit_linear_add(P, C, hi, _SubView(big, allb), 16, w_post, x, gate1)
            elif post == "lin":
                big = C.big
                for c in range(8):
                    P.dma(big[:, c, :], mT[c * 128:(c + 1) * 128, h0:h0 + NH], [], [big.sub(c)], r=True)
                emit_linear_add(P, C, hi, _SubView(big, [big.sub(c) for c in range(8)]), 8, w_post, x, gate1)
            elif post == "pool":
                emit_pool(P, C, hi, hp_ctx, hp_lat, inv_cnt)
                big = C.big
                for dc in range(8):
                    g, j = dc // 2, dc % 2
                    wb = C.wbuf()
                    wv = wb.t[:, 0:256].rearrange("p (a b) -> p a b", b=128)
                    P.dma(wv, pl_w[g, :, j * 128:(j + 1) * 128].rearrange("(kc p) n -> p kc n", p=128), [], [wb], r=True)
                    for ti in range(3):
                        n0, n1 = ti * NTILE, (ti + 1) * NTILE
                        pb = P.bank()
                        for kc in range(2):
                            P.mm(pb[:, 0:NTILE], wv[:, kc, :], big[:, 2 * g + kc, n0:n1], kc == 0, kc == 1,
                                 [wb, big.sub(2 * g + kc)], [pb])
                        for (a, b, w) in segs(hi, n0, n1):
                            P.stt(x[:, dc, a:b], pb[:, a - n0:b - n0], gate1[:, dc, w:w + 1], x[:, dc, a:b], ALU.mult, ALU.add,
                                  [pb, gate1, x], [x])
            if ffn is not None:
                h2 = C.hb
                emit_norm_mod(P, C, hi, x, h2, gs2, modT, 24)
                if ffn == "dense":
                    emit_ffn(P, C, hi, h2, x, gate2, w_in, w_out, F)
                else:
                    emit_moe_gates(P, C, hi, h2, router)
                    for e_ in range(8):
                        emit_gbc(P, C, e_)
                        emit_ffn(P, C, hi, h2, x, gate2, w_in[e_], w_out[e_], F, gbc=C.gbc)
            if nxt == "norm":
                for c in range(8):
                    P.dma(xT_out[c * 128:(c + 1) * 128, h0:h0 + NH], x[:, c, :], [x], [])
                emit_norm_mod(P, C, hi, x, C.hb, gs1n, modN, 0)
                for c in range(8):
                    P.dma(hT_out[c * 128:(c + 1) * 128, h0:h0 + NH], C.hb[:, c, :], [C.hb], [])
            else:
                emit_norm_mod(P, C, hi, x, C.hb, fg, None, 0)
                for c in range(8):
                    P.dma(out_T[c * 128:(c + 1) * 128, h0:h0 + NH], C.hb[:, c, :], [C.hb], [])
        P.emit()
    return nc


def rows128(v):
    return np.ascontiguousarray(np.asarray(v, np.float32).reshape(-1, 128))


def to_cores_T(lat, ctx):
    outs = []
    for core in range(NCORES):
        b, q = core // 4, core % 4
        a = np.concatenate([ctx[b, q * 64:(q + 1) * 64], lat[b, q * 2048:(q + 1) * 2048]], axis=0)
        outs.append(np.ascontiguousarray(a.T))
    return outs


def from_cores_T(arrs):
    Cc = arrs[0].shape[0]
    lat = np.empty((2, 8192, Cc), np.float32)
    ctx = np.empty((2, 256, Cc), np.float32)
    for core in range(NCORES):
        b, q = core // 4, core % 4
        a = arrs[core].T
        ctx[b, q * 64:(q + 1) * 64] = a[0:64]
        lat[b, q * 2048:(q + 1) * 2048] = a[64:]
    return lat, ctx


def cvec_for(c, c_ctx, core):
    b = core // 4
    return np.ascontiguousarray(np.concatenate([np.asarray(c[b], np.float32).reshape(8, 128),
                                                np.asarray(c_ctx, np.float32).reshape(8, 128)], axis=0))


_NC_CACHE = {}


def get_ts(layer, post, ffn, nxt):
    key = ("ts", post, ffn, nxt)
    if key not in _NC_CACHE:
        _NC_CACHE[key] = build_ts(layer, post, ffn, nxt)
    return _NC_CACHE[key]


def run(nc, in_maps):
    res = run_bass_kernel_spmd(nc, in_maps, core_ids=list(range(NCORES)))
    return res.results


def pool_inputs(h_lat, h_ctx):
    outs = []
    for core in range(NCORES):
        b, q = core // 4, core % 4
        lat = np.zeros((2048 + 16, D), np.float32)
        lo, hi = q * 2048 - 8, (q + 1) * 2048 + 8
        s0, s1 = max(lo, 0), min(hi, 8192)
        lat[s0 - lo:s1 - lo] = h_lat[b, s0:s1]
        cx = np.zeros((64 + 16, D), np.float32)
        lo, hi = q * 64 - 8, (q + 1) * 64 + 8
        s0, s1 = max(lo, 0), min(hi, 256)
        cx[s0 - lo:s1 - lo] = h_ctx[b, s0:s1]
        inv = np.empty((4, NT), np.float32)
        for g, w in enumerate((2, 4, 8, 16)):
            t = np.arange(q * 64, (q + 1) * 64)
            inv[g, 0:64] = 1.0 / (np.minimum(t + w // 2, 256) - np.maximum(t - w // 2, 0))
            t = np.arange(q * 2048, (q + 1) * 2048)
            inv[g, 64:] = 1.0 / (np.minimum(t + w // 2, 8192) - np.maximum(t - w // 2, 0))
        outs.append({"hp_ctx": np.ascontiguousarray(cx.T), "hp_lat": np.ascontiguousarray(lat.T), "inv_cnt": inv})
    return outs


NKEY = 8448
NQ = 8192


def build_attn():
    nc = bass.Bass("TRN2", target_bir_lowering=False)
    with ExitStack() as stack:
        nc.dge_precook = False
        P = Prog(nc, stack)
        P.init_psum(6)
        oacc = P.ps("oacc", [128, 512])
        bcp = P.ps("bcp", [128, 512])

        def din(name, shape):
            return nc.dram_tensor(name, list(shape), F32, kind="ExternalInput").ap()

        hT = din("hT", [D, NKEY])
        wq = din("wq", [D, 256])
        wk = din("wk", [D, 64])
        wv = din("wv", [D, 64])
        qg = din("qg", [64, 1])
        kg = din("kg", [64, 1])
        cosT = din("cosT", [64, NKEY])
        sinS = din("sinS", [64, NKEY])
        prot = din("prot", [64, 64])
        oT = nc.dram_tensor("oT", [256, NQ], F32, kind="ExternalOutput").ap()

        ident, ones = make_consts(P)
        wq_s = P.sb("wq_s", [128, 8, 256])
        wk_s = P.sb("wk_s", [128, 8, 64])
        wv_s = P.sb("wv_s", [128, 8, 64])
        P.dma(wq_s[:], wq.rearrange("(kc p) n -> p kc n", p=128), [], [wq_s], r=True)
        P.dma(wk_s[:], wk.rearrange("(kc p) n -> p kc n", p=128), [], [wk_s], r=True)
        P.dma(wv_s[:], wv.rearrange("(kc p) n -> p kc n", p=128), [], [wv_s], r=True)
        qg_s = P.sb("qg_s", [64, 1])
        kg_s = P.sb("kg_s", [64, 1])
        P.dma(qg_s[:], qg, [], [qg_s])
        P.dma(kg_s[:], kg, [], [kg_s])
        prot_s = P.sb("prot_s", [64, 64])
        P.dma(prot_s[:], prot, [], [prot_s], r=True)
        epsb = P.sb("epsb", [128, 1])
        P.memset(epsb[:], EPS, [epsb])
        KT = P.sb("KT", [64, NKEY])
        Vx = P.sb("Vx", [128, 66, 65])
        P.ts(R(Vx[:, :, 64:65]), Vx[:, :, 64:65], 0.0, ALU.mult, [], [Vx], s2=1.0, op1=ALU.add)
        hts = [P.sb(f"ht{i}", [128, 8, 512]) for i in range(2)]
        cst = [P.sb(f"cs{i}", [64, 512]) for i in range(2)]
        snt = [P.sb(f"sn{i}", [64, 512]) for i in range(2)]
        QTs = [P.sb(f"QT{i}", [64, 4, 512]) for i in range(2)]
        pts = [P.sb(f"pt{i}", [128, 512]) for i in range(5)]
        sqb = [P.sb(f"sqb{i}", [64, 512]) for i in range(2)]
        rsb = [P.sb(f"rsb{i}", [64, 512]) for i in range(2)]
        qnb = [P.sb(f"qnb{i}", [64, 512]) for i in range(2)]
        t1b = [P.sb(f"t1b{i}", [64, 512]) for i in range(2)]
        t2b = [P.sb(f"t2b{i}", [64, 512]) for i in range(2)]
        obuf = [P.sb(f"ob{i}", [64, 4, 512]) for i in range(2)]
        lrow = P.sb("lrow", [128, 512])
        bcs = P.sb("bcs", [64, 512])
        cnt = [0]

        def normrope(src_ps, w, g_s, cs, sn, dst_ap, dst_tt):
            i = cnt[0] % 2
            cnt[0] += 1
            sq, rs, qn, t1, t2 = sqb[i], rsb[i], qnb[i], t1b[i], t2b[i]
            P.act(R(sq[:, 0:w]), src_ps, AF.Square, [src_tt[0]], [sq])
            pb = P.bank()
            P.mm(pb[0:64, 0:w], ones[0:64, 0:64], sq[:, 0:w], True, True, [ones, sq], [pb])
            P.act(rs[:, 0:w], pb[0:64, 0:w], AF.Ln, [pb, epsb], [rs], bias=epsb[0:64, 0:1], scale=1.0 / 64)
            P.act(rs[:, 0:w], rs[:, 0:w], AF.Exp, [rs], [rs], scale=-0.5)
            P.stt(R(qn[:, 0:w]), src_ps, g_s[:, 0:1], rs[:, 0:w], ALU.mult, ALU.mult, [src_tt[0], g_s, rs], [qn])
            pr = P.bank()
            P.mm(pr[0:64, 0:w], prot_s[:, :], qn[:, 0:w], True, True, [prot_s, qn], [pr])
            P.tt(t1[:, 0:w], qn[:, 0:w], cs, ALU.mult, [qn, cs_tt[0]], [t1])
            P.tt(t2[:, 0:w], pr[0:64, 0:w], sn, ALU.mult, [pr, sn_tt[0]], [t2])
            P.tt(R(dst_ap), t1[:, 0:w], t2[:, 0:w], ALU.add, [t1, t2], [dst_tt])

        src_tt = [None]
        cs_tt = [None]
        sn_tt = [None]

        ntile_k = [(i * 512, 512) for i in range(16)] + [(8192, 256)]
        for ti, (c0, w) in enumerate(ntile_k):
            ht = hts[ti % 2]
            cs = cst[ti % 2]
            sn = snt[ti % 2]
            for kc in range(8):
                P.dma(ht[:, kc, 0:w], hT[kc * 128:(kc + 1) * 128, c0:c0 + w], [], [ht], r=True)
            P.dma(cs[:, 0:w], cosT[:, c0:c0 + w], [], [cs])
            P.dma(sn[:, 0:w], sinS[:, c0:c0 + w], [], [sn])
            pk = P.bank()
            for kc in range(8):
                P.mm(pk[0:64, 0:w], wk_s[:, kc, :], ht[:, kc, 0:w], kc == 0, kc == 7, [wk_s, ht], [pk])
            src_tt[0], cs_tt[0], sn_tt[0] = pk, cs, sn
            normrope(pk[0:64, 0:w], w, kg_s, cs[:, 0:w], sn[:, 0:w], KT[:, c0:c0 + w], KT.sub(ti))
            for j in range(w // 128):
                ch = c0 // 128 + j
                pv = P.bank()
                for kc in range(8):
                    P.mm(pv[:, 0:64], ht[:, kc, j * 128:(j + 1) * 128], wv_s[:, kc, :], kc == 0, kc == 7, [ht, wv_s], [pv])
                P.copy(R(Vx[:, ch, 0:64]), pv[:, 0:64], [pv], [Vx.sub(ch)], eng="act")
        KTall = [KT.sub(ti) for ti in range(len(ntile_k))]

        for ti in range(16):
            c0 = 256 + ti * 512
            ht = hts[ti % 2]
            cs = cst[ti % 2]
            sn = snt[ti % 2]
            QT = QTs[ti % 2]
            ob = obuf[ti % 2]
            for kc in range(8):
                P.dma(ht[:, kc, :], hT[kc * 128:(kc + 1) * 128, c0:c0 + 512], [], [ht], r=True)
            P.dma(cs[:, :], cosT[:, c0:c0 + 512], [], [cs])
            P.dma(sn[:, :], sinS[:, c0:c0 + 512], [], [sn])
            for hq in range(4):
                pq = P.bank()
                for kc in range(8):
                    P.mm(pq[0:64, :], wq_s[:, kc, hq * 64:(hq + 1) * 64], ht[:, kc, :], kc == 0, kc == 7, [wq_s, ht], [pq])
                src_tt[0], cs_tt[0], sn_tt[0] = pq, cs, sn
                normrope(pq[0:64, :], 512, qg_s, cs[:, :], sn[:, :], QT[:, hq, :], QT)
            for qb in range(4):
                rhs_q = QT[:, :, qb * 128:(qb + 1) * 128]
                PD = 3
                for st_ in range(66 + PD):
                    if st_ < 66:
                        ch = st_
                        pst = P.bank()
                        P.mm(pst[:, :].rearrange("p (h q) -> p h q", h=4), KT[:, ch * 128:(ch + 1) * 128], rhs_q, True, True,
                             [KT.sub(ch // 4), QT], [pst])
                        pt = pts[ch % 5]
                        P.act(R(pt[:, :]), pst[:, :], AF.Exp, [pst], [pt], scale=0.125)
                    if st_ >= PD:
                        ch = st_ - PD
                        pt = pts[ch % 5]
                        P.mm(oacc[0:65, :], Vx[:, ch, :], pt[:, :], ch == 0, ch == 65, [Vx.sub(ch), pt], [oacc])
                P.copy(lrow[64:65, :], oacc[64:65, :], [oacc], [lrow], eng="act")
                P.op("dve", lambda e: e.reciprocal(out=lrow[64:65, :], in_=lrow[64:65, :]), [lrow], [lrow])
                P.mm(bcp[0:64, :], ones[64:65, 0:64], lrow[64:65, :], True, True, [ones, lrow], [bcp], r=False)
                P.copy(bcs[:, :], bcp[0:64, :], [bcp], [bcs], eng="act")
                P.tt(ob[:, :, qb * 128:(qb + 1) * 128], oacc[0:64, :].rearrange("p (h q) -> p h q", h=4),
                     bcs[:, :].rearrange("p (h q) -> p h q", h=4), ALU.mult, [oacc, bcs], [ob])
            q0 = ti * 512
            for hq in range(4):
                P.dma(oT[hq * 64:(hq + 1) * 64, q0:q0 + 512], ob[:, hq, :], [ob], [])
        P.emit()
    return nc


def rope_tables():
    quarter = 16
    inv = (10000.0 ** (-np.arange(quarter, dtype=np.float32) / quarter)).astype(np.float32)
    t = np.arange(8192)
    rows = (t // 64).astype(np.float32)
    cols = (t % 64).astype(np.float32)
    cosT = np.ones((64, NKEY), np.float32)
    sinS = np.zeros((64, NKEY), np.float32)
    prot = np.zeros((64, 64), np.float32)
    for i in range(64):
        half, within = i // 32, i % 32
        j, first = within % 16, within < 16
        pos = rows if half == 0 else cols
        ang = (pos * inv[j]).astype(np.float32)
        cosT[i, 256:] = np.cos(ang)
        sinS[i, 256:] = -np.sin(ang) if first else np.sin(ang)
        prot[i + 16 if first else i - 16, i] = 1.0
    return cosT, sinS, prot


def build_mamba(debug=False):
    nc = bass.Bass("TRN2", target_bir_lowering=False)
    with ExitStack() as stack:
        nc.dge_precook = False
        P = Prog(nc, stack)
        P.debug = debug
        P.init_psum(6)
        pybanks = [P.ps("pyb0", [128, 512]), P.ps("pyb1", [128, 512])]
        pyi = [0]

        def din(name, shape):
            return nc.dram_tensor(name, list(shape), F32, kind="ExternalInput").ap()

        hT = din("hT", [D, NKEY])
        wz = din("wz", [D, 512])
        wxbc = din("wxbc", [D, 768])
        wdt = din("wdt", [D, 16])
        cw = din("cw", [128, 30])
        cb = din("cb", [128, 6])
        dtb = din("dtb", [1, 16])
        alog = din("alog", [1, 16])
        dsk = din("dsk", [128, 4])
        uT = nc.dram_tensor("uT", [512, NKEY], F32, kind="ExternalOutput").ap()
        ybs = nc.dram_tensor("ybs", [512, NKEY], F32, kind="Internal").ap()
        ybs_t = TT(ybs, "ybs")

        ident, ones = make_consts(P)
        val = P.sb("val", [128, 128])
        P.op("pool", lambda e: e.iota(val[:], pattern=[[1, 128]], base=0, channel_multiplier=-1,
                                      allow_small_or_imprecise_dtypes=True), (), [val])
        Uf = P.sb("Uf", [128, 128]); Tf = P.sb("Tf", [128, 128]); Ub = P.sb("Ub", [128, 128]); Tb = P.sb("Tb", [128, 128])
        P.ts(Uf[:], val[:], 0.0, ALU.is_lt, [val], [Uf])
        P.ts(Tf[:], val[:], 0.0, ALU.is_ge, [val], [Tf])
        P.ts(Ub[:], val[:], 0.0, ALU.is_gt, [val], [Ub])
        P.ts(Tb[:], val[:], 0.0, ALU.is_le, [val], [Tb])
        UU = [Uf, Ub]
        TTm = [Tf, Tb]

        wz_s = P.sb("wz_s", [128, 8, 512])
        wx_s = P.sb("wx_s", [128, 8, 768])
        wd_s = P.sb("wd_s", [128, 8, 16])
        P.dma(wz_s[:], wz.rearrange("(kc p) n -> p kc n", p=128), [], [wz_s], r=True)
        P.dma(wx_s[:], wxbc.rearrange("(kc p) n -> p kc n", p=128), [], [wx_s], r=True)
        P.dma(wd_s[:], wdt.rearrange("(kc p) n -> p kc n", p=128), [], [wd_s], r=True)
        cw_s = P.sb("cw_s", [128, 30]); cb_s = P.sb("cb_s", [128, 6]); dsk_s = P.sb("dsk_s", [128, 4])
        P.dma(cw_s[:], cw, [], [cw_s]); P.dma(cb_s[:], cb, [], [cb_s]); P.dma(dsk_s[:], dsk, [], [dsk_s])
        dtb_s = P.sb("dtb_s", [128, 16]); aneg = P.sb("aneg", [128, 16])
        P.dma(dtb_s[:], dtb.to_broadcast([128, 16]), [], [dtb_s])
        P.dma(aneg[:], alog.to_broadcast([128, 16]), [], [aneg])
        P.act(aneg[:], aneg[:], AF.Exp, [aneg], [aneg])
        P.ts(aneg[:], aneg[:], -1.0, ALU.mult, [aneg], [aneg])
        oneb = P.sb("oneb", [128, 1])
        P.memset(oneb[:], 1.0, [oneb])

        W = 256
        ht = [P.sb(f"ht{i}", [128, 8, W + 4]) for i in range(2)]
        raw = P.sb("raw", [128, 6, W + 4])
        acc = [P.sb(f"acc{i}", [128, W]) for i in range(2)]
        xTc = P.sb("xTc", [128, 4, W])
        BT = P.sb("BT", [128, W]); CT = P.sb("CT", [128, W])
        x_tok = P.sb("x_tok", [128, 2, 512]); B_tok = P.sb("B_tok", [128, 2, 128]); dt_tok = P.sb("dt_tok", [128, 2, 16])
        zs = P.sb("zs", [128, 4, W])
        ST = [P.sb(f"ST{d}", [128, 512]) for d in range(2)]
        for d_ in range(2):
            P.memset(ST[d_][:], 0.0, [ST[d_]])
        sm = [P.sb(f"sm{i}", [128, 16]) for i in range(8)]
        GM = P.sb("GM", [128, 128])
        lD = [P.sb(f"lD{i}", [128, 128]) for i in range(8)]
        Lx = [P.sb(f"Lx{i}", [128, 128]) for i in range(8)]
        WT = [P.sb(f"WT{i}", [128, 128]) for i in range(8)]
        A1 = [P.sb(f"A1{i}", [128, 128]) for i in range(8)]
        Ec = [P.sb(f"Ec{i}", [128, 128]) for i in range(8)]
        Cd = [P.sb(f"Cd{i}", [128, 128]) for i in range(8)]
        xdt = P.sb("xdt", [128, 512])
        ybuf = P.sb("ybuf", [128, 4, 128])
        ubuf = [P.sb(f"ubuf{i}", [128, 4, 128]) for i in range(2)]

        def stage_a(ti, c0, q0, q1, fwd):
            h = ht[ti % 2]
            lo, hi = max(c0 - 2, q0), min(c0 + W + 2, q1)
            off = lo - (c0 - 2)
            n = hi - lo
            for kc in range(8):
                P.dma(h[:, kc, off:off + n], hT[kc * 128:(kc + 1) * 128, lo:hi], [], [h], r=True)
            if off > 0:
                P.ts(R(h[:, :, 0:off]), h[:, :, 0:off], 0.0, ALU.mult, [], [h])
            for cc in range(6):
                pb = P.bank()
                for kc in range(8):
                    P.mm(pb[:, 0:n], wx_s[:, kc, cc * 128:(cc + 1) * 128], h[:, kc, off:off + n], kc == 0, kc == 7, [wx_s, h], [pb])
                if off > 0:
                    P.memset(raw[:, cc, 0:off], 0.0, [raw.sub(cc)])
                if off + n < W + 4:
                    P.memset(raw[:, cc, off + n:W + 4], 0.0, [raw.sub(cc)])
                P.copy(raw[:, cc, off:off + n], pb[:, 0:n], [pb], [raw.sub(cc)], eng="act")
                a_ = acc[cc % 2]
                P.ts(a_[:, :], raw[:, cc, 0:W], cw_s[:, cc * 5:cc * 5 + 1], ALU.mult, [raw.sub(cc), cw_s], [a_])
                for k in range(1, 5):
                    P.stt(a_[:, :], raw[:, cc, k:k + W], cw_s[:, cc * 5 + k:cc * 5 + k + 1], a_[:, :], ALU.mult, ALU.add,
                          [raw.sub(cc), cw_s, a_], [a_])
                if cc < 4:
                    P.act(xTc[:, cc, :], a_[:, :], AF.Silu, [a_, cb_s], [xTc.sub(cc)], bias=cb_s[:, cc:cc + 1])
                elif cc == 4:
                    P.act(BT[:, :], a_[:, :], AF.Silu, [a_, cb_s], [BT], bias=cb_s[:, cc:cc + 1])
                else:
                    P.act(CT[:, :], a_[:, :], AF.Silu, [a_, cb_s], [CT], bias=cb_s[:, cc:cc + 1])
            for j in range(2):
                pt = P.bank()
                for cc in range(4):
                    P.transpose(pt[:, cc * 128:(cc + 1) * 128], xTc[:, cc, j * 128:(j + 1) * 128], ident[:, :], [xTc.sub(cc), ident], [pt])
                P.copy(x_tok[:, j, :], pt[:, :], [pt], [x_tok.sub(j)], eng="act")
                pb = P.bank()
                P.transpose(pb[:, 0:128], BT[:, j * 128:(j + 1) * 128], ident[:, :], [BT, ident], [pb])
                P.copy(R(B_tok[:, j, :]), pb[:, 0:128], [pb], [B_tok.sub(j)])
                pd = P.bank()
                for kc in range(8):
                    P.mm(pd[:, 0:16], h[:, kc, 2 + j * 128:2 + (j + 1) * 128], wd_s[:, kc, :], kc == 0, kc == 7, [h, wd_s], [pd], r=False)
                xx, ax, ee, rr = sm[0], sm[1], sm[2], sm[3]
                P.tt(xx[:, :], pd[:, 0:16], dtb_s[:, :], ALU.add, [pd, dtb_s], [xx])
                P.stt(ax[:, :], xx[:, :], -1.0, xx[:, :], ALU.mult, ALU.max, [xx], [ax])
                P.act(ee[:, :], ax[:, :], AF.Exp, [ax], [ee], scale=-1.0)
                P.act(ee[:, :], ee[:, :], AF.Ln, [ee, oneb], [ee], bias=oneb[:, 0:1])
                P.ts(rr[:, :], xx[:, :], 0.0, ALU.max, [xx], [rr])
                P.tt(dt_tok[:, j, :], rr[:, :], ee[:, :], ALU.add, [rr, ee], [dt_tok.sub(j)])
            if fwd:
                P.dbg("d_xTc", xTc[:, :, :], [128, 4, W], [xTc])
                P.dbg("d_BT", BT[:, :], [128, W], [BT])
                P.dbg("d_CT", CT[:, :], [128, W], [CT])
                P.dbg("d_xtok", x_tok[:, :, :], [128, 2, 512], [x_tok])
                P.dbg("d_Btok", B_tok[:, :, :], [128, 2, 128], [B_tok])
                P.dbg("d_dt", dt_tok[:, :, :], [128, 2, 16], [dt_tok])
                P.dbg("d_raw", raw[:, :, :], [128, 6, W + 4], [raw])
            if fwd:
                for cc in range(4):
                    pz = P.bank()
                    for kc in range(8):
                        P.mm(pz[:, 0:W], wz_s[:, kc, cc * 128:(cc + 1) * 128], h[:, kc, 2:2 + W], kc == 0, kc == 7, [wz_s, h], [pz])
                    P.act(zs[:, cc, :], pz[:, 0:W], AF.Silu, [pz], [zs.sub(cc)])

        def chunk_step(d, j, col0, ui):
            S = ST[d]
            dts = dt_tok[:, j, d * 8:(d + 1) * 8]
            a_s, cum_s, w_s, et_s = sm[4], sm[5], sm[6], sm[7]
            P.tt(a_s[:, 0:8], dts, aneg[:, d * 8:(d + 1) * 8], ALU.mult, [dt_tok.sub(j), aneg], [a_s])
            pc = P.bank()
            P.mm(pc[:, 0:8], TTm[d][:, :], a_s[:, 0:8], True, True, [TTm[d], a_s], [pc], r=False)
            P.mm(pc[:, 8:16], ones[:, :], a_s[:, 0:8], True, True, [ones, a_s], [pc], r=False)
            P.copy(cum_s[:, 0:16], pc[:, 0:16], [pc], [cum_s])
            P.tt(w_s[:, 0:8], cum_s[:, 8:16], cum_s[:, 0:8], ALU.subtract, [cum_s], [w_s])
            P.act(w_s[:, 0:8], w_s[:, 0:8], AF.Exp, [w_s], [w_s])
            P.tt(w_s[:, 0:8], w_s[:, 0:8], dts, ALU.mult, [w_s, dt_tok.sub(j)], [w_s])
            P.act(et_s[:, 0:8], cum_s[:, 8:16], AF.Exp, [cum_s], [et_s])
            pg = P.bank()
            P.mm(pg[:, 0:128], BT[:, j * 128:(j + 1) * 128], CT[:, j * 128:(j + 1) * 128], True, True, [BT, CT], [pg], r=False)
            P.tt(GM[:, :], pg[:, 0:128], TTm[d][:, :], ALU.mult, [pg, TTm[d]], [GM])
            py = pybanks[pyi[0] % 2]
            pyi[0] += 1
            for e_ in range(8):
                P.ts(lD[e_][:, :], UU[d][:, :], a_s[:, e_:e_ + 1], ALU.mult, [UU[d], a_s], [lD[e_]])
                P.ts(A1[e_][:, :], ones[:, :], a_s[:, e_:e_ + 1], ALU.mult, [ones, a_s], [A1[e_]])
            pDE = []
            for e_ in range(8):
                if e_ % 2 == 0:
                    pb_ = P.bank()
                o_ = (e_ % 2) * 256
                P.mm(pb_[:, o_:o_ + 128], lD[e_][:, :], TTm[d][:, :], True, True, [lD[e_], TTm[d]], [pb_], r=False)
                P.mm(pb_[:, o_ + 128:o_ + 256], A1[e_][:, :], TTm[d][:, :], True, True, [A1[e_], TTm[d]], [pb_], r=False)
                pDE.append((pb_, o_))
            for e_ in range(8):
                pb_, o_ = pDE[e_]
                P.act(Lx[e_][:, :], pb_[:, o_:o_ + 128], AF.Exp, [pb_], [Lx[e_]])
                P.act(Ec[e_][:, :], pb_[:, o_ + 128:o_ + 256], AF.Exp, [pb_], [Ec[e_]])
            for e_ in range(8):
                P.stt(WT[e_][:, :], Lx[e_][:, :], dts[:, e_:e_ + 1], GM[:, :], ALU.mult, ALU.mult, [Lx[e_], dt_tok.sub(j), GM], [WT[e_]])
                P.tt(Cd[e_][:, :], CT[:, j * 128:(j + 1) * 128], Ec[e_][:, :], ALU.mult, [CT, Ec[e_]], [Cd[e_]])
            for e_ in range(8):
                p0 = (e_ % 2) * 64
                cc = e_ // 2
                P.mm(py[p0:p0 + 64, cc * 128:(cc + 1) * 128], x_tok[:, j, e_ * 64:(e_ + 1) * 64], WT[e_][:, :], True, False,
                     [x_tok.sub(j), WT[e_]], [py], r=False)
                P.mm(py[p0:p0 + 64, cc * 128:(cc + 1) * 128], S[:, e_ * 64:(e_ + 1) * 64], Cd[e_][:, :], False, True,
                     [S, Cd[e_]], [py], r=False)
            P.tt(R(xdt[:, :].rearrange("p (e q) -> p e q", e=8)), x_tok[:, j, :].rearrange("p (e q) -> p e q", e=8),
                 w_s[:, 0:8].unsqueeze(2).to_broadcast([128, 8, 64]), ALU.mult, [x_tok.sub(j), w_s], [xdt])
            pu = P.bank()
            P.mm(pu[:, :], B_tok[:, j, :], xdt[:, :], True, True, [B_tok.sub(j), xdt], [pu])
            P.tt(S[:, :].rearrange("p (e q) -> p e q", e=8), S[:, :].rearrange("p (e q) -> p e q", e=8),
                 et_s[:, 0:8].unsqueeze(2).to_broadcast([128, 8, 64]), ALU.mult, [S, et_s], [S])
            P.tt(S[:, :], S[:, :], pu[:, :], ALU.add, [S, pu], [S])
            if d == 0:
                P.dbg("d_GM", GM[:, :], [128, 128], [GM])
                P.dbg("d_WT", WT[7][:, :], [128, 128], [WT[7]])
                P.dbg("d_Lx", Lx[7][:, :], [128, 128], [Lx[7]])
                P.dbg("d_Cd", Cd[7][:, :], [128, 128], [Cd[7]])
                P.dbg("d_cum", cum_s[:, :], [128, 16], [cum_s])
                P.dbg("d_w", w_s[:, :], [128, 16], [w_s])
                P.dbg("d_S", S[:, :], [128, 512], [S])
            pyv = py[:, :].rearrange("p (c t) -> p c t", c=4)
            if d == 1:
                P.copy(ybuf[:, :, :], pyv, [py], [ybuf])
                P.dma(ybs[:, col0:col0 + 128].rearrange("(c p) t -> p c t", p=128), ybuf[:, :, :], [ybuf], [ybs_t])
            else:
                ub = ubuf[ui % 2]
                P.dma(ybuf[:, :, :], ybs[:, col0:col0 + 128].rearrange("(c p) t -> p c t", p=128), [ybs_t], [ybuf])
                P.tt(ybuf[:, :, :], ybuf[:, :, :], pyv, ALU.add, [ybuf, py], [ybuf])
                for cc in range(4):
                    P.stt(ub[:, cc, :], xTc[:, cc, j * 128:(j + 1) * 128], dsk_s[:, cc:cc + 1], ybuf[:, cc, :], ALU.mult, ALU.add,
                          [xTc.sub(cc), dsk_s, ybuf], [ub])
                P.dbg("d_ysum", ybuf[:, :, :], [128, 4, 128], [ybuf])
                P.dbg("d_zs", zs[:, :, :], [128, 4, W], [zs])
                P.tt(ub[:, :, :], ub[:, :, :], zs[:, :, j * 128:(j + 1) * 128], ALU.mult, [ub, zs], [ub])
                P.dma(uT[:, col0:col0 + 128].rearrange("(c p) t -> p c t", p=128), ub[:, :, :], [ub], [])

        tiles = [(0, 0, 256)] + [(256 + 256 * t, 256, NKEY) for t in range(32)]
        order_b = [tiles[0]] + tiles[:0:-1]
        ti = 0
        for (c0, q0, q1) in order_b:
            stage_a(ti, c0, q0, q1, False)
            for j in (1, 0):
                chunk_step(1, j, c0 + j * 128, 0)
            ti += 1
        ui = 0
        for (c0, q0, q1) in tiles:
            stage_a(ti, c0, q0, q1, True)
            for j in (0, 1):
                chunk_step(0, j, c0 + j * 128, ui)
                ui += 1
            ti += 1
        P.emit()
    return nc


def mamba_maps(inp, h_lat, h_ctx):
    W = np.asarray(inp["mb_in_w"][0], np.float32)
    cwf = np.asarray(inp["mb_conv_w"][0], np.float32)
    cbf = np.asarray(inp["mb_conv_b"][0], np.float32)
    maps = []
    for core in range(NCORES):
        b, g = core // 4, core % 4
        hT = np.ascontiguousarray(np.concatenate([h_ctx[b], h_lat[b]], axis=0).T)
        xcols = np.arange(g * 512, (g + 1) * 512)
        bcols = 2048 + np.arange(g * 128, (g + 1) * 128)
        ccols = 2048 + 512 + np.arange(g * 128, (g + 1) * 128)
        ch = np.concatenate([xcols, bcols, ccols])
        wxbc = np.ascontiguousarray(W[:, 2048 + ch])
        wz = np.ascontiguousarray(W[:, g * 512:(g + 1) * 512])
        dcols = np.concatenate([5120 + g * 8 + np.arange(8), 5120 + 32 + g * 8 + np.arange(8)])
        wdt = np.ascontiguousarray(W[:, dcols])
        cw = np.ascontiguousarray(cwf[:, ch].reshape(5, 6, 128).transpose(2, 1, 0).reshape(128, 30))
        cb = np.ascontiguousarray(cbf[ch].reshape(6, 128).T)
        dtb = np.ascontiguousarray(np.asarray(inp["mb_dt_bias"][0], np.float32)[:, g * 8:(g + 1) * 8].reshape(1, 16))
        alog = np.ascontiguousarray(np.asarray(inp["mb_a_log"][0], np.float32)[:, g * 8:(g + 1) * 8].reshape(1, 16))
        dsk = np.ascontiguousarray(np.repeat(np.asarray(inp["mb_d"][0], np.float32)[g * 8:(g + 1) * 8], 64).reshape(4, 128).T)
        maps.append({"hT": hT, "wz": wz, "wxbc": wxbc, "wdt": wdt, "cw": cw, "cb": cb, "dtb": dtb, "alog": alog, "dsk": dsk})
    return maps


def mamba_gather(res):
    u_lat = np.empty((2, 8192, 2048), np.float32)
    u_ctx = np.empty((2, 256, 2048), np.float32)
    for core in range(NCORES):
        b, g = core // 4, core % 4
        u = res[core]["uT"].T
        u_ctx[b, :, g * 512:(g + 1) * 512] = u[0:256]
        u_lat[b, :, g * 512:(g + 1) * 512] = u[256:]
    return u_lat, u_ctx


RW_SCALE = 0.606531
RW_EPS = 64e-5


def build_rwkv():
    nc = bass.Bass("TRN2", target_bir_lowering=False)
    with ExitStack() as stack:
        nc.dge_precook = False
        P = Prog(nc, stack)
        P.init_psum(8)

        def din(name, shape):
            return nc.dram_tensor(name, list(shape), F32, kind="ExternalInput").ap()

        hT = din("hT", [D, NKEY])
        mixr = din("mixr", [48, 128])
        wr = din("wr", [D, 256]); wk = din("wk", [D, 256]); wv = din("wv", [D, 256])
        w1 = din("w1", [2, D, 64]); w2 = din("w2", [2, 64, 256]); w0 = din("w0", [128, 4])
        a1 = din("a1", [2, D, 64]); a2 = din("a2", [2, 64, 256]); a0 = din("a0", [128, 4])
        g1 = din("g1", [D, 128]); g2 = din("g2", [128, 256])
        kkv = din("kkv", [128, 2]); kav = din("kav", [128, 2]); rkv = din("rkv", [128, 2])
        lng = din("lng", [2, 128, 64]); lnb = din("lnb", [2, 128, 64])
        oo = nc.dram_tensor("oo", [NKEY, 256], F32, kind="ExternalOutput").ap()
        ybs = nc.dram_tensor("ybs", [NKEY, 256], F32, kind="Internal").ap()
        ybs_t = TT(ybs, "ybs")

        ident, ones = make_consts(P)
        val = P.sb("val", [128, 64])
        for hs in range(2):
            P.op("pool", lambda e, hs=hs: e.iota(val[hs * 64:(hs + 1) * 64, :], pattern=[[1, 64]], base=0, channel_multiplier=-1,
                                                allow_small_or_imprecise_dtypes=True), (), [val])
        mgt = P.sb("mgt", [128, 64]); mge = P.sb("mge", [128, 64]); mlt = P.sb("mlt", [128, 64]); mle = P.sb("mle", [128, 64])
        identp = P.sb("identp", [128, 64])
        P.ts(mgt[:], val[:], 0.0, ALU.is_gt, [val], [mgt]); P.ts(mge[:], val[:], 0.0, ALU.is_ge, [val], [mge])
        P.ts(mlt[:], val[:], 0.0, ALU.is_lt, [val], [mlt]); P.ts(mle[:], val[:], 0.0, ALU.is_le, [val], [mle])
        P.ts(identp[:], val[:], 0.0, ALU.is_equal, [val], [identp])
        maskT = [P.sb("maskTf", [128, 128]), P.sb("maskTb", [128, 128])]
        P.copy(maskT[0][:, 0:64], mgt[:], [mgt], [maskT[0]]); P.copy(maskT[0][:, 64:128], mge[:], [mge], [maskT[0]])
        P.copy(maskT[1][:, 0:64], mlt[:], [mlt], [maskT[1]]); P.copy(maskT[1][:, 64:128], mle[:], [mle], [maskT[1]])
        maskA = [mlt, mgt]
        blk = P.sb("blk", [128, 128])
        P.memset(blk[:], 0.0, [blk])
        P.memset(blk[0:64, 0:64], 1.0, [blk]); P.memset(blk[64:128, 64:128], 1.0, [blk])
        half = P.sb("half", [128, 2])
        P.memset(half[:], 0.5, [half])
        tiny = P.sb("tiny", [128, 1])
        P.memset(tiny[:], RW_EPS, [tiny])

        def wload(name, ap, shape, rr=True):
            t = P.sb(name, shape)
            P.dma(t[:], ap, [], [t], r=rr)
            return t
        wr_s = wload("wr_s", wr.rearrange("(kc p) n -> p kc n", p=128), [128, 8, 256])
        wk_s = wload("wk_s", wk.rearrange("(kc p) n -> p kc n", p=128), [128, 8, 256])
        wv_s = wload("wv_s", wv.rearrange("(kc p) n -> p kc n", p=128), [128, 8, 256])
        g1_s = wload("g1_s", g1.rearrange("(kc p) n -> p kc n", p=128), [128, 8, 128])
        g2_s = wload("g2_s", g2, [128, 256])
        w1_s = [wload(f"w1_{d}", w1[d].rearrange("(kc p) n -> p kc n", p=128), [128, 8, 64]) for d in range(2)]
        a1_s = [wload(f"a1_{d}", a1[d].rearrange("(kc p) n -> p kc n", p=128), [128, 8, 64]) for d in range(2)]
        w2_s = [wload(f"w2_{d}", w2[d], [64, 256]) for d in range(2)]
        a2_s = [wload(f"a2_{d}", a2[d], [64, 256]) for d in range(2)]
        w0_s = wload("w0_s", w0, [128, 4], False); a0_s = wload("a0_s", a0, [128, 4], False)
        kk_s = wload("kk_s", kkv, [128, 2], False); ka_s = wload("ka_s", kav, [128, 2], False); rk_s = wload("rk_s", rkv, [128, 2], False)
        omka = P.sb("omka", [128, 2])
        P.ts(omka[:], ka_s[:], -1.0, ALU.mult, [ka_s], [omka], s2=1.0, op1=ALU.add)
        lng_s = [wload(f"lng{i}", lng[i], [128, 64], False) for i in range(2)]
        lnb_s = [wload(f"lnb{i}", lnb[i], [128, 64], False) for i in range(2)]
        mixc = P.sb("mixc", [128, 48])
        load_cols(P, ident, mixc, mixc[:], mixr, 48)

        W = 256
        NCH = 4
        ht = [P.sb(f"ht{i}", [128, 8, W + 2]) for i in range(2)]
        xx = P.sb("xx", [128, 8, W])
        xm = [P.sb(f"xm{i}", [128, 8, W]) for i in range(2)]
        rT = P.sb("rT", [128, 2, W]); kT = P.sb("kT", [128, 2, W]); kkT = P.sb("kkT", [128, 2, W])
        lwT = P.sb("lwT", [128, 2, W]); clT = P.sb("clT", [128, 2, W]); cleT = P.sb("cleT", [128, 2, W])
        aT = [P.sb(f"aT{d}", [128, 2, W]) for d in range(2)]
        kdT = [P.sb(f"kdT{d}", [128, 2, W]) for d in range(2)]
        bT = P.sb("bT", [128, 2, W])
        e1 = P.sb("e1", [128, 2, W]); e2 = P.sb("e2", [128, 2, W]); e3 = P.sb("e3", [128, 2, W])
        lam = P.sb("lam", [128, 2, NCH])
        KR = P.sb("KR", [128, 2, NCH, 128]); BK = P.sb("BK", [128, 2, NCH, 128])
        tw = P.sb("tw", [64, W]); t1 = P.sb("t1", [128, W])
        Vt = P.sb("Vt", [128, NCH, 2, 64]); Gt = P.sb("Gt", [128, NCH, 2, 64])
        prodT = P.sb("prodT", [128, 2, W]); bsc = P.sb("bsc", [128, NCH, 2, 2])
        sq = P.sb("sqk", [128, W]); rn = P.sb("rnk", [128, W])
        onesW = P.sb("onesW", [128, W])
        P.memset(onesW[:], 1.0, [onesW])
        ST = [[P.sb(f"ST{d}{hp}", [128, 64]) for hp in range(2)] for d in range(2)]
        for d_ in range(2):
            for hp in range(2):
                P.memset(ST[d_][hp][:], 0.0, [ST[d_][hp]])

        class Slot:
            pass
        slots = []
        for si in range(8):
            s_ = Slot()
            for nm, shp in (("AWu", [128, 128]), ("BWv", [128, 128]), ("PPa", [128, 128]), ("PPb", [128, 128]), ("X", [128, 64]),
                            ("RH", [128, 128]), ("UK", [128, 128]), ("BKt", [128, 128]), ("MpT", [128, 64]), ("Nc", [128, 64]),
                            ("R2T", [128, 64]), ("Y0", [128, 64])):
                setattr(s_, nm, P.sb(f"{nm}{si}", shp))
            slots.append(s_)
        ysb = [P.sb(f"ysb{i}", [128, 64]) for i in range(4)]
        ybb = [P.sb(f"ybb{i}", [128, 64]) for i in range(4)]
        stt6 = [P.sb(f"st6{i}", [128, 6]) for i in range(2)]
        mv = [P.sb(f"mv{i}", [128, 4]) for i in range(2)]

        def stage_a(ti, c0, q0, q1, dirs, fwd):
            h = ht[ti % 2]
            lo, hi = max(c0 - 1, q0), min(c0 + W + 1, q1)
            off = lo - (c0 - 1)
            n = hi - lo
            if off > 0:
                P.memset(h[:, :, 0:off], 0.0, [h])
            if off + n < W + 2:
                P.memset(h[:, :, off + n:W + 2], 0.0, [h])
            for kc in range(8):
                P.dma(h[:, kc, off:off + n], hT[kc * 128:(kc + 1) * 128, lo:hi], [], [h])
            P.tt(xx[:, :, :], h[:, :, 0:W], h[:, :, 2:W + 2], ALU.add, [h], [xx])
            P.stt(xx[:, :, :], xx[:, :, :], 0.5, h[:, :, 1:W + 1], ALU.mult, ALU.subtract, [xx, h], [xx])
            mi = [0]

            def mixed(j):
                m_ = xm[mi[0] % 2]
                mi[0] += 1
                for kc in range(8):
                    P.stt(R(m_[:, kc, :]), xx[:, kc, :], mixc[:, j * 8 + kc:j * 8 + kc + 1], h[:, kc, 1:W + 1], ALU.mult, ALU.add,
                          [xx, mixc, h], [m_])
                return m_

            def proj2(m_, w_s, dst):
                for oc in range(2):
                    pb = P.bank()
                    for kc in range(8):
                        P.mm(pb[:, 0:W], w_s[:, kc, oc * 128:(oc + 1) * 128], m_[:, kc, :], kc == 0, kc == 7, [w_s, m_], [pb])
                    P.copy(dst[:, oc, :], pb[:, 0:W], [pb], [dst], eng="act")

            def lora(m_, l1_s, l2_s, bias_s, d, dst, mid_func):
                pb = P.bank()
                for kc in range(8):
                    P.mm(pb[0:64, 0:W], l1_s[:, kc, :], m_[:, kc, :], kc == 0, kc == 7, [l1_s, m_], [pb])
                P.act(R(tw[:, :]), pb[0:64, 0:W], mid_func, [pb], [tw])
                for oc in range(2):
                    p2 = P.bank()
                    P.mm(p2[:, 0:W], l2_s[:, oc * 128:(oc + 1) * 128], tw[:, :], True, True, [l2_s, tw], [p2])
                    P.act(dst[:, oc, :], p2[:, 0:W], AF.Sigmoid, [p2, bias_s], [dst], bias=bias_s[:, d * 2 + oc:d * 2 + oc + 1])

            m_ = mixed(0)
            proj2(m_, wr_s, rT)
            m_ = mixed(1)
            dd = dirs[0] if len(dirs) == 1 else 0
            lora(m_, w1_s[dd], w2_s[dd], w0_s, dd, lwT, AF.Tanh)
            P.ts(lwT[:, :, :], lwT[:, :, :], -RW_SCALE, ALU.mult, [lwT], [lwT])
            m_ = mixed(2)
            proj2(m_, wk_s, kT)
            m_ = mixed(3)
            for j4 in range(NCH):
                pv = P.bank()
                for hq in range(4):
                    p0 = (hq % 2) * 64
                    hp = hq // 2
                    for kc in range(8):
                        P.mm(pv[p0:p0 + 64, hp * 64:(hp + 1) * 64], m_[:, kc, j4 * 64:(j4 + 1) * 64], wv_s[:, kc, hq * 64:(hq + 1) * 64],
                             kc == 0, kc == 7, [m_, wv_s], [pv], r=False)
                P.copy(Vt[:, j4, :, :], pv[:, 0:128].rearrange("p (a b) -> p a b", a=2), [pv], [Vt.sub(j4)], eng="act")
            m_ = mixed(4)
            adirs = [0, 1] if fwd else dirs
            for d in adirs:
                lora(m_, a1_s[d], a2_s[d], a0_s, d, aT[d], AF.Copy)
            if fwd:
                m_ = mixed(5)
                pb = P.bank()
                for kc in range(8):
                    P.mm(pb[:, 0:W], g1_s[:, kc, :], m_[:, kc, :], kc == 0, kc == 7, [g1_s, m_], [pb])
                P.act(t1[:, :], pb[:, 0:W], AF.Sigmoid, [pb], [t1])
                for j4 in range(NCH):
                    pg = P.bank()
                    for hq in range(4):
                        p0 = (hq % 2) * 64
                        hp = hq // 2
                        P.mm(pg[p0:p0 + 64, hp * 64:(hp + 1) * 64], t1[:, j4 * 64:(j4 + 1) * 64], g2_s[:, hq * 64:(hq + 1) * 64],
                             True, True, [t1, g2_s], [pg], r=False)
                    P.copy(Gt[:, j4, :, :], pg[:, 0:128].rearrange("p (a b) -> p a b", a=2), [pg], [Gt.sub(j4)], eng="act")
            for oc in range(2):
                P.ts(kkT[:, oc, :], kT[:, oc, :], kk_s[:, oc:oc + 1], ALU.mult, [kT, kk_s], [kkT])
                P.act(sq[:, :], kkT[:, oc, :], AF.Square, [kkT], [sq])
                pb = P.bank()
                P.mm(pb[:, 0:W], blk[:, :], sq[:, :], True, True, [blk, sq], [pb], r=False)
                P.ts(rn[:, :], pb[:, 0:W], 1e-24, ALU.max, [pb], [rn])
                P.act(rn[:, :], rn[:, :], AF.Ln, [rn], [rn])
                P.act(rn[:, :], rn[:, :], AF.Exp, [rn], [rn], scale=-0.5)
                P.tt(kkT[:, oc, :], kkT[:, oc, :], rn[:, :], ALU.mult, [kkT, rn], [kkT])
                for d in adirs:
                    P.ts(kdT[d][:, oc, :], aT[d][:, oc, :], ka_s[:, oc:oc + 1], ALU.mult, [aT[d], ka_s, omka], [kdT[d]],
                         s2=omka[:, oc:oc + 1], op1=ALU.add)
                    P.tt(kdT[d][:, oc, :], kdT[d][:, oc, :], kT[:, oc, :], ALU.mult, [kdT[d], kT], [kdT[d]])
            if fwd:
                P.tt(prodT[:, :, :], kdT[0][:, :, :], kdT[1][:, :, :], ALU.add, [kdT[0], kdT[1]], [prodT])
                for oc in range(2):
                    P.stt(prodT[:, oc, :], rT[:, oc, :], rk_s[:, oc:oc + 1], prodT[:, oc, :], ALU.mult, ALU.mult, [rT, rk_s, prodT], [prodT])
                for j4 in range(NCH):
                    pb = P.bank()
                    for hq in range(4):
                        p0 = (hq % 2) * 64
                        hp = hq // 2
                        P.mm(pb[p0:p0 + 64, hp * 2:hp * 2 + 2], prodT[p0:p0 + 64, hp, j4 * 64:(j4 + 1) * 64], half[p0:p0 + 64, 0:2],
                             True, True, [prodT, half], [pb], r=False)
                    P.copy(bsc[:, j4, :, :], pb[:, 0:4].rearrange("p (a b) -> p a b", a=2), [pb], [bsc.sub(j4)])
            d = dirs[0] if len(dirs) == 1 else 0
            P.tt(bT[:, :, :], kkT[:, :, :], aT[d][:, :, :], ALU.mult, [kkT, aT[d]], [bT])
            for oc in range(2):
                P.op("dve", lambda e, oc=oc: e.tensor_tensor_scan(out=clT[:, oc, :], data0=onesW[:, :], data1=lwT[:, oc, :], initial=0.0,
                                                                   op0=ALU.mult, op1=ALU.add), [onesW, lwT], [clT])
                for j4 in range(NCH - 1, 0, -1):
                    P.ts(clT[:, oc, j4 * 64:(j4 + 1) * 64], clT[:, oc, j4 * 64:(j4 + 1) * 64], clT[:, oc, j4 * 64 - 1:j4 * 64], ALU.subtract,
                         [clT], [clT])
                if d == 0:
                    P.tt(cleT[:, oc, :], clT[:, oc, :], lwT[:, oc, :], ALU.subtract, [clT, lwT], [cleT])
                else:
                    for j4 in range(NCH):
                        P.ts(cleT[:, oc, j4 * 64:(j4 + 1) * 64], clT[:, oc, j4 * 64:(j4 + 1) * 64], -1.0, ALU.mult, [clT], [cleT],
                             s2=clT[:, oc, j4 * 64 + 63:j4 * 64 + 64], op1=ALU.add)
                    P.tt(clT[:, oc, :], cleT[:, oc, :], lwT[:, oc, :], ALU.add, [cleT, lwT, clT], [clT])
            P.act(e1[:, :, :], cleT[:, :, :], AF.Exp, [cleT], [e1])
            P.act(e2[:, :, :], clT[:, :, :], AF.Exp, [clT], [e2], scale=-1.0)
            P.act(e3[:, :, :], clT[:, :, :], AF.Exp, [clT], [e3])
            for oc in range(2):
                e3v = e3[:, oc, :].rearrange("p (c t) -> p c t", c=NCH)
                P.copy(lam[:, oc, :], e3v[:, :, 63] if d == 0 else e3v[:, :, 0], [e3], [lam])
                kkv_ = kkT[:, oc, :].rearrange("p (c t) -> p c t", c=NCH)
                P.tt(KR[:, oc, :, 0:64], kkv_, e1[:, oc, :].rearrange("p (c t) -> p c t", c=NCH), ALU.mult, [kkT, e1], [KR])
                P.tt(KR[:, oc, :, 64:128], rT[:, oc, :].rearrange("p (c t) -> p c t", c=NCH), e3v, ALU.mult, [rT, e3], [KR])
                e2v = e2[:, oc, :].rearrange("p (c t) -> p c t", c=NCH)
                P.tt(BK[:, oc, :, 0:64], bT[:, oc, :].rearrange("p (c t) -> p c t", c=NCH), e2v, ALU.mult, [bT, e2], [BK])
                P.tt(BK[:, oc, :, 64:128], kdT[d][:, oc, :].rearrange("p (c t) -> p c t", c=NCH), e2v, ALU.mult, [kdT[d], e2], [BK])

        def pre(sl, hp, d, j4):
            kr = KR[:, hp, j4, :]
            bk = BK[:, hp, j4, :]
            V = Vt[:, j4, hp, :]
            hs2 = [(0, 64), (64, 128)]
            pA = P.bank()
            for (a, b) in hs2:
                P.mm(pA[a:b, 0:128], bk[a:b, 0:64], kr[a:b, :], True, True, [BK, KR], [pA], r=False)
                P.mm(pA[a:b, 128:256], bk[a:b, 64:128], kr[a:b, :], True, True, [BK, KR], [pA], r=False)
                P.mm(pA[a:b, 256:320], kr[a:b, 0:64], bk[a:b, 0:64], True, True, [BK, KR], [pA], r=False)
            yield
            P.tt(sl.AWu[:, :], pA[:, 0:128], maskT[d][:, :], ALU.mult, [pA, maskT[d]], [sl.AWu])
            P.tt(sl.BWv[:, :], pA[:, 128:256], maskT[d][:, :], ALU.mult, [pA, maskT[d]], [sl.BWv])
            P.stt(sl.PPa[:, 0:64], pA[:, 256:320], -1.0, maskA[d][:, :], ALU.mult, ALU.mult, [pA, maskA[d]], [sl.PPa])
            P.ts(sl.PPa[:, 64:128], sl.AWu[:, 0:64], -1.0, ALU.mult, [sl.AWu], [sl.PPa])
            P.tt(sl.X[:, :], identp[:, :], sl.PPa[:, 64:128], ALU.add, [identp, sl.PPa], [sl.X])
            yield
            cur, nxt = sl.PPa, sl.PPb
            for m in range(1, 6):
                pL = P.bank()
                for (a, b) in hs2:
                    P.mm(pL[a:b, 0:64], cur[a:b, 64:128], cur[a:b, 0:64], True, True, [cur], [pL], r=False)
                    if m < 5:
                        P.mm(pL[a:b, 64:128], cur[a:b, 0:64], cur[a:b, 64:128], True, True, [cur], [pL], r=False)
                yield
                if m < 5:
                    P.copy(nxt[:, :], pL[:, 0:128], [pL], [nxt], eng="act")
                else:
                    P.copy(nxt[:, 0:64], pL[:, 0:64], [pL], [nxt], eng="act")
                yield
                pX = P.bank()
                for (a, b) in hs2:
                    P.mm(pX[a:b, 0:64], nxt[a:b, 0:64], sl.X[a:b, :], True, True, [nxt, sl.X], [pX], r=False)
                yield
                P.tt(sl.X[:, :], sl.X[:, :], pX[:, 0:64], ALU.add, [sl.X, pX], [sl.X])
                yield
                cur, nxt = nxt, cur
            pR = P.bank()
            for (a, b) in hs2:
                P.mm(pR[a:b, 0:64], sl.BWv[a:b, 0:64], V[a:b, :], True, True, [sl.BWv, Vt.sub(j4)], [pR], r=False)
                P.mm(pR[a:b, 64:128], kr[a:b, 0:64], ident[a:b, a:b], True, True, [KR, ident], [pR], r=False)
            yield
            P.copy(sl.RH[:, :], pR[:, 0:128], [pR], [sl.RH], eng="act")
            yield
            pXR = P.bank()
            for (a, b) in hs2:
                P.mm(pXR[a:b, 0:128], sl.X[a:b, :], sl.RH[a:b, :], True, True, [sl.X, sl.RH], [pXR], r=False)
            yield
            P.ts(sl.UK[:, 0:64], pXR[:, 0:64], -1.0, ALU.mult, [pXR], [sl.UK])
            P.copy(sl.UK[:, 64:128], pXR[:, 64:128], [pXR], [sl.UK], eng="act")
            yield
            pT = P.bank()
            for (a, b) in hs2:
                P.mm(pT[a:b, 0:64], bk[a:b, 0:64], ident[a:b, a:b], True, True, [BK, ident], [pT], r=False)
                P.mm(pT[a:b, 64:128], bk[a:b, 64:128], ident[a:b, a:b], True, True, [BK, ident], [pT], r=False)
            yield
            P.copy(sl.BKt[:, :], pT[:, 0:128], [pT], [sl.BKt], eng="act")
            yield
            pM = P.bank()
            for (a, b) in hs2:
                P.mm(pM[a:b, 0:64], sl.UK[a:b, 64:128], sl.BKt[a:b, 0:64], True, True, [sl.UK, sl.BKt], [pM], r=False)
                P.mm(pM[a:b, 64:128], sl.BKt[a:b, 0:64], sl.UK[a:b, 0:64], True, False, [sl.UK, sl.BKt], [pM], r=False)
                P.mm(pM[a:b, 64:128], sl.BKt[a:b, 64:128], V[a:b, :], False, True, [sl.BKt, Vt.sub(j4)], [pM], r=False)
                P.mm(pM[a:b, 128:192], sl.UK[a:b, 64:128], sl.AWu[a:b, 64:128], True, True, [sl.UK, sl.AWu], [pM], r=False)
            yield
            P.tt(sl.MpT[:, :], identp[:, :], pM[:, 0:64], ALU.subtract, [identp, pM], [sl.MpT])
            P.ts(sl.Nc[:, :], pM[:, 64:128], lam[:, hp, j4:j4 + 1], ALU.mult, [pM, lam], [sl.Nc])
            P.tt(sl.R2T[:, :], kr[:, 64:128], pM[:, 128:192], ALU.subtract, [KR, pM], [sl.R2T])
            yield
            pY = P.bank()
            for (a, b) in hs2:
                P.mm(pY[a:b, 0:64], sl.AWu[a:b, 64:128], sl.UK[a:b, 0:64], True, False, [sl.AWu, sl.UK], [pY], r=False)
                P.mm(pY[a:b, 0:64], sl.BWv[a:b, 64:128], V[a:b, :], False, True, [sl.BWv, Vt.sub(j4)], [pY], r=False)
            yield
            P.copy(sl.Y0[:, :], pY[:, 0:64], [pY], [sl.Y0], eng="act")

        def seq(sl, hp, d, j4, col0, k):
            S = ST[d][hp]
            hs2 = [(0, 64), (64, 128)]
            pYs = P.bank()
            for (a, b) in hs2:
                P.mm(pYs[a:b, 0:64], sl.R2T[a:b, :], S[a:b, :], True, True, [sl.R2T, S], [pYs], r=False)
            ys = ysb[k % 4]
            P.tt(ys[:, :], sl.Y0[:, :], pYs[:, 0:64], ALU.add, [sl.Y0, pYs], [ys])
            pS = P.bank()
            for (a, b) in hs2:
                P.mm(pS[a:b, 0:64], sl.MpT[a:b, :], S[a:b, :], True, True, [sl.MpT, S], [pS], r=False)
            P.stt(S[:, :], pS[:, 0:64], lam[:, hp, j4:j4 + 1], sl.Nc[:, :], ALU.mult, ALU.add, [pS, lam, sl.Nc], [S])
            if d == 1:
                for hs, (a, b) in enumerate(hs2):
                    hq = hp * 2 + hs
                    P.dma(ybs[col0:col0 + 64, hq * 64:(hq + 1) * 64], ys[a:b, :], [ys], [ybs_t])
            else:
                yb = ybb[k % 4]
                for hs, (a, b) in enumerate(hs2):
                    hq = hp * 2 + hs
                    P.dma(yb[a:b, :], ybs[col0:col0 + 64, hq * 64:(hq + 1) * 64], [ybs_t], [yb])
                P.tt(ys[:, :], ys[:, :], yb[:, :], ALU.add, [ys, yb], [ys])
                s6 = stt6[k % 2]
                m2 = mv[k % 2]
                P.op("dve", lambda e, s6=s6, ys=ys: e.bn_stats(out=s6[:, :], in_=ys[:, :]), [ys], [s6])
                P.op("dve", lambda e, s6=s6, m2=m2: e.bn_aggr(out=m2[:, 0:2], in_=s6[:, :]), [s6], [m2])
                P.act(m2[:, 2:3], m2[:, 1:2], AF.Sqrt, [m2, tiny], [m2], bias=tiny[:, 0:1])
                P.op("dve", lambda e, m2=m2: e.reciprocal(out=m2[:, 3:4], in_=m2[:, 2:3]), [m2], [m2])
                P.ts(ys[:, :], ys[:, :], m2[:, 0:1], ALU.subtract, [ys, m2], [ys], s2=m2[:, 3:4], op1=ALU.mult)
                P.tt(ys[:, :], ys[:, :], lng_s[hp][:, :], ALU.mult, [ys, lng_s[hp]], [ys])
                P.tt(ys[:, :], ys[:, :], lnb_s[hp][:, :], ALU.add, [ys, lnb_s[hp]], [ys])
                P.stt(yb[:, :], Vt[:, j4, hp, :], bsc[:, j4, hp, 0:1], ys[:, :], ALU.mult, ALU.add, [Vt.sub(j4), bsc.sub(j4), ys], [yb])
                P.tt(yb[:, :], yb[:, :], Gt[:, j4, hp, :], ALU.mult, [yb, Gt.sub(j4)], [yb])
                for hs, (a, b) in enumerate(hs2):
                    hq = hp * 2 + hs
                    P.dma(oo[col0:col0 + 64, hq * 64:(hq + 1) * 64], yb[a:b, :], [yb], [])

        tiles = [(0, 0, 256)] + [(256 + 256 * t, 256, NKEY) for t in range(32)]
        order_b = [tiles[0]] + tiles[:0:-1]
        ti = 0
        k = 0
        for d, order, chunks in ((1, order_b, (3, 2, 1, 0)), (0, tiles, (0, 1, 2, 3))):
            for (c0, q0, q1) in order:
                stage_a(ti, c0, q0, q1, [d], d == 0)
                ti += 1
                units = [(j4, hp) for j4 in chunks for hp in range(2)]
                gens = [pre(slots[ui], hp, d, j4) for ui, (j4, hp) in enumerate(units)]
                live = list(gens)
                while live:
                    nxt_live = []
                    for g_ in live:
                        try:
                            next(g_)
                            nxt_live.append(g_)
                        except StopIteration:
                            pass
                    live = nxt_live
                for ui, (j4, hp) in enumerate(units):
                    seq(slots[ui], hp, d, j4, c0 + j4 * 64, k)
                    k += 1
        P.emit()
    return nc


def rwkv_maps(inp, h_lat, h_ctx):
    f = lambda k: np.asarray(inp[k][0], np.float32)
    rkv, w0, w1, w2 = f("rw_rkv_w"), f("rw_w0"), f("rw_w1"), f("rw_w2")
    a0, a1, a2, g1, g2 = f("rw_a0"), f("rw_a1"), f("rw_a2"), f("rw_g1"), f("rw_g2")
    k_k, k_a, r_k, ln_g, ln_b = f("rw_k_k"), f("rw_k_a"), f("rw_r_k").reshape(-1), f("rw_ln_g"), f("rw_ln_b")
    mixr = rows128(f("rw_mix"))
    maps = []
    for core in range(NCORES):
        b, hg = core // 4, core % 4
        sl = slice(hg * 256, (hg + 1) * 256)
        hT = np.ascontiguousarray(np.concatenate([h_ctx[b], h_lat[b]], axis=0).T)

        def pc(v):
            return np.ascontiguousarray(v.reshape(2, 128).T)

        def pc2(v2):
            return np.ascontiguousarray(v2.reshape(2, 2, 128).transpose(2, 0, 1).reshape(128, 4))

        def pairrows(v):
            hh = v.reshape(2, 2, 1, 64)
            return np.ascontiguousarray(np.broadcast_to(hh, (2, 2, 64, 64)).reshape(2, 128, 64))
        maps.append({
            "hT": hT, "mixr": mixr,
            "wr": np.ascontiguousarray(rkv[0][:, sl]), "wk": np.ascontiguousarray(rkv[1][:, sl]), "wv": np.ascontiguousarray(rkv[2][:, sl]),
            "w1": w1, "w2": np.ascontiguousarray(w2[:, :, sl]), "w0": pc2(w0[:, sl]),
            "a1": a1, "a2": np.ascontiguousarray(a2[:, :, sl]), "a0": pc2(a0[:, sl]),
            "g1": g1, "g2": np.ascontiguousarray(g2[:, sl]),
            "kkv": pc(k_k[sl]), "kav": pc(k_a[sl]), "rkv": pc(r_k[sl]),
            "lng": pairrows(ln_g[sl]), "lnb": pairrows(ln_b[sl]),
        })
    return maps


def rwkv_gather(res):
    o_lat = np.empty((2, 8192, 1024), np.float32)
    o_ctx = np.empty((2, 256, 1024), np.float32)
    for core in range(NCORES):
        b, hg = core // 4, core % 4
        o = res[core]["oo"]
        o_ctx[b, :, hg * 256:(hg + 1) * 256] = o[0:256]
        o_lat[b, :, hg * 256:(hg + 1) * 256] = o[256:]
    return o_lat, o_ctx


def _get(key, builder):
    if key not in _NC_CACHE:
        _NC_CACHE[key] = builder()
    return _NC_CACHE[key]


def attn_maps(inp, h_lat, h_ctx):
    cosT, sinS, prot = rope_tables()
    Wq = np.asarray(inp["at_qkv_w"][0], np.float32)
    qg = np.asarray(inp["at_q_g"][0], np.float32).reshape(64, 1)
    kg = np.asarray(inp["at_k_g"][0], np.float32).reshape(64, 1)
    maps = []
    for core in range(NCORES):
        b, kv = core // 4, core % 4
        hT = np.ascontiguousarray(np.concatenate([h_ctx[b], h_lat[b]], axis=0).T)
        maps.append({"hT": hT, "wq": np.ascontiguousarray(Wq[:, kv * 256:(kv + 1) * 256]),
                     "wk": np.ascontiguousarray(Wq[:, 1024 + kv * 64:1024 + (kv + 1) * 64]),
                     "wv": np.ascontiguousarray(Wq[:, 1280 + kv * 64:1280 + (kv + 1) * 64]),
                     "qg": qg, "kg": kg, "cosT": cosT, "sinS": sinS, "prot": prot})
    return maps


def attn_gather(res):
    o = np.zeros((2, 8192, 1024), np.float32)
    for core in range(NCORES):
        b, kv = core // 4, core % 4
        o[b, :, kv * 256:(kv + 1) * 256] = res[core]["oT"].T
    return o


def kernel(**inp):
    f32 = lambda a: np.asarray(a, np.float32)
    x, c, ctx, c_ctx = f32(inp["x"]), f32(inp["c"]), f32(inp["ctx"]), f32(inp["c_ctx"])
    mod_w, mod_b = f32(inp["mod_w"]), f32(inp["mod_b"])
    n1g, n2g = f32(inp["norm1_g"]), f32(inp["norm2_g"])
    cv = [cvec_for(c, c_ctx, core) for core in range(NCORES)]

    def nxt_params(i):
        return {"mod_w_n": mod_w[i], "mod_b_n": rows128(mod_b[i]), "n1g_n": rows128(n1g[i])}

    def cur_params(i):
        return {"mod_w": mod_w[i], "mod_b": rows128(mod_b[i]), "n2g": rows128(n2g[i])}

    xs = to_cores_T(x, ctx)
    res = run(_get(("ts", None, None, "norm"), lambda: build_ts(0, None, None, "norm")),
              [dict(xT=xs[k], cvec=cv[k], **nxt_params(0)) for k in range(NCORES)])
    h_lat, h_ctx = from_cores_T([r["hT_out"] for r in res])
    mres = run(_get("mamba", build_mamba), mamba_maps(inp, h_lat, h_ctx))
    u_lat, u_ctx = mamba_gather(mres)
    ms = to_cores_T(u_lat, u_ctx)
    res = run(_get(("ts", "mamba", "dense", "norm"), lambda: build_ts(0, "mamba", "dense", "norm")),
              [dict(xT=xs[k], mT=ms[k], cvec=cv[k], mb_ng=rows128(f32(inp["mb_norm_g"])[0]), w_post=f32(inp["mb_out_w"])[0],
                    w_in=f32(inp["ff_in_w"])[0], w_out=f32(inp["ff_out_w"])[0], **cur_params(0), **nxt_params(1)) for k in range(NCORES)])
    xs = [r["xT_out"] for r in res]
    h_lat, h_ctx = from_cores_T([r["hT_out"] for r in res])
    rres = run(_get("rwkv", build_rwkv), rwkv_maps(inp, h_lat, h_ctx))
    o_lat, o_ctx = rwkv_gather(rres)
    ms = to_cores_T(o_lat, o_ctx)
    ts_moe_n = _get(("ts", "lin", "moe", "norm"), lambda: build_ts(1, "lin", "moe", "norm"))
    res = run(ts_moe_n,
              [dict(xT=xs[k], mT=ms[k], cvec=cv[k], w_post=f32(inp["rw_out_w"])[0], router=f32(inp["moe_router_w"])[0],
                    w_in=f32(inp["moe_in_w"])[0], w_out=f32(inp["moe_out_w"])[0], **cur_params(1), **nxt_params(2)) for k in range(NCORES)])
    xs = [r["xT_out"] for r in res]
    h_lat, h_ctx = from_cores_T([r["hT_out"] for r in res])
    pin = pool_inputs(h_lat, h_ctx)
    res = run(_get(("ts", "pool", "dense", "norm"), lambda: build_ts(2, "pool", "dense", "norm")),
              [dict(xT=xs[k], cvec=cv[k], pl_w=f32(inp["pl_w"])[0], pl_scale=rows128(f32(inp["pl_scale"])[0]),
                    w_in=f32(inp["ff_in_w"])[1], w_out=f32(inp["ff_out_w"])[1], **pin[k], **cur_params(2), **nxt_params(3))
               for k in range(NCORES)])
    xs = [r["xT_out"] for r in res]
    h_lat, h_ctx = from_cores_T([r["hT_out"] for r in res])
    ares = run(_get("attn", build_attn), attn_maps(inp, h_lat, h_ctx))
    o_lat = attn_gather(ares)
    ms = to_cores_T(o_lat, np.zeros((2, 256, 1024), np.float32))
    res = run(_get(("ts", "lin", "moe", "final"), lambda: build_ts(3, "lin", "moe", "final")),
              [dict(xT=xs[k], mT=ms[k], cvec=cv[k], w_post=f32(inp["at_out_w"])[0], router=f32(inp["moe_router_w"])[1],
                    w_in=f32(inp["moe_in_w"])[1], w_out=f32(inp["moe_out_w"])[1], fin_g=rows128(f32(inp["final_g"])), **cur_params(3))
               for k in range(NCORES)])
    out_lat, _ = from_cores_T([r["out_T"] for r in res])
    return out_lat
```

```python
import numpy as np
from contextlib import ExitStack
import concourse.bass as bass
import concourse.mybir as mybir
from concourse.bass_utils import run_bass_kernel_spmd

F32 = mybir.dt.float32
F32R = mybir.dt.float32r
I32 = mybir.dt.int32
AF = mybir.ActivationFunctionType
ALU = mybir.AluOpType
AX = mybir.AxisListType


def R(ap):
    return ap.bitcast(F32R)

D = 1024
KC = 8
NCORES = 8
EPS = 1e-6


class Buf:
    __slots__ = ("lw", "rd", "name", "parent")

    def __init__(self, name="", parent=None):
        self.lw = None
        self.rd = {}
        self.name = name
        self.parent = parent


class TT:
    def __init__(self, t, name):
        self.t = t
        self.name = name
        self.b = Buf(name)
        self.subs = {}

    def sub(self, key):
        if key not in self.subs:
            self.subs[key] = Buf(f"{self.name}/{key}", self.b)
        return self.subs[key]

    def __getitem__(self, idx):
        return self.t[idx]


class _SubView:
    def __init__(self, tt, subs):
        self.tt = tt
        self.subs = subs

    def __getitem__(self, idx):
        return self.tt.t[idx]


def _bufs(lst):
    out = []
    for x in lst:
        if x is None:
            continue
        if isinstance(x, TT):
            out.append(x.b)
            out.extend(x.subs.values())
        elif isinstance(x, _SubView):
            out.extend(x.subs)
        else:
            out.append(x)
    return out


class Prog:
    NDS = 24

    def __init__(self, nc, stack):
        self.nc = nc
        self.stack = stack
        self.eng = {"pe": nc.tensor, "dve": nc.vector, "act": nc.scalar, "pool": nc.gpsimd, "sp": nc.sync}
        self.sem = {k: stack.enter_context(nc.semaphore("s_" + k)) for k in self.eng}
        self.cnt = {k: 0 for k in self.eng}
        self.waited = {k: {} for k in self.eng}
        self.ops = {k: [] for k in self.eng}
        self.dsem = [stack.enter_context(nc.semaphore(f"d{i}")) for i in range(self.NDS)]
        self.dval = [0] * self.NDS
        self.dnext = 0
        self.nalloc = 0
        self.psum_banks = None
        self.psum_next = 0

    def sb(self, name, shape, dtype=F32):
        self.nalloc += 1
        t = self.stack.enter_context(self.nc.sbuf_tensor(f"{name}_{self.nalloc}", list(shape), dtype))
        return TT(t, name)

    def ps(self, name, shape, dtype=F32):
        self.nalloc += 1
        t = self.stack.enter_context(self.nc.psum_tensor(f"{name}_{self.nalloc}", list(shape), dtype))
        return TT(t, name)

    def init_psum(self, n=8):
        self.psum_banks = [self.ps(f"bank{i}", [128, 512]) for i in range(n)]

    def bank(self):
        b = self.psum_banks[self.psum_next % len(self.psum_banks)]
        self.psum_next += 1
        return b

    def dram(self, name, shape, dtype=F32, kind="Internal"):
        t = self.nc.dram_tensor(name, list(shape), dtype, kind=kind)
        return TT(t.ap(), name)

    def _collect(self, engine, reads, writes, extra=None):
        need = {}

        def add(ev):
            if ev is None:
                return
            k, v = ev
            if need.get(k, 0) < v:
                need[k] = v

        for b in reads:
            add(b.lw)
            if b.parent is not None:
                add(b.parent.lw)
        for b in writes:
            add(b.lw)
            for k, v in b.rd.items():
                add((k, v))
            if b.parent is not None:
                add(b.parent.lw)
                for k, v in b.parent.rd.items():
                    add((k, v))
        if extra:
            for ev in extra:
                add(ev)
        if engine == "pe":
            need.pop(("e", "pe"), None)
        w = self.waited[engine]
        waits = []
        for k, v in need.items():
            if w.get(k, 0) < v:
                waits.append((k, v))
                w[k] = v
        return waits

    def _mark(self, ev, reads, writes):
        k, v = ev
        for b in reads:
            if b.rd.get(k, 0) < v:
                b.rd[k] = v
        for b in writes:
            b.lw = ev
            b.rd = {}

    def op(self, engine, fn, reads=(), writes=()):
        reads = _bufs(reads)
        writes = _bufs(writes)
        waits = self._collect(engine, reads, writes)
        self.cnt[engine] += 1
        ev = (("e", engine), self.cnt[engine])
        self._mark(ev, reads, writes)
        self.ops[engine].append((waits, fn, None, self.cnt[engine]))

    def dma(self, out, in_, reads=(), writes=(), queue="sp", r=False, **kw):
        if r:
            out = out.bitcast(F32R)
            in_ = in_.bitcast(F32R)
        reads = _bufs(reads)
        writes = _bufs(writes)
        i = self.dnext
        self.dnext = (i + 1) % self.NDS
        extra = [(("d", i), self.dval[i])] if self.dval[i] else None
        waits = self._collect(queue, reads, writes, extra)
        self.dval[i] += 16
        ev = (("d", i), self.dval[i])
        self._mark(ev, reads, writes)
        self.ops[queue].append((waits, lambda e: e.dma_start(out=out, in_=in_, **kw), i, None))

    def dbg(self, name, ap, shape, reads):
        if not getattr(self, "debug", False):
            return
        if name in getattr(self, "_dbg_done", set()):
            return
        self.__dict__.setdefault("_dbg_done", set()).add(name)
        t = self.nc.dram_tensor(name, list(shape), F32, kind="ExternalOutput").ap()
        self.dma(t, ap, reads, [])

    def _semof(self, k):
        return self.sem[k[1]] if k[0] == "e" else self.dsem[k[1]]

    def emit(self):
        fin = []
        for i in range(self.NDS):
            if self.dval[i] and self.waited["sp"].get(("d", i), 0) < self.dval[i]:
                fin.append((("d", i), self.dval[i]))
        for k in self.eng:
            if k != "sp" and self.cnt[k]:
                fin.append((("e", k), self.cnt[k]))
        needed = {k: set() for k in self.eng}
        for engine in self.eng:
            for waits, fn, di, idx in self.ops[engine]:
                for k, v in waits:
                    if k[0] == "e":
                        needed[k[1]].add(v)
        for k, v in fin:
            if k[0] == "e":
                needed[k[1]].add(v)
        rank = {}
        for k in self.eng:
            srt = sorted(needed[k])
            rank[k] = {v: i + 1 for i, v in enumerate(srt)}
            assert len(srt) < 30000, (k, len(srt))
        self.sem_counts = {k: len(rank[k]) for k in self.eng}

        def semval(k, v):
            return rank[k[1]][v] if k[0] == "e" else v

        with self.nc.Block() as block:
            def mk(engine):
                def body(e):
                    for waits, fn, di, idx in self.ops[engine]:
                        for k, v in waits:
                            e.wait_ge(self._semof(k), semval(k, v))
                        inst = fn(e)
                        if di is None:
                            if idx in rank[engine]:
                                inst.then_inc(self.sem[engine], 1)
                        else:
                            inst.then_inc(self.dsem[di], 16)
                    if engine == "sp":
                        for k, v in fin:
                            e.wait_ge(self._semof(k), semval(k, v))
                return body
            block.sync(mk("sp"))
            block.tensor(mk("pe"))
            block.vector(mk("dve"))
            block.scalar(mk("act"))
            block.gpsimd(mk("pool"))

    def mm(self, out, lhsT, rhs, start, stop, reads, writes, r=True):
        if r:
            lhsT = lhsT.bitcast(F32R)
            rhs = rhs.bitcast(F32R)
        self.op("pe", lambda e: e.matmul(out, lhsT, rhs, start=start, stop=stop), reads, writes)

    def transpose(self, out, in_, ident, reads, writes):
        self.op("pe", lambda e: e.transpose(out, in_, ident), reads, writes)

    def act(self, out, in_, func, reads, writes, bias=None, scale=None):
        kw = {}
        if bias is not None:
            kw["bias"] = bias
        if scale is not None:
            kw["scale"] = scale
        self.op("act", lambda e: e.activation(out=out, in_=in_, func=func, **kw), reads, writes)

    def tt(self, out, in0, in1, op, reads, writes, eng="dve"):
        self.op(eng, lambda e: e.tensor_tensor(out=out, in0=in0, in1=in1, op=op), reads, writes)

    def ts(self, out, in0, s1, op0, reads, writes, s2=None, op1=None, eng="dve"):
        if op1 is None:
            self.op(eng, lambda e: e.tensor_scalar(out=out, in0=in0, scalar1=s1, scalar2=None, op0=op0), reads, writes)
        else:
            self.op(eng, lambda e: e.tensor_scalar(out=out, in0=in0, scalar1=s1, scalar2=s2, op0=op0, op1=op1), reads, writes)

    def stt(self, out, in0, scalar, in1, op0, op1, reads, writes):
        self.op("dve", lambda e: e.scalar_tensor_tensor(out=out, in0=in0, scalar=scalar, in1=in1, op0=op0, op1=op1), reads, writes)

    def copy(self, out, in_, reads, writes, eng="dve"):
        if eng == "act":
            self.op("act", lambda e: e.copy(out=out, in_=in_), reads, writes)
        else:
            self.op(eng, lambda e: e.tensor_copy(out=out, in_=in_), reads, writes)

    def memset(self, ap, val, writes, eng="dve"):
        self.op(eng, lambda e: e.memset(ap, val), (), writes)


def make_consts(P):
    ident = P.sb("ident", [128, 128])
    ones = P.sb("ones", [128, 128])
    tmp = P.sb("iota_tmp", [128, 128])
    P.op("pool", lambda e: e.iota(tmp[:], pattern=[[1, 128]], base=0, channel_multiplier=-1,
                                  allow_small_or_imprecise_dtypes=True), (), [tmp])
    P.ts(ident[:], tmp[:], 0.0, ALU.is_equal, [tmp], [ident])
    P.ts(R(ones[:]), tmp[:], 0.0, ALU.mult, [tmp], [ones], s2=1.0, op1=ALU.add)
    return ident, ones


def load_cols(P, ident, dst, dst_ap, src_rows_ap, n):
    rows = P.sb("lc_rows", [n, 128])
    P.dma(rows[:], src_rows_ap, [], [rows])
    pb = P.bank()
    P.transpose(pb[:, 0:n], rows[:], ident[0:n, 0:n], [rows, ident], [pb])
    P.copy(dst_ap, pb[:, 0:n], [pb], [dst])


NT = 2112
NH = 1056
NTILE = 352
HALVES = [(0, [(0, 64, 1), (64, 1056, 0)]), (1056, [(0, 1056, 0)])]


def segs(half_idx, a, b):
    out = []
    for (c0, c1, w) in HALVES[half_idx][1]:
        lo, hi = max(a, c0), min(b, c1)
        if lo < hi:
            out.append((lo, hi, w))
    return out


class Ctx:
    pass


def emit_mod(P, C, mod_w_ap, mod_b_rows_ap, name):
    modT = P.sb(name, [128, 48, 2])
    bcols = P.sb(name + "_b", [128, 48])
    load_cols(P, C.ident, bcols, bcols[:], mod_b_rows_ap, 48)
    for blk in range(12):
        wb = C.wbuf()
        wv = wb.t[:, 0:4096].rearrange("p (a b) -> p a b", b=512)
        P.dma(wv, mod_w_ap[:, blk * 512:(blk + 1) * 512].rearrange("(kc p) n -> p kc n", p=128), [], [wb], r=True)
        pb = P.bank()
        for j in range(4):
            for kc in range(8):
                P.mm(pb[:, 2 * j:2 * j + 2], wv[:, kc, j * 128:(j + 1) * 128], C.sT[:, kc, :], kc == 0, kc == 7,
                     [wb, C.sT], [pb], r=False)
        P.tt(modT[:, blk * 4:(blk + 1) * 4, :], pb[:, 0:8].rearrange("p (a b) -> p a b", b=2),
             bcols[:, blk * 4:(blk + 1) * 4].unsqueeze(2).to_broadcast([128, 4, 2]), ALU.add, [pb, bcols], [modT])
    return modT


def emit_scale_vec(P, C, modT, g_rows_ap, sc_chunk0, name):
    g = P.sb(name + "_g", [128, 8])
    load_cols(P, C.ident, g, g[:], g_rows_ap, 8)
    gs = P.sb(name, [128, 8, 2])
    P.ts(gs[:], modT[:, sc_chunk0:sc_chunk0 + 8, :], 1.0, ALU.add, [modT], [gs])
    P.tt(gs[:], gs[:], g[:].unsqueeze(2).to_broadcast([128, 8, 2]), ALU.mult, [gs, g], [gs])
    return gs


def emit_rstd(P, C, src, nch, n0, n1, dst, inv_d, eps):
    pb = P.bank()
    w = n1 - n0
    for c in range(nch):
        sq = C.sqbuf()
        P.act(R(sq[:, 0:w]), src[:, c, n0:n1], AF.Square, [src], [sq])
        P.mm(pb[:, 0:w], C.ones[:, :], sq[:, 0:w], c == 0, c == nch - 1, [C.ones, sq], [pb])
    P.act(dst[:, n0:n1], pb[:, 0:w], AF.Ln, [pb, C.epsb], [dst], bias=C.epsb[:, 0:1] if eps == EPS else C.epsb[:, 1:2], scale=inv_d)
    P.act(dst[:, n0:n1], dst[:, n0:n1], AF.Exp, [dst], [dst], scale=-0.5)


def emit_norm_mod(P, C, hi, x, dst, gs, modT, shift_chunk0):
    for ti in range(3):
        n0, n1 = ti * NTILE, (ti + 1) * NTILE
        emit_rstd(P, C, x, 8, n0, n1, C.rstd, 1.0 / D, EPS)
        for c in range(8):
            for (a, b, w) in segs(hi, n0, n1):
                if modT is not None:
                    P.stt(R(dst[:, c, a:b]), x[:, c, a:b], gs[:, c, w:w + 1], C.rstd[:, a:b], ALU.mult, ALU.mult,
                          [x, gs, C.rstd], [dst])
                    P.act(R(dst[:, c, a:b]), dst[:, c, a:b], AF.Identity, [dst, modT], [dst],
                          bias=modT[:, shift_chunk0 + c, w:w + 1])
                else:
                    P.stt(R(dst[:, c, a:b]), x[:, c, a:b], gs[:, c:c + 1], C.rstd[:, a:b], ALU.mult, ALU.mult,
                          [x, gs, C.rstd], [dst])


def emit_linear_add(P, C, hi, src, kchunks, w_ap, x, gate):
    for dc in range(8):
        wb = C.wbuf()
        wv = wb.t[:, 0:kchunks * 128].rearrange("p (a b) -> p a b", b=128)
        P.dma(wv, w_ap[:, dc * 128:(dc + 1) * 128].rearrange("(kc p) n -> p kc n", p=128), [], [wb], r=True)
        for ti in range(3):
            n0, n1 = ti * NTILE, (ti + 1) * NTILE
            pb = P.bank()
            for kc in range(kchunks):
                P.mm(pb[:, 0:NTILE], wv[:, kc, :], src[:, kc, n0:n1], kc == 0, kc == kchunks - 1, [wb, src], [pb])
            for (a, b, w) in segs(hi, n0, n1):
                P.stt(x[:, dc, a:b], pb[:, a - n0:b - n0], gate[:, dc, w:w + 1], x[:, dc, a:b], ALU.mult, ALU.add,
                      [pb, gate, x], [x])


def emit_ffn(P, C, hi, h2, x, gate2, w_in_ap, w_out_ap, F, gbc=None):
    FC = F // 128
    G = FC // 2
    act = C.big
    for gi in range(2):
        f0 = gi * G
        fl = 0
        while fl < G:
            nb = min(4, G - fl)
            wg = C.wbuf()
            wu = C.wbuf()
            wgv = wg.t[:, 0:8 * nb * 128].rearrange("p (a b) -> p a b", b=nb * 128)
            wuv = wu.t[:, 0:8 * nb * 128].rearrange("p (a b) -> p a b", b=nb * 128)
            c0 = (f0 + fl) * 128
            P.dma(wgv, w_in_ap[:, c0:c0 + nb * 128].rearrange("(kc p) n -> p kc n", p=128), [], [wg], r=True)
            P.dma(wuv, w_in_ap[:, F + c0:F + c0 + nb * 128].rearrange("(kc p) n -> p kc n", p=128), [], [wu], r=True)
            for j in range(nb):
                for ti in range(3):
                    n0, n1 = ti * NTILE, (ti + 1) * NTILE
                    pg = P.bank()
                    pu = P.bank()
                    for kc in range(8):
                        P.mm(pg[:, 0:NTILE], wgv[:, kc, j * 128:(j + 1) * 128], h2[:, kc, n0:n1], kc == 0, kc == 7, [wg, h2], [pg])
                    for kc in range(8):
                        P.mm(pu[:, 0:NTILE], wuv[:, kc, j * 128:(j + 1) * 128], h2[:, kc, n0:n1], kc == 0, kc == 7, [wu, h2], [pu])
                    sg = C.sgbuf()
                    P.act(sg[:, 0:NTILE], pg[:, 0:NTILE], AF.Silu, [pg], [sg])
                    P.tt(R(act[:, fl + j, n0:n1]), sg[:, 0:NTILE], pu[:, 0:NTILE], ALU.mult, [sg, pu], [act.sub(fl + j)])
            fl += nb
        for dc in range(8):
            wb = C.wbuf()
            wv = wb.t[:, 0:G * 128].rearrange("p (a b) -> p a b", b=128)
            P.dma(wv, w_out_ap[f0 * 128:(f0 + G) * 128, dc * 128:(dc + 1) * 128].rearrange("(kc p) n -> p kc n", p=128), [], [wb], r=True)
            for ti in range(3):
                n0, n1 = ti * NTILE, (ti + 1) * NTILE
                pb = P.bank()
                for k in range(G):
                    P.mm(pb[:, 0:NTILE], wv[:, k, :], act[:, k, n0:n1], k == 0, k == G - 1, [wb, act.sub(k)], [pb])
                for (a, b, w) in segs(hi, n0, n1):
                    if gbc is None:
                        P.stt(x[:, dc, a:b], pb[:, a - n0:b - n0], gate2[:, dc, w:w + 1], x[:, dc, a:b], ALU.mult, ALU.add,
                              [pb, gate2, x], [x])
                    else:
                        tmp = C.sgbuf()
                        P.tt(tmp[:, 0:b - a], pb[:, a - n0:b - n0], gbc[:, a:b], ALU.mult, [pb, gbc], [tmp])
                        P.stt(x[:, dc, a:b], tmp[:, 0:b - a], gate2[:, dc, w:w + 1], x[:, dc, a:b], ALU.mult, ALU.add,
                              [tmp, gate2, x], [x])


def emit_pool(P, C, hi, hp_ctx, hp_lat, inv_ap):
    h0 = HALVES[hi][0]
    inv = C.hb
    for g in range(4):
        P.dma(inv[:, g, :], inv_ap[g:g + 1, h0:h0 + NH].to_broadcast([128, NH]), [], [inv], r=True)
    for c in range(8):
        g = c // 2
        w = 2 << g
        for (a, b, isctx) in HALVES[hi][1]:
            n = b - a
            hh = C.poolbuf[0]
            if isctx:
                src = hp_ctx[c * 128:(c + 1) * 128, 0:n + 16]
            else:
                l0 = h0 + a - 64
                src = hp_lat[c * 128:(c + 1) * 128, l0:l0 + n + 16]
            P.dma(hh[:, 0:n + 16], src, [], [hh])
            cur = hh
            sh = 1
            k = 0
            while sh < w:
                nxt = C.poolbuf[1 + (k % 2)]
                lo = 2 * sh - 1
                P.tt(nxt[:, lo:n + 16], cur[:, lo:n + 16], cur[:, lo - sh:n + 16 - sh], ALU.add, [cur], [nxt])
                cur = nxt
                sh *= 2
                k += 1
            off = 8 + w // 2 - 1
            P.tt(R(C.big[:, c, a:b]), cur[:, off:off + n], inv[:, g, a:b], ALU.mult, [cur, inv], [C.big.sub(c)])
            P.tt(R(C.big[:, c, a:b]), C.big[:, c, a:b], hh[:, 8:8 + n], ALU.subtract, [C.big.sub(c), hh], [C.big.sub(c)])


def emit_moe_gates(P, C, hi, h2, router_ap):
    rw = P.sb("router", [128, 8, 8])
    P.dma(rw[:], router_ap.rearrange("(kc p) e -> p kc e", p=128), [], [rw])
    t0 = 0
    while t0 < NH:
        m = min(128, NH - t0)
        pb = P.bank()
        for kc in range(8):
            P.mm(pb[0:m, 0:8], h2[:, kc, t0:t0 + m], rw[:, kc, :], kc == 0, kc == 7, [h2, rw], [pb], r=False)
        lg = P.sb("lg", [128, 8])
        P.copy(lg[0:m, :], pb[0:m, 0:8], [pb], [lg])
        mx = P.sb("mx", [128, 8])
        P.op("dve", lambda e, mx=mx, lg=lg, m=m: e.max(out=mx[0:m, :], in_=lg[0:m, :]), [lg], [mx])
        dd = P.sb("dd", [128, 4])
        P.tt(dd[0:m, 0:1], mx[0:m, 1:2], mx[0:m, 0:1], ALU.subtract, [mx], [dd])
        P.act(dd[0:m, 1:2], dd[0:m, 0:1], AF.Exp, [dd], [dd])
        P.ts(dd[0:m, 1:2], dd[0:m, 1:2], 1.0, ALU.add, [dd], [dd])
        P.op("dve", lambda e, dd=dd, m=m: e.reciprocal(out=dd[0:m, 2:3], in_=dd[0:m, 1:2]), [dd], [dd])
        P.ts(dd[0:m, 3:4], dd[0:m, 2:3], -1.0, ALU.mult, [dd], [dd], s2=1.0, op1=ALU.add)
        g1 = P.sb("g1", [128, 8])
        g2 = P.sb("g2", [128, 8])
        P.ts(g1[0:m, :], lg[0:m, :], mx[0:m, 0:1], ALU.is_equal, [lg, mx, dd], [g1], s2=dd[0:m, 2:3], op1=ALU.mult)
        P.ts(g2[0:m, :], lg[0:m, :], mx[0:m, 1:2], ALU.is_equal, [lg, mx, dd], [g2], s2=dd[0:m, 3:4], op1=ALU.mult)
        P.tt(g1[0:m, :], g1[0:m, :], g2[0:m, :], ALU.add, [g1, g2], [g1])
        pt = P.bank()
        P.transpose(pt[0:8, 0:m], g1[0:m, :], C.ident[0:m, 0:m], [g1, C.ident], [pt])
        P.copy(C.gatesT[:, t0:t0 + m], pt[0:8, 0:m], [pt], [C.gatesT])
        t0 += m


def emit_gbc(P, C, e_idx):
    for ti in range(3):
        n0, n1 = ti * NTILE, (ti + 1) * NTILE
        pb = P.bank()
        P.mm(pb[:, 0:NTILE], C.sel[:, e_idx, :], C.gatesT[:, n0:n1], True, True, [C.sel, C.gatesT], [pb], r=False)
        P.copy(C.gbc[:, n0:n1], pb[:, 0:NTILE], [pb], [C.gbc], eng="act")


def build_ts(layer, post, ffn, nxt):
    nc = bass.Bass("TRN2", target_bir_lowering=False)
    with ExitStack() as stack:
        nc.dge_precook = False
        P = Prog(nc, stack)
        P.init_psum(8)
        C = Ctx()

        def din(name, shape):
            return nc.dram_tensor(name, list(shape), F32, kind="ExternalInput").ap()

        def dout(name, shape):
            return nc.dram_tensor(name, list(shape), F32, kind="ExternalOutput").ap()

        xT = din("xT", [D, NT])
        cvec = din("cvec", [16, 128])
        if post is not None or ffn is not None:
            mod_w = din("mod_w", [D, 6 * D])
            mod_b = din("mod_b", [48, 128])
            n2g = din("n2g", [8, 128])
        if post == "mamba":
            mT = din("mT", [2048, NT])
            mb_ng = din("mb_ng", [16, 128])
            w_post = din("w_post", [2048, D])
        elif post == "lin":
            mT = din("mT", [D, NT])
            w_post = din("w_post", [D, D])
        elif post == "pool":
            hp_ctx = din("hp_ctx", [D, 80])
            hp_lat = din("hp_lat", [D, 2048 + 16])
            inv_cnt = din("inv_cnt", [4, NT])
            pl_w = din("pl_w", [4, 256, 256])
            pl_scale = din("pl_scale", [8, 128])
        if ffn == "dense":
            F = 2816
            w_in = din("w_in", [D, 2 * F])
            w_out = din("w_out", [F, D])
        elif ffn == "moe":
            F = 3584
            router = din("router", [D, 8])
            w_in = din("w_in", [8, D, 2 * F])
            w_out = din("w_out", [8, F, D])
        if nxt == "norm":
            mod_w_n = din("mod_w_n", [D, 6 * D])
            mod_b_n = din("mod_b_n", [48, 128])
            n1g_n = din("n1g_n", [8, 128])
            xT_out = dout("xT_out", [D, NT])
            hT_out = dout("hT_out", [D, NT])
        else:
            fin_g = din("fin_g", [8, 128])
            out_T = dout("out_T", [D, NT])

        C.ident, C.ones = make_consts(P)
        wbufs = [P.sb(f"wb{i}", [128, 4096]) for i in range(3)]
        wi = [0]

        def wbuf():
            b = wbufs[wi[0] % 3]
            wi[0] += 1
            return b
        C.wbuf = wbuf
        sqs = [P.sb(f"sq{i}", [128, NTILE]) for i in range(3)]
        si = [0]

        def sqbuf():
            b = sqs[si[0] % 3]
            si[0] += 1
            return b
        C.sqbuf = sqbuf
        sgs = [P.sb(f"sg{i}", [128, NTILE]) for i in range(3)]
        gi_ = [0]

        def sgbuf():
            b = sgs[gi_[0] % 3]
            gi_[0] += 1
            return b
        C.sgbuf = sgbuf
        C.rstd = P.sb("rstd", [128, NH])
        C.epsb = P.sb("epsb", [128, 2])
        P.memset(C.epsb[:, 0:1], EPS, [C.epsb])
        P.memset(C.epsb[:, 1:2], EPS, [C.epsb])
        craw = P.sb("craw", [128, 16])
        load_cols(P, C.ident, craw, craw[:], cvec, 16)
        C.sT = P.sb("sT", [128, 8, 2])
        P.act(C.sT[:, :, 0], craw[:, 0:8], AF.Silu, [craw], [C.sT])
        P.act(C.sT[:, :, 1], craw[:, 8:16], AF.Silu, [craw], [C.sT])

        x = P.sb("x", [128, 8, NH])
        C.hb = P.sb("hb", [128, 8, NH])
        need_big = post is not None or ffn is not None
        if need_big:
            nbig = 16 if post == "mamba" else (14 if ffn == "moe" else 11)
            C.big = P.sb("big", [128, nbig, NH])
        if post == "pool":
            C.poolbuf = [P.sb(f"pb{i}", [128, NH + 16]) for i in range(3)]
        if ffn == "moe":
            C.gatesT = P.sb("gatesT", [8, NH])
            C.gbc = P.sb("gbc", [128, NH])
            C.sel = P.sb("sel", [8, 8, 128])
            P.memset(C.sel[:], 0.0, [C.sel])
            for e_ in range(8):
                P.ts(C.sel[:, e_, :], C.ones[0:8, :], C.ident[0:8, e_:e_ + 1], ALU.mult, [C.ones, C.ident, C.sel], [C.sel])

        if post is not None or ffn is not None:
            modT = emit_mod(P, C, mod_w, mod_b, "modT")
            gs2 = emit_scale_vec(P, C, modT, n2g, 32, "gs2")
            gate1 = P.sb("gate1", [128, 8, 2])
            P.copy(gate1[:], modT[:, 16:24, :], [modT], [gate1])
            gate2 = P.sb("gate2", [128, 8, 2])
            P.copy(gate2[:], modT[:, 40:48, :], [modT], [gate2])
            if post == "pool":
                psc = P.sb("psc", [128, 8])
                load_cols(P, C.ident, psc, psc[:], pl_scale, 8)
                P.tt(gate1[:], gate1[:], psc[:].unsqueeze(2).to_broadcast([128, 8, 2]), ALU.mult, [gate1, psc], [gate1])
            if post == "mamba":
                mng = P.sb("mng", [128, 16])
                load_cols(P, C.ident, mng, mng[:], mb_ng, 16)
        if nxt == "norm":
            modN = emit_mod(P, C, mod_w_n, mod_b_n, "modN")
            gs1n = emit_scale_vec(P, C, modN, n1g_n, 8, "gs1n")
        else:
            fg = P.sb("fg", [128, 8])
            load_cols(P, C.ident, fg, fg[:], fin_g, 8)

        for hi in range(2):
            h0 = HALVES[hi][0]
            for c in range(8):
                P.dma(x[:, c, :], xT[c * 128:(c + 1) * 128, h0:h0 + NH], [], [x])
            if post == "mamba":
                big = C.big
                for c in range(16):
                    P.dma(big[:, c, :], mT[c * 128:(c + 1) * 128, h0:h0 + NH], [], [big.sub(c)], r=True)
                allb = [big.sub(c) for c in range(16)]
                for ti in range(3):
                    n0, n1 = ti * NTILE, (ti + 1) * NTILE
                    pb = P.bank()
                    for c in range(16):
                        sq = C.sqbuf()
                        P.act(R(sq[:, 0:NTILE]), big[:, c, n0:n1], AF.Square, [big.sub(c)], [sq])
                        P.mm(pb[:, 0:NTILE], C.ones[:, :], sq[:, 0:NTILE], c == 0, c == 15, [C.ones, sq], [pb])
                    P.act(C.rstd[:, n0:n1], pb[:, 0:NTILE], AF.Ln, [pb, C.epsb], [C.rstd], bias=C.epsb[:, 0:1], scale=1.0 / 2048)
                    P.act(C.rstd[:, n0:n1], C.rstd[:, n0:n1], AF.Exp, [C.rstd], [C.rstd], scale=-0.5)
                    for c in range(16):
                        P.stt(R(big[:, c, n0:n1]), big[:, c, n0:n1], mng[:, c:c + 1], C.rstd[:, n0:n1], ALU.mult, ALU.mult,
                              [big.sub(c), mng, C.rstd], [big.sub(c)])
                emit_linear_add(P, C, hi, _SubView(big, allb), 16, w_post, x, gate1)
            elif post == "lin":
                big = C.big
                for c in range(8):
                    P.dma(big[:, c, :], mT[c * 128:(c + 1) * 128, h0:h0 + NH], [], [big.sub(c)], r=True)
                emit_linear_add(P, C, hi, _SubView(big, [big.sub(c) for c in range(8)]), 8, w_post, x, gate1)
            elif post == "pool":
                emit_pool(P, C, hi, hp_ctx, hp_lat, inv_cnt)
                big = C.big
                for dc in range(8):
                    g, j = dc // 2, dc % 2
                    wb = C.wbuf()
                    wv = wb.t[:, 0:256].rearrange("p (a b) -> p a b", b=128)
                    P.dma(wv, pl_w[g, :, j * 128:(j + 1) * 128].rearrange("(kc p) n -> p kc n", p=128), [], [wb], r=True)
                    for ti in range(3):
                        n0, n1 = ti * NTILE, (ti + 1) * NTILE
                        pb = P.bank()
                        for kc in range(2):
                            P.mm(pb[:, 0:NTILE], wv[:, kc, :], big[:, 2 * g + kc, n0:n1], kc == 0, kc == 1,
                                 [wb, big.sub(2 * g + kc)], [pb])
                        for (a, b, w) in segs(hi, n0, n1):
                            P.stt(x[:, dc, a:b], pb[:, a - n0:b - n0], gate1[:, dc, w:w + 1], x[:, dc, a:b], ALU.mult, ALU.add,
                                  [pb, gate1, x], [x])
            if ffn is not None:
                h2 = C.hb
                emit_norm_mod(P, C, hi, x, h2, gs2, modT, 24)
                if ffn == "dense":
                    emit_ffn(P, C, hi, h2, x, gate2, w_in, w_out, F)
                else:
                    emit_moe_gates(P, C, hi, h2, router)
                    for e_ in range(8):
                        emit_gbc(P, C, e_)
                        emit_ffn(P, C, hi, h2, x, gate2, w_in[e_], w_out[e_], F, gbc=C.gbc)
            if nxt == "norm":
                for c in range(8):
                    P.dma(xT_out[c * 128:(c + 1) * 128, h0:h0 + NH], x[:, c, :], [x], [])
                emit_norm_mod(P, C, hi, x, C.hb, gs1n, modN, 0)
                for c in range(8):
                    P.dma(hT_out[c * 128:(c + 1) * 128, h0:h0 + NH], C.hb[:, c, :], [C.hb], [])
            else:
                emit_norm_mod(P, C, hi, x, C.hb, fg, None, 0)
                for c in range(8):
                    P.dma(out_T[c * 128:(c + 1) * 128, h0:h0 + NH], C.hb[:, c, :], [C.hb], [])
        P.emit()
    return nc


def rows128(v):
    return np.ascontiguousarray(np.asarray(v, np.float32).reshape(-1, 128))


def to_cores_T(lat, ctx):
    outs = []
    for core in range(NCORES):
        b, q = core // 4, core % 4
        a = np.concatenate([ctx[b, q * 64:(q + 1) * 64], lat[b, q * 2048:(q + 1) * 2048]], axis=0)
        outs.append(np.ascontiguousarray(a.T))
    return outs


def from_cores_T(arrs):
    Cc = arrs[0].shape[0]
    lat = np.empty((2, 8192, Cc), np.float32)
    ctx = np.empty((2, 256, Cc), np.float32)
    for core in range(NCORES):
        b, q = core // 4, core % 4
        a = arrs[core].T
        ctx[b, q * 64:(q + 1) * 64] = a[0:64]
        lat[b, q * 2048:(q + 1) * 2048] = a[64:]
    return lat, ctx


def cvec_for(c, c_ctx, core):
    b = core // 4
    return np.ascontiguousarray(np.concatenate([np.asarray(c[b], np.float32).reshape(8, 128),
                                                np.asarray(c_ctx, np.float32).reshape(8, 128)], axis=0))


_NC_CACHE = {}


def get_ts(layer, post, ffn, nxt):
    key = ("ts", post, ffn, nxt)
    if key not in _NC_CACHE:
        _NC_CACHE[key] = build_ts(layer, post, ffn, nxt)
    return _NC_CACHE[key]


def run(nc, in_maps):
    res = run_bass_kernel_spmd(nc, in_maps, core_ids=list(range(NCORES)))
    return res.results


def pool_inputs(h_lat, h_ctx):
    outs = []
    for core in range(NCORES):
        b, q = core // 4, core % 4
        lat = np.zeros((2048 + 16, D), np.float32)
        lo, hi = q * 2048 - 8, (q + 1) * 2048 + 8
        s0, s1 = max(lo, 0), min(hi, 8192)
        lat[s0 - lo:s1 - lo] = h_lat[b, s0:s1]
        cx = np.zeros((64 + 16, D), np.float32)
        lo, hi = q * 64 - 8, (q + 1) * 64 + 8
        s0, s1 = max(lo, 0), min(hi, 256)
        cx[s0 - lo:s1 - lo] = h_ctx[b, s0:s1]
        inv = np.empty((4, NT), np.float32)
        for g, w in enumerate((2, 4, 8, 16)):
            t = np.arange(q * 64, (q + 1) * 64)
            inv[g, 0:64] = 1.0 / (np.minimum(t + w // 2, 256) - np.maximum(t - w // 2, 0))
            t = np.arange(q * 2048, (q + 1) * 2048)
            inv[g, 64:] = 1.0 / (np.minimum(t + w // 2, 8192) - np.maximum(t - w // 2, 0))
        outs.append({"hp_ctx": np.ascontiguousarray(cx.T), "hp_lat": np.ascontiguousarray(lat.T), "inv_cnt": inv})
    return outs


NKEY = 8448
NQ = 8192


def build_attn():
    nc = bass.Bass("TRN2", target_bir_lowering=False)
    with ExitStack() as stack:
        nc.dge_precook = False
        P = Prog(nc, stack)
        P.init_psum(3)
        oacc = P.ps("oacc", [128, 512])
        sgrp = [P.ps(f"sgrp{i}", [128, 1024]) for i in range(2)]

        def din(name, shape):
            return nc.dram_tensor(name, list(shape), F32, kind="ExternalInput").ap()

        hT = din("hT", [D, NKEY])
        wq = din("wq", [D, 256])
        wk = din("wk", [D, 64])
        wv = din("wv", [D, 64])
        qg = din("qg", [64, 1])
        kg = din("kg", [64, 1])
        cosT = din("cosT", [64, NKEY])
        sinS = din("sinS", [64, NKEY])
        prot = din("prot", [64, 64])
        oT = nc.dram_tensor("oT", [256, NQ], F32, kind="ExternalOutput").ap()

        ident, ones = make_consts(P)
        wq_s = P.sb("wq_s", [128, 8, 256])
        wk_s = P.sb("wk_s", [128, 8, 64])
        wv_s = P.sb("wv_s", [128, 8, 64])
        P.dma(wq_s[:], wq.rearrange("(kc p) n -> p kc n", p=128), [], [wq_s], r=True)
        P.dma(wk_s[:], wk.rearrange("(kc p) n -> p kc n", p=128), [], [wk_s], r=True)
        P.dma(wv_s[:], wv.rearrange("(kc p) n -> p kc n", p=128), [], [wv_s], r=True)
        qg_s = P.sb("qg_s", [64, 1])
        kg_s = P.sb("kg_s", [64, 1])
        P.dma(qg_s[:], qg, [], [qg_s])
        P.dma(kg_s[:], kg, [], [kg_s])
        prot_s = P.sb("prot_s", [64, 64])
        P.dma(prot_s[:], prot, [], [prot_s], r=True)
        epsb = P.sb("epsb", [128, 1])
        P.memset(epsb[:], EPS, [epsb])
        KT = P.sb("KT", [64, NKEY])
        Vx = P.sb("Vx", [128, 66, 65])
        P.ts(R(Vx[:, :, 64:65]), Vx[:, :, 64:65], 0.0, ALU.mult, [], [Vx], s2=1.0, op1=ALU.add)
        hts = [P.sb(f"ht{i}", [128, 8, 512]) for i in range(2)]
        cst = [P.sb(f"cs{i}", [64, 512]) for i in range(2)]
        snt = [P.sb(f"sn{i}", [64, 512]) for i in range(2)]
        QTs = [P.sb(f"QT{i}", [64, 4, 512]) for i in range(2)]
        pts = [P.sb(f"pt{i}", [128, 1024]) for i in range(3)]
        sqb = [P.sb(f"sqb{i}", [64, 512]) for i in range(2)]
        rsb = [P.sb(f"rsb{i}", [64, 512]) for i in range(2)]
        qnb = [P.sb(f"qnb{i}", [64, 512]) for i in range(2)]
        t1b = [P.sb(f"t1b{i}", [64, 512]) for i in range(2)]
        t2b = [P.sb(f"t2b{i}", [64, 512]) for i in range(2)]
        obuf = [P.sb(f"ob{i}", [64, 4, 512]) for i in range(2)]
        lrow = P.sb("lrow", [128, 512])
        bcs = P.sb("bcs", [64, 512])
        cnt = [0]

        def normrope(src_ps, w, g_s, cs, sn, dst_ap, dst_tt):
            i = cnt[0] % 2
            cnt[0] += 1
            sq, rs, qn, t1, t2 = sqb[i], rsb[i], qnb[i], t1b[i], t2b[i]
            P.act(R(sq[:, 0:w]), src_ps, AF.Square, [src_tt[0]], [sq])
            pb = P.bank()
            P.mm(pb[0:64, 0:w], ones[0:64, 0:64], sq[:, 0:w], True, True, [ones, sq], [pb])
            P.act(rs[:, 0:w], pb[0:64, 0:w], AF.Ln, [pb, epsb], [rs], bias=epsb[0:64, 0:1], scale=1.0 / 64)
            P.act(rs[:, 0:w], rs[:, 0:w], AF.Exp, [rs], [rs], scale=-0.5)
            P.stt(R(qn[:, 0:w]), src_ps, g_s[:, 0:1], rs[:, 0:w], ALU.mult, ALU.mult, [src_tt[0], g_s, rs], [qn])
            pr = P.bank()
            P.mm(pr[0:64, 0:w], prot_s[:, :], qn[:, 0:w], True, True, [prot_s, qn], [pr])
            P.tt(t1[:, 0:w], qn[:, 0:w], cs, ALU.mult, [qn, cs_tt[0]], [t1])
            P.tt(t2[:, 0:w], pr[0:64, 0:w], sn, ALU.mult, [pr, sn_tt[0]], [t2])
            P.tt(R(dst_ap), t1[:, 0:w], t2[:, 0:w], ALU.add, [t1, t2], [dst_tt])

        src_tt = [None]
        cs_tt = [None]
        sn_tt = [None]

        ntile_k = [(i * 512, 512) for i in range(16)] + [(8192, 256)]
        for ti, (c0, w) in enumerate(ntile_k):
            ht = hts[ti % 2]
            cs = cst[ti % 2]
            sn = snt[ti % 2]
            for kc in range(8):
                P.dma(ht[:, kc, 0:w], hT[kc * 128:(kc + 1) * 128, c0:c0 + w], [], [ht], r=True)
            P.dma(cs[:, 0:w], cosT[:, c0:c0 + w], [], [cs])
            P.dma(sn[:, 0:w], sinS[:, c0:c0 + w], [], [sn])
            pk = P.bank()
            for kc in range(8):
                P.mm(pk[0:64, 0:w], wk_s[:, kc, :], ht[:, kc, 0:w], kc == 0, kc == 7, [wk_s, ht], [pk])
            src_tt[0], cs_tt[0], sn_tt[0] = pk, cs, sn
            normrope(pk[0:64, 0:w], w, kg_s, cs[:, 0:w], sn[:, 0:w], KT[:, c0:c0 + w], KT.sub(ti))
            for j in range(w // 128):
                ch = c0 // 128 + j
                pv = P.bank()
                for kc in range(8):
                    P.mm(pv[:, 0:64], ht[:, kc, j * 128:(j + 1) * 128], wv_s[:, kc, :], kc == 0, kc == 7, [ht, wv_s], [pv])
                P.copy(R(Vx[:, ch, 0:64]), pv[:, 0:64], [pv], [Vx.sub(ch)], eng="act")
        KTall = [KT.sub(ti) for ti in range(len(ntile_k))]

        for ti in range(16):
            c0 = 256 + ti * 512
            ht = hts[ti % 2]
            cs = cst[ti % 2]
            sn = snt[ti % 2]
            QT = QTs[ti % 2]
            ob = obuf[ti % 2]
            for kc in range(8):
                P.dma(ht[:, kc, :], hT[kc * 128:(kc + 1) * 128, c0:c0 + 512], [], [ht], r=True)
            P.dma(cs[:, :], cosT[:, c0:c0 + 512], [], [cs])
            P.dma(sn[:, :], sinS[:, c0:c0 + 512], [], [sn])
            for hq in range(4):
                pq = P.bank()
                for kc in range(8):
                    P.mm(pq[0:64, :], wq_s[:, kc, hq * 64:(hq + 1) * 64], ht[:, kc, :], kc == 0, kc == 7, [wq_s, ht], [pq])
                src_tt[0], cs_tt[0], sn_tt[0] = pq, cs, sn
                normrope(pq[0:64, :], 512, qg_s, cs[:, :], sn[:, :], QT[:, hq, :], QT)
            for qb in range(4):
                rhs_q = QT[:, :, qb * 128:(qb + 1) * 128]
                for st_ in range(33 + 1):
                    if st_ < 33:
                        sg = sgrp[st_ % 2]
                        for u_ in range(2):
                            ch = 2 * st_ + u_
                            P.mm(sg[:, u_ * 512:(u_ + 1) * 512].rearrange("p (h q) -> p h q", h=4), KT[:, ch * 128:(ch + 1) * 128], rhs_q, True, True,
                                 [KT.sub(ch // 4), QT], [sg])
                        pt = pts[st_ % 3]
                        P.act(R(pt[:, :]), sg[:, :], AF.Exp, [sg], [pt], scale=0.125)
                    if st_ >= 1:
                        pp = st_ - 1
                        pt = pts[pp % 3]
                        for u_ in range(2):
                            ch = 2 * pp + u_
                            P.mm(oacc[0:65, :], Vx[:, ch, :], pt[:, u_ * 512:(u_ + 1) * 512], ch == 0, ch == 65, [Vx.sub(ch), pt], [oacc])
                P.copy(lrow[64:65, :], oacc[64:65, :], [oacc], [lrow], eng="act")
                P.op("dve", lambda e: e.reciprocal(out=lrow[64:65, :], in_=lrow[64:65, :]), [lrow], [lrow])
                bcp = P.bank()
                P.mm(bcp[0:64, :], ones[64:65, 0:64], lrow[64:65, :], True, True, [ones, lrow], [bcp], r=False)
                P.copy(bcs[:, :], bcp[0:64, :], [bcp], [bcs], eng="act")
                P.tt(ob[:, :, qb * 128:(qb + 1) * 128], oacc[0:64, :].rearrange("p (h q) -> p h q", h=4),
                     bcs[:, :].rearrange("p (h q) -> p h q", h=4), ALU.mult, [oacc, bcs], [ob])
            q0 = ti * 512
            for hq in range(4):
                P.dma(oT[hq * 64:(hq + 1) * 64, q0:q0 + 512], ob[:, hq, :], [ob], [])
        P.emit()
    return nc


def rope_tables():
    quarter = 16
    inv = (10000.0 ** (-np.arange(quarter, dtype=np.float32) / quarter)).astype(np.float32)
    t = np.arange(8192)
    rows = (t // 64).astype(np.float32)
    cols = (t % 64).astype(np.float32)
    cosT = np.ones((64, NKEY), np.float32)
    sinS = np.zeros((64, NKEY), np.float32)
    prot = np.zeros((64, 64), np.float32)
    for i in range(64):
        half, within = i // 32, i % 32
        j, first = within % 16, within < 16
        pos = rows if half == 0 else cols
        ang = (pos * inv[j]).astype(np.float32)
        cosT[i, 256:] = np.cos(ang)
        sinS[i, 256:] = -np.sin(ang) if first else np.sin(ang)
        prot[i + 16 if first else i - 16, i] = 1.0
    return cosT, sinS, prot


def build_mamba(debug=False):
    nc = bass.Bass("TRN2", target_bir_lowering=False)
    with ExitStack() as stack:
        nc.dge_precook = False
        P = Prog(nc, stack)
        P.debug = debug
        P.init_psum(6)
        pybanks = [P.ps("pyb0", [128, 512]), P.ps("pyb1", [128, 512])]
        pyi = [0]

        def din(name, shape):
            return nc.dram_tensor(name, list(shape), F32, kind="ExternalInput").ap()

        hT = din("hT", [D, NKEY])
        wz = din("wz", [D, 512])
        wxbc = din("wxbc", [D, 768])
        wdt = din("wdt", [D, 16])
        cw = din("cw", [128, 30])
        cb = din("cb", [128, 6])
        dtb = din("dtb", [1, 16])
        alog = din("alog", [1, 16])
        dsk = din("dsk", [128, 4])
        uT = nc.dram_tensor("uT", [512, NKEY], F32, kind="ExternalOutput").ap()
        ybs = nc.dram_tensor("ybs", [512, NKEY], F32, kind="Internal").ap()
        ybs_t = TT(ybs, "ybs")

        ident, ones = make_consts(P)
        val = P.sb("val", [128, 128])
        P.op("pool", lambda e: e.iota(val[:], pattern=[[1, 128]], base=0, channel_multiplier=-1,
                                      allow_small_or_imprecise_dtypes=True), (), [val])
        Uf = P.sb("Uf", [128, 128]); Tf = P.sb("Tf", [128, 128]); Ub = P.sb("Ub", [128, 128]); Tb = P.sb("Tb", [128, 128])
        P.ts(Uf[:], val[:], 0.0, ALU.is_lt, [val], [Uf])
        P.ts(Tf[:], val[:], 0.0, ALU.is_ge, [val], [Tf])
        P.ts(Ub[:], val[:], 0.0, ALU.is_gt, [val], [Ub])
        P.ts(Tb[:], val[:], 0.0, ALU.is_le, [val], [Tb])
        UU = [Uf, Ub]
        TTm = [Tf, Tb]

        wz_s = P.sb("wz_s", [128, 8, 512])
        wx_s = P.sb("wx_s", [128, 8, 768])
        wd_s = P.sb("wd_s", [128, 8, 16])
        P.dma(wz_s[:], wz.rearrange("(kc p) n -> p kc n", p=128), [], [wz_s], r=True)
        P.dma(wx_s[:], wxbc.rearrange("(kc p) n -> p kc n", p=128), [], [wx_s], r=True)
        P.dma(wd_s[:], wdt.rearrange("(kc p) n -> p kc n", p=128), [], [wd_s], r=True)
        cw_s = P.sb("cw_s", [128, 30]); cb_s = P.sb("cb_s", [128, 6]); dsk_s = P.sb("dsk_s", [128, 4])
        P.dma(cw_s[:], cw, [], [cw_s]); P.dma(cb_s[:], cb, [], [cb_s]); P.dma(dsk_s[:], dsk, [], [dsk_s])
        dtb_s = P.sb("dtb_s", [128, 16]); aneg = P.sb("aneg", [128, 16])
        P.dma(dtb_s[:], dtb.to_broadcast([128, 16]), [], [dtb_s])
        P.dma(aneg[:], alog.to_broadcast([128, 16]), [], [aneg])
        P.act(aneg[:], aneg[:], AF.Exp, [aneg], [aneg])
        P.ts(aneg[:], aneg[:], -1.0, ALU.mult, [aneg], [aneg])
        oneb = P.sb("oneb", [128, 1])
        P.memset(oneb[:], 1.0, [oneb])

        W = 256
        ht = [P.sb(f"ht{i}", [128, 8, W + 4]) for i in range(2)]
        raw = P.sb("raw", [128, 6, W + 4])
        acc = [P.sb(f"acc{i}", [128, W]) for i in range(2)]
        xTc = P.sb("xTc", [128, 4, W])
        BT = P.sb("BT", [128, W]); CT = P.sb("CT", [128, W])
        x_tok = P.sb("x_tok", [128, 2, 512]); B_tok = P.sb("B_tok", [128, 2, 128]); dt_tok = P.sb("dt_tok", [128, 2, 16])
        zs = P.sb("zs", [128, 4, W])
        ST = [P.sb(f"ST{d}", [128, 512]) for d in range(2)]
        for d_ in range(2):
            P.memset(ST[d_][:], 0.0, [ST[d_]])
        sm = [P.sb(f"sm{i}", [128, 16]) for i in range(8)]
        GM = P.sb("GM", [128, 128])
        lD = [P.sb(f"lD{i}", [128, 128]) for i in range(8)]
        Lx = [P.sb(f"Lx{i}", [128, 128]) for i in range(8)]
        WT = [P.sb(f"WT{i}", [128, 128]) for i in range(8)]
        A1 = [P.sb(f"A1{i}", [128, 128]) for i in range(8)]
        Ec = [P.sb(f"Ec{i}", [128, 128]) for i in range(8)]
        Cd = [P.sb(f"Cd{i}", [128, 128]) for i in range(8)]
        xdt = P.sb("xdt", [128, 512])
        ybuf = P.sb("ybuf", [128, 4, 128])
        ubuf = [P.sb(f"ubuf{i}", [128, 4, 128]) for i in range(2)]

        def stage_a(ti, c0, q0, q1, fwd):
            h = ht[ti % 2]
            lo, hi = max(c0 - 2, q0), min(c0 + W + 2, q1)
            off = lo - (c0 - 2)
            n = hi - lo
            for kc in range(8):
                P.dma(h[:, kc, off:off + n], hT[kc * 128:(kc + 1) * 128, lo:hi], [], [h], r=True)
            for cc in range(6):
                pb = P.bank()
                for kc in range(8):
                    P.mm(pb[:, 0:n], wx_s[:, kc, cc * 128:(cc + 1) * 128], h[:, kc, off:off + n], kc == 0, kc == 7, [wx_s, h], [pb])
                if off > 0:
                    P.memset(raw[:, cc, 0:off], 0.0, [raw.sub(cc)])
                if off + n < W + 4:
                    P.memset(raw[:, cc, off + n:W + 4], 0.0, [raw.sub(cc)])
                P.copy(raw[:, cc, off:off + n], pb[:, 0:n], [pb], [raw.sub(cc)], eng="act")
                a_ = acc[cc % 2]
                P.ts(a_[:, :], raw[:, cc, 0:W], cw_s[:, cc * 5:cc * 5 + 1], ALU.mult, [raw.sub(cc), cw_s], [a_])
                for k in range(1, 5):
                    P.stt(a_[:, :], raw[:, cc, k:k + W], cw_s[:, cc * 5 + k:cc * 5 + k + 1], a_[:, :], ALU.mult, ALU.add,
                          [raw.sub(cc), cw_s, a_], [a_])
                if cc < 4:
                    P.act(xTc[:, cc, :], a_[:, :], AF.Silu, [a_, cb_s], [xTc.sub(cc)], bias=cb_s[:, cc:cc + 1])
                elif cc == 4:
                    P.act(BT[:, :], a_[:, :], AF.Silu, [a_, cb_s], [BT], bias=cb_s[:, cc:cc + 1])
                else:
                    P.act(CT[:, :], a_[:, :], AF.Silu, [a_, cb_s], [CT], bias=cb_s[:, cc:cc + 1])
            for j in range(2):
                pt = P.bank()
                for cc in range(4):
                    P.transpose(pt[:, cc * 128:(cc + 1) * 128], xTc[:, cc, j * 128:(j + 1) * 128], ident[:, :], [xTc.sub(cc), ident], [pt])
                P.copy(x_tok[:, j, :], pt[:, :], [pt], [x_tok.sub(j)], eng="act")
                pb = P.bank()
                P.transpose(pb[:, 0:128], BT[:, j * 128:(j + 1) * 128], ident[:, :], [BT, ident], [pb])
                P.copy(R(B_tok[:, j, :]), pb[:, 0:128], [pb], [B_tok.sub(j)])
                pd = P.bank()
                for kc in range(8):
                    P.mm(pd[:, 0:16], h[:, kc, 2 + j * 128:2 + (j + 1) * 128], wd_s[:, kc, :], kc == 0, kc == 7, [h, wd_s], [pd], r=False)
                xx, ax, ee, rr = sm[0], sm[1], sm[2], sm[3]
                P.tt(xx[:, :], pd[:, 0:16], dtb_s[:, :], ALU.add, [pd, dtb_s], [xx])
                P.stt(ax[:, :], xx[:, :], -1.0, xx[:, :], ALU.mult, ALU.max, [xx], [ax])
                P.act(ee[:, :], ax[:, :], AF.Exp, [ax], [ee], scale=-1.0)
                P.act(ee[:, :], ee[:, :], AF.Ln, [ee, oneb], [ee], bias=oneb[:, 0:1])
                P.ts(rr[:, :], xx[:, :], 0.0, ALU.max, [xx], [rr])
                P.tt(dt_tok[:, j, :], rr[:, :], ee[:, :], ALU.add, [rr, ee], [dt_tok.sub(j)])
            if fwd:
                P.dbg("d_xTc", xTc[:, :, :], [128, 4, W], [xTc])
                P.dbg("d_BT", BT[:, :], [128, W], [BT])
                P.dbg("d_CT", CT[:, :], [128, W], [CT])
                P.dbg("d_xtok", x_tok[:, :, :], [128, 2, 512], [x_tok])
                P.dbg("d_Btok", B_tok[:, :, :], [128, 2, 128], [B_tok])
                P.dbg("d_dt", dt_tok[:, :, :], [128, 2, 16], [dt_tok])
                P.dbg("d_raw", raw[:, :, :], [128, 6, W + 4], [raw])
            if fwd:
                for cc in range(4):
                    pz = P.bank()
                    for kc in range(8):
                        P.mm(pz[:, 0:W], wz_s[:, kc, cc * 128:(cc + 1) * 128], h[:, kc, 2:2 + W], kc == 0, kc == 7, [wz_s, h], [pz])
                    P.act(zs[:, cc, :], pz[:, 0:W], AF.Silu, [pz], [zs.sub(cc)])

        def chunk_step(d, j, col0, ui):
            S = ST[d]
            dts = dt_tok[:, j, d * 8:(d + 1) * 8]
            a_s, cum_s, w_s, et_s = sm[4], sm[5], sm[6], sm[7]
            P.tt(a_s[:, 0:8], dts, aneg[:, d * 8:(d + 1) * 8], ALU.mult, [dt_tok.sub(j), aneg], [a_s])
            pc = P.bank()
            P.mm(pc[:, 0:8], TTm[d][:, :], a_s[:, 0:8], True, True, [TTm[d], a_s], [pc], r=False)
            P.mm(pc[:, 8:16], ones[:, :], a_s[:, 0:8], True, True, [ones, a_s], [pc], r=False)
            P.copy(cum_s[:, 0:16], pc[:, 0:16], [pc], [cum_s])
            P.tt(w_s[:, 0:8], cum_s[:, 8:16], cum_s[:, 0:8], ALU.subtract, [cum_s], [w_s])
            P.act(w_s[:, 0:8], w_s[:, 0:8], AF.Exp, [w_s], [w_s])
            P.tt(w_s[:, 0:8], w_s[:, 0:8], dts, ALU.mult, [w_s, dt_tok.sub(j)], [w_s])
            P.act(et_s[:, 0:8], cum_s[:, 8:16], AF.Exp, [cum_s], [et_s])
            pg = P.bank()
            P.mm(pg[:, 0:128], BT[:, j * 128:(j + 1) * 128], CT[:, j * 128:(j + 1) * 128], True, True, [BT, CT], [pg], r=False)
            P.tt(GM[:, :], pg[:, 0:128], TTm[d][:, :], ALU.mult, [pg, TTm[d]], [GM])
            py = pybanks[pyi[0] % 2]
            pyi[0] += 1
            for e_ in range(8):
                P.ts(lD[e_][:, :], UU[d][:, :], a_s[:, e_:e_ + 1], ALU.mult, [UU[d], a_s], [lD[e_]])
                P.ts(A1[e_][:, :], ones[:, :], a_s[:, e_:e_ + 1], ALU.mult, [ones, a_s], [A1[e_]])
            pDE = []
            for e_ in range(8):
                if e_ % 2 == 0:
                    pb_ = P.bank()
                o_ = (e_ % 2) * 256
                P.mm(pb_[:, o_:o_ + 128], lD[e_][:, :], TTm[d][:, :], True, True, [lD[e_], TTm[d]], [pb_], r=False)
                P.mm(pb_[:, o_ + 128:o_ + 256], A1[e_][:, :], TTm[d][:, :], True, True, [A1[e_], TTm[d]], [pb_], r=False)
                pDE.append((pb_, o_))
            for e_ in range(8):
                pb_, o_ = pDE[e_]
                P.act(Lx[e_][:, :], pb_[:, o_:o_ + 128], AF.Exp, [pb_], [Lx[e_]])
                P.act(Ec[e_][:, :], pb_[:, o_ + 128:o_ + 256], AF.Exp, [pb_], [Ec[e_]])
            for e_ in range(8):
                P.stt(WT[e_][:, :], Lx[e_][:, :], dts[:, e_:e_ + 1], GM[:, :], ALU.mult, ALU.mult, [Lx[e_], dt_tok.sub(j), GM], [WT[e_]])
                P.tt(Cd[e_][:, :], CT[:, j * 128:(j + 1) * 128], Ec[e_][:, :], ALU.mult, [CT, Ec[e_]], [Cd[e_]])
            for e_ in range(8):
                p0 = (e_ % 2) * 64
                cc = e_ // 2
                P.mm(py[p0:p0 + 64, cc * 128:(cc + 1) * 128], x_tok[:, j, e_ * 64:(e_ + 1) * 64], WT[e_][:, :], True, False,
                     [x_tok.sub(j), WT[e_]], [py], r=False)
                P.mm(py[p0:p0 + 64, cc * 128:(cc + 1) * 128], S[:, e_ * 64:(e_ + 1) * 64], Cd[e_][:, :], False, True,
                     [S, Cd[e_]], [py], r=False)
            P.tt(R(xdt[:, :].rearrange("p (e q) -> p e q", e=8)), x_tok[:, j, :].rearrange("p (e q) -> p e q", e=8),
                 w_s[:, 0:8].unsqueeze(2).to_broadcast([128, 8, 64]), ALU.mult, [x_tok.sub(j), w_s], [xdt])
            pu = P.bank()
            P.mm(pu[:, :], B_tok[:, j, :], xdt[:, :], True, True, [B_tok.sub(j), xdt], [pu])
            P.tt(S[:, :].rearrange("p (e q) -> p e q", e=8), S[:, :].rearrange("p (e q) -> p e q", e=8),
                 et_s[:, 0:8].unsqueeze(2).to_broadcast([128, 8, 64]), ALU.mult, [S, et_s], [S])
            P.tt(S[:, :], S[:, :], pu[:, :], ALU.add, [S, pu], [S])
            if d == 0:
                P.dbg("d_GM", GM[:, :], [128, 128], [GM])
                P.dbg("d_WT", WT[7][:, :], [128, 128], [WT[7]])
                P.dbg("d_Lx", Lx[7][:, :], [128, 128], [Lx[7]])
                P.dbg("d_Cd", Cd[7][:, :], [128, 128], [Cd[7]])
                P.dbg("d_cum", cum_s[:, :], [128, 16], [cum_s])
                P.dbg("d_w", w_s[:, :], [128, 16], [w_s])
                P.dbg("d_S", S[:, :], [128, 512], [S])
            pyv = py[:, :].rearrange("p (c t) -> p c t", c=4)
            if d == 1:
                P.copy(ybuf[:, :, :], pyv, [py], [ybuf])
                P.dma(ybs[:, col0:col0 + 128].rearrange("(c p) t -> p c t", p=128), ybuf[:, :, :], [ybuf], [ybs_t])
            else:
                ub = ubuf[ui % 2]
                P.dma(ybuf[:, :, :], ybs[:, col0:col0 + 128].rearrange("(c p) t -> p c t", p=128), [ybs_t], [ybuf])
                P.tt(ybuf[:, :, :], ybuf[:, :, :], pyv, ALU.add, [ybuf, py], [ybuf])
                for cc in range(4):
                    P.stt(ub[:, cc, :], xTc[:, cc, j * 128:(j + 1) * 128], dsk_s[:, cc:cc + 1], ybuf[:, cc, :], ALU.mult, ALU.add,
                          [xTc.sub(cc), dsk_s, ybuf], [ub])
                P.dbg("d_ysum", ybuf[:, :, :], [128, 4, 128], [ybuf])
                P.dbg("d_zs", zs[:, :, :], [128, 4, W], [zs])
                P.tt(ub[:, :, :], ub[:, :, :], zs[:, :, j * 128:(j + 1) * 128], ALU.mult, [ub, zs], [ub])
                P.dma(uT[:, col0:col0 + 128].rearrange("(c p) t -> p c t", p=128), ub[:, :, :], [ub], [])

        tiles = [(0, 0, 256)] + [(256 + 256 * t, 256, NKEY) for t in range(32)]
        order_b = [tiles[0]] + tiles[:0:-1]
        ti = 0
        for (c0, q0, q1) in order_b:
            stage_a(ti, c0, q0, q1, False)
            for j in (1, 0):
                chunk_step(1, j, c0 + j * 128, 0)
            ti += 1
        ui = 0
        for (c0, q0, q1) in tiles:
            stage_a(ti, c0, q0, q1, True)
            for j in (0, 1):
                chunk_step(0, j, c0 + j * 128, ui)
                ui += 1
            ti += 1
        P.emit()
    return nc


def mamba_maps(inp, h_lat, h_ctx):
    W = np.asarray(inp["mb_in_w"][0], np.float32)
    cwf = np.asarray(inp["mb_conv_w"][0], np.float32)
    cbf = np.asarray(inp["mb_conv_b"][0], np.float32)
    maps = []
    for core in range(NCORES):
        b, g = core // 4, core % 4
        hT = np.ascontiguousarray(np.concatenate([h_ctx[b], h_lat[b]], axis=0).T)
        xcols = np.arange(g * 512, (g + 1) * 512)
        bcols = 2048 + np.arange(g * 128, (g + 1) * 128)
        ccols = 2048 + 512 + np.arange(g * 128, (g + 1) * 128)
        ch = np.concatenate([xcols, bcols, ccols])
        wxbc = np.ascontiguousarray(W[:, 2048 + ch])
        wz = np.ascontiguousarray(W[:, g * 512:(g + 1) * 512])
        dcols = np.concatenate([5120 + g * 8 + np.arange(8), 5120 + 32 + g * 8 + np.arange(8)])
        wdt = np.ascontiguousarray(W[:, dcols])
        cw = np.ascontiguousarray(cwf[:, ch].reshape(5, 6, 128).transpose(2, 1, 0).reshape(128, 30))
        cb = np.ascontiguousarray(cbf[ch].reshape(6, 128).T)
        dtb = np.ascontiguousarray(np.asarray(inp["mb_dt_bias"][0], np.float32)[:, g * 8:(g + 1) * 8].reshape(1, 16))
        alog = np.ascontiguousarray(np.asarray(inp["mb_a_log"][0], np.float32)[:, g * 8:(g + 1) * 8].reshape(1, 16))
        dsk = np.ascontiguousarray(np.repeat(np.asarray(inp["mb_d"][0], np.float32)[g * 8:(g + 1) * 8], 64).reshape(4, 128).T)
        maps.append({"hT": hT, "wz": wz, "wxbc": wxbc, "wdt": wdt, "cw": cw, "cb": cb, "dtb": dtb, "alog": alog, "dsk": dsk})
    return maps


def mamba_gather(res):
    u_lat = np.empty((2, 8192, 2048), np.float32)
    u_ctx = np.empty((2, 256, 2048), np.float32)
    for core in range(NCORES):
        b, g = core // 4, core % 4
        u = res[core]["uT"].T
        u_ctx[b, :, g * 512:(g + 1) * 512] = u[0:256]
        u_lat[b, :, g * 512:(g + 1) * 512] = u[256:]
    return u_lat, u_ctx


RW_SCALE = 0.606531
RW_EPS = 64e-5


def build_rwkv():
    nc = bass.Bass("TRN2", target_bir_lowering=False)
    with ExitStack() as stack:
        nc.dge_precook = False
        P = Prog(nc, stack)
        P.init_psum(8)

        def din(name, shape):
            return nc.dram_tensor(name, list(shape), F32, kind="ExternalInput").ap()

        hT = din("hT", [D, NKEY])
        mixr = din("mixr", [48, 128])
        wr = din("wr", [D, 256]); wk = din("wk", [D, 256]); wv = din("wv", [D, 256])
        w1 = din("w1", [2, D, 64]); w2 = din("w2", [2, 64, 256]); w0 = din("w0", [128, 4])
        a1 = din("a1", [2, D, 64]); a2 = din("a2", [2, 64, 256]); a0 = din("a0", [128, 4])
        g1 = din("g1", [D, 128]); g2 = din("g2", [128, 256])
        kkv = din("kkv", [128, 2]); kav = din("kav", [128, 2]); rkv = din("rkv", [128, 2])
        lng = din("lng", [2, 128, 64]); lnb = din("lnb", [2, 128, 64])
        oo = nc.dram_tensor("oo", [NKEY, 256], F32, kind="ExternalOutput").ap()
        ybs = nc.dram_tensor("ybs", [NKEY, 256], F32, kind="Internal").ap()
        ybs_t = TT(ybs, "ybs")

        ident, ones = make_consts(P)
        val = P.sb("val", [128, 64])
        for hs in range(2):
            P.op("pool", lambda e, hs=hs: e.iota(val[hs * 64:(hs + 1) * 64, :], pattern=[[1, 64]], base=0, channel_multiplier=-1,
                                                allow_small_or_imprecise_dtypes=True), (), [val])
        mgt = P.sb("mgt", [128, 64]); mge = P.sb("mge", [128, 64]); mlt = P.sb("mlt", [128, 64]); mle = P.sb("mle", [128, 64])
        identp = P.sb("identp", [128, 64])
        P.ts(mgt[:], val[:], 0.0, ALU.is_gt, [val], [mgt]); P.ts(mge[:], val[:], 0.0, ALU.is_ge, [val], [mge])
        P.ts(mlt[:], val[:], 0.0, ALU.is_lt, [val], [mlt]); P.ts(mle[:], val[:], 0.0, ALU.is_le, [val], [mle])
        P.ts(identp[:], val[:], 0.0, ALU.is_equal, [val], [identp])
        maskT = [P.sb("maskTf", [128, 128]), P.sb("maskTb", [128, 128])]
        P.copy(maskT[0][:, 0:64], mgt[:], [mgt], [maskT[0]]); P.copy(maskT[0][:, 64:128], mge[:], [mge], [maskT[0]])
        P.copy(maskT[1][:, 0:64], mlt[:], [mlt], [maskT[1]]); P.copy(maskT[1][:, 64:128], mle[:], [mle], [maskT[1]])
        maskA = [mlt, mgt]
        blk = P.sb("blk", [128, 128])
        P.memset(blk[:], 0.0, [blk])
        P.memset(blk[0:64, 0:64], 1.0, [blk]); P.memset(blk[64:128, 64:128], 1.0, [blk])
        half = P.sb("half", [128, 2])
        P.memset(half[:], 0.5, [half])
        tiny = P.sb("tiny", [128, 1])
        P.memset(tiny[:], RW_EPS, [tiny])

        def wload(name, ap, shape, rr=True):
            t = P.sb(name, shape)
            P.dma(t[:], ap, [], [t], r=rr)
            return t
        wr_s = wload("wr_s", wr.rearrange("(kc p) n -> p kc n", p=128), [128, 8, 256])
        wk_s = wload("wk_s", wk.rearrange("(kc p) n -> p kc n", p=128), [128, 8, 256])
        wv_s = wload("wv_s", wv.rearrange("(kc p) n -> p kc n", p=128), [128, 8, 256])
        g1_s = wload("g1_s", g1.rearrange("(kc p) n -> p kc n", p=128), [128, 8, 128])
        g2_s = wload("g2_s", g2, [128, 256])
        w1_s = [wload(f"w1_{d}", w1[d].rearrange("(kc p) n -> p kc n", p=128), [128, 8, 64]) for d in range(2)]
        a1_s = [wload(f"a1_{d}", a1[d].rearrange("(kc p) n -> p kc n", p=128), [128, 8, 64]) for d in range(2)]
        w2_s = [wload(f"w2_{d}", w2[d], [64, 256]) for d in range(2)]
        a2_s = [wload(f"a2_{d}", a2[d], [64, 256]) for d in range(2)]
        w0_s = wload("w0_s", w0, [128, 4], False); a0_s = wload("a0_s", a0, [128, 4], False)
        kk_s = wload("kk_s", kkv, [128, 2], False); ka_s = wload("ka_s", kav, [128, 2], False); rk_s = wload("rk_s", rkv, [128, 2], False)
        omka = P.sb("omka", [128, 2])
        P.ts(omka[:], ka_s[:], -1.0, ALU.mult, [ka_s], [omka], s2=1.0, op1=ALU.add)
        lng_s = [wload(f"lng{i}", lng[i], [128, 64], False) for i in range(2)]
        lnb_s = [wload(f"lnb{i}", lnb[i], [128, 64], False) for i in range(2)]
        mixc = P.sb("mixc", [128, 48])
        load_cols(P, ident, mixc, mixc[:], mixr, 48)

        W = 256
        NCH = 4
        ht = [P.sb(f"ht{i}", [128, 8, W + 2]) for i in range(2)]
        xx = P.sb("xx", [128, 8, W])
        xm = [P.sb(f"xm{i}", [128, 8, W]) for i in range(2)]
        rT = P.sb("rT", [128, 2, W]); kT = P.sb("kT", [128, 2, W]); kkT = P.sb("kkT", [128, 2, W])
        lwT = P.sb("lwT", [128, 2, W]); clT = P.sb("clT", [128, 2, W]); cleT = P.sb("cleT", [128, 2, W])
        aT = [P.sb(f"aT{d}", [128, 2, W]) for d in range(2)]
        kdT = [P.sb(f"kdT{d}", [128, 2, W]) for d in range(2)]
        bT = P.sb("bT", [128, 2, W])
        e1 = P.sb("e1", [128, 2, W]); e2 = P.sb("e2", [128, 2, W]); e3 = P.sb("e3", [128, 2, W])
        lam2 = [P.sb(f"lam{i}", [128, 2, NCH]) for i in range(3)]
        KR2 = [P.sb(f"KR{i}", [128, 2, NCH, 128]) for i in range(2)]
        BK2 = [P.sb(f"BK{i}", [128, 2, NCH, 128]) for i in range(2)]
        tw = P.sb("tw", [64, W]); t1 = P.sb("t1", [128, W])
        Vt2 = [P.sb(f"Vt{i}", [128, NCH, 2, 64]) for i in range(3)]
        Gt2 = [P.sb(f"Gt{i}", [128, NCH, 2, 64]) for i in range(3)]
        prodT = P.sb("prodT", [128, 2, W])
        bsc2 = [P.sb(f"bsc{i}", [128, NCH, 2, 2]) for i in range(3)]
        sq = P.sb("sqk", [128, W]); rn = P.sb("rnk", [128, W])
        onesW = P.sb("onesW", [128, W])
        P.memset(onesW[:], 1.0, [onesW])
        ST = [[P.sb(f"ST{d}{hp}", [128, 64]) for hp in range(2)] for d in range(2)]
        for d_ in range(2):
            for hp in range(2):
                P.memset(ST[d_][hp][:], 0.0, [ST[d_][hp]])

        class Slot:
            pass
        scratch = []
        for si in range(8):
            s_ = {}
            for nm, shp in (("AWu", [128, 128]), ("BWv", [128, 128]), ("PPa", [128, 128]), ("PPb", [128, 128]), ("X", [128, 64]),
                            ("RH", [128, 128]), ("UK", [128, 128]), ("BKt", [128, 128])):
                s_[nm] = P.sb(f"{nm}{si}", shp)
            scratch.append(s_)
        slots2 = []
        for par in range(2):
            row = []
            for si in range(8):
                s_ = Slot()
                for nm, t_ in scratch[si].items():
                    setattr(s_, nm, t_)
                for nm in ("MpT", "Nc", "R2T", "Y0"):
                    setattr(s_, nm, P.sb(f"{nm}{par}{si}", [128, 64]))
                row.append(s_)
            slots2.append(row)
        ysb = [P.sb(f"ysb{i}", [128, 64]) for i in range(4)]
        ybb = [P.sb(f"ybb{i}", [128, 64]) for i in range(4)]
        stt6 = [P.sb(f"st6{i}", [128, 6]) for i in range(2)]
        mv = [P.sb(f"mv{i}", [128, 4]) for i in range(2)]

        def stage_a(ti, c0, q0, q1, dirs, fwd):
            par = ti % 2
            p3 = ti % 3
            lam, KR, BK, Vt, Gt, bsc = lam2[p3], KR2[par], BK2[par], Vt2[p3], Gt2[p3], bsc2[p3]
            h = ht[ti % 2]
            lo, hi = max(c0 - 1, q0), min(c0 + W + 1, q1)
            off = lo - (c0 - 1)
            n = hi - lo
            if off > 0:
                P.memset(h[:, :, 0:off], 0.0, [h])
            if off + n < W + 2:
                P.memset(h[:, :, off + n:W + 2], 0.0, [h])
            for kc in range(8):
                P.dma(h[:, kc, off:off + n], hT[kc * 128:(kc + 1) * 128, lo:hi], [], [h])
            P.tt(xx[:, :, :], h[:, :, 0:W], h[:, :, 2:W + 2], ALU.add, [h], [xx])
            P.stt(xx[:, :, :], xx[:, :, :], 0.5, h[:, :, 1:W + 1], ALU.mult, ALU.subtract, [xx, h], [xx])
            mi = [0]

            def mixed(j):
                m_ = xm[mi[0] % 2]
                mi[0] += 1
                for kc in range(8):
                    P.stt(R(m_[:, kc, :]), xx[:, kc, :], mixc[:, j * 8 + kc:j * 8 + kc + 1], h[:, kc, 1:W + 1], ALU.mult, ALU.add,
                          [xx, mixc, h], [m_])
                return m_

            def proj2(m_, w_s, dst):
                for oc in range(2):
                    pb = P.bank()
                    for kc in range(8):
                        P.mm(pb[:, 0:W], w_s[:, kc, oc * 128:(oc + 1) * 128], m_[:, kc, :], kc == 0, kc == 7, [w_s, m_], [pb])
                    P.copy(dst[:, oc, :], pb[:, 0:W], [pb], [dst], eng="act")

            def lora(m_, l1_s, l2_s, bias_s, d, dst, mid_func):
                pb = P.bank()
                for kc in range(8):
                    P.mm(pb[0:64, 0:W], l1_s[:, kc, :], m_[:, kc, :], kc == 0, kc == 7, [l1_s, m_], [pb])
                P.act(R(tw[:, :]), pb[0:64, 0:W], mid_func, [pb], [tw])
                for oc in range(2):
                    p2 = P.bank()
                    P.mm(p2[:, 0:W], l2_s[:, oc * 128:(oc + 1) * 128], tw[:, :], True, True, [l2_s, tw], [p2])
                    P.act(dst[:, oc, :], p2[:, 0:W], AF.Sigmoid, [p2, bias_s], [dst], bias=bias_s[:, d * 2 + oc:d * 2 + oc + 1])

            yield
            m_ = mixed(0)
            proj2(m_, wr_s, rT)
            yield
            m_ = mixed(1)
            dd = dirs[0] if len(dirs) == 1 else 0
            lora(m_, w1_s[dd], w2_s[dd], w0_s, dd, lwT, AF.Tanh)
            P.ts(lwT[:, :, :], lwT[:, :, :], -RW_SCALE, ALU.mult, [lwT], [lwT])
            yield
            m_ = mixed(2)
            proj2(m_, wk_s, kT)
            yield
            m_ = mixed(3)
            for j4 in range(NCH):
                yield
                pv = P.bank()
                for hq in range(4):
                    p0 = (hq % 2) * 64
                    hp = hq // 2
                    for kc in range(8):
                        P.mm(pv[p0:p0 + 64, hp * 64:(hp + 1) * 64], m_[:, kc, j4 * 64:(j4 + 1) * 64], wv_s[:, kc, hq * 64:(hq + 1) * 64],
                             kc == 0, kc == 7, [m_, wv_s], [pv], r=False)
                P.copy(Vt[:, j4, :, :], pv[:, 0:128].rearrange("p (a b) -> p a b", a=2), [pv], [Vt.sub(j4)], eng="act")
            yield
            m_ = mixed(4)
            adirs = [0, 1] if fwd else dirs
            for d in adirs:
                lora(m_, a1_s[d], a2_s[d], a0_s, d, aT[d], AF.Copy)
            if fwd:
                yield
                m_ = mixed(5)
                pb = P.bank()
                for kc in range(8):
                    P.mm(pb[:, 0:W], g1_s[:, kc, :], m_[:, kc, :], kc == 0, kc == 7, [g1_s, m_], [pb])
                P.act(t1[:, :], pb[:, 0:W], AF.Sigmoid, [pb], [t1])
                for j4 in range(NCH):
                    pg = P.bank()
                    for hq in range(4):
                        p0 = (hq % 2) * 64
                        hp = hq // 2
                        P.mm(pg[p0:p0 + 64, hp * 64:(hp + 1) * 64], t1[:, j4 * 64:(j4 + 1) * 64], g2_s[:, hq * 64:(hq + 1) * 64],
                             True, True, [t1, g2_s], [pg], r=False)
                    P.copy(Gt[:, j4, :, :], pg[:, 0:128].rearrange("p (a b) -> p a b", a=2), [pg], [Gt.sub(j4)], eng="act")
            yield
            for oc in range(2):
                P.ts(kkT[:, oc, :], kT[:, oc, :], kk_s[:, oc:oc + 1], ALU.mult, [kT, kk_s], [kkT])
                P.act(sq[:, :], kkT[:, oc, :], AF.Square, [kkT], [sq])
                pb = P.bank()
                P.mm(pb[:, 0:W], blk[:, :], sq[:, :], True, True, [blk, sq], [pb], r=False)
                P.ts(rn[:, :], pb[:, 0:W], 1e-24, ALU.max, [pb], [rn])
                P.act(rn[:, :], rn[:, :], AF.Ln, [rn], [rn])
                P.act(rn[:, :], rn[:, :], AF.Exp, [rn], [rn], scale=-0.5)
                P.tt(kkT[:, oc, :], kkT[:, oc, :], rn[:, :], ALU.mult, [kkT, rn], [kkT])
                for d in adirs:
                    P.ts(kdT[d][:, oc, :], aT[d][:, oc, :], ka_s[:, oc:oc + 1], ALU.mult, [aT[d], ka_s, omka], [kdT[d]],
                         s2=omka[:, oc:oc + 1], op1=ALU.add)
                    P.tt(kdT[d][:, oc, :], kdT[d][:, oc, :], kT[:, oc, :], ALU.mult, [kdT[d], kT], [kdT[d]])
            yield
            if fwd:
                P.tt(prodT[:, :, :], kdT[0][:, :, :], kdT[1][:, :, :], ALU.add, [kdT[0], kdT[1]], [prodT])
                for oc in range(2):
                    P.stt(prodT[:, oc, :], rT[:, oc, :], rk_s[:, oc:oc + 1], prodT[:, oc, :], ALU.mult, ALU.mult, [rT, rk_s, prodT], [prodT])
                for j4 in range(NCH):
                    pb = P.bank()
                    for hq in range(4):
                        p0 = (hq % 2) * 64
                        hp = hq // 2
                        P.mm(pb[p0:p0 + 64, hp * 2:hp * 2 + 2], prodT[p0:p0 + 64, hp, j4 * 64:(j4 + 1) * 64], half[p0:p0 + 64, 0:2],
                             True, True, [prodT, half], [pb], r=False)
                    P.copy(bsc[:, j4, :, :], pb[:, 0:4].rearrange("p (a b) -> p a b", a=2), [pb], [bsc.sub(j4)])
            yield
            d = dirs[0] if len(dirs) == 1 else 0
            P.tt(bT[:, :, :], kkT[:, :, :], aT[d][:, :, :], ALU.mult, [kkT, aT[d]], [bT])
            for oc in range(2):
                P.op("dve", lambda e, oc=oc: e.tensor_tensor_scan(out=clT[:, oc, :], data0=onesW[:, :], data1=lwT[:, oc, :], initial=0.0,
                                                                   op0=ALU.mult, op1=ALU.add), [onesW, lwT], [clT])
                for j4 in range(NCH - 1, 0, -1):
                    P.ts(clT[:, oc, j4 * 64:(j4 + 1) * 64], clT[:, oc, j4 * 64:(j4 + 1) * 64], clT[:, oc, j4 * 64 - 1:j4 * 64], ALU.subtract,
                         [clT], [clT])
                if d == 0:
                    P.tt(cleT[:, oc, :], clT[:, oc, :], lwT[:, oc, :], ALU.subtract, [clT, lwT], [cleT])
                else:
                    for j4 in range(NCH):
                        P.ts(cleT[:, oc, j4 * 64:(j4 + 1) * 64], clT[:, oc, j4 * 64:(j4 + 1) * 64], -1.0, ALU.mult, [clT], [cleT],
                             s2=clT[:, oc, j4 * 64 + 63:j4 * 64 + 64], op1=ALU.add)
                    P.tt(clT[:, oc, :], cleT[:, oc, :], lwT[:, oc, :], ALU.add, [cleT, lwT, clT], [clT])
            yield
            P.act(e1[:, :, :], cleT[:, :, :], AF.Exp, [cleT], [e1])
            P.act(e2[:, :, :], clT[:, :, :], AF.Exp, [clT], [e2], scale=-1.0)
            P.act(e3[:, :, :], clT[:, :, :], AF.Exp, [clT], [e3])
            for oc in range(2):
                e3v = e3[:, oc, :].rearrange("p (c t) -> p c t", c=NCH)
                P.copy(lam[:, oc, :], e3v[:, :, 63] if d == 0 else e3v[:, :, 0], [e3], [lam])
                kkv_ = kkT[:, oc, :].rearrange("p (c t) -> p c t", c=NCH)
                P.tt(KR[:, oc, :, 0:64], kkv_, e1[:, oc, :].rearrange("p (c t) -> p c t", c=NCH), ALU.mult, [kkT, e1], [KR])
                P.tt(KR[:, oc, :, 64:128], rT[:, oc, :].rearrange("p (c t) -> p c t", c=NCH), e3v, ALU.mult, [rT, e3], [KR])
                e2v = e2[:, oc, :].rearrange("p (c t) -> p c t", c=NCH)
                P.tt(BK[:, oc, :, 0:64], bT[:, oc, :].rearrange("p (c t) -> p c t", c=NCH), e2v, ALU.mult, [bT, e2], [BK])
                P.tt(BK[:, oc, :, 64:128], kdT[d][:, oc, :].rearrange("p (c t) -> p c t", c=NCH), e2v, ALU.mult, [kdT[d], e2], [BK])

        def pre(sl, hp, d, j4, par, p3):
            lam, KR, BK, Vt = lam2[p3], KR2[par], BK2[par], Vt2[p3]
            kr = KR[:, hp, j4, :]
            bk = BK[:, hp, j4, :]
            V = Vt[:, j4, hp, :]
            hs2 = [(0, 64), (64, 128)]
            pA = P.bank()
            for (a, b) in hs2:
                P.mm(pA[a:b, 0:128], bk[a:b, 0:64], kr[a:b, :], True, True, [BK, KR], [pA], r=False)
                P.mm(pA[a:b, 128:256], bk[a:b, 64:128], kr[a:b, :], True, True, [BK, KR], [pA], r=False)
                P.mm(pA[a:b, 256:320], kr[a:b, 0:64], bk[a:b, 0:64], True, True, [BK, KR], [pA], r=False)
            P.tt(sl.AWu[:, :], pA[:, 0:128], maskT[d][:, :], ALU.mult, [pA, maskT[d]], [sl.AWu])
            P.tt(sl.BWv[:, :], pA[:, 128:256], maskT[d][:, :], ALU.mult, [pA, maskT[d]], [sl.BWv])
            P.stt(sl.PPa[:, 0:64], pA[:, 256:320], -1.0, maskA[d][:, :], ALU.mult, ALU.mult, [pA, maskA[d]], [sl.PPa])
            P.ts(sl.PPa[:, 64:128], sl.AWu[:, 0:64], -1.0, ALU.mult, [sl.AWu], [sl.PPa])
            P.tt(sl.X[:, :], identp[:, :], sl.PPa[:, 64:128], ALU.add, [identp, sl.PPa], [sl.X])
            yield
            cur, nxt = sl.PPa, sl.PPb
            for m in range(1, 6):
                pL = P.bank()
                for (a, b) in hs2:
                    P.mm(pL[a:b, 0:64], cur[a:b, 64:128], cur[a:b, 0:64], True, True, [cur], [pL], r=False)
                    if m < 5:
                        P.mm(pL[a:b, 64:128], cur[a:b, 0:64], cur[a:b, 64:128], True, True, [cur], [pL], r=False)
                if m < 5:
                    P.copy(nxt[:, :], pL[:, 0:128], [pL], [nxt], eng="act")
                else:
                    P.copy(nxt[:, 0:64], pL[:, 0:64], [pL], [nxt], eng="act")
                yield
                pX = P.bank()
                for (a, b) in hs2:
                    P.mm(pX[a:b, 0:64], nxt[a:b, 0:64], sl.X[a:b, :], True, True, [nxt, sl.X], [pX], r=False)
                P.tt(sl.X[:, :], sl.X[:, :], pX[:, 0:64], ALU.add, [sl.X, pX], [sl.X])
                yield
                cur, nxt = nxt, cur
            pR = P.bank()
            for (a, b) in hs2:
                P.mm(pR[a:b, 0:64], sl.BWv[a:b, 0:64], V[a:b, :], True, True, [sl.BWv, Vt.sub(j4)], [pR], r=False)
                P.mm(pR[a:b, 64:128], kr[a:b, 0:64], ident[a:b, a:b], True, True, [KR, ident], [pR], r=False)
            P.copy(sl.RH[:, :], pR[:, 0:128], [pR], [sl.RH], eng="act")
            yield
            pXR = P.bank()
            for (a, b) in hs2:
                P.mm(pXR[a:b, 0:128], sl.X[a:b, :], sl.RH[a:b, :], True, True, [sl.X, sl.RH], [pXR], r=False)
            P.ts(sl.UK[:, 0:64], pXR[:, 0:64], -1.0, ALU.mult, [pXR], [sl.UK])
            P.copy(sl.UK[:, 64:128], pXR[:, 64:128], [pXR], [sl.UK], eng="act")
            yield
            pT = P.bank()
            for (a, b) in hs2:
                P.mm(pT[a:b, 0:64], bk[a:b, 0:64], ident[a:b, a:b], True, True, [BK, ident], [pT], r=False)
                P.mm(pT[a:b, 64:128], bk[a:b, 64:128], ident[a:b, a:b], True, True, [BK, ident], [pT], r=False)
            P.copy(sl.BKt[:, :], pT[:, 0:128], [pT], [sl.BKt], eng="act")
            yield
            pM = P.bank()
            for (a, b) in hs2:
                P.mm(pM[a:b, 0:64], sl.UK[a:b, 64:128], sl.BKt[a:b, 0:64], True, True, [sl.UK, sl.BKt], [pM], r=False)
                P.mm(pM[a:b, 64:128], sl.BKt[a:b, 0:64], sl.UK[a:b, 0:64], True, False, [sl.UK, sl.BKt], [pM], r=False)
                P.mm(pM[a:b, 64:128], sl.BKt[a:b, 64:128], V[a:b, :], False, True, [sl.BKt, Vt.sub(j4)], [pM], r=False)
                P.mm(pM[a:b, 128:192], sl.UK[a:b, 64:128], sl.AWu[a:b, 64:128], True, True, [sl.UK, sl.AWu], [pM], r=False)
            P.tt(sl.MpT[:, :], identp[:, :], pM[:, 0:64], ALU.subtract, [identp, pM], [sl.MpT])
            P.ts(sl.Nc[:, :], pM[:, 64:128], lam[:, hp, j4:j4 + 1], ALU.mult, [pM, lam], [sl.Nc])
            P.tt(sl.R2T[:, :], kr[:, 64:128], pM[:, 128:192], ALU.subtract, [KR, pM], [sl.R2T])
            yield
            pY = P.bank()
            for (a, b) in hs2:
                P.mm(pY[a:b, 0:64], sl.AWu[a:b, 64:128], sl.UK[a:b, 0:64], True, False, [sl.AWu, sl.UK], [pY], r=False)
                P.mm(pY[a:b, 0:64], sl.BWv[a:b, 64:128], V[a:b, :], False, True, [sl.BWv, Vt.sub(j4)], [pY], r=False)
            P.copy(sl.Y0[:, :], pY[:, 0:64], [pY], [sl.Y0], eng="act")

        def seq(sl, hp, d, j4, col0, k, p3):
            lam, Vt, Gt, bsc = lam2[p3], Vt2[p3], Gt2[p3], bsc2[p3]
            S = ST[d][hp]
            hs2 = [(0, 64), (64, 128)]
            pYs = P.bank()
            for (a, b) in hs2:
                P.mm(pYs[a:b, 0:64], sl.R2T[a:b, :], S[a:b, :], True, True, [sl.R2T, S], [pYs], r=False)
            ys = ysb[k % 4]
            P.tt(ys[:, :], sl.Y0[:, :], pYs[:, 0:64], ALU.add, [sl.Y0, pYs], [ys])
            pS = P.bank()
            for (a, b) in hs2:
                P.mm(pS[a:b, 0:64], sl.MpT[a:b, :], S[a:b, :], True, True, [sl.MpT, S], [pS], r=False)
            P.stt(S[:, :], pS[:, 0:64], lam[:, hp, j4:j4 + 1], sl.Nc[:, :], ALU.mult, ALU.add, [pS, lam, sl.Nc], [S])
            if d == 1:
                for hs, (a, b) in enumerate(hs2):
                    hq = hp * 2 + hs
                    P.dma(ybs[col0:col0 + 64, hq * 64:(hq + 1) * 64], ys[a:b, :], [ys], [ybs_t])
            else:
                yb = ybb[k % 4]
                for hs, (a, b) in enumerate(hs2):
                    hq = hp * 2 + hs
                    P.dma(yb[a:b, :], ybs[col0:col0 + 64, hq * 64:(hq + 1) * 64], [ybs_t], [yb])
                P.tt(ys[:, :], ys[:, :], yb[:, :], ALU.add, [ys, yb], [ys])
                s6 = stt6[k % 2]
                m2 = mv[k % 2]
                P.op("dve", lambda e, s6=s6, ys=ys: e.bn_stats(out=s6[:, :], in_=ys[:, :]), [ys], [s6])
                P.op("dve", lambda e, s6=s6, m2=m2: e.bn_aggr(out=m2[:, 0:2], in_=s6[:, :]), [s6], [m2])
                P.act(m2[:, 2:3], m2[:, 1:2], AF.Sqrt, [m2, tiny], [m2], bias=tiny[:, 0:1])
                P.op("dve", lambda e, m2=m2: e.reciprocal(out=m2[:, 3:4], in_=m2[:, 2:3]), [m2], [m2])
                P.ts(ys[:, :], ys[:, :], m2[:, 0:1], ALU.subtract, [ys, m2], [ys], s2=m2[:, 3:4], op1=ALU.mult)
                P.tt(ys[:, :], ys[:, :], lng_s[hp][:, :], ALU.mult, [ys, lng_s[hp]], [ys])
                P.tt(ys[:, :], ys[:, :], lnb_s[hp][:, :], ALU.add, [ys, lnb_s[hp]], [ys])
                P.stt(yb[:, :], Vt[:, j4, hp, :], bsc[:, j4, hp, 0:1], ys[:, :], ALU.mult, ALU.add, [Vt.sub(j4), bsc.sub(j4), ys], [yb])
                P.tt(yb[:, :], yb[:, :], Gt[:, j4, hp, :], ALU.mult, [yb, Gt.sub(j4)], [yb])
                for hs, (a, b) in enumerate(hs2):
                    hq = hp * 2 + hs
                    P.dma(oo[col0:col0 + 64, hq * 64:(hq + 1) * 64], yb[a:b, :], [yb], [])

        tiles = [(0, 0, 256)] + [(256 + 256 * t, 256, NKEY) for t in range(32)]
        order_b = [tiles[0]] + tiles[:0:-1]
        work = []
        for d, order, chunks in ((1, order_b, (3, 2, 1, 0)), (0, tiles, (0, 1, 2, 3))):
            for (c0, q0, q1) in order:
                work.append((d, c0, q0, q1, chunks))
        nW = len(work)

        def drain(g_):
            for _ in g_:
                pass

        def units_of(wi):
            d, c0, q0, q1, chunks = work[wi]
            return [(j4, hp) for j4 in chunks for hp in range(2)]
        kcnt = [0]
        drain(stage_a(0, work[0][1], work[0][2], work[0][3], [work[0][0]], work[0][0] == 0))
        for i in range(nW + 1):
            gA = None
            if i + 1 < nW:
                d_, c0_, q0_, q1_, _ = work[i + 1]
                gA = stage_a(i + 1, c0_, q0_, q1_, [d_], d_ == 0)
            gB = []
            if i < nW:
                d_ = work[i][0]
                gB = [pre(slots2[i % 2][ui], hp, d_, j4, i % 2, i % 3) for ui, (j4, hp) in enumerate(units_of(i))]
            cS = []
            if i >= 1:
                d_, c0_ = work[i - 1][0], work[i - 1][1]
                cS = [(slots2[(i - 1) % 2][ui], hp, d_, j4, c0_ + j4 * 64) for ui, (j4, hp) in enumerate(units_of(i - 1))]
            tick = 0
            while gA is not None or gB or cS:
                if gA is not None:
                    try:
                        next(gA)
                    except StopIteration:
                        gA = None
                nb = []
                for g_ in gB:
                    try:
                        next(g_)
                        nb.append(g_)
                    except StopIteration:
                        pass
                gB = nb
                if cS and tick % 3 == 1:
                    sl_, hp_, dd_, j4_, col_ = cS.pop(0)
                    seq(sl_, hp_, dd_, j4_, col_, kcnt[0], (i - 1) % 3)
                    kcnt[0] += 1
                tick += 1
        P.emit()
    return nc


def rwkv_maps(inp, h_lat, h_ctx):
    f = lambda k: np.asarray(inp[k][0], np.float32)
    rkv, w0, w1, w2 = f("rw_rkv_w"), f("rw_w0"), f("rw_w1"), f("rw_w2")
    a0, a1, a2, g1, g2 = f("rw_a0"), f("rw_a1"), f("rw_a2"), f("rw_g1"), f("rw_g2")
    k_k, k_a, r_k, ln_g, ln_b = f("rw_k_k"), f("rw_k_a"), f("rw_r_k").reshape(-1), f("rw_ln_g"), f("rw_ln_b")
    mixr = rows128(f("rw_mix"))
    maps = []
    for core in range(NCORES):
        b, hg = core // 4, core % 4
        sl = slice(hg * 256, (hg + 1) * 256)
        hT = np.ascontiguousarray(np.concatenate([h_ctx[b], h_lat[b]], axis=0).T)

        def pc(v):
            return np.ascontiguousarray(v.reshape(2, 128).T)

        def pc2(v2):
            return np.ascontiguousarray(v2.reshape(2, 2, 128).transpose(2, 0, 1).reshape(128, 4))

        def pairrows(v):
            hh = v.reshape(2, 2, 1, 64)
            return np.ascontiguousarray(np.broadcast_to(hh, (2, 2, 64, 64)).reshape(2, 128, 64))
        maps.append({
            "hT": hT, "mixr": mixr,
            "wr": np.ascontiguousarray(rkv[0][:, sl]), "wk": np.ascontiguousarray(rkv[1][:, sl]), "wv": np.ascontiguousarray(rkv[2][:, sl]),
            "w1": w1, "w2": np.ascontiguousarray(w2[:, :, sl]), "w0": pc2(w0[:, sl]),
            "a1": a1, "a2": np.ascontiguousarray(a2[:, :, sl]), "a0": pc2(a0[:, sl]),
            "g1": g1, "g2": np.ascontiguousarray(g2[:, sl]),
            "kkv": pc(k_k[sl]), "kav": pc(k_a[sl]), "rkv": pc(r_k[sl]),
            "lng": pairrows(ln_g[sl]), "lnb": pairrows(ln_b[sl]),
        })
    return maps


def rwkv_gather(res):
    o_lat = np.empty((2, 8192, 1024), np.float32)
    o_ctx = np.empty((2, 256, 1024), np.float32)
    for core in range(NCORES):
        b, hg = core // 4, core % 4
        o = res[core]["oo"]
        o_ctx[b, :, hg * 256:(hg + 1) * 256] = o[0:256]
        o_lat[b, :, hg * 256:(hg + 1) * 256] = o[256:]
    return o_lat, o_ctx


def _get(key, builder):
    if key not in _NC_CACHE:
        _NC_CACHE[key] = builder()
    return _NC_CACHE[key]


def attn_maps(inp, h_lat, h_ctx):
    cosT, sinS, prot = rope_tables()
    Wq = np.asarray(inp["at_qkv_w"][0], np.float32)
    qg = np.asarray(inp["at_q_g"][0], np.float32).reshape(64, 1)
    kg = np.asarray(inp["at_k_g"][0], np.float32).reshape(64, 1)
    maps = []
    for core in range(NCORES):
        b, kv = core // 4, core % 4
        hT = np.ascontiguousarray(np.concatenate([h_ctx[b], h_lat[b]], axis=0).T)
        maps.append({"hT": hT, "wq": np.ascontiguousarray(Wq[:, kv * 256:(kv + 1) * 256]),
                     "wk": np.ascontiguousarray(Wq[:, 1024 + kv * 64:1024 + (kv + 1) * 64]),
                     "wv": np.ascontiguousarray(Wq[:, 1280 + kv * 64:1280 + (kv + 1) * 64]),
                     "qg": qg, "kg": kg, "cosT": cosT, "sinS": sinS, "prot": prot})
    return maps


def attn_gather(res):
    o = np.zeros((2, 8192, 1024), np.float32)
    for core in range(NCORES):
        b, kv = core // 4, core % 4
        o[b, :, kv * 256:(kv + 1) * 256] = res[core]["oT"].T
    return o


def kernel(**inp):
    f32 = lambda a: np.asarray(a, np.float32)
    x, c, ctx, c_ctx = f32(inp["x"]), f32(inp["c"]), f32(inp["ctx"]), f32(inp["c_ctx"])
    mod_w, mod_b = f32(inp["mod_w"]), f32(inp["mod_b"])
    n1g, n2g = f32(inp["norm1_g"]), f32(inp["norm2_g"])
    cv = [cvec_for(c, c_ctx, core) for core in range(NCORES)]

    def nxt_params(i):
        return {"mod_w_n": mod_w[i], "mod_b_n": rows128(mod_b[i]), "n1g_n": rows128(n1g[i])}

    def cur_params(i):
        return {"mod_w": mod_w[i], "mod_b": rows128(mod_b[i]), "n2g": rows128(n2g[i])}

    xs = to_cores_T(x, ctx)
    res = run(_get(("ts", None, None, "norm"), lambda: build_ts(0, None, None, "norm")),
              [dict(xT=xs[k], cvec=cv[k], **nxt_params(0)) for k in range(NCORES)])
    h_lat, h_ctx = from_cores_T([r["hT_out"] for r in res])
    mres = run(_get("mamba", build_mamba), mamba_maps(inp, h_lat, h_ctx))
    u_lat, u_ctx = mamba_gather(mres)
    ms = to_cores_T(u_lat, u_ctx)
    res = run(_get(("ts", "mamba", "dense", "norm"), lambda: build_ts(0, "mamba", "dense", "norm")),
              [dict(xT=xs[k], mT=ms[k], cvec=cv[k], mb_ng=rows128(f32(inp["mb_norm_g"])[0]), w_post=f32(inp["mb_out_w"])[0],
                    w_in=f32(inp["ff_in_w"])[0], w_out=f32(inp["ff_out_w"])[0], **cur_params(0), **nxt_params(1)) for k in range(NCORES)])
    xs = [r["xT_out"] for r in res]
    h_lat, h_ctx = from_cores_T([r["hT_out"] for r in res])
    rres = run(_get("rwkv", build_rwkv), rwkv_maps(inp, h_lat, h_ctx))
    o_lat, o_ctx = rwkv_gather(rres)
    ms = to_cores_T(o_lat, o_ctx)
    ts_moe_n = _get(("ts", "lin", "moe", "norm"), lambda: build_ts(1, "lin", "moe", "norm"))
    res = run(ts_moe_n,
              [dict(xT=xs[k], mT=ms[k], cvec=cv[k], w_post=f32(inp["rw_out_w"])[0], router=f32(inp["moe_router_w"])[0],
                    w_in=f32(inp["moe_in_w"])[0], w_out=f32(inp["moe_out_w"])[0], **cur_params(1), **nxt_params(2)) for k in range(NCORES)])
    xs = [r["xT_out"] for r in res]
    h_lat, h_ctx = from_cores_T([r["hT_out"] for r in res])
    pin = pool_inputs(h_lat, h_ctx)
    res = run(_get(("ts", "pool", "dense", "norm"), lambda: build_ts(2, "pool", "dense", "norm")),
              [dict(xT=xs[k], cvec=cv[k], pl_w=f32(inp["pl_w"])[0], pl_scale=rows128(f32(inp["pl_scale"])[0]),
                    w_in=f32(inp["ff_in_w"])[1], w_out=f32(inp["ff_out_w"])[1], **pin[k], **cur_params(2), **nxt_params(3))
               for k in range(NCORES)])
    xs = [r["xT_out"] for r in res]
    h_lat, h_ctx = from_cores_T([r["hT_out"] for r in res])
    ares = run(_get("attn", build_attn), attn_maps(inp, h_lat, h_ctx))
    o_lat = attn_gather(ares)
    ms = to_cores_T(o_lat, np.zeros((2, 256, 1024), np.float32))
    res = run(_get(("ts", "lin", "moe", "final"), lambda: build_ts(3, "lin", "moe", "final")),
              [dict(xT=xs[k], mT=ms[k], cvec=cv[k], w_post=f32(inp["at_out_w"])[0], router=f32(inp["moe_router_w"])[1],
                    w_in=f32(inp["moe_in_w"])[1], w_out=f32(inp["moe_out_w"])[1], fin_g=rows128(f32(inp["final_g"])), **cur_params(3))
               for k in range(NCORES)])
    out_lat, _ = from_cores_T([r["out_T"] for r in res])
    return out_lat
```

```python
import numpy as np
from contextlib import ExitStack
import concourse.bass as bass
import concourse.mybir as mybir
from concourse.bass_utils import run_bass_kernel_spmd

F32 = mybir.dt.float32
F32R = mybir.dt.float32r
I32 = mybir.dt.int32
AF = mybir.ActivationFunctionType
ALU = mybir.AluOpType
AX = mybir.AxisListType


def R(ap):
    return ap.bitcast(F32R)

D = 1024
KC = 8
NCORES = 8
EPS = 1e-6


class Buf:
    __slots__ = ("lw", "rd", "name", "parent")

    def __init__(self, name="", parent=None):
        self.lw = None
        self.rd = {}
        self.name = name
        self.parent = parent


class TT:
    def __init__(self, t, name):
        self.t = t
        self.name = name
        self.b = Buf(name)
        self.subs = {}

    def sub(self, key):
        if key not in self.subs:
            self.subs[key] = Buf(f"{self.name}/{key}", self.b)
        return self.subs[key]

    def __getitem__(self, idx):
        return self.t[idx]


class _SubView:
    def __init__(self, tt, subs):
        self.tt = tt
        self.subs = subs

    def __getitem__(self, idx):
        return self.tt.t[idx]


def _bufs(lst):
    out = []
    for x in lst:
        if x is None:
            continue
        if isinstance(x, TT):
            out.append(x.b)
            out.extend(x.subs.values())
        elif isinstance(x, _SubView):
            out.extend(x.subs)
        else:
            out.append(x)
    return out


class Prog:
    NDS = 24

    def __init__(self, nc, stack):
        self.nc = nc
        self.stack = stack
        self.eng = {"pe": nc.tensor, "dve": nc.vector, "act": nc.scalar, "pool": nc.gpsimd, "sp": nc.sync}
        self.sem = {k: stack.enter_context(nc.semaphore("s_" + k)) for k in self.eng}
        self.cnt = {k: 0 for k in self.eng}
        self.waited = {k: {} for k in self.eng}
        self.ops = {k: [] for k in self.eng}
        self.dsem = [stack.enter_context(nc.semaphore(f"d{i}")) for i in range(self.NDS)]
        self.dval = [0] * self.NDS
        self.dnext = 0
        self.nalloc = 0
        self.psum_banks = None
        self.psum_next = 0

    def sb(self, name, shape, dtype=F32):
        self.nalloc += 1
        t = self.stack.enter_context(self.nc.sbuf_tensor(f"{name}_{self.nalloc}", list(shape), dtype))
        return TT(t, name)

    def ps(self, name, shape, dtype=F32):
        self.nalloc += 1
        t = self.stack.enter_context(self.nc.psum_tensor(f"{name}_{self.nalloc}", list(shape), dtype))
        return TT(t, name)

    def init_psum(self, n=8):
        self.psum_banks = [self.ps(f"bank{i}", [128, 512]) for i in range(n)]

    def bank(self):
        b = self.psum_banks[self.psum_next % len(self.psum_banks)]
        self.psum_next += 1
        return b

    def dram(self, name, shape, dtype=F32, kind="Internal"):
        t = self.nc.dram_tensor(name, list(shape), dtype, kind=kind)
        return TT(t.ap(), name)

    def _collect(self, engine, reads, writes, extra=None):
        need = {}

        def add(ev):
            if ev is None:
                return
            k, v = ev
            if need.get(k, 0) < v:
                need[k] = v

        for b in reads:
            add(b.lw)
            if b.parent is not None:
                add(b.parent.lw)
        for b in writes:
            add(b.lw)
            for k, v in b.rd.items():
                add((k, v))
            if b.parent is not None:
                add(b.parent.lw)
                for k, v in b.parent.rd.items():
                    add((k, v))
        if extra:
            for ev in extra:
                add(ev)
        if engine == "pe":
            need.pop(("e", "pe"), None)
        w = self.waited[engine]
        waits = []
        for k, v in need.items():
            if w.get(k, 0) < v:
                waits.append((k, v))
                w[k] = v
        return waits

    def _mark(self, ev, reads, writes):
        k, v = ev
        for b in reads:
            if b.rd.get(k, 0) < v:
                b.rd[k] = v
        for b in writes:
            b.lw = ev
            b.rd = {}

    def op(self, engine, fn, reads=(), writes=()):
        reads = _bufs(reads)
        writes = _bufs(writes)
        waits = self._collect(engine, reads, writes)
        self.cnt[engine] += 1
        ev = (("e", engine), self.cnt[engine])
        self._mark(ev, reads, writes)
        self.ops[engine].append((waits, fn, None, self.cnt[engine]))

    def dma(self, out, in_, reads=(), writes=(), queue="sp", r=False, **kw):
        if r:
            out = out.bitcast(F32R)
            in_ = in_.bitcast(F32R)
        reads = _bufs(reads)
        writes = _bufs(writes)
        i = self.dnext
        self.dnext = (i + 1) % self.NDS
        extra = [(("d", i), self.dval[i])] if self.dval[i] else None
        waits = self._collect(queue, reads, writes, extra)
        self.dval[i] += 16
        ev = (("d", i), self.dval[i])
        self._mark(ev, reads, writes)
        self.ops[queue].append((waits, lambda e: e.dma_start(out=out, in_=in_, **kw), i, None))

    def dbg(self, name, ap, shape, reads):
        if not getattr(self, "debug", False):
            return
        if name in getattr(self, "_dbg_done", set()):
            return
        self.__dict__.setdefault("_dbg_done", set()).add(name)
        t = self.nc.dram_tensor(name, list(shape), F32, kind="ExternalOutput").ap()
        self.dma(t, ap, reads, [])

    def _semof(self, k):
        return self.sem[k[1]] if k[0] == "e" else self.dsem[k[1]]

    def emit(self):
        fin = []
        for i in range(self.NDS):
            if self.dval[i] and self.waited["sp"].get(("d", i), 0) < self.dval[i]:
                fin.append((("d", i), self.dval[i]))
        for k in self.eng:
            if k != "sp" and self.cnt[k]:
                fin.append((("e", k), self.cnt[k]))
        needed = {k: set() for k in self.eng}
        for engine in self.eng:
            for waits, fn, di, idx in self.ops[engine]:
                for k, v in waits:
                    if k[0] == "e":
                        needed[k[1]].add(v)
        for k, v in fin:
            if k[0] == "e":
                needed[k[1]].add(v)
        rank = {}
        for k in self.eng:
            srt = sorted(needed[k])
            rank[k] = {v: i + 1 for i, v in enumerate(srt)}
            assert len(srt) < 30000, (k, len(srt))
        self.sem_counts = {k: len(rank[k]) for k in self.eng}

        def semval(k, v):
            return rank[k[1]][v] if k[0] == "e" else v

        with self.nc.Block() as block:
            def mk(engine):
                def body(e):
                    for waits, fn, di, idx in self.ops[engine]:
                        for k, v in waits:
                            e.wait_ge(self._semof(k), semval(k, v))
                        inst = fn(e)
                        if di is None:
                            if idx in rank[engine]:
                                inst.then_inc(self.sem[engine], 1)
                        else:
                            inst.then_inc(self.dsem[di], 16)
                    if engine == "sp":
                        for k, v in fin:
                            e.wait_ge(self._semof(k), semval(k, v))
                return body
            block.sync(mk("sp"))
            block.tensor(mk("pe"))
            block.vector(mk("dve"))
            block.scalar(mk("act"))
            block.gpsimd(mk("pool"))

    def mm(self, out, lhsT, rhs, start, stop, reads, writes, r=True):
        if r:
            lhsT = lhsT.bitcast(F32R)
            rhs = rhs.bitcast(F32R)
        self.op("pe", lambda e: e.matmul(out, lhsT, rhs, start=start, stop=stop), reads, writes)

    def transpose(self, out, in_, ident, reads, writes):
        self.op("pe", lambda e: e.transpose(out, in_, ident), reads, writes)

    def act(self, out, in_, func, reads, writes, bias=None, scale=None):
        kw = {}
        if bias is not None:
            kw["bias"] = bias
        if scale is not None:
            kw["scale"] = scale
        self.op("act", lambda e: e.activation(out=out, in_=in_, func=func, **kw), reads, writes)

    def tt(self, out, in0, in1, op, reads, writes, eng="dve"):
        self.op(eng, lambda e: e.tensor_tensor(out=out, in0=in0, in1=in1, op=op), reads, writes)

    def ts(self, out, in0, s1, op0, reads, writes, s2=None, op1=None, eng="dve"):
        if op1 is None:
            self.op(eng, lambda e: e.tensor_scalar(out=out, in0=in0, scalar1=s1, scalar2=None, op0=op0), reads, writes)
        else:
            self.op(eng, lambda e: e.tensor_scalar(out=out, in0=in0, scalar1=s1, scalar2=s2, op0=op0, op1=op1), reads, writes)

    def stt(self, out, in0, scalar, in1, op0, op1, reads, writes):
        self.op("dve", lambda e: e.scalar_tensor_tensor(out=out, in0=in0, scalar=scalar, in1=in1, op0=op0, op1=op1), reads, writes)

    def copy(self, out, in_, reads, writes, eng="dve"):
        if eng == "act":
            self.op("act", lambda e: e.copy(out=out, in_=in_), reads, writes)
        else:
            self.op(eng, lambda e: e.tensor_copy(out=out, in_=in_), reads, writes)

    def memset(self, ap, val, writes, eng="dve"):
        self.op(eng, lambda e: e.memset(ap, val), (), writes)


def make_consts(P):
    ident = P.sb("ident", [128, 128])
    ones = P.sb("ones", [128, 128])
    tmp = P.sb("iota_tmp", [128, 128])
    P.op("pool", lambda e: e.iota(tmp[:], pattern=[[1, 128]], base=0, channel_multiplier=-1,
                                  allow_small_or_imprecise_dtypes=True), (), [tmp])
    P.ts(ident[:], tmp[:], 0.0, ALU.is_equal, [tmp], [ident])
    P.ts(R(ones[:]), tmp[:], 0.0, ALU.mult, [tmp], [ones], s2=1.0, op1=ALU.add)
    return ident, ones


def load_cols(P, ident, dst, dst_ap, src_rows_ap, n):
    rows = P.sb("lc_rows", [n, 128])
    P.dma(rows[:], src_rows_ap, [], [rows])
    pb = P.bank()
    P.transpose(pb[:, 0:n], rows[:], ident[0:n, 0:n], [rows, ident], [pb])
    P.copy(dst_ap, pb[:, 0:n], [pb], [dst])


NT = 2112
NH = 1056
NTILE = 352
HALVES = [(0, [(0, 64, 1), (64, 1056, 0)]), (1056, [(0, 1056, 0)])]


def segs(half_idx, a, b):
    out = []
    for (c0, c1, w) in HALVES[half_idx][1]:
        lo, hi = max(a, c0), min(b, c1)
        if lo < hi:
            out.append((lo, hi, w))
    return out


class Ctx:
    pass


def emit_mod(P, C, mod_w_ap, mod_b_rows_ap, name, blocks=tuple(range(12))):
    modT = P.sb(name, [128, 48, 2])
    bcols = P.sb(name + "_b", [128, 48])
    load_cols(P, C.ident, bcols, bcols[:], mod_b_rows_ap, 48)
    for blk in blocks:
        wb = C.wbuf()
        wv = wb.t[:, 0:4096].rearrange("p (a b) -> p a b", b=512)
        P.dma(wv, mod_w_ap[:, blk * 512:(blk + 1) * 512].rearrange("(kc p) n -> p kc n", p=128), [], [wb], r=True)
        pb = P.bank()
        for j in range(4):
            for kc in range(8):
                P.mm(pb[:, 2 * j:2 * j + 2], wv[:, kc, j * 128:(j + 1) * 128], C.sT[:, kc, :], kc == 0, kc == 7,
                     [wb, C.sT], [pb], r=False)
        P.tt(modT[:, blk * 4:(blk + 1) * 4, :], pb[:, 0:8].rearrange("p (a b) -> p a b", b=2),
             bcols[:, blk * 4:(blk + 1) * 4].unsqueeze(2).to_broadcast([128, 4, 2]), ALU.add, [pb, bcols], [modT])
    return modT


def emit_scale_vec(P, C, modT, g_rows_ap, sc_chunk0, name):
    g = P.sb(name + "_g", [128, 8])
    load_cols(P, C.ident, g, g[:], g_rows_ap, 8)
    gs = P.sb(name, [128, 8, 2])
    P.ts(gs[:], modT[:, sc_chunk0:sc_chunk0 + 8, :], 1.0, ALU.add, [modT], [gs])
    P.tt(gs[:], gs[:], g[:].unsqueeze(2).to_broadcast([128, 8, 2]), ALU.mult, [gs, g], [gs])
    return gs


def emit_rstd(P, C, src, nch, n0, n1, dst, inv_d, eps):
    pb = P.bank()
    w = n1 - n0
    for c in range(nch):
        sq = C.sqbuf()
        P.act(R(sq[:, 0:w]), src[:, c, n0:n1], AF.Square, [src], [sq])
        P.mm(pb[:, 0:w], C.ones[:, :], sq[:, 0:w], c == 0, c == nch - 1, [C.ones, sq], [pb])
    P.act(dst[:, n0:n1], pb[:, 0:w], AF.Ln, [pb, C.epsb], [dst], bias=C.epsb[:, 0:1] if eps == EPS else C.epsb[:, 1:2], scale=inv_d)
    P.act(dst[:, n0:n1], dst[:, n0:n1], AF.Exp, [dst], [dst], scale=-0.5)


def emit_norm_mod(P, C, hi, x, dst, gs, modT, shift_chunk0):
    for ti in range(3):
        n0, n1 = ti * NTILE, (ti + 1) * NTILE
        emit_rstd(P, C, x, 8, n0, n1, C.rstd, 1.0 / D, EPS)
        for c in range(8):
            for (a, b, w) in segs(hi, n0, n1):
                if modT is not None:
                    P.stt(R(dst[:, c, a:b]), x[:, c, a:b], gs[:, c, w:w + 1], C.rstd[:, a:b], ALU.mult, ALU.mult,
                          [x, gs, C.rstd], [dst])
                    P.act(R(dst[:, c, a:b]), dst[:, c, a:b], AF.Identity, [dst, modT], [dst],
                          bias=modT[:, shift_chunk0 + c, w:w + 1])
                else:
                    P.stt(R(dst[:, c, a:b]), x[:, c, a:b], gs[:, c:c + 1], C.rstd[:, a:b], ALU.mult, ALU.mult,
                          [x, gs, C.rstd], [dst])


def emit_linear_add(P, C, hi, src, kchunks, w_ap, x, gate):
    for dc in range(8):
        wb = C.wbuf()
        wv = wb.t[:, 0:kchunks * 128].rearrange("p (a b) -> p a b", b=128)
        P.dma(wv, w_ap[:, dc * 128:(dc + 1) * 128].rearrange("(kc p) n -> p kc n", p=128), [], [wb], r=True)
        for ti in range(3):
            n0, n1 = ti * NTILE, (ti + 1) * NTILE
            pb = P.bank()
            for kc in range(kchunks):
                P.mm(pb[:, 0:NTILE], wv[:, kc, :], src[:, kc, n0:n1], kc == 0, kc == kchunks - 1, [wb, src], [pb])
            for (a, b, w) in segs(hi, n0, n1):
                P.stt(x[:, dc, a:b], pb[:, a - n0:b - n0], gate[:, dc, w:w + 1], x[:, dc, a:b], ALU.mult, ALU.add,
                      [pb, gate, x], [x])


def emit_ffn(P, C, hi, h2, x, gate2, w_in_ap, w_out_ap, F, gbc=None):
    FC = F // 128
    G = FC // 2
    act = C.big
    for gi in range(2):
        f0 = gi * G
        fl = 0
        while fl < G:
            nb = min(4, G - fl)
            wg = C.wbuf()
            wu = C.wbuf()
            wgv = wg.t[:, 0:8 * nb * 128].rearrange("p (a b) -> p a b", b=nb * 128)
            wuv = wu.t[:, 0:8 * nb * 128].rearrange("p (a b) -> p a b", b=nb * 128)
            c0 = (f0 + fl) * 128
            P.dma(wgv, w_in_ap[:, c0:c0 + nb * 128].rearrange("(kc p) n -> p kc n", p=128), [], [wg], r=True)
            P.dma(wuv, w_in_ap[:, F + c0:F + c0 + nb * 128].rearrange("(kc p) n -> p kc n", p=128), [], [wu], r=True)
            for j in range(nb):
                for ti in range(3):
                    n0, n1 = ti * NTILE, (ti + 1) * NTILE
                    pg = P.bank()
                    pu = P.bank()
                    for kc in range(8):
                        P.mm(pg[:, 0:NTILE], wgv[:, kc, j * 128:(j + 1) * 128], h2[:, kc, n0:n1], kc == 0, kc == 7, [wg, h2], [pg])
                    for kc in range(8):
                        P.mm(pu[:, 0:NTILE], wuv[:, kc, j * 128:(j + 1) * 128], h2[:, kc, n0:n1], kc == 0, kc == 7, [wu, h2], [pu])
                    sg = C.sgbuf()
                    P.act(sg[:, 0:NTILE], pg[:, 0:NTILE], AF.Silu, [pg], [sg])
                    P.tt(R(act[:, fl + j, n0:n1]), sg[:, 0:NTILE], pu[:, 0:NTILE], ALU.mult, [sg, pu], [act.sub(fl + j)])
            fl += nb
        for dc in range(8):
            wb = C.wbuf()
            wv = wb.t[:, 0:G * 128].rearrange("p (a b) -> p a b", b=128)
            P.dma(wv, w_out_ap[f0 * 128:(f0 + G) * 128, dc * 128:(dc + 1) * 128].rearrange("(kc p) n -> p kc n", p=128), [], [wb], r=True)
            for ti in range(3):
                n0, n1 = ti * NTILE, (ti + 1) * NTILE
                pb = P.bank()
                for k in range(G):
                    P.mm(pb[:, 0:NTILE], wv[:, k, :], act[:, k, n0:n1], k == 0, k == G - 1, [wb, act.sub(k)], [pb])
                for (a, b, w) in segs(hi, n0, n1):
                    if gbc is None:
                        P.stt(x[:, dc, a:b], pb[:, a - n0:b - n0], gate2[:, dc, w:w + 1], x[:, dc, a:b], ALU.mult, ALU.add,
                              [pb, gate2, x], [x])
                    else:
                        tmp = C.sgbuf()
                        P.tt(tmp[:, 0:b - a], pb[:, a - n0:b - n0], gbc[:, a:b], ALU.mult, [pb, gbc], [tmp])
                        P.stt(x[:, dc, a:b], tmp[:, 0:b - a], gate2[:, dc, w:w + 1], x[:, dc, a:b], ALU.mult, ALU.add,
                              [tmp, gate2, x], [x])


def emit_pool(P, C, hi, hp_ctx, hp_lat, inv_ap):
    h0 = HALVES[hi][0]
    inv = C.hb
    for g in range(4):
        P.dma(inv[:, g, :], inv_ap[g:g + 1, h0:h0 + NH].to_broadcast([128, NH]), [], [inv], r=True)
    for c in range(8):
        g = c // 2
        w = 2 << g
        for (a, b, isctx) in HALVES[hi][1]:
            n = b - a
            hh = C.poolbuf[0]
            if isctx:
                src = hp_ctx[c * 128:(c + 1) * 128, 0:n + 16]
            else:
                l0 = h0 + a - 64
                src = hp_lat[c * 128:(c + 1) * 128, l0:l0 + n + 16]
            P.dma(hh[:, 0:n + 16], src, [], [hh])
            cur = hh
            sh = 1
            k = 0
            while sh < w:
                nxt = C.poolbuf[1 + (k % 2)]
                lo = 2 * sh - 1
                P.tt(nxt[:, lo:n + 16], cur[:, lo:n + 16], cur[:, lo - sh:n + 16 - sh], ALU.add, [cur], [nxt])
                cur = nxt
                sh *= 2
                k += 1
            off = 8 + w // 2 - 1
            P.tt(R(C.big[:, c, a:b]), cur[:, off:off + n], inv[:, g, a:b], ALU.mult, [cur, inv], [C.big.sub(c)])
            P.tt(R(C.big[:, c, a:b]), C.big[:, c, a:b], hh[:, 8:8 + n], ALU.subtract, [C.big.sub(c), hh], [C.big.sub(c)])


def emit_moe_gates(P, C, hi, h2, router_ap):
    rw = P.sb("router", [128, 8, 8])
    P.dma(rw[:], router_ap.rearrange("(kc p) e -> p kc e", p=128), [], [rw])
    t0 = 0
    while t0 < NH:
        m = min(128, NH - t0)
        pb = P.bank()
        for kc in range(8):
            P.mm(pb[0:m, 0:8], h2[:, kc, t0:t0 + m], rw[:, kc, :], kc == 0, kc == 7, [h2, rw], [pb], r=False)
        lg = P.sb("lg", [128, 8])
        P.copy(lg[0:m, :], pb[0:m, 0:8], [pb], [lg])
        mx = P.sb("mx", [128, 8])
        P.op("dve", lambda e, mx=mx, lg=lg, m=m: e.max(out=mx[0:m, :], in_=lg[0:m, :]), [lg], [mx])
        dd = P.sb("dd", [128, 4])
        P.tt(dd[0:m, 0:1], mx[0:m, 1:2], mx[0:m, 0:1], ALU.subtract, [mx], [dd])
        P.act(dd[0:m, 1:2], dd[0:m, 0:1], AF.Exp, [dd], [dd])
        P.ts(dd[0:m, 1:2], dd[0:m, 1:2], 1.0, ALU.add, [dd], [dd])
        P.op("dve", lambda e, dd=dd, m=m: e.reciprocal(out=dd[0:m, 2:3], in_=dd[0:m, 1:2]), [dd], [dd])
        P.ts(dd[0:m, 3:4], dd[0:m, 2:3], -1.0, ALU.mult, [dd], [dd], s2=1.0, op1=ALU.add)
        g1 = P.sb("g1", [128, 8])
        g2 = P.sb("g2", [128, 8])
        P.ts(g1[0:m, :], lg[0:m, :], mx[0:m, 0:1], ALU.is_equal, [lg, mx, dd], [g1], s2=dd[0:m, 2:3], op1=ALU.mult)
        P.ts(g2[0:m, :], lg[0:m, :], mx[0:m, 1:2], ALU.is_equal, [lg, mx, dd], [g2], s2=dd[0:m, 3:4], op1=ALU.mult)
        P.tt(g1[0:m, :], g1[0:m, :], g2[0:m, :], ALU.add, [g1, g2], [g1])
        pt = P.bank()
        P.transpose(pt[0:8, 0:m], g1[0:m, :], C.ident[0:m, 0:m], [g1, C.ident], [pt])
        P.copy(C.gatesT[:, t0:t0 + m], pt[0:8, 0:m], [pt], [C.gatesT])
        t0 += m


def emit_gbc(P, C, e_idx):
    for ti in range(3):
        n0, n1 = ti * NTILE, (ti + 1) * NTILE
        pb = P.bank()
        P.mm(pb[:, 0:NTILE], C.sel[:, e_idx, :], C.gatesT[:, n0:n1], True, True, [C.sel, C.gatesT], [pb], r=False)
        P.copy(C.gbc[:, n0:n1], pb[:, 0:NTILE], [pb], [C.gbc], eng="act")


def build_ts(layer, post, ffn, nxt):
    nc = bass.Bass("TRN2", target_bir_lowering=False)
    with ExitStack() as stack:
        nc.dge_precook = False
        P = Prog(nc, stack)
        P.init_psum(8)
        C = Ctx()

        def din(name, shape):
            return nc.dram_tensor(name, list(shape), F32, kind="ExternalInput").ap()

        def dout(name, shape):
            return nc.dram_tensor(name, list(shape), F32, kind="ExternalOutput").ap()

        xT = din("xT", [D, NT])
        cvec = din("cvec", [16, 128])
        if post is not None or ffn is not None:
            mod_w = din("mod_w", [D, 6 * D])
            mod_b = din("mod_b", [48, 128])
            n2g = din("n2g", [8, 128])
        if post == "mamba":
            mT = din("mT", [2048, NT])
            mb_ng = din("mb_ng", [16, 128])
            w_post = din("w_post", [2048, D])
        elif post == "lin":
            mT = din("mT", [D, NT])
            w_post = din("w_post", [D, D])
        elif post == "pool":
            hp_ctx = din("hp_ctx", [D, 80])
            hp_lat = din("hp_lat", [D, 2048 + 16])
            inv_cnt = din("inv_cnt", [4, NT])
            pl_w = din("pl_w", [4, 256, 256])
            pl_scale = din("pl_scale", [8, 128])
        if ffn == "dense":
            F = 2816
            w_in = din("w_in", [D, 2 * F])
            w_out = din("w_out", [F, D])
        elif ffn == "moe":
            F = 3584
            router = din("router", [D, 8])
            w_in = din("w_in", [8, D, 2 * F])
            w_out = din("w_out", [8, F, D])
        if nxt == "norm":
            mod_w_n = din("mod_w_n", [D, 6 * D])
            mod_b_n = din("mod_b_n", [48, 128])
            n1g_n = din("n1g_n", [8, 128])
            xT_out = dout("xT_out", [D, NT])
            hT_out = dout("hT_out", [D, NT])
        else:
            fin_g = din("fin_g", [8, 128])
            out_T = dout("out_T", [D, NT])

        C.ident, C.ones = make_consts(P)
        wbufs = [P.sb(f"wb{i}", [128, 4096]) for i in range(3)]
        wi = [0]

        def wbuf():
            b = wbufs[wi[0] % 3]
            wi[0] += 1
            return b
        C.wbuf = wbuf
        sqs = [P.sb(f"sq{i}", [128, NTILE]) for i in range(3)]
        si = [0]

        def sqbuf():
            b = sqs[si[0] % 3]
            si[0] += 1
            return b
        C.sqbuf = sqbuf
        sgs = [P.sb(f"sg{i}", [128, NTILE]) for i in range(3)]
        gi_ = [0]

        def sgbuf():
            b = sgs[gi_[0] % 3]
            gi_[0] += 1
            return b
        C.sgbuf = sgbuf
        C.rstd = P.sb("rstd", [128, NH])
        C.epsb = P.sb("epsb", [128, 2])
        P.memset(C.epsb[:, 0:1], EPS, [C.epsb])
        P.memset(C.epsb[:, 1:2], EPS, [C.epsb])
        craw = P.sb("craw", [128, 16])
        load_cols(P, C.ident, craw, craw[:], cvec, 16)
        C.sT = P.sb("sT", [128, 8, 2])
        P.act(C.sT[:, :, 0], craw[:, 0:8], AF.Silu, [craw], [C.sT])
        P.act(C.sT[:, :, 1], craw[:, 8:16], AF.Silu, [craw], [C.sT])

        x = P.sb("x", [128, 8, NH])
        C.hb = P.sb("hb", [128, 8, NH])
        need_big = post is not None or ffn is not None
        if need_big:
            nbig = 16 if post == "mamba" else (14 if ffn == "moe" else 11)
            C.big = P.sb("big", [128, nbig, NH])
        if post == "pool":
            C.poolbuf = [P.sb(f"pb{i}", [128, NH + 16]) for i in range(3)]
        if ffn == "moe":
            C.gatesT = P.sb("gatesT", [8, NH])
            C.gbc = P.sb("gbc", [128, NH])
            C.sel = P.sb("sel", [8, 8, 128])
            P.memset(C.sel[:], 0.0, [C.sel])
            for e_ in range(8):
                P.ts(C.sel[:, e_, :], C.ones[0:8, :], C.ident[0:8, e_:e_ + 1], ALU.mult, [C.ones, C.ident, C.sel], [C.sel])

        if post is not None or ffn is not None:
            modT = emit_mod(P, C, mod_w, mod_b, "modT", blocks=tuple(range(4, 12)))
            gs2 = emit_scale_vec(P, C, modT, n2g, 32, "gs2")
            gate1 = P.sb("gate1", [128, 8, 2])
            P.copy(gate1[:], modT[:, 16:24, :], [modT], [gate1])
            gate2 = P.sb("gate2", [128, 8, 2])
            P.copy(gate2[:], modT[:, 40:48, :], [modT], [gate2])
            if post == "pool":
                psc = P.sb("psc", [128, 8])
                load_cols(P, C.ident, psc, psc[:], pl_scale, 8)
                P.tt(gate1[:], gate1[:], psc[:].unsqueeze(2).to_broadcast([128, 8, 2]), ALU.mult, [gate1, psc], [gate1])
            if post == "mamba":
                mng = P.sb("mng", [128, 16])
                load_cols(P, C.ident, mng, mng[:], mb_ng, 16)
        if nxt == "norm":
            modN = emit_mod(P, C, mod_w_n, mod_b_n, "modN", blocks=(0, 1, 2, 3))
            gs1n = emit_scale_vec(P, C, modN, n1g_n, 8, "gs1n")
        else:
            fg = P.sb("fg", [128, 8])
            load_cols(P, C.ident, fg, fg[:], fin_g, 8)

        for hi in range(2):
            h0 = HALVES[hi][0]
            for c in range(8):
                P.dma(x[:, c, :], xT[c * 128:(c + 1) * 128, h0:h0 + NH], [], [x])
            if post == "mamba":
                big = C.big
                for c in range(16):
                    P.dma(big[:, c, :], mT[c * 128:(c + 1) * 128, h0:h0 + NH], [], [big.sub(c)], r=True)
                allb = [big.sub(c) for c in range(16)]
                for ti in range(3):
                    n0, n1 = ti * NTILE, (ti + 1) * NTILE
                    pb = P.bank()
                    for c in range(16):
                        sq = C.sqbuf()
                        P.act(R(sq[:, 0:NTILE]), big[:, c, n0:n1], AF.Square, [big.sub(c)], [sq])
                        P.mm(pb[:, 0:NTILE], C.ones[:, :], sq[:, 0:NTILE], c == 0, c == 15, [C.ones, sq], [pb])
                    P.act(C.rstd[:, n0:n1], pb[:, 0:NTILE], AF.Ln, [pb, C.epsb], [C.rstd], bias=C.epsb[:, 0:1], scale=1.0 / 2048)
                    P.act(C.rstd[:, n0:n1], C.rstd[:, n0:n1], AF.Exp, [C.rstd], [C.rstd], scale=-0.5)
                    for c in range(16):
                        P.stt(R(big[:, c, n0:n1]), big[:, c, n0:n1], mng[:, c:c + 1], C.rstd[:, n0:n1], ALU.mult, ALU.mult,
                              [big.sub(c), mng, C.rstd], [big.sub(c)])
                emit_linear_add(P, C, hi, _SubView(big, allb), 16, w_post, x, gate1)
            elif post == "lin":
                big = C.big
                for c in range(8):
                    P.dma(big[:, c, :], mT[c * 128:(c + 1) * 128, h0:h0 + NH], [], [big.sub(c)], r=True)
                emit_linear_add(P, C, hi, _SubView(big, [big.sub(c) for c in range(8)]), 8, w_post, x, gate1)
            elif post == "pool":
                emit_pool(P, C, hi, hp_ctx, hp_lat, inv_cnt)
                big = C.big
                for dc in range(8):
                    g, j = dc // 2, dc % 2
                    wb = C.wbuf()
                    wv = wb.t[:, 0:256].rearrange("p (a b) -> p a b", b=128)
                    P.dma(wv, pl_w[g, :, j * 128:(j + 1) * 128].rearrange("(kc p) n -> p kc n", p=128), [], [wb], r=True)
                    for ti in range(3):
                        n0, n1 = ti * NTILE, (ti + 1) * NTILE
                        pb = P.bank()
                        for kc in range(2):
                            P.mm(pb[:, 0:NTILE], wv[:, kc, :], big[:, 2 * g + kc, n0:n1], kc == 0, kc == 1,
                                 [wb, big.sub(2 * g + kc)], [pb])
                        for (a, b, w) in segs(hi, n0, n1):
                            P.stt(x[:, dc, a:b], pb[:, a - n0:b - n0], gate1[:, dc, w:w + 1], x[:, dc, a:b], ALU.mult, ALU.add,
                                  [pb, gate1, x], [x])
            if ffn is not None:
                h2 = C.hb
                emit_norm_mod(P, C, hi, x, h2, gs2, modT, 24)
                if ffn == "dense":
                    emit_ffn(P, C, hi, h2, x, gate2, w_in, w_out, F)
                else:
                    emit_moe_gates(P, C, hi, h2, router)
                    for e_ in range(8):
                        emit_gbc(P, C, e_)
                        emit_ffn(P, C, hi, h2, x, gate2, w_in[e_], w_out[e_], F, gbc=C.gbc)
            if nxt == "norm":
                for c in range(8):
                    P.dma(xT_out[c * 128:(c + 1) * 128, h0:h0 + NH], x[:, c, :], [x], [])
                emit_norm_mod(P, C, hi, x, C.hb, gs1n, modN, 0)
                for c in range(8):
                    P.dma(hT_out[c * 128:(c + 1) * 128, h0:h0 + NH], C.hb[:, c, :], [C.hb], [])
            else:
                emit_norm_mod(P, C, hi, x, C.hb, fg, None, 0)
                for c in range(8):
                    P.dma(out_T[c * 128:(c + 1) * 128, h0:h0 + NH], C.hb[:, c, :], [C.hb], [])
        P.emit()
    return nc


def rows128(v):
    return np.ascontiguousarray(np.asarray(v, np.float32).reshape(-1, 128))


def to_cores_T(lat, ctx):
    outs = []
    for core in range(NCORES):
        b, q = core // 4, core % 4
        a = np.concatenate([ctx[b, q * 64:(q + 1) * 64], lat[b, q * 2048:(q + 1) * 2048]], axis=0)
        outs.append(np.ascontiguousarray(a.T))
    return outs


def from_cores_T(arrs):
    Cc = arrs[0].shape[0]
    lat = np.empty((2, 8192, Cc), np.float32)
    ctx = np.empty((2, 256, Cc), np.float32)
    for core in range(NCORES):
        b, q = core // 4, core % 4
        a = arrs[core].T
        ctx[b, q * 64:(q + 1) * 64] = a[0:64]
        lat[b, q * 2048:(q + 1) * 2048] = a[64:]
    return lat, ctx


def cvec_for(c, c_ctx, core):
    b = core // 4
    return np.ascontiguousarray(np.concatenate([np.asarray(c[b], np.float32).reshape(8, 128),
                                                np.asarray(c_ctx, np.float32).reshape(8, 128)], axis=0))


_NC_CACHE = {}


def get_ts(layer, post, ffn, nxt):
    key = ("ts", post, ffn, nxt)
    if key not in _NC_CACHE:
        _NC_CACHE[key] = build_ts(layer, post, ffn, nxt)
    return _NC_CACHE[key]


def run(nc, in_maps):
    res = run_bass_kernel_spmd(nc, in_maps, core_ids=list(range(NCORES)))
    return res.results


def pool_inputs(h_lat, h_ctx):
    outs = []
    for core in range(NCORES):
        b, q = core // 4, core % 4
        lat = np.zeros((2048 + 16, D), np.float32)
        lo, hi = q * 2048 - 8, (q + 1) * 2048 + 8
        s0, s1 = max(lo, 0), min(hi, 8192)
        lat[s0 - lo:s1 - lo] = h_lat[b, s0:s1]
        cx = np.zeros((64 + 16, D), np.float32)
        lo, hi = q * 64 - 8, (q + 1) * 64 + 8
        s0, s1 = max(lo, 0), min(hi, 256)
        cx[s0 - lo:s1 - lo] = h_ctx[b, s0:s1]
        inv = np.empty((4, NT), np.float32)
        for g, w in enumerate((2, 4, 8, 16)):
            t = np.arange(q * 64, (q + 1) * 64)
            inv[g, 0:64] = 1.0 / (np.minimum(t + w // 2, 256) - np.maximum(t - w // 2, 0))
            t = np.arange(q * 2048, (q + 1) * 2048)
            inv[g, 64:] = 1.0 / (np.minimum(t + w // 2, 8192) - np.maximum(t - w // 2, 0))
        outs.append({"hp_ctx": np.ascontiguousarray(cx.T), "hp_lat": np.ascontiguousarray(lat.T), "inv_cnt": inv})
    return outs


NKEY = 8448
NQ = 8192


def build_attn():
    nc = bass.Bass("TRN2", target_bir_lowering=False)
    with ExitStack() as stack:
        nc.dge_precook = False
        P = Prog(nc, stack)
        P.init_psum(3)
        oacc = P.ps("oacc", [128, 512])
        sgrp = [P.ps(f"sgrp{i}", [128, 1024]) for i in range(2)]

        def din(name, shape):
            return nc.dram_tensor(name, list(shape), F32, kind="ExternalInput").ap()

        hT = din("hT", [D, NKEY])
        wq = din("wq", [D, 256])
        wk = din("wk", [D, 64])
        wv = din("wv", [D, 64])
        qg = din("qg", [64, 1])
        kg = din("kg", [64, 1])
        cosT = din("cosT", [64, NKEY])
        sinS = din("sinS", [64, NKEY])
        prot = din("prot", [64, 64])
        oT = nc.dram_tensor("oT", [256, NQ], F32, kind="ExternalOutput").ap()

        ident, ones = make_consts(P)
        wq_s = P.sb("wq_s", [128, 8, 256])
        wk_s = P.sb("wk_s", [128, 8, 64])
        wv_s = P.sb("wv_s", [128, 8, 64])
        P.dma(wq_s[:], wq.rearrange("(kc p) n -> p kc n", p=128), [], [wq_s], r=True)
        P.dma(wk_s[:], wk.rearrange("(kc p) n -> p kc n", p=128), [], [wk_s], r=True)
        P.dma(wv_s[:], wv.rearrange("(kc p) n -> p kc n", p=128), [], [wv_s], r=True)
        qg_s = P.sb("qg_s", [64, 1])
        kg_s = P.sb("kg_s", [64, 1])
        P.dma(qg_s[:], qg, [], [qg_s])
        P.dma(kg_s[:], kg, [], [kg_s])
        prot_s = P.sb("prot_s", [64, 64])
        P.dma(prot_s[:], prot, [], [prot_s], r=True)
        epsb = P.sb("epsb", [128, 1])
        P.memset(epsb[:], EPS, [epsb])
        KT = P.sb("KT", [64, NKEY])
        Vx = P.sb("Vx", [128, 66, 65])
        P.ts(R(Vx[:, :, 64:65]), Vx[:, :, 64:65], 0.0, ALU.mult, [], [Vx], s2=1.0, op1=ALU.add)
        hts = [P.sb(f"ht{i}", [128, 8, 512]) for i in range(2)]
        cst = [P.sb(f"cs{i}", [64, 512]) for i in range(2)]
        snt = [P.sb(f"sn{i}", [64, 512]) for i in range(2)]
        QTs = [P.sb(f"QT{i}", [64, 4, 512]) for i in range(2)]
        pts = [P.sb(f"pt{i}", [128, 1024]) for i in range(3)]
        sqb = [P.sb(f"sqb{i}", [64, 512]) for i in range(2)]
        rsb = [P.sb(f"rsb{i}", [64, 512]) for i in range(2)]
        qnb = [P.sb(f"qnb{i}", [64, 512]) for i in range(2)]
        t1b = [P.sb(f"t1b{i}", [64, 512]) for i in range(2)]
        t2b = [P.sb(f"t2b{i}", [64, 512]) for i in range(2)]
        obuf = [P.sb(f"ob{i}", [64, 4, 512]) for i in range(2)]
        lrow = P.sb("lrow", [128, 512])
        bcs = P.sb("bcs", [64, 512])
        cnt = [0]

        def normrope(src_ps, w, g_s, cs, sn, dst_ap, dst_tt):
            i = cnt[0] % 2
            cnt[0] += 1
            sq, rs, qn, t1, t2 = sqb[i], rsb[i], qnb[i], t1b[i], t2b[i]
            P.act(R(sq[:, 0:w]), src_ps, AF.Square, [src_tt[0]], [sq])
            pb = P.bank()
            P.mm(pb[0:64, 0:w], ones[0:64, 0:64], sq[:, 0:w], True, True, [ones, sq], [pb])
            P.act(rs[:, 0:w], pb[0:64, 0:w], AF.Ln, [pb, epsb], [rs], bias=epsb[0:64, 0:1], scale=1.0 / 64)
            P.act(rs[:, 0:w], rs[:, 0:w], AF.Exp, [rs], [rs], scale=-0.5)
            P.stt(R(qn[:, 0:w]), src_ps, g_s[:, 0:1], rs[:, 0:w], ALU.mult, ALU.mult, [src_tt[0], g_s, rs], [qn])
            pr = P.bank()
            P.mm(pr[0:64, 0:w], prot_s[:, :], qn[:, 0:w], True, True, [prot_s, qn], [pr])
            P.tt(t1[:, 0:w], qn[:, 0:w], cs, ALU.mult, [qn, cs_tt[0]], [t1])
            P.tt(t2[:, 0:w], pr[0:64, 0:w], sn, ALU.mult, [pr, sn_tt[0]], [t2])
            P.tt(R(dst_ap), t1[:, 0:w], t2[:, 0:w], ALU.add, [t1, t2], [dst_tt])

        src_tt = [None]
        cs_tt = [None]
        sn_tt = [None]

        ntile_k = [(i * 512, 512) for i in range(16)] + [(8192, 256)]
        for ti, (c0, w) in enumerate(ntile_k):
            ht = hts[ti % 2]
            cs = cst[ti % 2]
            sn = snt[ti % 2]
            for kc in range(8):
                P.dma(ht[:, kc, 0:w], hT[kc * 128:(kc + 1) * 128, c0:c0 + w], [], [ht], r=True)
            P.dma(cs[:, 0:w], cosT[:, c0:c0 + w], [], [cs])
            P.dma(sn[:, 0:w], sinS[:, c0:c0 + w], [], [sn])
            pk = P.bank()
            for kc in range(8):
                P.mm(pk[0:64, 0:w], wk_s[:, kc, :], ht[:, kc, 0:w], kc == 0, kc == 7, [wk_s, ht], [pk])
            src_tt[0], cs_tt[0], sn_tt[0] = pk, cs, sn
            normrope(pk[0:64, 0:w], w, kg_s, cs[:, 0:w], sn[:, 0:w], KT[:, c0:c0 + w], KT.sub(ti))
            for j in range(w // 128):
                ch = c0 // 128 + j
                pv = P.bank()
                for kc in range(8):
                    P.mm(pv[:, 0:64], ht[:, kc, j * 128:(j + 1) * 128], wv_s[:, kc, :], kc == 0, kc == 7, [ht, wv_s], [pv])
                P.copy(R(Vx[:, ch, 0:64]), pv[:, 0:64], [pv], [Vx.sub(ch)], eng="act")
        KTall = [KT.sub(ti) for ti in range(len(ntile_k))]

        for ti in range(16):
            c0 = 256 + ti * 512
            ht = hts[ti % 2]
            cs = cst[ti % 2]
            sn = snt[ti % 2]
            QT = QTs[ti % 2]
            ob = obuf[ti % 2]
            for kc in range(8):
                P.dma(ht[:, kc, :], hT[kc * 128:(kc + 1) * 128, c0:c0 + 512], [], [ht], r=True)
            P.dma(cs[:, :], cosT[:, c0:c0 + 512], [], [cs])
            P.dma(sn[:, :], sinS[:, c0:c0 + 512], [], [sn])
            for hq in range(4):
                pq = P.bank()
                for kc in range(8):
                    P.mm(pq[0:64, :], wq_s[:, kc, hq * 64:(hq + 1) * 64], ht[:, kc, :], kc == 0, kc == 7, [wq_s, ht], [pq])
                src_tt[0], cs_tt[0], sn_tt[0] = pq, cs, sn
                normrope(pq[0:64, :], 512, qg_s, cs[:, :], sn[:, :], QT[:, hq, :], QT)
            for qb in range(4):
                rhs_q = QT[:, :, qb * 128:(qb + 1) * 128]
                for st_ in range(33 + 1):
                    if st_ < 33:
                        sg = sgrp[st_ % 2]
                        for u_ in range(2):
                            ch = 2 * st_ + u_
                            P.mm(sg[:, u_ * 512:(u_ + 1) * 512].rearrange("p (h q) -> p h q", h=4), KT[:, ch * 128:(ch + 1) * 128], rhs_q, True, True,
                                 [KT.sub(ch // 4), QT], [sg])
                        pt = pts[st_ % 3]
                        P.act(R(pt[:, :]), sg[:, :], AF.Exp, [sg], [pt], scale=0.125)
                    if st_ >= 1:
                        pp = st_ - 1
                        pt = pts[pp % 3]
                        for u_ in range(2):
                            ch = 2 * pp + u_
                            P.mm(oacc[0:65, :], Vx[:, ch, :], pt[:, u_ * 512:(u_ + 1) * 512], ch == 0, ch == 65, [Vx.sub(ch), pt], [oacc])
                P.copy(lrow[64:65, :], oacc[64:65, :], [oacc], [lrow], eng="act")
                P.op("dve", lambda e: e.reciprocal(out=lrow[64:65, :], in_=lrow[64:65, :]), [lrow], [lrow])
                bcp = P.bank()
                P.mm(bcp[0:64, :], ones[64:65, 0:64], lrow[64:65, :], True, True, [ones, lrow], [bcp], r=False)
                P.copy(bcs[:, :], bcp[0:64, :], [bcp], [bcs], eng="act")
                P.tt(ob[:, :, qb * 128:(qb + 1) * 128], oacc[0:64, :].rearrange("p (h q) -> p h q", h=4),
                     bcs[:, :].rearrange("p (h q) -> p h q", h=4), ALU.mult, [oacc, bcs], [ob])
            q0 = ti * 512
            for hq in range(4):
                P.dma(oT[hq * 64:(hq + 1) * 64, q0:q0 + 512], ob[:, hq, :], [ob], [])
        P.emit()
    return nc


def rope_tables():
    quarter = 16
    inv = (10000.0 ** (-np.arange(quarter, dtype=np.float32) / quarter)).astype(np.float32)
    t = np.arange(8192)
    rows = (t // 64).astype(np.float32)
    cols = (t % 64).astype(np.float32)
    cosT = np.ones((64, NKEY), np.float32)
    sinS = np.zeros((64, NKEY), np.float32)
    prot = np.zeros((64, 64), np.float32)
    for i in range(64):
        half, within = i // 32, i % 32
        j, first = within % 16, within < 16
        pos = rows if half == 0 else cols
        ang = (pos * inv[j]).astype(np.float32)
        cosT[i, 256:] = np.cos(ang)
        sinS[i, 256:] = -np.sin(ang) if first else np.sin(ang)
        prot[i + 16 if first else i - 16, i] = 1.0
    return cosT, sinS, prot


def build_mamba(debug=False):
    nc = bass.Bass("TRN2", target_bir_lowering=False)
    with ExitStack() as stack:
        nc.dge_precook = False
        P = Prog(nc, stack)
        P.debug = debug
        P.init_psum(6)
        pybanks = [P.ps("pyb0", [128, 512]), P.ps("pyb1", [128, 512])]
        pyi = [0]

        def din(name, shape):
            return nc.dram_tensor(name, list(shape), F32, kind="ExternalInput").ap()

        hT = din("hT", [D, NKEY])
        wz = din("wz", [D, 512])
        wxbc = din("wxbc", [D, 768])
        wdt = din("wdt", [D, 16])
        cw = din("cw", [128, 30])
        cb = din("cb", [128, 6])
        dtb = din("dtb", [1, 16])
        alog = din("alog", [1, 16])
        dsk = din("dsk", [128, 4])
        uT = nc.dram_tensor("uT", [512, NKEY], F32, kind="ExternalOutput").ap()
        ybs = nc.dram_tensor("ybs", [512, NKEY], F32, kind="Internal").ap()
        ybs_t = TT(ybs, "ybs")

        ident, ones = make_consts(P)
        val = P.sb("val", [128, 128])
        P.op("pool", lambda e: e.iota(val[:], pattern=[[1, 128]], base=0, channel_multiplier=-1,
                                      allow_small_or_imprecise_dtypes=True), (), [val])
        Uf = P.sb("Uf", [128, 128]); Tf = P.sb("Tf", [128, 128]); Ub = P.sb("Ub", [128, 128]); Tb = P.sb("Tb", [128, 128])
        P.ts(Uf[:], val[:], 0.0, ALU.is_lt, [val], [Uf])
        P.ts(Tf[:], val[:], 0.0, ALU.is_ge, [val], [Tf])
        P.ts(Ub[:], val[:], 0.0, ALU.is_gt, [val], [Ub])
        P.ts(Tb[:], val[:], 0.0, ALU.is_le, [val], [Tb])
        UU = [Uf, Ub]
        TTm = [Tf, Tb]

        wz_s = P.sb("wz_s", [128, 8, 512])
        wx_s = P.sb("wx_s", [128, 8, 768])
        wd_s = P.sb("wd_s", [128, 8, 16])
        P.dma(wz_s[:], wz.rearrange("(kc p) n -> p kc n", p=128), [], [wz_s], r=True)
        P.dma(wx_s[:], wxbc.rearrange("(kc p) n -> p kc n", p=128), [], [wx_s], r=True)
        P.dma(wd_s[:], wdt.rearrange("(kc p) n -> p kc n", p=128), [], [wd_s], r=True)
        cw_s = P.sb("cw_s", [128, 30]); cb_s = P.sb("cb_s", [128, 6]); dsk_s = P.sb("dsk_s", [128, 4])
        P.dma(cw_s[:], cw, [], [cw_s]); P.dma(cb_s[:], cb, [], [cb_s]); P.dma(dsk_s[:], dsk, [], [dsk_s])
        dtb_s = P.sb("dtb_s", [128, 16]); aneg = P.sb("aneg", [128, 16])
        P.dma(dtb_s[:], dtb.to_broadcast([128, 16]), [], [dtb_s])
        P.dma(aneg[:], alog.to_broadcast([128, 16]), [], [aneg])
        P.act(aneg[:], aneg[:], AF.Exp, [aneg], [aneg])
        P.ts(aneg[:], aneg[:], -1.0, ALU.mult, [aneg], [aneg])
        oneb = P.sb("oneb", [128, 1])
        P.memset(oneb[:], 1.0, [oneb])

        W = 256
        ht = [P.sb(f"ht{i}", [128, 8, W + 4]) for i in range(2)]
        raw = P.sb("raw", [128, 6, W + 4])
        acc = [P.sb(f"acc{i}", [128, W]) for i in range(2)]
        xTc = P.sb("xTc", [128, 4, W])
        BT = P.sb("BT", [128, W]); CT = P.sb("CT", [128, W])
        x_tok = P.sb("x_tok", [128, 2, 512]); B_tok = P.sb("B_tok", [128, 2, 128]); dt_tok = P.sb("dt_tok", [128, 2, 16])
        zs = P.sb("zs", [128, 4, W])
        ST = [P.sb(f"ST{d}", [128, 512]) for d in range(2)]
        for d_ in range(2):
            P.memset(ST[d_][:], 0.0, [ST[d_]])
        sm = [P.sb(f"sm{i}", [128, 16]) for i in range(8)]
        GM = P.sb("GM", [128, 128])
        lD = [P.sb(f"lD{i}", [128, 128]) for i in range(8)]
        Lx = [P.sb(f"Lx{i}", [128, 128]) for i in range(8)]
        WT = [P.sb(f"WT{i}", [128, 128]) for i in range(8)]
        A1 = [P.sb(f"A1{i}", [128, 128]) for i in range(8)]
        Ec = [P.sb(f"Ec{i}", [128, 128]) for i in range(8)]
        Cd = [P.sb(f"Cd{i}", [128, 128]) for i in range(8)]
        xdt = P.sb("xdt", [128, 512])
        ybuf = P.sb("ybuf", [128, 4, 128])
        ubuf = [P.sb(f"ubuf{i}", [128, 4, 128]) for i in range(2)]

        def stage_a(ti, c0, q0, q1, fwd):
            h = ht[ti % 2]
            lo, hi = max(c0 - 2, q0), min(c0 + W + 2, q1)
            off = lo - (c0 - 2)
            n = hi - lo
            for kc in range(8):
                P.dma(h[:, kc, off:off + n], hT[kc * 128:(kc + 1) * 128, lo:hi], [], [h], r=True)
            for cc in range(6):
                pb = P.bank()
                for kc in range(8):
                    P.mm(pb[:, 0:n], wx_s[:, kc, cc * 128:(cc + 1) * 128], h[:, kc, off:off + n], kc == 0, kc == 7, [wx_s, h], [pb])
                if off > 0:
                    P.memset(raw[:, cc, 0:off], 0.0, [raw.sub(cc)])
                if off + n < W + 4:
                    P.memset(raw[:, cc, off + n:W + 4], 0.0, [raw.sub(cc)])
                P.copy(raw[:, cc, off:off + n], pb[:, 0:n], [pb], [raw.sub(cc)], eng="act")
                a_ = acc[cc % 2]
                P.ts(a_[:, :], raw[:, cc, 0:W], cw_s[:, cc * 5:cc * 5 + 1], ALU.mult, [raw.sub(cc), cw_s], [a_])
                for k in range(1, 5):
                    P.stt(a_[:, :], raw[:, cc, k:k + W], cw_s[:, cc * 5 + k:cc * 5 + k + 1], a_[:, :], ALU.mult, ALU.add,
                          [raw.sub(cc), cw_s, a_], [a_])
                if cc < 4:
                    P.act(xTc[:, cc, :], a_[:, :], AF.Silu, [a_, cb_s], [xTc.sub(cc)], bias=cb_s[:, cc:cc + 1])
                elif cc == 4:
                    P.act(BT[:, :], a_[:, :], AF.Silu, [a_, cb_s], [BT], bias=cb_s[:, cc:cc + 1])
                else:
                    P.act(CT[:, :], a_[:, :], AF.Silu, [a_, cb_s], [CT], bias=cb_s[:, cc:cc + 1])
            for j in range(2):
                pt = P.bank()
                for cc in range(4):
                    P.transpose(pt[:, cc * 128:(cc + 1) * 128], xTc[:, cc, j * 128:(j + 1) * 128], ident[:, :], [xTc.sub(cc), ident], [pt])
                P.copy(x_tok[:, j, :], pt[:, :], [pt], [x_tok.sub(j)], eng="act")
                pb = P.bank()
                P.transpose(pb[:, 0:128], BT[:, j * 128:(j + 1) * 128], ident[:, :], [BT, ident], [pb])
                P.copy(R(B_tok[:, j, :]), pb[:, 0:128], [pb], [B_tok.sub(j)])
                pd = P.bank()
                for kc in range(8):
                    P.mm(pd[:, 0:16], h[:, kc, 2 + j * 128:2 + (j + 1) * 128], wd_s[:, kc, :], kc == 0, kc == 7, [h, wd_s], [pd], r=False)
                xx, ax, ee, rr = sm[0], sm[1], sm[2], sm[3]
                P.tt(xx[:, :], pd[:, 0:16], dtb_s[:, :], ALU.add, [pd, dtb_s], [xx])
                P.stt(ax[:, :], xx[:, :], -1.0, xx[:, :], ALU.mult, ALU.max, [xx], [ax])
                P.act(ee[:, :], ax[:, :], AF.Exp, [ax], [ee], scale=-1.0)
                P.act(ee[:, :], ee[:, :], AF.Ln, [ee, oneb], [ee], bias=oneb[:, 0:1])
                P.ts(rr[:, :], xx[:, :], 0.0, ALU.max, [xx], [rr])
                P.tt(dt_tok[:, j, :], rr[:, :], ee[:, :], ALU.add, [rr, ee], [dt_tok.sub(j)])
            if fwd:
                P.dbg("d_xTc", xTc[:, :, :], [128, 4, W], [xTc])
                P.dbg("d_BT", BT[:, :], [128, W], [BT])
                P.dbg("d_CT", CT[:, :], [128, W], [CT])
                P.dbg("d_xtok", x_tok[:, :, :], [128, 2, 512], [x_tok])
                P.dbg("d_Btok", B_tok[:, :, :], [128, 2, 128], [B_tok])
                P.dbg("d_dt", dt_tok[:, :, :], [128, 2, 16], [dt_tok])
                P.dbg("d_raw", raw[:, :, :], [128, 6, W + 4], [raw])
            if fwd:
                for cc in range(4):
                    pz = P.bank()
                    for kc in range(8):
                        P.mm(pz[:, 0:W], wz_s[:, kc, cc * 128:(cc + 1) * 128], h[:, kc, 2:2 + W], kc == 0, kc == 7, [wz_s, h], [pz])
                    P.act(zs[:, cc, :], pz[:, 0:W], AF.Silu, [pz], [zs.sub(cc)])

        def chunk_step(d, j, col0, ui):
            S = ST[d]
            dts = dt_tok[:, j, d * 8:(d + 1) * 8]
            a_s, cum_s, w_s, et_s = sm[4], sm[5], sm[6], sm[7]
            P.tt(a_s[:, 0:8], dts, aneg[:, d * 8:(d + 1) * 8], ALU.mult, [dt_tok.sub(j), aneg], [a_s])
            pc = P.bank()
            P.mm(pc[:, 0:8], TTm[d][:, :], a_s[:, 0:8], True, True, [TTm[d], a_s], [pc], r=False)
            P.mm(pc[:, 8:16], ones[:, :], a_s[:, 0:8], True, True, [ones, a_s], [pc], r=False)
            P.copy(cum_s[:, 0:16], pc[:, 0:16], [pc], [cum_s])
            P.tt(w_s[:, 0:8], cum_s[:, 8:16], cum_s[:, 0:8], ALU.subtract, [cum_s], [w_s])
            P.act(w_s[:, 0:8], w_s[:, 0:8], AF.Exp, [w_s], [w_s])
            P.tt(w_s[:, 0:8], w_s[:, 0:8], dts, ALU.mult, [w_s, dt_tok.sub(j)], [w_s])
            P.act(et_s[:, 0:8], cum_s[:, 8:16], AF.Exp, [cum_s], [et_s])
            pg = P.bank()
            P.mm(pg[:, 0:128], BT[:, j * 128:(j + 1) * 128], CT[:, j * 128:(j + 1) * 128], True, True, [BT, CT], [pg], r=False)
            P.tt(GM[:, :], pg[:, 0:128], TTm[d][:, :], ALU.mult, [pg, TTm[d]], [GM])
            py = pybanks[pyi[0] % 2]
            pyi[0] += 1
            for e_ in range(8):
                P.ts(lD[e_][:, :], UU[d][:, :], a_s[:, e_:e_ + 1], ALU.mult, [UU[d], a_s], [lD[e_]])
                P.ts(A1[e_][:, :], ones[:, :], a_s[:, e_:e_ + 1], ALU.mult, [ones, a_s], [A1[e_]])
            pDE = []
            for e_ in range(8):
                if e_ % 2 == 0:
                    pb_ = P.bank()
                o_ = (e_ % 2) * 256
                P.mm(pb_[:, o_:o_ + 128], lD[e_][:, :], TTm[d][:, :], True, True, [lD[e_], TTm[d]], [pb_], r=False)
                P.mm(pb_[:, o_ + 128:o_ + 256], A1[e_][:, :], TTm[d][:, :], True, True, [A1[e_], TTm[d]], [pb_], r=False)
                pDE.append((pb_, o_))
            for e_ in range(8):
                pb_, o_ = pDE[e_]
                P.act(Lx[e_][:, :], pb_[:, o_:o_ + 128], AF.Exp, [pb_], [Lx[e_]])
                P.act(Ec[e_][:, :], pb_[:, o_ + 128:o_ + 256], AF.Exp, [pb_], [Ec[e_]])
            for e_ in range(8):
                P.stt(WT[e_][:, :], Lx[e_][:, :], dts[:, e_:e_ + 1], GM[:, :], ALU.mult, ALU.mult, [Lx[e_], dt_tok.sub(j), GM], [WT[e_]])
                P.tt(Cd[e_][:, :], CT[:, j * 128:(j + 1) * 128], Ec[e_][:, :], ALU.mult, [CT, Ec[e_]], [Cd[e_]])
            for e_ in range(8):
                p0 = (e_ % 2) * 64
                cc = e_ // 2
                P.mm(py[p0:p0 + 64, cc * 128:(cc + 1) * 128], x_tok[:, j, e_ * 64:(e_ + 1) * 64], WT[e_][:, :], True, False,
                     [x_tok.sub(j), WT[e_]], [py], r=False)
                P.mm(py[p0:p0 + 64, cc * 128:(cc + 1) * 128], S[:, e_ * 64:(e_ + 1) * 64], Cd[e_][:, :], False, True,
                     [S, Cd[e_]], [py], r=False)
            P.tt(R(xdt[:, :].rearrange("p (e q) -> p e q", e=8)), x_tok[:, j, :].rearrange("p (e q) -> p e q", e=8),
                 w_s[:, 0:8].unsqueeze(2).to_broadcast([128, 8, 64]), ALU.mult, [x_tok.sub(j), w_s], [xdt])
            pu = P.bank()
            P.mm(pu[:, :], B_tok[:, j, :], xdt[:, :], True, True, [B_tok.sub(j), xdt], [pu])
            P.tt(S[:, :].rearrange("p (e q) -> p e q", e=8), S[:, :].rearrange("p (e q) -> p e q", e=8),
                 et_s[:, 0:8].unsqueeze(2).to_broadcast([128, 8, 64]), ALU.mult, [S, et_s], [S])
            P.tt(S[:, :], S[:, :], pu[:, :], ALU.add, [S, pu], [S])
            if d == 0:
                P.dbg("d_GM", GM[:, :], [128, 128], [GM])
                P.dbg("d_WT", WT[7][:, :], [128, 128], [WT[7]])
                P.dbg("d_Lx", Lx[7][:, :], [128, 128], [Lx[7]])
                P.dbg("d_Cd", Cd[7][:, :], [128, 128], [Cd[7]])
                P.dbg("d_cum", cum_s[:, :], [128, 16], [cum_s])
                P.dbg("d_w", w_s[:, :], [128, 16], [w_s])
                P.dbg("d_S", S[:, :], [128, 512], [S])
            pyv = py[:, :].rearrange("p (c t) -> p c t", c=4)
            if d == 1:
                P.copy(ybuf[:, :, :], pyv, [py], [ybuf])
                P.dma(ybs[:, col0:col0 + 128].rearrange("(c p) t -> p c t", p=128), ybuf[:, :, :], [ybuf], [ybs_t])
            else:
                ub = ubuf[ui % 2]
                P.dma(ybuf[:, :, :], ybs[:, col0:col0 + 128].rearrange("(c p) t -> p c t", p=128), [ybs_t], [ybuf])
                P.tt(ybuf[:, :, :], ybuf[:, :, :], pyv, ALU.add, [ybuf, py], [ybuf])
                for cc in range(4):
                    P.stt(ub[:, cc, :], xTc[:, cc, j * 128:(j + 1) * 128], dsk_s[:, cc:cc + 1], ybuf[:, cc, :], ALU.mult, ALU.add,
                          [xTc.sub(cc), dsk_s, ybuf], [ub])
                P.dbg("d_ysum", ybuf[:, :, :], [128, 4, 128], [ybuf])
                P.dbg("d_zs", zs[:, :, :], [128, 4, W], [zs])
                P.tt(ub[:, :, :], ub[:, :, :], zs[:, :, j * 128:(j + 1) * 128], ALU.mult, [ub, zs], [ub])
                P.dma(uT[:, col0:col0 + 128].rearrange("(c p) t -> p c t", p=128), ub[:, :, :], [ub], [])

        tiles = [(0, 0, 256)] + [(256 + 256 * t, 256, NKEY) for t in range(32)]
        order_b = [tiles[0]] + tiles[:0:-1]
        ti = 0
        for (c0, q0, q1) in order_b:
            stage_a(ti, c0, q0, q1, False)
            for j in (1, 0):
                chunk_step(1, j, c0 + j * 128, 0)
            ti += 1
        ui = 0
        for (c0, q0, q1) in tiles:
            stage_a(ti, c0, q0, q1, True)
            for j in (0, 1):
                chunk_step(0, j, c0 + j * 128, ui)
                ui += 1
            ti += 1
        P.emit()
    return nc


def mamba_maps(inp, h_lat, h_ctx):
    W = np.asarray(inp["mb_in_w"][0], np.float32)
    cwf = np.asarray(inp["mb_conv_w"][0], np.float32)
    cbf = np.asarray(inp["mb_conv_b"][0], np.float32)
    maps = []
    for core in range(NCORES):
        b, g = core // 4, core % 4
        hT = np.ascontiguousarray(np.concatenate([h_ctx[b], h_lat[b]], axis=0).T)
        xcols = np.arange(g * 512, (g + 1) * 512)
        bcols = 2048 + np.arange(g * 128, (g + 1) * 128)
        ccols = 2048 + 512 + np.arange(g * 128, (g + 1) * 128)
        ch = np.concatenate([xcols, bcols, ccols])
        wxbc = np.ascontiguousarray(W[:, 2048 + ch])
        wz = np.ascontiguousarray(W[:, g * 512:(g + 1) * 512])
        dcols = np.concatenate([5120 + g * 8 + np.arange(8), 5120 + 32 + g * 8 + np.arange(8)])
        wdt = np.ascontiguousarray(W[:, dcols])
        cw = np.ascontiguousarray(cwf[:, ch].reshape(5, 6, 128).transpose(2, 1, 0).reshape(128, 30))
        cb = np.ascontiguousarray(cbf[ch].reshape(6, 128).T)
        dtb = np.ascontiguousarray(np.asarray(inp["mb_dt_bias"][0], np.float32)[:, g * 8:(g + 1) * 8].reshape(1, 16))
        alog = np.ascontiguousarray(np.asarray(inp["mb_a_log"][0], np.float32)[:, g * 8:(g + 1) * 8].reshape(1, 16))
        dsk = np.ascontiguousarray(np.repeat(np.asarray(inp["mb_d"][0], np.float32)[g * 8:(g + 1) * 8], 64).reshape(4, 128).T)
        maps.append({"hT": hT, "wz": wz, "wxbc": wxbc, "wdt": wdt, "cw": cw, "cb": cb, "dtb": dtb, "alog": alog, "dsk": dsk})
    return maps


def mamba_gather(res):
    u_lat = np.empty((2, 8192, 2048), np.float32)
    u_ctx = np.empty((2, 256, 2048), np.float32)
    for core in range(NCORES):
        b, g = core // 4, core % 4
        u = res[core]["uT"].T
        u_ctx[b, :, g * 512:(g + 1) * 512] = u[0:256]
        u_lat[b, :, g * 512:(g + 1) * 512] = u[256:]
    return u_lat, u_ctx


RW_SCALE = 0.606531
RW_EPS = 64e-5


def build_rwkv():
    nc = bass.Bass("TRN2", target_bir_lowering=False)
    with ExitStack() as stack:
        nc.dge_precook = False
        P = Prog(nc, stack)
        P.init_psum(8)

        def din(name, shape):
            return nc.dram_tensor(name, list(shape), F32, kind="ExternalInput").ap()

        hT = din("hT", [D, NKEY])
        mixr = din("mixr", [48, 128])
        wr = din("wr", [D, 256]); wk = din("wk", [D, 256]); wv = din("wv", [D, 256])
        w1 = din("w1", [2, D, 64]); w2 = din("w2", [2, 64, 256]); w0 = din("w0", [128, 4])
        a1 = din("a1", [2, D, 64]); a2 = din("a2", [2, 64, 256]); a0 = din("a0", [128, 4])
        g1 = din("g1", [D, 128]); g2 = din("g2", [128, 256])
        kkv = din("kkv", [128, 2]); kav = din("kav", [128, 2]); rkv = din("rkv", [128, 2])
        lng = din("lng", [2, 128, 64]); lnb = din("lnb", [2, 128, 64])
        oo = nc.dram_tensor("oo", [NKEY, 256], F32, kind="ExternalOutput").ap()
        ybs = nc.dram_tensor("ybs", [NKEY, 256], F32, kind="Internal").ap()
        ybs_t = TT(ybs, "ybs")

        ident, ones = make_consts(P)
        val = P.sb("val", [128, 64])
        for hs in range(2):
            P.op("pool", lambda e, hs=hs: e.iota(val[hs * 64:(hs + 1) * 64, :], pattern=[[1, 64]], base=0, channel_multiplier=-1,
                                                allow_small_or_imprecise_dtypes=True), (), [val])
        mgt = P.sb("mgt", [128, 64]); mge = P.sb("mge", [128, 64]); mlt = P.sb("mlt", [128, 64]); mle = P.sb("mle", [128, 64])
        identp = P.sb("identp", [128, 64])
        P.ts(mgt[:], val[:], 0.0, ALU.is_gt, [val], [mgt]); P.ts(mge[:], val[:], 0.0, ALU.is_ge, [val], [mge])
        P.ts(mlt[:], val[:], 0.0, ALU.is_lt, [val], [mlt]); P.ts(mle[:], val[:], 0.0, ALU.is_le, [val], [mle])
        P.ts(identp[:], val[:], 0.0, ALU.is_equal, [val], [identp])
        maskT = [P.sb("maskTf", [128, 128]), P.sb("maskTb", [128, 128])]
        P.copy(maskT[0][:, 0:64], mgt[:], [mgt], [maskT[0]]); P.copy(maskT[0][:, 64:128], mge[:], [mge], [maskT[0]])
        P.copy(maskT[1][:, 0:64], mlt[:], [mlt], [maskT[1]]); P.copy(maskT[1][:, 64:128], mle[:], [mle], [maskT[1]])
        maskA = [mlt, mgt]
        blk = P.sb("blk", [128, 128])
        P.memset(blk[:], 0.0, [blk])
        P.memset(blk[0:64, 0:64], 1.0, [blk]); P.memset(blk[64:128, 64:128], 1.0, [blk])
        half = P.sb("half", [128, 2])
        P.memset(half[:], 0.5, [half])
        tiny = P.sb("tiny", [128, 1])
        P.memset(tiny[:], RW_EPS, [tiny])

        def wload(name, ap, shape, rr=True):
            t = P.sb(name, shape)
            P.dma(t[:], ap, [], [t], r=rr)
            return t
        wr_s = wload("wr_s", wr.rearrange("(kc p) n -> p kc n", p=128), [128, 8, 256])
        wk_s = wload("wk_s", wk.rearrange("(kc p) n -> p kc n", p=128), [128, 8, 256])
        wv_s = wload("wv_s", wv.rearrange("(kc p) n -> p kc n", p=128), [128, 8, 256])
        g1_s = wload("g1_s", g1.rearrange("(kc p) n -> p kc n", p=128), [128, 8, 128])
        g2_s = wload("g2_s", g2, [128, 256])
        w1_s = [wload(f"w1_{d}", w1[d].rearrange("(kc p) n -> p kc n", p=128), [128, 8, 64]) for d in range(2)]
        a1_s = [wload(f"a1_{d}", a1[d].rearrange("(kc p) n -> p kc n", p=128), [128, 8, 64]) for d in range(2)]
        w2_s = [wload(f"w2_{d}", w2[d], [64, 256]) for d in range(2)]
        a2_s = [wload(f"a2_{d}", a2[d], [64, 256]) for d in range(2)]
        w0_s = wload("w0_s", w0, [128, 4], False); a0_s = wload("a0_s", a0, [128, 4], False)
        kk_s = wload("kk_s", kkv, [128, 2], False); ka_s = wload("ka_s", kav, [128, 2], False); rk_s = wload("rk_s", rkv, [128, 2], False)
        omka = P.sb("omka", [128, 2])
        P.ts(omka[:], ka_s[:], -1.0, ALU.mult, [ka_s], [omka], s2=1.0, op1=ALU.add)
        lng_s = [wload(f"lng{i}", lng[i], [128, 64], False) for i in range(2)]
        lnb_s = [wload(f"lnb{i}", lnb[i], [128, 64], False) for i in range(2)]
        mixc = P.sb("mixc", [128, 48])
        load_cols(P, ident, mixc, mixc[:], mixr, 48)

        W = 256
        NCH = 4
        ht = [P.sb(f"ht{i}", [128, 8, W + 2]) for i in range(2)]
        xx = P.sb("xx", [128, 8, W])
        xm = [P.sb(f"xm{i}", [128, 8, W]) for i in range(2)]
        rT = P.sb("rT", [128, 2, W]); kT = P.sb("kT", [128, 2, W]); kkT = P.sb("kkT", [128, 2, W])
        lwT = P.sb("lwT", [128, 2, W]); clT = P.sb("clT", [128, 2, W]); cleT = P.sb("cleT", [128, 2, W])
        aT = [P.sb(f"aT{d}", [128, 2, W]) for d in range(2)]
        kdT = [P.sb(f"kdT{d}", [128, 2, W]) for d in range(2)]
        bT = P.sb("bT", [128, 2, W])
        e1 = P.sb("e1", [128, 2, W]); e2 = P.sb("e2", [128, 2, W]); e3 = P.sb("e3", [128, 2, W])
        lam2 = [P.sb(f"lam{i}", [128, 2, NCH]) for i in range(3)]
        KR2 = [P.sb(f"KR{i}", [128, 2, NCH, 128]) for i in range(2)]
        BK2 = [P.sb(f"BK{i}", [128, 2, NCH, 128]) for i in range(2)]
        tw = P.sb("tw", [64, W]); t1 = P.sb("t1", [128, W])
        Vt2 = [P.sb(f"Vt{i}", [128, NCH, 2, 64]) for i in range(3)]
        Gt2 = [P.sb(f"Gt{i}", [128, NCH, 2, 64]) for i in range(3)]
        prodT = P.sb("prodT", [128, 2, W])
        bsc2 = [P.sb(f"bsc{i}", [128, NCH, 2, 2]) for i in range(3)]
        sq = P.sb("sqk", [128, W]); rn = P.sb("rnk", [128, W])
        onesW = P.sb("onesW", [128, W])
        P.memset(onesW[:], 1.0, [onesW])
        ST = [[P.sb(f"ST{d}{hp}", [128, 64]) for hp in range(2)] for d in range(2)]
        for d_ in range(2):
            for hp in range(2):
                P.memset(ST[d_][hp][:], 0.0, [ST[d_][hp]])

        class Slot:
            pass
        scratch = []
        for si in range(8):
            s_ = {}
            for nm, shp in (("AWu", [128, 128]), ("BWv", [128, 128]), ("PPa", [128, 128]), ("PPb", [128, 128]), ("X", [128, 64]),
                            ("RH", [128, 128]), ("UK", [128, 128]), ("BKt", [128, 128])):
                s_[nm] = P.sb(f"{nm}{si}", shp)
            scratch.append(s_)
        slots2 = []
        for par in range(2):
            row = []
            for si in range(8):
                s_ = Slot()
                for nm, t_ in scratch[si].items():
                    setattr(s_, nm, t_)
                for nm in ("MpT", "Nc", "R2T", "Y0"):
                    setattr(s_, nm, P.sb(f"{nm}{par}{si}", [128, 64]))
                row.append(s_)
            slots2.append(row)
        ysb = [P.sb(f"ysb{i}", [128, 64]) for i in range(4)]
        ybb = [P.sb(f"ybb{i}", [128, 64]) for i in range(4)]
        stt6 = [P.sb(f"st6{i}", [128, 6]) for i in range(2)]
        mv = [P.sb(f"mv{i}", [128, 4]) for i in range(2)]

        def stage_a(ti, c0, q0, q1, dirs, fwd):
            par = ti % 2
            p3 = ti % 3
            lam, KR, BK, Vt, Gt, bsc = lam2[p3], KR2[par], BK2[par], Vt2[p3], Gt2[p3], bsc2[p3]
            h = ht[ti % 2]
            lo, hi = max(c0 - 1, q0), min(c0 + W + 1, q1)
            off = lo - (c0 - 1)
            n = hi - lo
            if off > 0:
                P.memset(h[:, :, 0:off], 0.0, [h])
            if off + n < W + 2:
                P.memset(h[:, :, off + n:W + 2], 0.0, [h])
            for kc in range(8):
                P.dma(h[:, kc, off:off + n], hT[kc * 128:(kc + 1) * 128, lo:hi], [], [h])
            P.tt(xx[:, :, :], h[:, :, 0:W], h[:, :, 2:W + 2], ALU.add, [h], [xx])
            P.stt(xx[:, :, :], xx[:, :, :], 0.5, h[:, :, 1:W + 1], ALU.mult, ALU.subtract, [xx, h], [xx])
            mi = [0]

            def mixed(j):
                m_ = xm[mi[0] % 2]
                mi[0] += 1
                for kc in range(8):
                    P.stt(R(m_[:, kc, :]), xx[:, kc, :], mixc[:, j * 8 + kc:j * 8 + kc + 1], h[:, kc, 1:W + 1], ALU.mult, ALU.add,
                          [xx, mixc, h], [m_])
                return m_

            def proj2(m_, w_s, dst):
                for oc in range(2):
                    pb = P.bank()
                    for kc in range(8):
                        P.mm(pb[:, 0:W], w_s[:, kc, oc * 128:(oc + 1) * 128], m_[:, kc, :], kc == 0, kc == 7, [w_s, m_], [pb])
                    P.copy(dst[:, oc, :], pb[:, 0:W], [pb], [dst], eng="act")

            def lora(m_, l1_s, l2_s, bias_s, d, dst, mid_func):
                pb = P.bank()
                for kc in range(8):
                    P.mm(pb[0:64, 0:W], l1_s[:, kc, :], m_[:, kc, :], kc == 0, kc == 7, [l1_s, m_], [pb])
                P.act(R(tw[:, :]), pb[0:64, 0:W], mid_func, [pb], [tw])
                for oc in range(2):
                    p2 = P.bank()
                    P.mm(p2[:, 0:W], l2_s[:, oc * 128:(oc + 1) * 128], tw[:, :], True, True, [l2_s, tw], [p2])
                    P.act(dst[:, oc, :], p2[:, 0:W], AF.Sigmoid, [p2, bias_s], [dst], bias=bias_s[:, d * 2 + oc:d * 2 + oc + 1])

            yield
            m_ = mixed(0)
            proj2(m_, wr_s, rT)
            yield
            m_ = mixed(1)
            dd = dirs[0] if len(dirs) == 1 else 0
            lora(m_, w1_s[dd], w2_s[dd], w0_s, dd, lwT, AF.Tanh)
            P.ts(lwT[:, :, :], lwT[:, :, :], -RW_SCALE, ALU.mult, [lwT], [lwT])
            yield
            m_ = mixed(2)
            proj2(m_, wk_s, kT)
            yield
            m_ = mixed(3)
            for j4 in range(NCH):
                yield
                pv = P.bank()
                for hq in range(4):
                    p0 = (hq % 2) * 64
                    hp = hq // 2
                    for kc in range(8):
                        P.mm(pv[p0:p0 + 64, hp * 64:(hp + 1) * 64], m_[:, kc, j4 * 64:(j4 + 1) * 64], wv_s[:, kc, hq * 64:(hq + 1) * 64],
                             kc == 0, kc == 7, [m_, wv_s], [pv], r=False)
                P.copy(Vt[:, j4, :, :], pv[:, 0:128].rearrange("p (a b) -> p a b", a=2), [pv], [Vt.sub(j4)], eng="act")
            yield
            m_ = mixed(4)
            adirs = [0, 1] if fwd else dirs
            for d in adirs:
                lora(m_, a1_s[d], a2_s[d], a0_s, d, aT[d], AF.Copy)
            if fwd:
                yield
                m_ = mixed(5)
                pb = P.bank()
                for kc in range(8):
                    P.mm(pb[:, 0:W], g1_s[:, kc, :], m_[:, kc, :], kc == 0, kc == 7, [g1_s, m_], [pb])
                P.act(t1[:, :], pb[:, 0:W], AF.Sigmoid, [pb], [t1])
                for j4 in range(NCH):
                    pg = P.bank()
                    for hq in range(4):
                        p0 = (hq % 2) * 64
                        hp = hq // 2
                        P.mm(pg[p0:p0 + 64, hp * 64:(hp + 1) * 64], t1[:, j4 * 64:(j4 + 1) * 64], g2_s[:, hq * 64:(hq + 1) * 64],
                             True, True, [t1, g2_s], [pg], r=False)
                    P.copy(Gt[:, j4, :, :], pg[:, 0:128].rearrange("p (a b) -> p a b", a=2), [pg], [Gt.sub(j4)], eng="act")
            yield
            for oc in range(2):
                P.ts(kkT[:, oc, :], kT[:, oc, :], kk_s[:, oc:oc + 1], ALU.mult, [kT, kk_s], [kkT])
                P.act(sq[:, :], kkT[:, oc, :], AF.Square, [kkT], [sq])
                pb = P.bank()
                P.mm(pb[:, 0:W], blk[:, :], sq[:, :], True, True, [blk, sq], [pb], r=False)
                P.ts(rn[:, :], pb[:, 0:W], 1e-24, ALU.max, [pb], [rn])
                P.act(rn[:, :], rn[:, :], AF.Ln, [rn], [rn])
                P.act(rn[:, :], rn[:, :], AF.Exp, [rn], [rn], scale=-0.5)
                P.tt(kkT[:, oc, :], kkT[:, oc, :], rn[:, :], ALU.mult, [kkT, rn], [kkT])
                for d in adirs:
                    P.ts(kdT[d][:, oc, :], aT[d][:, oc, :], ka_s[:, oc:oc + 1], ALU.mult, [aT[d], ka_s, omka], [kdT[d]],
                         s2=omka[:, oc:oc + 1], op1=ALU.add)
                    P.tt(kdT[d][:, oc, :], kdT[d][:, oc, :], kT[:, oc, :], ALU.mult, [kdT[d], kT], [kdT[d]])
            yield
            if fwd:
                P.tt(prodT[:, :, :], kdT[0][:, :, :], kdT[1][:, :, :], ALU.add, [kdT[0], kdT[1]], [prodT])
                for oc in range(2):
                    P.stt(prodT[:, oc, :], rT[:, oc, :], rk_s[:, oc:oc + 1], prodT[:, oc, :], ALU.mult, ALU.mult, [rT, rk_s, prodT], [prodT])
                for j4 in range(NCH):
                    pb = P.bank()
                    for hq in range(4):
                        p0 = (hq % 2) * 64
                        hp = hq // 2
                        P.mm(pb[p0:p0 + 64, hp * 2:hp * 2 + 2], prodT[p0:p0 + 64, hp, j4 * 64:(j4 + 1) * 64], half[p0:p0 + 64, 0:2],
                             True, True, [prodT, half], [pb], r=False)
                    P.copy(bsc[:, j4, :, :], pb[:, 0:4].rearrange("p (a b) -> p a b", a=2), [pb], [bsc.sub(j4)])
            yield
            d = dirs[0] if len(dirs) == 1 else 0
            P.tt(bT[:, :, :], kkT[:, :, :], aT[d][:, :, :], ALU.mult, [kkT, aT[d]], [bT])
            for oc in range(2):
                P.op("dve", lambda e, oc=oc: e.tensor_tensor_scan(out=clT[:, oc, :], data0=onesW[:, :], data1=lwT[:, oc, :], initial=0.0,
                                                                   op0=ALU.mult, op1=ALU.add), [onesW, lwT], [clT])
                for j4 in range(NCH - 1, 0, -1):
                    P.ts(clT[:, oc, j4 * 64:(j4 + 1) * 64], clT[:, oc, j4 * 64:(j4 + 1) * 64], clT[:, oc, j4 * 64 - 1:j4 * 64], ALU.subtract,
                         [clT], [clT])
                if d == 0:
                    P.tt(cleT[:, oc, :], clT[:, oc, :], lwT[:, oc, :], ALU.subtract, [clT, lwT], [cleT])
                else:
                    for j4 in range(NCH):
                        P.ts(cleT[:, oc, j4 * 64:(j4 + 1) * 64], clT[:, oc, j4 * 64:(j4 + 1) * 64], -1.0, ALU.mult, [clT], [cleT],
                             s2=clT[:, oc, j4 * 64 + 63:j4 * 64 + 64], op1=ALU.add)
                    P.tt(clT[:, oc, :], cleT[:, oc, :], lwT[:, oc, :], ALU.add, [cleT, lwT, clT], [clT])
            yield
            P.act(e1[:, :, :], cleT[:, :, :], AF.Exp, [cleT], [e1])
            P.act(e2[:, :, :], clT[:, :, :], AF.Exp, [clT], [e2], scale=-1.0)
            P.act(e3[:, :, :], clT[:, :, :], AF.Exp, [clT], [e3])
            for oc in range(2):
                e3v = e3[:, oc, :].rearrange("p (c t) -> p c t", c=NCH)
                P.copy(lam[:, oc, :], e3v[:, :, 63] if d == 0 else e3v[:, :, 0], [e3], [lam])
                kkv_ = kkT[:, oc, :].rearrange("p (c t) -> p c t", c=NCH)
                P.tt(KR[:, oc, :, 0:64], kkv_, e1[:, oc, :].rearrange("p (c t) -> p c t", c=NCH), ALU.mult, [kkT, e1], [KR])
                P.tt(KR[:, oc, :, 64:128], rT[:, oc, :].rearrange("p (c t) -> p c t", c=NCH), e3v, ALU.mult, [rT, e3], [KR])
                e2v = e2[:, oc, :].rearrange("p (c t) -> p c t", c=NCH)
                P.tt(BK[:, oc, :, 0:64], bT[:, oc, :].rearrange("p (c t) -> p c t", c=NCH), e2v, ALU.mult, [bT, e2], [BK])
                P.tt(BK[:, oc, :, 64:128], kdT[d][:, oc, :].rearrange("p (c t) -> p c t", c=NCH), e2v, ALU.mult, [kdT[d], e2], [BK])

        def pre(sl, hp, d, j4, par, p3):
            lam, KR, BK, Vt = lam2[p3], KR2[par], BK2[par], Vt2[p3]
            kr = KR[:, hp, j4, :]
            bk = BK[:, hp, j4, :]
            V = Vt[:, j4, hp, :]
            hs2 = [(0, 64), (64, 128)]
            pA = P.bank()
            for (a, b) in hs2:
                P.mm(pA[a:b, 0:128], bk[a:b, 0:64], kr[a:b, :], True, True, [BK, KR], [pA], r=False)
                P.mm(pA[a:b, 128:256], bk[a:b, 64:128], kr[a:b, :], True, True, [BK, KR], [pA], r=False)
                P.mm(pA[a:b, 256:320], kr[a:b, 0:64], bk[a:b, 0:64], True, True, [BK, KR], [pA], r=False)
            P.tt(sl.AWu[:, :], pA[:, 0:128], maskT[d][:, :], ALU.mult, [pA, maskT[d]], [sl.AWu])
            P.tt(sl.BWv[:, :], pA[:, 128:256], maskT[d][:, :], ALU.mult, [pA, maskT[d]], [sl.BWv])
            P.stt(sl.PPa[:, 0:64], pA[:, 256:320], -1.0, maskA[d][:, :], ALU.mult, ALU.mult, [pA, maskA[d]], [sl.PPa])
            P.ts(sl.PPa[:, 64:128], sl.AWu[:, 0:64], -1.0, ALU.mult, [sl.AWu], [sl.PPa])
            P.tt(sl.X[:, :], identp[:, :], sl.PPa[:, 64:128], ALU.add, [identp, sl.PPa], [sl.X])
            yield
            cur, nxt = sl.PPa, sl.PPb
            for m in range(1, 6):
                pL = P.bank()
                for (a, b) in hs2:
                    P.mm(pL[a:b, 0:64], cur[a:b, 64:128], cur[a:b, 0:64], True, True, [cur], [pL], r=False)
                    if m < 5:
                        P.mm(pL[a:b, 64:128], cur[a:b, 0:64], cur[a:b, 64:128], True, True, [cur], [pL], r=False)
                if m < 5:
                    P.copy(nxt[:, :], pL[:, 0:128], [pL], [nxt], eng="act")
                else:
                    P.copy(nxt[:, 0:64], pL[:, 0:64], [pL], [nxt], eng="act")
                yield
                pX = P.bank()
                for (a, b) in hs2:
                    P.mm(pX[a:b, 0:64], nxt[a:b, 0:64], sl.X[a:b, :], True, True, [nxt, sl.X], [pX], r=False)
                P.tt(sl.X[:, :], sl.X[:, :], pX[:, 0:64], ALU.add, [sl.X, pX], [sl.X])
                yield
                cur, nxt = nxt, cur
            pR = P.bank()
            for (a, b) in hs2:
                P.mm(pR[a:b, 0:64], sl.BWv[a:b, 0:64], V[a:b, :], True, True, [sl.BWv, Vt.sub(j4)], [pR], r=False)
                P.mm(pR[a:b, 64:128], kr[a:b, 0:64], ident[a:b, a:b], True, True, [KR, ident], [pR], r=False)
            P.copy(sl.RH[:, :], pR[:, 0:128], [pR], [sl.RH], eng="act")
            yield
            pXR = P.bank()
            for (a, b) in hs2:
                P.mm(pXR[a:b, 0:128], sl.X[a:b, :], sl.RH[a:b, :], True, True, [sl.X, sl.RH], [pXR], r=False)
            P.ts(sl.UK[:, 0:64], pXR[:, 0:64], -1.0, ALU.mult, [pXR], [sl.UK])
            P.copy(sl.UK[:, 64:128], pXR[:, 64:128], [pXR], [sl.UK], eng="act")
            yield
            pT = P.bank()
            for (a, b) in hs2:
                P.mm(pT[a:b, 0:64], bk[a:b, 0:64], ident[a:b, a:b], True, True, [BK, ident], [pT], r=False)
                P.mm(pT[a:b, 64:128], bk[a:b, 64:128], ident[a:b, a:b], True, True, [BK, ident], [pT], r=False)
            P.copy(sl.BKt[:, :], pT[:, 0:128], [pT], [sl.BKt], eng="act")
            yield
            pM = P.bank()
            for (a, b) in hs2:
                P.mm(pM[a:b, 0:64], sl.UK[a:b, 64:128], sl.BKt[a:b, 0:64], True, True, [sl.UK, sl.BKt], [pM], r=False)
                P.mm(pM[a:b, 64:128], sl.BKt[a:b, 0:64], sl.UK[a:b, 0:64], True, False, [sl.UK, sl.BKt], [pM], r=False)
                P.mm(pM[a:b, 64:128], sl.BKt[a:b, 64:128], V[a:b, :], False, True, [sl.BKt, Vt.sub(j4)], [pM], r=False)
                P.mm(pM[a:b, 128:192], sl.UK[a:b, 64:128], sl.AWu[a:b, 64:128], True, True, [sl.UK, sl.AWu], [pM], r=False)
            P.tt(sl.MpT[:, :], identp[:, :], pM[:, 0:64], ALU.subtract, [identp, pM], [sl.MpT])
            P.ts(sl.Nc[:, :], pM[:, 64:128], lam[:, hp, j4:j4 + 1], ALU.mult, [pM, lam], [sl.Nc])
            P.tt(sl.R2T[:, :], kr[:, 64:128], pM[:, 128:192], ALU.subtract, [KR, pM], [sl.R2T])
            yield
            pY = P.bank()
            for (a, b) in hs2:
                P.mm(pY[a:b, 0:64], sl.AWu[a:b, 64:128], sl.UK[a:b, 0:64], True, False, [sl.AWu, sl.UK], [pY], r=False)
                P.mm(pY[a:b, 0:64], sl.BWv[a:b, 64:128], V[a:b, :], False, True, [sl.BWv, Vt.sub(j4)], [pY], r=False)
            P.copy(sl.Y0[:, :], pY[:, 0:64], [pY], [sl.Y0], eng="act")

        def seq(sl, hp, d, j4, col0, k, p3):
            lam, Vt, Gt, bsc = lam2[p3], Vt2[p3], Gt2[p3], bsc2[p3]
            S = ST[d][hp]
            hs2 = [(0, 64), (64, 128)]
            pYs = P.bank()
            for (a, b) in hs2:
                P.mm(pYs[a:b, 0:64], sl.R2T[a:b, :], S[a:b, :], True, True, [sl.R2T, S], [pYs], r=False)
            ys = ysb[k % 4]
            P.tt(ys[:, :], sl.Y0[:, :], pYs[:, 0:64], ALU.add, [sl.Y0, pYs], [ys])
            pS = P.bank()
            for (a, b) in hs2:
                P.mm(pS[a:b, 0:64], sl.MpT[a:b, :], S[a:b, :], True, True, [sl.MpT, S], [pS], r=False)
            P.stt(S[:, :], pS[:, 0:64], lam[:, hp, j4:j4 + 1], sl.Nc[:, :], ALU.mult, ALU.add, [pS, lam, sl.Nc], [S])
            if d == 1:
                for hs, (a, b) in enumerate(hs2):
                    hq = hp * 2 + hs
                    P.dma(ybs[col0:col0 + 64, hq * 64:(hq + 1) * 64], ys[a:b, :], [ys], [ybs_t])
            else:
                yb = ybb[k % 4]
                for hs, (a, b) in enumerate(hs2):
                    hq = hp * 2 + hs
                    P.dma(yb[a:b, :], ybs[col0:col0 + 64, hq * 64:(hq + 1) * 64], [ybs_t], [yb])
                P.tt(ys[:, :], ys[:, :], yb[:, :], ALU.add, [ys, yb], [ys])
                s6 = stt6[k % 2]
                m2 = mv[k % 2]
                P.op("dve", lambda e, s6=s6, ys=ys: e.bn_stats(out=s6[:, :], in_=ys[:, :]), [ys], [s6])
                P.op("dve", lambda e, s6=s6, m2=m2: e.bn_aggr(out=m2[:, 0:2], in_=s6[:, :]), [s6], [m2])
                P.act(m2[:, 2:3], m2[:, 1:2], AF.Sqrt, [m2, tiny], [m2], bias=tiny[:, 0:1])
                P.op("dve", lambda e, m2=m2: e.reciprocal(out=m2[:, 3:4], in_=m2[:, 2:3]), [m2], [m2])
                P.ts(ys[:, :], ys[:, :], m2[:, 0:1], ALU.subtract, [ys, m2], [ys], s2=m2[:, 3:4], op1=ALU.mult)
                P.tt(ys[:, :], ys[:, :], lng_s[hp][:, :], ALU.mult, [ys, lng_s[hp]], [ys])
                P.tt(ys[:, :], ys[:, :], lnb_s[hp][:, :], ALU.add, [ys, lnb_s[hp]], [ys])
                P.stt(yb[:, :], Vt[:, j4, hp, :], bsc[:, j4, hp, 0:1], ys[:, :], ALU.mult, ALU.add, [Vt.sub(j4), bsc.sub(j4), ys], [yb])
                P.tt(yb[:, :], yb[:, :], Gt[:, j4, hp, :], ALU.mult, [yb, Gt.sub(j4)], [yb])
                for hs, (a, b) in enumerate(hs2):
                    hq = hp * 2 + hs
                    P.dma(oo[col0:col0 + 64, hq * 64:(hq + 1) * 64], yb[a:b, :], [yb], [])

        tiles = [(0, 0, 256)] + [(256 + 256 * t, 256, NKEY) for t in range(32)]
        order_b = [tiles[0]] + tiles[:0:-1]
        work = []
        for d, order, chunks in ((1, order_b, (3, 2, 1, 0)), (0, tiles, (0, 1, 2, 3))):
            for (c0, q0, q1) in order:
                work.append((d, c0, q0, q1, chunks))
        nW = len(work)

        def drain(g_):
            for _ in g_:
                pass

        def units_of(wi):
            d, c0, q0, q1, chunks = work[wi]
            return [(j4, hp) for j4 in chunks for hp in range(2)]
        kcnt = [0]
        drain(stage_a(0, work[0][1], work[0][2], work[0][3], [work[0][0]], work[0][0] == 0))
        for i in range(nW + 1):
            gA = None
            if i + 1 < nW:
                d_, c0_, q0_, q1_, _ = work[i + 1]
                gA = stage_a(i + 1, c0_, q0_, q1_, [d_], d_ == 0)
            gB = []
            if i < nW:
                d_ = work[i][0]
                gB = [pre(slots2[i % 2][ui], hp, d_, j4, i % 2, i % 3) for ui, (j4, hp) in enumerate(units_of(i))]
            cS = []
            if i >= 1:
                d_, c0_ = work[i - 1][0], work[i - 1][1]
                cS = [(slots2[(i - 1) % 2][ui], hp, d_, j4, c0_ + j4 * 64) for ui, (j4, hp) in enumerate(units_of(i - 1))]
            tick = 0
            while gA is not None or gB or cS:
                if gA is not None:
                    try:
                        next(gA)
                    except StopIteration:
                        gA = None
                nb = []
                for g_ in gB:
                    try:
                        next(g_)
                        nb.append(g_)
                    except StopIteration:
                        pass
                gB = nb
                if cS:
                    sl_, hp_, dd_, j4_, col_ = cS.pop(0)
                    seq(sl_, hp_, dd_, j4_, col_, kcnt[0], (i - 1) % 3)
                    kcnt[0] += 1
                tick += 1
        P.emit()
    return nc


def rwkv_maps(inp, h_lat, h_ctx):
    f = lambda k: np.asarray(inp[k][0], np.float32)
    rkv, w0, w1, w2 = f("rw_rkv_w"), f("rw_w0"), f("rw_w1"), f("rw_w2")
    a0, a1, a2, g1, g2 = f("rw_a0"), f("rw_a1"), f("rw_a2"), f("rw_g1"), f("rw_g2")
    k_k, k_a, r_k, ln_g, ln_b = f("rw_k_k"), f("rw_k_a"), f("rw_r_k").reshape(-1), f("rw_ln_g"), f("rw_ln_b")
    mixr = rows128(f("rw_mix"))
    maps = []
    for core in range(NCORES):
        b, hg = core // 4, core % 4
        sl = slice(hg * 256, (hg + 1) * 256)
        hT = np.ascontiguousarray(np.concatenate([h_ctx[b], h_lat[b]], axis=0).T)

        def pc(v):
            return np.ascontiguousarray(v.reshape(2, 128).T)

        def pc2(v2):
            return np.ascontiguousarray(v2.reshape(2, 2, 128).transpose(2, 0, 1).reshape(128, 4))

        def pairrows(v):
            hh = v.reshape(2, 2, 1, 64)
            return np.ascontiguousarray(np.broadcast_to(hh, (2, 2, 64, 64)).reshape(2, 128, 64))
        maps.append({
            "hT": hT, "mixr": mixr,
            "wr": np.ascontiguousarray(rkv[0][:, sl]), "wk": np.ascontiguousarray(rkv[1][:, sl]), "wv": np.ascontiguousarray(rkv[2][:, sl]),
            "w1": w1, "w2": np.ascontiguousarray(w2[:, :, sl]), "w0": pc2(w0[:, sl]),
            "a1": a1, "a2": np.ascontiguousarray(a2[:, :, sl]), "a0": pc2(a0[:, sl]),
            "g1": g1, "g2": np.ascontiguousarray(g2[:, sl]),
            "kkv": pc(k_k[sl]), "kav": pc(k_a[sl]), "rkv": pc(r_k[sl]),
            "lng": pairrows(ln_g[sl]), "lnb": pairrows(ln_b[sl]),
        })
    return maps


def rwkv_gather(res):
    o_lat = np.empty((2, 8192, 1024), np.float32)
    o_ctx = np.empty((2, 256, 1024), np.float32)
    for core in range(NCORES):
        b, hg = core // 4, core % 4
        o = res[core]["oo"]
        o_ctx[b, :, hg * 256:(hg + 1) * 256] = o[0:256]
        o_lat[b, :, hg * 256:(hg + 1) * 256] = o[256:]
    return o_lat, o_ctx


def _get(key, builder):
    if key not in _NC_CACHE:
        _NC_CACHE[key] = builder()
    return _NC_CACHE[key]


def attn_maps(inp, h_lat, h_ctx):
    cosT, sinS, prot = rope_tables()
    Wq = np.asarray(inp["at_qkv_w"][0], np.float32)
    qg = np.asarray(inp["at_q_g"][0], np.float32).reshape(64, 1)
    kg = np.asarray(inp["at_k_g"][0], np.float32).reshape(64, 1)
    maps = []
    for core in range(NCORES):
        b, kv = core // 4, core % 4
        hT = np.ascontiguousarray(np.concatenate([h_ctx[b], h_lat[b]], axis=0).T)
        maps.append({"hT": hT, "wq": np.ascontiguousarray(Wq[:, kv * 256:(kv + 1) * 256]),
                     "wk": np.ascontiguousarray(Wq[:, 1024 + kv * 64:1024 + (kv + 1) * 64]),
                     "wv": np.ascontiguousarray(Wq[:, 1280 + kv * 64:1280 + (kv + 1) * 64]),
                     "qg": qg, "kg": kg, "cosT": cosT, "sinS": sinS, "prot": prot})
    return maps


def attn_gather(res):
    o = np.zeros((2, 8192, 1024), np.float32)
    for core in range(NCORES):
        b, kv = core // 4, core % 4
        o[b, :, kv * 256:(kv + 1) * 256] = res[core]["oT"].T
    return o


def kernel(**inp):
    f32 = lambda a: np.asarray(a, np.float32)
    x, c, ctx, c_ctx = f32(inp["x"]), f32(inp["c"]), f32(inp["ctx"]), f32(inp["c_ctx"])
    mod_w, mod_b = f32(inp["mod_w"]), f32(inp["mod_b"])
    n1g, n2g = f32(inp["norm1_g"]), f32(inp["norm2_g"])
    cv = [cvec_for(c, c_ctx, core) for core in range(NCORES)]

    def nxt_params(i):
        return {"mod_w_n": mod_w[i], "mod_b_n": rows128(mod_b[i]), "n1g_n": rows128(n1g[i])}

    def cur_params(i):
        return {"mod_w": mod_w[i], "mod_b": rows128(mod_b[i]), "n2g": rows128(n2g[i])}

    xs = to_cores_T(x, ctx)
    res = run(_get(("ts", None, None, "norm"), lambda: build_ts(0, None, None, "norm")),
              [dict(xT=xs[k], cvec=cv[k], **nxt_params(0)) for k in range(NCORES)])
    h_lat, h_ctx = from_cores_T([r["hT_out"] for r in res])
    mres = run(_get("mamba", build_mamba), mamba_maps(inp, h_lat, h_ctx))
    u_lat, u_ctx = mamba_gather(mres)
    ms = to_cores_T(u_lat, u_ctx)
    res = run(_get(("ts", "mamba", "dense", "norm"), lambda: build_ts(0, "mamba", "dense", "norm")),
              [dict(xT=xs[k], mT=ms[k], cvec=cv[k], mb_ng=rows128(f32(inp["mb_norm_g"])[0]), w_post=f32(inp["mb_out_w"])[0],
                    w_in=f32(inp["ff_in_w"])[0], w_out=f32(inp["ff_out_w"])[0], **cur_params(0), **nxt_params(1)) for k in range(NCORES)])
    xs = [r["xT_out"] for r in res]
    h_lat, h_ctx = from_cores_T([r["hT_out"] for r in res])
    rres = run(_get("rwkv", build_rwkv), rwkv_maps(inp, h_lat, h_ctx))
    o_lat, o_ctx = rwkv_gather(rres)
    ms = to_cores_T(o_lat, o_ctx)
    ts_moe_n = _get(("ts", "lin", "moe", "norm"), lambda: build_ts(1, "lin", "moe", "norm"))
    res = run(ts_moe_n,
              [dict(xT=xs[k], mT=ms[k], cvec=cv[k], w_post=f32(inp["rw_out_w"])[0], router=f32(inp["moe_router_w"])[0],
                    w_in=f32(inp["moe_in_w"])[0], w_out=f32(inp["moe_out_w"])[0], **cur_params(1), **nxt_params(2)) for k in range(NCORES)])
    xs = [r["xT_out"] for r in res]
    h_lat, h_ctx = from_cores_T([r["hT_out"] for r in res])
    pin = pool_inputs(h_lat, h_ctx)
    res = run(_get(("ts", "pool", "dense", "norm"), lambda: build_ts(2, "pool", "dense", "norm")),
              [dict(xT=xs[k], cvec=cv[k], pl_w=f32(inp["pl_w"])[0], pl_scale=rows128(f32(inp["pl_scale"])[0]),
                    w_in=f32(inp["ff_in_w"])[1], w_out=f32(inp["ff_out_w"])[1], **pin[k], **cur_params(2), **nxt_params(3))
               for k in range(NCORES)])
    xs = [r["xT_out"] for r in res]
    h_lat, h_ctx = from_cores_T([r["hT_out"] for r in res])
    ares = run(_get("attn", build_attn), attn_maps(inp, h_lat, h_ctx))
    o_lat = attn_gather(ares)
    ms = to_cores_T(o_lat, np.zeros((2, 256, 1024), np.float32))
    res = run(_get(("ts", "lin", "moe", "final"), lambda: build_ts(3, "lin", "moe", "final")),
              [dict(xT=xs[k], mT=ms[k], cvec=cv[k], w_post=f32(inp["at_out_w"])[0], router=f32(inp["moe_router_w"])[1],
                    w_in=f32(inp["moe_in_w"])[1], w_out=f32(inp["moe_out_w"])[1], fin_g=rows128(f32(inp["final_g"])), **cur_params(3))
               for k in range(NCORES)])
    out_lat, _ = from_cores_T([r["out_T"] for r in res])
    return out_lat
```
